# Optimizing a Trainium2 kernel written in Bass

```python
import math
import jax, jax.numpy as jnp
from jax import lax
import numpy as np

D_MODEL = 1024
BATCH = 16
SEQ = 2048
DEPTH = 2

CHUNK = 64
Q_BLOCK = 128
D_MIX = D_MODEL
SB_HEADS = 8
SB_HEAD_DIM = 64
DSA_HEADS = 8
DSA_HEAD_DIM = 64
DSA_KV_RANK = 128
IDX_HEADS = 8
IDX_DIM = 64
DSA_MAX_TOPK = 256
D_FF = 4 * D_MODEL
N_BUCKETS = 32
MAX_DISTANCE = 128
EPS = 1e-6

SB_QKV = 3 * SB_HEADS * SB_HEAD_DIM
DSA_Q = DSA_HEADS * DSA_KV_RANK
DSA_KV = DSA_KV_RANK
IDX_Q = IDX_HEADS * IDX_DIM
IDX_K = IDX_DIM
IDX_W = IDX_HEADS
D_IN_PROJ = SB_QKV + DSA_Q + DSA_KV + IDX_Q + IDX_K + IDX_W
SPLITS = [SB_QKV, SB_QKV + DSA_Q, SB_QKV + DSA_Q + DSA_KV,
          SB_QKV + DSA_Q + DSA_KV + IDX_Q, SB_QKV + DSA_Q + DSA_KV + IDX_Q + IDX_K]

kernel_name = "hybrid_stickbreak_dsa_adaln_trunk"


def rmsnorm(x, g):
    xf = x.astype(jnp.float32)
    y = xf * lax.rsqrt(jnp.mean(xf * xf, axis=-1, keepdims=True) + EPS)
    return (y * g.astype(jnp.float32)).astype(x.dtype)


def t5_bucket(rel):
    nb = N_BUCKETS // 2
    max_exact = nb // 2
    base = jnp.where(rel > 0, nb, 0)
    n = jnp.abs(rel)
    nf = jnp.maximum(n, max_exact).astype(jnp.float32)
    large = max_exact + (jnp.log(nf / max_exact) / math.log(MAX_DISTANCE / max_exact)
                         * (nb - max_exact)).astype(jnp.int32)
    large = jnp.minimum(large, nb - 1)
    return base + jnp.where(n < max_exact, n, large)


def stick_breaking_attention(q, k, v):
    B, S, H, Dh = q.shape
    nb = S // Q_BLOCK
    qb = q.reshape(B, nb, Q_BLOCK, H, Dh).transpose(1, 0, 2, 3, 4)
    kpos = jnp.arange(S)
    scale = Dh ** -0.5

    def block(args):
        qi, i = args
        qpos = i * Q_BLOCK + jnp.arange(Q_BLOCK)
        z = jnp.einsum('bqhd,bshd->bhqs', qi, k).astype(jnp.float32) * scale
        causal = (kpos[None, :] < qpos[:, None])[None, None]
        log_beta = jax.nn.log_sigmoid(z)
        log_1m = jnp.where(causal, log_beta - z, 0.0)
        after = lax.cumsum(log_1m, axis=3, reverse=True) - log_1m
        w = jnp.where(causal, jnp.exp(log_beta + after), 0.0)
        return jnp.einsum('bhqs,bshd->bqhd', w.astype(v.dtype), v)

    out = lax.map(block, (qb, jnp.arange(nb)))
    return out.transpose(1, 0, 2, 3, 4).reshape(B, S, H, Dh)


def dsa_attention(q, kv, q_idx, k_idx, w_idx, w_uv, rel_bias):
    B, S, H, R = q.shape
    topk = min(DSA_MAX_TOPK, S // 4)
    nb = S // Q_BLOCK
    kchunk = jnp.arange(S) // CHUNK
    qb = q.reshape(B, nb, Q_BLOCK, H, R).transpose(1, 0, 2, 3, 4)
    qib = q_idx.reshape(B, nb, Q_BLOCK, IDX_HEADS, IDX_DIM).transpose(1, 0, 2, 3, 4)
    wib = w_idx.reshape(B, nb, Q_BLOCK, IDX_HEADS).transpose(1, 0, 2, 3)
    bias_tab = rel_bias.astype(jnp.float32)

    def block(args):
        qi, qii, wi, i = args
        qpos = i * Q_BLOCK + jnp.arange(Q_BLOCK)
        qchunk = qpos // CHUNK
        admissible = kchunk[None, :] <= qchunk[:, None]
        sc = jnp.einsum('bqjd,bsd->bqjs', qii, k_idx).astype(jnp.float32) * (IDX_DIM ** -0.5)
        score = jnp.einsum('bqjs,bqj->bqs', jax.nn.relu(sc),
                           wi.astype(jnp.float32) * (IDX_HEADS ** -0.5))
        score = jnp.where(admissible[None], score, -jnp.inf)
        _, idx = lax.top_k(score, topk)
        kv_sel = jax.vmap(lambda a, ix: a[ix])(kv, idx)
        valid = (idx // CHUNK) <= qchunk[None, :, None]
        bias = bias_tab[t5_bucket(idx - qpos[None, :, None])]
        logits = (jnp.einsum('bqhr,bqkr->bqhk', qi, kv_sel).astype(jnp.float32) * (R ** -0.5)
                  + bias.transpose(0, 1, 3, 2))
        logits = jnp.where(valid[:, :, None, :], logits, -jnp.inf)
        p = jax.nn.softmax(logits, axis=-1)
        o = jnp.einsum('bqhk,bqkr->bqhr', p.astype(kv.dtype), kv_sel)
        return jnp.einsum('bqhr,hrd->bqhd', o, w_uv)

    out = lax.map(block, (qb, qib, wib, jnp.arange(nb)))
    return out.transpose(1, 0, 2, 3, 4).reshape(B, S, H, DSA_HEAD_DIM)


def setup_inputs(seed: int = 0) -> dict:
    key = jax.random.key(seed)
    ks = jax.random.split(key, 20)
    f32 = jnp.float32

    def nrm(k, shape, scale):
        return jax.random.normal(k, shape, f32) * scale

    def gain(k, shape):
        return 1.0 + 0.02 * jax.random.normal(k, shape, f32)

    return {
        "x": nrm(ks[0], (BATCH, SEQ, D_MODEL), 1.0),
        "c": nrm(ks[1], (BATCH, D_MODEL), 1.0),
        "w_mod": nrm(ks[2], (DEPTH, D_MODEL, 6 * D_MODEL), 0.5 * D_MODEL ** -0.5),
        "b_mod": nrm(ks[3], (DEPTH, 6 * D_MODEL), 0.02),
        "g_attn": gain(ks[4], (DEPTH, D_MODEL)),
        "w_in": nrm(ks[5], (DEPTH, D_MODEL, D_IN_PROJ), D_MODEL ** -0.5),
        "kv_norm_g": gain(ks[6], (DEPTH, DSA_KV_RANK)),
        "w_uv": nrm(ks[7], (DEPTH, DSA_HEADS, DSA_KV_RANK, DSA_HEAD_DIM), DSA_KV_RANK ** -0.5),
        "g_out_a": gain(ks[8], (DEPTH, SB_HEADS * SB_HEAD_DIM)),
        "g_out_b": gain(ks[9], (DEPTH, DSA_HEADS * DSA_HEAD_DIM)),
        "w_out": nrm(ks[10], (DEPTH, D_MIX, D_MODEL), D_MIX ** -0.5),
        "g_mlp": gain(ks[11], (DEPTH, D_MODEL)),
        "w_up": nrm(ks[12], (DEPTH, D_MODEL, D_FF), D_MODEL ** -0.5),
        "w_down": nrm(ks[13], (DEPTH, D_FF, D_MODEL), D_FF ** -0.5),
        "rel_bias": nrm(ks[14], (N_BUCKETS, DSA_HEADS), 0.2),
        "g_final": gain(ks[15], (D_MODEL,)),
    }


def reference(x, c, w_mod, b_mod, g_attn, w_in, kv_norm_g, w_uv, g_out_a, g_out_b,
              w_out, g_mlp, w_up, w_down, rel_bias, g_final):
    B, S, D = x.shape
    c_act = jax.nn.silu(c)
    for l in range(DEPTH):
        mod = c_act @ w_mod[l] + b_mod[l]
        sh1, sc1, ga1, sh2, sc2, ga2 = [m[:, None, :] for m in jnp.split(mod, 6, axis=-1)]

        h = rmsnorm(x, g_attn[l]) * (1.0 + sc1) + sh1
        proj = h @ w_in[l]
        sb_qkv, dsa_q, dsa_kv, idx_q, idx_k, idx_w = jnp.split(proj, SPLITS, axis=-1)
        q_a, k_a, v_a = [t.reshape(B, S, SB_HEADS, SB_HEAD_DIM)
                         for t in jnp.split(sb_qkv, 3, axis=-1)]
        o_a = stick_breaking_attention(q_a, k_a, v_a).reshape(B, S, SB_HEADS * SB_HEAD_DIM)

        kv_lat = rmsnorm(dsa_kv, kv_norm_g[l])
        o_b = dsa_attention(dsa_q.reshape(B, S, DSA_HEADS, DSA_KV_RANK), kv_lat,
                            idx_q.reshape(B, S, IDX_HEADS, IDX_DIM), idx_k, idx_w,
                            w_uv[l], rel_bias).reshape(B, S, DSA_HEADS * DSA_HEAD_DIM)

        o = jnp.concatenate([rmsnorm(o_a, g_out_a[l]), rmsnorm(o_b, g_out_b[l])], axis=-1)
        x = x + ga1 * (o @ w_out[l])

        h = rmsnorm(x, g_mlp[l]) * (1.0 + sc2) + sh2
        x = x + ga2 * (jnp.square(jax.nn.relu(h @ w_up[l])) @ w_down[l])
    return rmsnorm(x, g_final)
```

```python
import math
from contextlib import ExitStack

import numpy as np
import concourse.bass as bass
import concourse.mybir as mybir
from concourse.bass_utils import run_bass_kernel_spmd

F32 = mybir.dt.float32
BF16 = mybir.dt.bfloat16
AF = mybir.ActivationFunctionType
ALU = mybir.AluOpType
AX = mybir.AxisListType

S = 2048
D = 1024
NSEQ = 2
NT = S // 128
DFF = 4096
DIN = 3272
EPS = 1e-6
KBIS = 16
TOPK = 256
EPOCH = 30000
LIM_SEQ = NSEQ
LIM_QB = NT
NO_POOL = False
ACC_DEPTH = 2
FLUSH_BG_BEFORE_C = False


class Buf:
    __slots__ = ("name", "last_w", "readers", "dsem", "bg")

    def __init__(self, name, bg=False):
        self.name = name
        self.last_w = None
        self.readers = []
        self.dsem = {}
        self.bg = bg


class Tracker:
    def __init__(self, nc):
        self.nc = nc
        self.eng = {"pe": nc.tensor, "act": nc.scalar, "dve": nc.vector,
                    "pool": nc.gpsimd, "sp": nc.sync}
        self.cnt = {e: 0 for e in self.eng}
        self.sems = {e: [] for e in self.eng}
        self.seen = {e: {} for e in self.eng}
        self.nsem = 0
        self.dma_bufs = []
        self.free_dsems = {"hw": [], "sw": []}
        self.nwaits = 0
        self.ninstr = 0

    def _newsem(self, name):
        self.nsem += 1
        return self.nc.alloc_semaphore(name=name)

    def _wait(self, e, tok):
        sem, val, src = tok
        key = id(sem)
        if self.seen[e].get(key, 0) >= val:
            return
        self.seen[e][key] = val
        self.eng[e].wait_ge(sem, val)
        self.nwaits += 1

    def _deps(self, e, reads, writes):
        for b in reads:
            if b.last_w is not None and not (b.last_w[2] == e and e == "pe"):
                self._wait(e, b.last_w)
        for b in writes:
            if b.last_w is not None and not (b.last_w[2] == e and e == "pe"):
                self._wait(e, b.last_w)
            for t in b.readers:
                if t[2] == e:
                    continue
                self._wait(e, t)

    def _commit(self, tok, reads, writes):
        for b in reads:
            b.readers.append(tok)
            if len(b.readers) > 12:
                d = {}
                for t in b.readers:
                    k = id(t[0])
                    if k not in d or d[k][1] < t[1]:
                        d[k] = t
                b.readers = list(d.values())
        for b in writes:
            b.last_w = tok
            b.readers = []

    def op(self, e, fn, reads=(), writes=()):
        self._deps(e, reads, writes)
        n = self.cnt[e]
        ep, v = divmod(n, EPOCH)
        while len(self.sems[e]) <= ep:
            self.sems[e].append(self._newsem(f"c_{e}_{len(self.sems[e])}"))
        sem = self.sems[e][ep]
        ins = fn(self.eng[e])
        ins.then_inc(sem, 1)
        self.cnt[e] = n + 1
        self.ninstr += 1
        tok = (sem, v + 1, e)
        self._commit(tok, reads, writes)
        return tok

    def dma(self, q, out, in_, sb, reads=(), writes=(), **kw):
        self._deps(q, reads, writes)
        kind = "sw" if q == "pool" else "hw"
        if kind not in sb.dsem:
            if self.free_dsems[kind]:
                sb.dsem[kind] = list(self.free_dsems[kind].pop())
            else:
                sb.dsem[kind] = [self._newsem(f"d{kind}_{sb.name}_{self.nsem}"), 0]
            self.dma_bufs.append((sb, kind))
        ent = sb.dsem[kind]
        ent[1] += 16
        ins = self.eng[q].dma_start(out=out, in_=in_, **kw)
        ins.then_inc(ent[0], 16)
        self.ninstr += 1
        tok = (ent[0], ent[1], "dma")
        self._commit(tok, reads, writes)
        return tok

    def barrier(self):
        toks = []
        for f in self.eng:
            n = self.cnt[f]
            if n == 0:
                continue
            ep, v = divmod(n - 1, EPOCH)
            toks.append((self.sems[f][ep], v + 1, f))
        for b, kind in self.dma_bufs:
            if not b.bg:
                toks.append((b.dsem[kind][0], b.dsem[kind][1], "dma"))
        for e in self.eng:
            for t in toks:
                if t[2] == e:
                    continue
                self._wait(e, t)
        keep = []
        for b, kind in self.dma_bufs:
            if b.bg:
                keep.append((b, kind))
            else:
                self.free_dsems[kind].append(tuple(b.dsem.pop(kind)))
        self.dma_bufs = keep


def build_program(nlayers=2, debug=False, phases="ABCDE"):
    nc = bass.Bass("TRN2", target_bir_lowering=False)
    T = Tracker(nc)
    uid = [0]

    def din(name, shape, dt=F32):
        return nc.dram_tensor(name, list(shape), dt, kind="ExternalInput").ap()

    def dscr(name, shape, dt):
        kind = "ExternalOutput" if debug else "Internal"
        return nc.dram_tensor(name, list(shape), dt, kind=kind).ap()

    x_in = din("x", [NSEQ, S, D])
    cT = din("cT", [128, 8, NSEQ])
    w_mod = din("w_mod", [2, D, 6 * D])
    b_modT = din("b_modT", [128, 2, 48, NSEQ])
    g_attnT = din("g_attnT", [128, 2, 8, NSEQ])
    g_mlpT = din("g_mlpT", [128, 2, 8, NSEQ])
    w_in = din("w_in", [2, D, DIN])
    kvg_bc = din("kvg_bc", [128, 2, 128])
    w_uv = din("w_uv", [2, 8, 128, 64])
    goa_bc = din("goa_bc", [128, 2, 512])
    gob_bc = din("gob_bc", [128, 2, 512])
    w_out = din("w_out", [2, D, D])
    w_up = din("w_up", [2, D, DFF])
    w_down = din("w_down", [2, DFF, D])
    biasn = din("biasn", [128, 2, 8, 128])
    bfar_bc = din("bfar_bc", [128, 8, 128])
    gfin_bc = din("gfin_bc", [128, D])
    c_ident = din("c_ident", [128, 128])
    c_tri8 = din("c_tri8", [128, 128])
    c_neg8 = din("c_neg8", [128, 128])
    c_cmask = din("c_cmask", [128, 8, 128])
    c_adm = din("c_adm", [128, 8, 128])
    c_admneg = din("c_admneg", [128, 8, 128])
    c_pow2 = din("c_pow2", [128, KBIS])
    c_ones = din("c_ones", [128, 128])
    out_d = nc.dram_tensor("out", [NSEQ, S, D], F32, kind="ExternalOutput").ap()

    xres = dscr("xres", [NSEQ, S, D], F32)
    featT = dscr("featT", [NSEQ, 21, 128, S], BF16)
    v_scr = dscr("v_scr", [NSEQ, S, 512], BF16)
    kv_tok = dscr("kv_tok", [NSEQ, S, 128], BF16)
    kvT_scr = dscr("kvT_scr", [NSEQ, 128, S], BF16)
    widx = dscr("widx", [NSEQ, S, 8], F32)
    ocat = dscr("ocat", [NSEQ, S, D], BF16)

    wb_in = [nc.dram_tensor(f"wb_in{l}", [D, DIN], BF16, kind="Internal").ap() for l in range(2)]
    wb_out = [nc.dram_tensor(f"wb_out{l}", [D, D], BF16, kind="Internal").ap() for l in range(2)]
    wb_up = [nc.dram_tensor(f"wb_up{l}", [D, DFF], BF16, kind="Internal").ap() for l in range(2)]
    wb_dn = [nc.dram_tensor(f"wb_dn{l}", [DFF, D], BF16, kind="Internal").ap() for l in range(2)]
    B_wb_in = [Buf(f"wb_in{l}", bg=True) for l in range(2)]
    B_wb_out = [Buf(f"wb_out{l}", bg=True) for l in range(2)]
    B_wb_up = [Buf(f"wb_up{l}", bg=True) for l in range(2)]
    B_wb_dn = [Buf(f"wb_dn{l}", bg=True) for l in range(2)]

    bgq = []

    def convert_weights():
        for l in range(nlayers):
            for (dst, src, bb, rows, step) in ((wb_in[l], w_in[l], B_wb_in[l], D, 256), (wb_out[l], w_out[l], B_wb_out[l], D, 512),
                                               (wb_up[l], w_up[l], B_wb_up[l], D, 256), (wb_dn[l], w_down[l], B_wb_dn[l], DFF, 1024)):
                for r0 in range(0, rows, step):
                    f = (lambda dst=dst, src=src, bb=bb, r0=r0, step=step:
                         T.dma("pool", dst[r0:r0 + step, :], src[r0:r0 + step, :], bb, writes=[bb]))
                    if l == 0 and dst is wb_in[0]:
                        f()
                    else:
                        bgq.append((l, f))

    def bg_step(n=1):
        for _ in range(n):
            if bgq:
                bgq.pop(0)[1]()

    def bg_flush(layer):
        while bgq and bgq[0][0] <= layer:
            bgq.pop(0)[1]()

    def salloc(es, name, shape, dt):
        uid[0] += 1
        return es.enter_context(nc.sbuf_tensor(f"{name}_{uid[0]}", list(shape), dt))

    ges = ExitStack()
    pb = [ges.enter_context(nc.psum_tensor(f"pb{i}", [128, 512], F32)) for i in range(8)]
    PB = [Buf(f"pb{i}") for i in range(8)]

    def pbf(i):
        return pb[i][:].bitcast(BF16)

    ident_f = salloc(ges, "identf", [128, 128], F32); b_identf = Buf("identf")
    ident_b = salloc(ges, "identb", [128, 128], BF16); b_identb = Buf("identb")
    ones_f = salloc(ges, "onesf", [128, 128], F32); b_onesf = Buf("onesf")
    ones_b = salloc(ges, "onesb", [128, 128], BF16); b_onesb = Buf("onesb")
    modT = salloc(ges, "modT", [128, 2, 48, NSEQ], F32); b_modT_ = Buf("modT")
    gm1T = salloc(ges, "gm1T", [128, 2, 8, NSEQ], F32); b_gm1 = Buf("gm1T")
    gm2T = salloc(ges, "gm2T", [128, 2, 8, NSEQ], F32); b_gm2 = Buf("gm2T")
    stat = salloc(ges, "stat", [128, 8, 4], F32)
    STB = [Buf(f"stat{i}") for i in range(8)]
    junk = salloc(ges, "junk", [128, 2048], BF16); b_junk = Buf("junk")
    stat_i = [0]

    T.dma("sp", ident_f[:], c_ident, b_identf, writes=[b_identf])
    T.dma("pool", ident_b[:], c_ident, b_identb, writes=[b_identb])
    T.dma("sp", ones_f[:], c_ones, b_onesf, writes=[b_onesf])
    T.dma("pool", ones_b[:], c_ones, b_onesb, writes=[b_onesb])

    def prologue(preA=None):
        with ExitStack() as es:
            sT = salloc(es, "sT", [128, 8, NSEQ], F32); b_sT = Buf("sT")
            cTs = salloc(es, "cTs", [128, 8, NSEQ], F32); b_cTs = Buf("cTs")
            bm = salloc(es, "bm", [128, 2, 48, NSEQ], F32); b_bm = Buf("bm")
            ga = salloc(es, "ga", [128, 2, 8, NSEQ], F32); b_ga = Buf("ga")
            gmm = salloc(es, "gmm", [128, 2, 8, NSEQ], F32); b_gmm = Buf("gmm")
            NW = 4
            wms = [salloc(es, f"wm{i}", [128, 8, 512], F32) for i in range(NW)]
            b_wms = [Buf(f"wm{i}") for i in range(NW)]
            modrow = salloc(es, "modrow", [NSEQ, 6 * D], F32); b_mrow = Buf("modrow")
            T.dma("sp", cTs[:], cT, b_cTs, writes=[b_cTs])
            T.dma("sp", bm[:], b_modT, b_bm, writes=[b_bm])
            T.dma("sp", ga[:], g_attnT, b_ga, writes=[b_ga])
            T.dma("sp", gmm[:], g_mlpT, b_gmm, writes=[b_gmm])
            T.op("act", lambda e: e.activation(out=sT[:], in_=cTs[:], func=AF.Silu),
                 reads=[b_cTs], writes=[b_sT])
            it = 0
            for l in range(nlayers):
                wl = w_mod[l].rearrange("(c p) n -> p c n", p=128)
                for ns in range(12):
                    sl = it % NW
                    bank = it % 2
                    it += 1
                    T.dma("sp", wms[sl][:], wl[:, :, ns * 512:(ns + 1) * 512], b_wms[sl],
                          writes=[b_wms[sl]])
                    for c in range(8):
                        T.op("pe", lambda e: e.matmul(
                            pb[bank][0:NSEQ, 0:512], lhsT=sT[:, c, :], rhs=wms[sl][:, c, :],
                            start=(c == 0), stop=(c == 7)),
                            reads=[b_wms[sl], b_sT], writes=[PB[bank]])
                    T.op("act", lambda e: e.activation(out=modrow[:, ns * 512:(ns + 1) * 512],
                                                       in_=pb[bank][0:NSEQ, 0:512], func=AF.Copy),
                         reads=[PB[bank]], writes=[b_mrow])
                for j in range(48):
                    T.op("pe", lambda e: e.transpose(pb[2][:, j * NSEQ:(j + 1) * NSEQ],
                                                     modrow[0:NSEQ, j * 128:(j + 1) * 128],
                                                     ident_f[0:NSEQ, 0:NSEQ]),
                         reads=[b_mrow, b_identf], writes=[PB[2]])
                T.op("dve", lambda e: e.tensor_tensor(
                    out=modT[:, l, :, :],
                    in0=pb[2][:, 0:48 * NSEQ].rearrange("p (j b) -> p j b", b=NSEQ),
                    in1=bm[:, l, :, :], op=ALU.add),
                    reads=[PB[2], b_bm], writes=[b_modT_])
                T.op("dve", lambda e: e.scalar_tensor_tensor(
                    out=gm1T[:, l], in0=modT[:, l, 8:16, :], scalar=1.0, in1=ga[:, l],
                    op0=ALU.add, op1=ALU.mult), reads=[b_modT_, b_ga], writes=[b_gm1])
                T.op("dve", lambda e: e.scalar_tensor_tensor(
                    out=gm2T[:, l], in0=modT[:, l, 32:40, :], scalar=1.0, in1=gmm[:, l],
                    op0=ALU.add, op1=ALU.mult), reads=[b_modT_, b_gmm], writes=[b_gm2])
            if preA is not None:
                load_A_weights(0, *preA)
            T.barrier()

    def next_stat():
        i = stat_i[0] % 8
        stat_i[0] += 1
        return stat[:, i, :], STB[i]

    def rstd_from(src_ap, src_bufs, n, from_psum=False):
        st, sbuf_ = next_stat()
        T.op("act", lambda e: e.activation(out=junk[:, 0:n], in_=src_ap, func=AF.Square,
                                           accum_out=st[:, 0:1]),
             reads=src_bufs, writes=[b_junk, sbuf_])
        T.op("dve", lambda e: e.tensor_scalar(out=st[:, 1:2], in0=st[:, 0:1], scalar1=1.0 / n,
                                              scalar2=EPS, op0=ALU.mult, op1=ALU.add),
             reads=[sbuf_], writes=[sbuf_])
        T.op("act", lambda e: e.activation(out=st[:, 2:3], in_=st[:, 1:2], func=AF.Sqrt),
             reads=[sbuf_], writes=[sbuf_])
        T.op("dve", lambda e: e.reciprocal(out=st[:, 3:4], in_=st[:, 2:3]),
             reads=[sbuf_], writes=[sbuf_])
        return st[:, 3:4], sbuf_

    def norm_to_T(xt_ap, bx, gmT_ap, shT_ap, bmods, xn, bxn, tbank, hT_dst, bhT):
        rstd, brs = rstd_from(xt_ap, [bx], D)
        T.op("dve", lambda e: e.tensor_scalar(out=xn[:], in0=xt_ap, scalar1=rstd, scalar2=None,
                                              op0=ALU.mult), reads=[bx, brs], writes=[bxn])
        pv = pbf(tbank).rearrange("p (c t) -> p c t", c=8)
        for c in range(8):
            T.op("pe", lambda e: e.transpose(pv[:, c, :], xn[:, c * 128:(c + 1) * 128], ident_b[:]),
                 reads=[bxn, b_identb], writes=[PB[tbank]])
        for c in range(8):
            if c % 2 == 0:
                T.op("act", lambda e: e.activation(out=hT_dst(c), in_=pv[:, c, :], func=AF.Identity,
                                                   scale=gmT_ap(c), bias=shT_ap(c)),
                     reads=[PB[tbank]] + bmods, writes=[bhT])
            else:
                T.op("dve", lambda e: e.tensor_scalar(out=hT_dst(c), in0=pv[:, c, :],
                                                      scalar1=gmT_ap(c), scalar2=shT_ap(c),
                                                      op0=ALU.mult, op1=ALU.add),
                     reads=[PB[tbank]] + bmods, writes=[bhT])

    def ga_bcast(es, l, chunk0, name):
        gb = salloc(es, name, [128, NSEQ, D], F32); bgb = Buf(name)
        dg = salloc(es, name + "dg", [128, 2, 128], F32); bdg = [Buf(name + "dg0"), Buf(name + "dg1")]
        k = 0
        for b in range(NSEQ):
            for half in range(2):
                bank = 6 + half
                for cc in range(4):
                    c = half * 4 + cc
                    sl = k % 2
                    k += 1
                    T.op("dve", lambda e: e.tensor_scalar(
                        out=dg[:, sl, :], in0=ident_f[:], scalar1=modT[:, l, chunk0 + c, b:b + 1],
                        scalar2=None, op0=ALU.mult), reads=[b_identf, b_modT_], writes=[bdg[sl]])
                    T.op("pe", lambda e: e.matmul(pb[bank][:, cc * 128:(cc + 1) * 128], lhsT=ones_f[:],
                                                  rhs=dg[:, sl, :], start=(cc == 0), stop=True,
                                                  skip_group_check=True),
                         reads=[b_onesf, bdg[sl]], writes=[PB[bank]])
                T.op("act", lambda e: e.activation(out=gb[:, b, half * 512:(half + 1) * 512],
                                                   in_=pb[bank][:], func=AF.Copy),
                     reads=[PB[bank]], writes=[bgb])
        return gb, bgb

    def load_A_weights(l, Wf, bWf, Wt, bWt):
        wl = wb_in[l].rearrange("(c p) n -> p c n", p=128)
        for (a, b, o) in [(0, 1024, 0), (1536, 2560, 1024), (2688, 3200, 2048),
                          (3200, 3264, 2560), (3200, 3264, 2624)]:
            for c in range(8):
                T.dma("sp", Wf[:, c, o:o + (b - a)], wl[:, c, a:b], bWf, reads=[B_wb_in[l]], writes=[bWf])
        for (a, b, o) in [(1024, 1536, 0), (2560, 2688, 512), (3264, 3272, 640)]:
            T.dma("sp", Wt[:, :, o:o + (b - a)], wl[:, :, a:b], bWt, reads=[B_wb_in[l]], writes=[bWt])

    def phase_A(l, pre=None):
        with ExitStack() as es:
            if pre is None:
                bg_flush(l)
                Wf = salloc(es, "Wf", [128, 8, 2688], BF16); bWf = Buf("Wf")
                Wt = salloc(es, "Wt", [128, 8, 648], BF16); bWt = Buf("Wt")
                load_A_weights(l, Wf, bWf, Wt, bWt)
            else:
                Wf, bWf, Wt, bWt = pre
            kvg = salloc(es, "kvg", [128, 128], F32); bkvg = Buf("kvg")
            T.dma("sp", kvg[:], kvg_bc[:, l, :], bkvg, writes=[bkvg])
            xts = [salloc(es, f"xt{i}", [128, D], F32) for i in range(3)]
            bxts = [Buf(f"xt{i}") for i in range(3)]
            xns = [salloc(es, f"xn{i}", [128, D], BF16) for i in range(2)]
            bxns = [Buf(f"xn{i}") for i in range(2)]
            hTs = [salloc(es, f"hT{i}", [128, 8, 512], BF16) for i in range(2)]
            bhTs = [Buf(f"hT{i}") for i in range(2)]
            vts = [salloc(es, f"vt{i}", [128, 512], BF16) for i in range(2)]
            bvts = [Buf(f"vt{i}") for i in range(2)]
            kvn = [salloc(es, f"kvn{i}", [128, 128], BF16) for i in range(2)]
            bkvn = [Buf(f"kvn{i}") for i in range(2)]
            kvTt = [salloc(es, f"kvTt{i}", [128, 128], BF16) for i in range(2)]
            bkvTt = [Buf(f"kvTt{i}") for i in range(2)]
            wis = [salloc(es, f"wis{i}", [128, 8], F32) for i in range(2)]
            bwis = [Buf(f"wis{i}") for i in range(2)]
            fos = [salloc(es, f"fo{i}", [128, 512], BF16) for i in range(3)]
            bfos = [Buf(f"fo{i}") for i in range(3)]
            src = x_in if l == 0 else xres
            cnt = {"ti": 0, "fi": 0}
            groups = [(s, g) for s in range(NSEQ) for g in range(4)]

            def prep_a(gi, j):
                s, g = groups[gi]
                hT = hTs[gi % 2]; bhT = bhTs[gi % 2]
                tt = g * 4 + j
                t0 = tt * 128
                ti = cnt["ti"]
                cnt["ti"] += 1
                xt = xts[ti % 3]; bxt = bxts[ti % 3]
                xn = xns[ti % 2]; bxn = bxns[ti % 2]
                T.dma("sp", xt[:], src[s, t0:t0 + 128, :], bxt, writes=[bxt])
                norm_to_T(xt[:], bxt,
                          lambda c: gm1T[:, l, c, s:s + 1],
                          lambda c: modT[:, l, 0 + c, s:s + 1],
                          [b_gm1, b_modT_], xn, bxn, ti % 2,
                          lambda c: hT[:, c, j * 128:(j + 1) * 128], bhT)
                return ti

            def prep_b(gi, j, ti):
                s, g = groups[gi]
                hT = hTs[gi % 2]; bhT = bhTs[gi % 2]
                t0 = (g * 4 + j) * 128
                k2 = ti % 2
                for c in range(8):
                    T.op("pe", lambda e: e.matmul(pb[2][:, 0:512], lhsT=hT[:, c, j * 128:(j + 1) * 128],
                                                  rhs=Wt[:, c, 0:512], start=(c == 0), stop=(c == 7)),
                         reads=[bhT, bWt], writes=[PB[2]])
                for c in range(8):
                    T.op("pe", lambda e: e.matmul(pb[3][:, 0:136], lhsT=hT[:, c, j * 128:(j + 1) * 128],
                                                  rhs=Wt[:, c, 512:648], start=(c == 0), stop=(c == 7)),
                         reads=[bhT, bWt], writes=[PB[3]])
                T.op("act", lambda e: e.activation(out=vts[k2][:], in_=pb[2][:, 0:512], func=AF.Copy),
                     reads=[PB[2]], writes=[bvts[k2]])
                T.dma("pool", v_scr[s, t0:t0 + 128, :], vts[k2][:], bvts[k2], reads=[bvts[k2]])
                rs2, brs2 = rstd_from(pb[3][:, 0:128], [PB[3]], 128)
                T.op("dve", lambda e: e.scalar_tensor_tensor(
                    out=kvn[k2][:], in0=pb[3][:, 0:128], scalar=rs2, in1=kvg[:],
                    op0=ALU.mult, op1=ALU.mult), reads=[PB[3], brs2, bkvg], writes=[bkvn[k2]])
                T.op("dve", lambda e: e.tensor_copy(out=wis[k2][:], in_=pb[3][:, 128:136]),
                     reads=[PB[3]], writes=[bwis[k2]])
                T.dma("pool", kv_tok[s, t0:t0 + 128, :], kvn[k2][:], bkvn[k2], reads=[bkvn[k2]])
                T.dma("pool", widx[s, t0:t0 + 128, :], wis[k2][:], bwis[k2], reads=[bwis[k2]])
                T.op("pe", lambda e: e.transpose(pbf(4)[:, 0:128], kvn[k2][:], ident_b[:]),
                     reads=[bkvn[k2], b_identb], writes=[PB[4]])
                T.op("act", lambda e: e.activation(out=kvTt[k2][:], in_=pbf(4)[:, 0:128], func=AF.Copy),
                     reads=[PB[4]], writes=[bkvTt[k2]])
                T.dma("pool", kvT_scr[s, :, t0:t0 + 128], kvTt[k2][:], bkvTt[k2], reads=[bkvTt[k2]])

            def fm_chunk(gi, ch):
                s, g = groups[gi]
                hT = hTs[gi % 2]; bhT = bhTs[gi % 2]
                fi = cnt["fi"]
                cnt["fi"] += 1
                bank = 5 + (fi % 3)
                fo = fos[fi % 3]; bfo = bfos[fi % 3]
                for c in range(8):
                    T.op("pe", lambda e: e.matmul(pb[bank][:, 0:512], lhsT=Wf[:, c, ch * 128:(ch + 1) * 128],
                                                  rhs=hT[:, c, :], start=(c == 0), stop=(c == 7)),
                         reads=[bhT, bWf], writes=[PB[bank]])
                if fi % 2 == 0:
                    T.op("act", lambda e: e.activation(out=fo[:], in_=pb[bank][:, 0:512], func=AF.Copy),
                         reads=[PB[bank]], writes=[bfo])
                else:
                    T.op("dve", lambda e: e.tensor_copy(out=fo[:], in_=pb[bank][:, 0:512]),
                         reads=[PB[bank]], writes=[bfo])
                T.dma("pool", featT[s, ch, :, g * 512:(g + 1) * 512], fo[:], bfo, reads=[bfo])

            for j in range(4):
                ti0 = prep_a(0, j)
                prep_b(0, j, ti0)
            for gi in range(len(groups)):
                pend_b = None
                for ch in range(21):
                    fm_chunk(gi, ch)
                    if gi + 1 < len(groups):
                        if ch in (1, 6, 11, 16):
                            j = (1, 6, 11, 16).index(ch)
                            pend_b = (j, prep_a(gi + 1, j))
                        if ch in (4, 9, 14, 19) and pend_b is not None:
                            prep_b(gi + 1, pend_b[0], pend_b[1])
                            pend_b = None
            T.barrier()

    def phase_B(l):
        with ExitStack() as es:
            qTs = [salloc(es, f"qT{i}", [128, 4, S], BF16) for i in range(NSEQ)]
            kTs = [salloc(es, f"kT{i}", [128, 4, S], BF16) for i in range(NSEQ)]
            vvs = [salloc(es, f"vv{i}", [128, NT, 512], BF16) for i in range(NSEQ)]
            bqTs = [Buf(f"qT{i}") for i in range(NSEQ)]
            bkTs = [Buf(f"kT{i}") for i in range(NSEQ)]
            bvvs = [Buf(f"vv{i}") for i in range(NSEQ)]
            def load_seq_B(s_, gate=()):
                g = list(gate)
                T.dma("sp", qTs[s_][:], featT[s_, 0:4].rearrange("c p t -> p c t"), bqTs[s_], reads=g, writes=[bqTs[s_]])
                T.dma("sp", kTs[s_][:], featT[s_, 4:8].rearrange("c p t -> p c t"), bkTs[s_], reads=g, writes=[bkTs[s_]])
                T.dma("sp", vvs[s_][:], v_scr[s_].rearrange("(n p) f -> p n f", p=128), bvvs[s_], reads=g, writes=[bvvs[s_]])
            load_seq_B(0)
            cur = {}
            tri8 = salloc(es, "tri8", [128, 128], BF16); btri = Buf("tri8")
            neg8 = salloc(es, "neg8", [128, 128], BF16); bneg = Buf("neg8")
            cm = salloc(es, "cm", [128, 512], BF16); bcm = Buf("cm")
            goa = salloc(es, "goa", [128, 512], F32); bgoa = Buf("goa")
            T.dma("pool", tri8[:], c_tri8, btri, writes=[btri])
            T.dma("pool", neg8[:], c_neg8, bneg, writes=[bneg])
            T.dma("pool", cm[:], c_cmask[:, 0:4, :].rearrange("p h t -> p (h t)"), bcm, writes=[bcm])
            T.dma("sp", goa[:], goa_bc[:, l, :], bgoa, writes=[bgoa])
            e32 = [[salloc(es, f"e32{g}{i}", [128, 512], F32) for i in range(2)] for g in range(2)]
            be32 = [[Buf(f"e32{g}{i}") for i in range(2)] for g in range(2)]
            spb = [[salloc(es, f"spb{g}{i}", [128, 512], BF16) for i in range(2)] for g in range(2)]
            bspb = [[Buf(f"spb{g}{i}") for i in range(2)] for g in range(2)]
            wb = [[salloc(es, f"wb{g}{i}", [128, 512], BF16) for i in range(2)] for g in range(2)]
            bwb = [[Buf(f"wb{g}{i}") for i in range(2)] for g in range(2)]
            sps = [salloc(es, f"sps{g}", [128, 512], F32) for g in range(2)]
            bsps = [Buf(f"sps{g}") for g in range(2)]
            spsb = [[salloc(es, f"spsb{g}{i}", [128, 512], BF16) for i in range(2)] for g in range(2)]
            bspsb = [[Buf(f"spsb{g}{i}") for i in range(2)] for g in range(2)]
            oan = [salloc(es, f"oan{i}", [128, 512], BF16) for i in range(2)]
            boan = [Buf(f"oan{i}") for i in range(2)]
            ZB = [[0, 1], [2, 3]]
            AB = [4, 5]
            OB = [6, 7]
            carry_slot = [0, 0]

            def zmm(bank, hg, qb, kb, start_first):
                for i in range(4):
                    h = 2 * i + hg
                    ch = h // 2
                    r0 = (h % 2) * 64
                    T.op("pe", lambda e: e.matmul(
                        pb[bank][:, i * 128:(i + 1) * 128],
                        lhsT=cur['kT'][r0:r0 + 64, ch, kb * 128:(kb + 1) * 128],
                        rhs=cur['qT'][r0:r0 + 64, ch, qb * 128:(qb + 1) * 128],
                        start=(start_first and i == 0), stop=(i == 3),
                        skip_group_check=True),
                        reads=[cur['bkT'], cur['bqT']], writes=[PB[bank]])

            def stage_Z(n, qb, kb):
                sl = n % 2
                for hg in range(2):
                    zmm(ZB[hg][sl], hg, qb, kb, True)
                for hg in range(2):
                    zb = ZB[hg][sl]
                    T.op("act", lambda e: e.activation(out=e32[hg][sl][:], in_=pb[zb][:], func=AF.Exp, scale=0.125),
                         reads=[PB[zb]], writes=[be32[hg][sl]])
                    T.op("act", lambda e: e.activation(out=spb[hg][sl][:], in_=e32[hg][sl][:], func=AF.Ln, bias=1.0),
                         reads=[be32[hg][sl]], writes=[bspb[hg][sl]])
                    if kb == qb:
                        T.op("dve", lambda e: e.tensor_tensor(out=spb[hg][sl][:], in0=spb[hg][sl][:], in1=cm[:], op=ALU.mult),
                             reads=[bspb[hg][sl], bcm], writes=[bspb[hg][sl]])

            def stage_A(n, qb, kb):
                sl = n % 2
                for hg in range(2):
                    ab = AB[hg]
                    T.op("pe", lambda e: e.matmul(pb[ab][:], lhsT=tri8[:], rhs=spb[hg][sl][:], start=True, stop=False,
                                                  skip_group_check=True),
                         reads=[btri, bspb[hg][sl]], writes=[PB[ab]])
                    if kb < qb:
                        cs = carry_slot[hg]
                        T.op("pe", lambda e: e.matmul(pb[ab][:], lhsT=neg8[:], rhs=spsb[hg][cs][:], start=False, stop=False,
                                                      skip_group_check=True),
                             reads=[bneg, bspsb[hg][cs]], writes=[PB[ab]])
                for hg in range(2):
                    zmm(AB[hg], hg, qb, kb, False)
                for hg in range(2):
                    ab = AB[hg]
                    T.op("act", lambda e: e.activation(out=wb[hg][sl][:], in_=pb[ab][:], func=AF.Exp, scale=0.125),
                         reads=[PB[ab]], writes=[bwb[hg][sl]])
                    if kb == qb:
                        T.op("dve", lambda e: e.tensor_tensor(out=wb[hg][sl][:], in0=wb[hg][sl][:], in1=cm[:], op=ALU.mult),
                             reads=[bwb[hg][sl], bcm], writes=[bwb[hg][sl]])
                    if kb > 0:
                        if kb == qb:
                            T.op("dve", lambda e: e.tensor_copy(out=sps[hg][:], in_=spb[hg][sl][:]),
                                 reads=[bspb[hg][sl]], writes=[bsps[hg]])
                        else:
                            T.op("dve", lambda e: e.tensor_tensor(out=sps[hg][:], in0=sps[hg][:], in1=spb[hg][sl][:], op=ALU.add),
                                 reads=[bsps[hg], bspb[hg][sl]], writes=[bsps[hg]])
                        carry_slot[hg] ^= 1
                        cs = carry_slot[hg]
                        T.op("dve", lambda e: e.tensor_copy(out=spsb[hg][cs][:], in_=sps[hg][:]),
                             reads=[bsps[hg]], writes=[bspsb[hg][cs]])

            def stage_PV(n, s, qb, kb):
                sl = n % 2
                ob = OB[qb % 2]
                for hg in range(2):
                    for i in range(4):
                        h = 2 * i + hg
                        T.op("pe", lambda e: e.matmul(
                            pb[ob][:, h * 64:(h + 1) * 64], lhsT=wb[hg][sl][:, i * 128:(i + 1) * 128],
                            rhs=cur['vv'][:, kb, h * 64:(h + 1) * 64], start=(kb == qb and hg == 0 and i == 0), stop=False,
                            skip_group_check=True),
                            reads=[bwb[hg][sl], cur['bvv']], writes=[PB[ob]])
                if kb == 0:
                    osl = qb % 2
                    rs, brs = rstd_from(pb[ob][:], [PB[ob]], 512)
                    T.op("dve", lambda e: e.scalar_tensor_tensor(out=oan[osl][:], in0=pb[ob][:], scalar=rs, in1=goa[:],
                                                                 op0=ALU.mult, op1=ALU.mult),
                         reads=[PB[ob], brs, bgoa], writes=[boan[osl]])
                    T.dma("pool", ocat[s, qb * 128:(qb + 1) * 128, 0:512], oan[osl][:], boan[osl], reads=[boan[osl]])
                    bg_step(1)
                    if s == 0 and LIM_SEQ > 1 and qb == min(3, LIM_QB - 1):
                        load_seq_B(1, gate=[boan[osl]])

            for s in range(LIM_SEQ):
                cur.update(qT=qTs[s], kT=kTs[s], vv=vvs[s], bqT=bqTs[s], bkT=bkTs[s], bvv=bvvs[s])
                its = [(qb, kb) for qb in range(LIM_QB) for kb in range(qb, -1, -1)]
                N = len(its)
                for t in range(N + 2):
                    if t < N:
                        stage_Z(t, *its[t])
                    if 0 <= t - 1 < N:
                        stage_A(t - 1, *its[t - 1])
                    if 0 <= t - 2 < N:
                        stage_PV(t - 2, s, *its[t - 2])
            T.barrier()

    def phase_C(l):
        if FLUSH_BG_BEFORE_C:
            bg_flush(99)
        with ExitStack() as es:
            cin = []
            for s_ in range(NSEQ):
                d_ = dict(
                    dqT=salloc(es, f"dqT{s_}", [128, 8, S], BF16), bdq=Buf(f"dqT{s_}"),
                    iqT=salloc(es, f"iqT{s_}", [128, 4, S], BF16), biq=Buf(f"iqT{s_}"),
                    ikT=salloc(es, f"ikT{s_}", [128, S], BF16), bik=Buf(f"ikT{s_}"),
                    kvT=salloc(es, f"kvT{s_}", [128, S], BF16), bkvT=Buf(f"kvT{s_}"),
                    kvt=salloc(es, f"kvt{s_}", [128, NT, 128], BF16), bkvt=Buf(f"kvt{s_}"),
                    wi=salloc(es, f"wi{s_}", [128, NT, 8], F32), bwi=Buf(f"wi{s_}"))
                cin.append(d_)
            cur = {}
            wuv = salloc(es, "wuv", [128, 8, 64], BF16); bwuv = Buf("wuv")
            gob = salloc(es, "gob", [128, 512], F32); bgob = Buf("gob")
            BN = salloc(es, "BN", [128, 2, 1024], BF16); bBN = Buf("BN")
            id4 = salloc(es, "id4", [128, 512], BF16); bid4 = Buf("id4")
            pw2 = salloc(es, "pw2", [128, KBIS], F32); bpw2 = Buf("pw2")
            T.dma("pool", wuv[:], w_uv[l].rearrange("h r d -> r h d"), bwuv, writes=[bwuv])
            T.dma("sp", gob[:], gob_bc[:, l, :], bgob, writes=[bgob])
            T.dma("sp", pw2[:], c_pow2, bpw2, writes=[bpw2])
            for i in range(4):
                T.dma("pool", id4[:, i * 128:(i + 1) * 128], c_ident, bid4, writes=[bid4])
            score = [salloc(es, f"score{i}", [128, S], F32) for i in range(2)]
            bsc = [Buf(f"score{i}") for i in range(2)]
            nmask = [salloc(es, f"nmask{i}", [128, S], BF16) for i in range(2)]
            bnm = [Buf(f"nmask{i}") for i in range(2)]
            PP = [salloc(es, f"PP{i}", [128, 1024], BF16) for i in range(2)]
            bPP = [Buf(f"PP{i}") for i in range(2)]
            oTs = salloc(es, "oTs", [128, 1024], BF16); boTs = Buf("oTs")
            bis = salloc(es, "bis", [128, 8 + 2 * KBIS], F32); bbis = Buf("bis")
            rden = salloc(es, "rden", [128, 8], F32); brden = Buf("rden")
            obf = salloc(es, "obf", [128, 512], F32); bobf = Buf("obf")
            obn = [salloc(es, f"obn{i}", [128, 512], BF16) for i in range(2)]
            bobn = [Buf(f"obn{i}") for i in range(2)]
            lsc = 128 ** -0.5
            isc = (64 ** -0.5) * (8 ** -0.5)
            with ExitStack() as es2:
                bn = salloc(es2, "bn", [128, 2, 1024], F32); bbn = Buf("bn")
                bf = salloc(es2, "bf", [128, 1024], F32); bbf = Buf("bf")
                adn = salloc(es2, "adn", [128, 1024], F32); badn = Buf("adn")
                T.dma("sp", bn[:], biasn.rearrange("p o h t -> p o (h t)"), bbn, writes=[bbn])
                T.dma("sp", bf[:], bfar_bc.rearrange("p h t -> p (h t)"), bbf, writes=[bbf])
                T.dma("sp", adn[:], c_admneg.rearrange("p h t -> p (h t)"), badn, writes=[badn])
                for o in range(2):
                    T.op("dve", lambda e: e.tensor_tensor(out=bn[:, o, :], in0=bn[:, o, :], in1=bf[:], op=ALU.subtract),
                         reads=[bbn, bbf], writes=[bbn])
                    if o == 0:
                        T.op("dve", lambda e: e.scalar_tensor_tensor(out=BN[:, o, :], in0=bn[:, o, :], scalar=1.0 / lsc, in1=adn[:],
                                                                     op0=ALU.mult, op1=ALU.add),
                             reads=[bbn, badn], writes=[bBN])
                    else:
                        T.op("dve", lambda e: e.tensor_scalar(out=BN[:, o, :], in0=bn[:, o, :], scalar1=1.0 / lsc, scalar2=None,
                                                              op0=ALU.mult), reads=[bbn], writes=[bBN])
                T.barrier()
            cnt_i = {"ii": 0, "pi": 0, "oi": 0, "lp": 0, "ib": 0}
            IBS = (2, 3)
            SB = 4
            NRB = 2 * ACC_DEPTH + 2
            Rb = [salloc(es, f"Rb{i}", [128, 512], BF16) for i in range(NRB)]
            bRb = [Buf(f"Rb{i}") for i in range(NRB)]
            dg = [salloc(es, f"dg{i}", [128, 8, 128], BF16) for i in range(2)]
            bdg = [Buf(f"dg{i}") for i in range(2)]
            absw = salloc(es, "absw", [128, NT, 8], F32); babsw = Buf("absw")
            sgn = salloc(es, "sgn", [128, NT, 8], F32); bsgn = Buf("sgn")
            OTB = (5, 6)
            DB = 7

            pend_acc = []

            def flush_acc(keep=0):
                while len(pend_acc) > keep:
                    (qb, c0, w, j, rsl, dsl) = pend_acc.pop(0)
                    sc_ = score[qb % 2]; bsc_ = bsc[qb % 2]
                    T.op("pe", lambda e: e.matmul(pb[SB][:, 0:w], lhsT=dg[dsl][:, j, :], rhs=Rb[rsl][:, 0:w],
                                                  start=(j == 0), stop=(j == 7), skip_group_check=True),
                         reads=[bdg[dsl], bRb[rsl]], writes=[PB[SB]])
                    if j == 7:
                        T.op("act", lambda e: e.activation(out=sc_[:, c0:c0 + w], in_=pb[SB][:, 0:w], func=AF.Copy),
                             reads=[PB[SB]], writes=[bsc_])

            def make_diag(qb):
                dsl = qb % 2
                for j in range(8):
                    T.op("act", lambda e: e.activation(out=dg[dsl][:, j, :], in_=ident_b[:], func=AF.Identity,
                                                       scale=sgn[:, qb, j:j + 1]),
                         reads=[b_identb, bsgn], writes=[bdg[dsl]])

            def idx_unit(s, qb, c, j):
                n = (qb + 1) * 128
                q0 = qb * 128
                c0 = c * 512
                w = min(512, n - c0)
                rsl = cnt_i["ii"] % NRB
                cnt_i["ii"] += 1
                ch = j // 2
                r0 = (j % 2) * 64
                IB = IBS[cnt_i["ib"] % 2]
                cnt_i["ib"] += 1
                T.op("pe", lambda e: e.matmul(pb[IB][:, 0:w], lhsT=cur['iqT'][r0:r0 + 64, ch, q0:q0 + 128],
                                              rhs=cur['ikT'][r0:r0 + 64, c0:c0 + w], start=True, stop=True),
                     reads=[cur['biq'], cur['bik']], writes=[PB[IB]])
                T.op("act", lambda e: e.activation(out=Rb[rsl][:, 0:w], in_=pb[IB][:, 0:w], func=AF.Relu,
                                                   scale=absw[:, qb, j:j + 1]),
                     reads=[PB[IB], babsw], writes=[bRb[rsl]])
                flush_acc(keep=ACC_DEPTH - 1)
                pend_acc.append((qb, c0, w, j, rsl, qb % 2))

            def select(qb):
                n = (qb + 1) * 128
                nm = nmask[qb % 2]; bn_ = bnm[qb % 2]
                score_ = score[qb % 2]; bsc_ = bsc[qb % 2]
                T.op("dve", lambda e: e.memset(score_[0:64, n - 64:n], -1.0e30), reads=[bsc_], writes=[bsc_])
                if qb >= 2:
                    hi = bis[:, 0:1]; lo = bis[:, 1:2]; w0 = bis[:, 2:3]; mid = bis[:, 3:4]
                    cnt = bis[:, 4:5]; sv = bis[:, 5:6]; thr = bis[:, 6:7]
                    H = bis[:, 8:8 + KBIS]; H2 = bis[:, 8 + KBIS:8 + 2 * KBIS]
                    T.op("dve", lambda e: e.tensor_reduce(out=hi, in_=score_[:, 0:n], axis=AX.X, op=ALU.max),
                         reads=[bsc_], writes=[bbis])
                    T.op("dve", lambda e: e.tensor_reduce(out=lo, in_=score_[:, 0:n - 64], axis=AX.X, op=ALU.min),
                         reads=[bsc_], writes=[bbis])
                    T.op("dve", lambda e: e.tensor_tensor(out=w0, in0=hi, in1=lo, op=ALU.subtract),
                         reads=[bbis], writes=[bbis])
                    T.op("dve", lambda e: e.tensor_scalar(out=H, in0=pw2[:], scalar1=w0, scalar2=None, op0=ALU.mult),
                         reads=[bbis, bpw2], writes=[bbis])
                    T.op("dve", lambda e: e.tensor_scalar(out=H2, in0=H, scalar1=2.0, scalar2=None, op0=ALU.mult),
                         reads=[bbis], writes=[bbis])
                    T.op("dve", lambda e: e.tensor_tensor(out=mid, in0=lo, in1=bis[:, 8:9], op=ALU.add),
                         reads=[bbis], writes=[bbis])
                    for k in range(KBIS):
                        T.op("dve", lambda e: e.tensor_scalar(out=junk[:, 0:n], in0=score_[:, 0:n], scalar1=mid, scalar2=None,
                                                              op0=ALU.is_ge, op1=ALU.add, accum_out=cnt),
                             reads=[bsc_, bbis], writes=[b_junk, bbis])
                        if k < KBIS - 1:
                            T.op("dve", lambda e: e.tensor_scalar(out=sv, in0=cnt, scalar1=float(TOPK),
                                                                  scalar2=bis[:, 8 + KBIS + k + 1:8 + KBIS + k + 2],
                                                                  op0=ALU.is_ge, op1=ALU.mult),
                                 reads=[bbis], writes=[bbis])
                            T.op("dve", lambda e: e.scalar_tensor_tensor(out=mid, in0=sv, scalar=bis[:, 8 + k + 1:8 + k + 2],
                                                                         in1=mid, op0=ALU.subtract, op1=ALU.add),
                                 reads=[bbis], writes=[bbis])
                        else:
                            T.op("dve", lambda e: e.tensor_scalar(out=sv, in0=cnt, scalar1=float(TOPK),
                                                                  scalar2=bis[:, 8 + k:8 + k + 1],
                                                                  op0=ALU.is_ge, op1=ALU.mult),
                                 reads=[bbis], writes=[bbis])
                            T.op("dve", lambda e: e.scalar_tensor_tensor(out=thr, in0=sv, scalar=bis[:, 8 + k:8 + k + 1],
                                                                         in1=mid, op0=ALU.subtract, op1=ALU.add),
                                 reads=[bbis], writes=[bbis])
                    T.op("dve", lambda e: e.tensor_scalar(out=nm[:, 0:n], in0=score_[:, 0:n], scalar1=thr, scalar2=-1.0e5,
                                                          op0=ALU.is_lt, op1=ALU.mult), reads=[bsc_, bbis], writes=[bn_])
                else:
                    T.op("dve", lambda e: e.tensor_scalar(out=nm[:, 0:n], in0=score_[:, 0:n], scalar1=-1.0e29, scalar2=-1.0e5,
                                                          op0=ALU.is_lt, op1=ALU.mult), reads=[bsc_], writes=[bn_])

            def att_logits(s, qb, kb):
                q0 = qb * 128
                nm = nmask[qb % 2]; bn_ = bnm[qb % 2]
                lp = cnt_i["lp"] % 2
                cnt_i["lp"] += 1
                P = PP[lp]; bP = bPP[lp]
                off = qb - kb
                for (bank, h0) in ((0, 0), (1, 4)):
                    T.op("pe", lambda e: e.matmul(
                        pb[bank][:].rearrange("p (h t) -> p h t", h=4),
                        lhsT=cur['kvT'][:, kb * 128:(kb + 1) * 128], rhs=cur['dqT'][:, h0:h0 + 4, q0:q0 + 128],
                        start=True, stop=False, skip_group_check=True), reads=[cur['bkvT'], cur['bdq']], writes=[PB[bank]])
                    T.op("pe", lambda e: e.matmul(pb[bank][:], lhsT=nm[:, kb * 128:(kb + 1) * 128], rhs=id4[:],
                                                  start=False, stop=(off >= 2), skip_group_check=True),
                         reads=[bn_, bid4], writes=[PB[bank]])
                    if off < 2:
                        T.op("pe", lambda e: e.matmul(pb[bank][:], lhsT=ident_b[:], rhs=BN[:, off, h0 * 128:(h0 + 4) * 128],
                                                      start=False, stop=True, skip_group_check=True),
                             reads=[b_identb, bBN], writes=[PB[bank]])
                    T.op("act", lambda e: e.activation(out=P[:, h0 * 128:(h0 + 4) * 128], in_=pb[bank][:], func=AF.Exp, scale=lsc),
                         reads=[PB[bank]], writes=[bP])
                return lp

            def att_pv(s, qb, kb, lp):
                P = PP[lp]; bP = bPP[lp]
                for (bank, h0) in ((OTB[0], 0), (OTB[1], 4)):
                    T.op("pe", lambda e: e.matmul(pb[bank][:], lhsT=cur['kvt'][:, kb, :], rhs=P[:, h0 * 128:(h0 + 4) * 128],
                                                  start=(kb == 0), stop=(kb == qb)),
                         reads=[cur['bkvt'], bP], writes=[PB[bank]])
                for h in range(8):
                    T.op("pe", lambda e: e.matmul(pb[DB][:, h:h + 1], lhsT=P[:, h * 128:(h + 1) * 128], rhs=ones_b[:, 0:1],
                                                  start=(kb == 0 and h == 0), stop=(kb == qb), skip_group_check=True),
                         reads=[bP, b_onesb], writes=[PB[DB]])

            def epilogue(s, qb):
                q0 = qb * 128
                T.op("act", lambda e: e.activation(out=oTs[:, 0:512], in_=pb[OTB[0]][:], func=AF.Copy), reads=[PB[OTB[0]]], writes=[boTs])
                T.op("act", lambda e: e.activation(out=oTs[:, 512:1024], in_=pb[OTB[1]][:], func=AF.Copy), reads=[PB[OTB[1]]], writes=[boTs])
                T.op("dve", lambda e: e.reciprocal(out=rden[:], in_=pb[DB][:, 0:8]), reads=[PB[DB]], writes=[brden])
                eb = IBS[cnt_i["ib"] % 2]
                cnt_i["ib"] += 1
                for h in range(8):
                    T.op("pe", lambda e: e.matmul(pb[eb][:, h * 64:(h + 1) * 64], lhsT=oTs[:, h * 128:(h + 1) * 128],
                                                  rhs=wuv[:, h, :], start=(h == 0), stop=True, skip_group_check=True),
                         reads=[boTs, bwuv], writes=[PB[eb]])
                for h in range(8):
                    T.op("dve", lambda e: e.tensor_scalar(out=obf[:, h * 64:(h + 1) * 64], in0=pb[eb][:, h * 64:(h + 1) * 64],
                                                          scalar1=rden[:, h:h + 1], scalar2=None, op0=ALU.mult),
                         reads=[PB[eb], brden], writes=[bobf])
                rs, brs = rstd_from(obf[:], [bobf], 512)
                osl = cnt_i["oi"] % 2
                cnt_i["oi"] += 1
                T.op("dve", lambda e: e.scalar_tensor_tensor(out=obn[osl][:], in0=obf[:], scalar=rs, in1=gob[:],
                                                             op0=ALU.mult, op1=ALU.mult),
                     reads=[bobf, brs, bgob], writes=[bobn[osl]])
                T.dma("pool", ocat[s, q0:q0 + 128, 512:1024], obn[osl][:], bobn[osl], reads=[bobn[osl]])
                if s == 0 and LIM_SEQ > 1 and qb == min(3, LIM_QB - 1):
                    load_seq_C(1, gate=[bobn[osl]])

            def load_seq_C(s_, gate=()):
                d_ = cin[s_]
                g = list(gate)
                T.dma("sp", d_["iqT"][:], featT[s_, 16:20].rearrange("c p t -> p c t"), d_["biq"], reads=g, writes=[d_["biq"]])
                T.dma("sp", d_["ikT"][:], featT[s_, 20], d_["bik"], reads=g, writes=[d_["bik"]])
                T.dma("sp", d_["wi"][:], widx[s_].rearrange("(n p) j -> p n j", p=128), d_["bwi"], reads=g, writes=[d_["bwi"]])
                T.dma("sp", d_["kvT"][:], kvT_scr[s_], d_["bkvT"], reads=g, writes=[d_["bkvT"]])
                T.dma("sp", d_["kvt"][:], kv_tok[s_].rearrange("(n p) r -> p n r", p=128), d_["bkvt"], reads=g, writes=[d_["bkvt"]])
                T.dma("sp", d_["dqT"][:], featT[s_, 8:16].rearrange("c p t -> p c t"), d_["bdq"], reads=g, writes=[d_["bdq"]])
            load_seq_C(0)
            for s in range(LIM_SEQ):
                cur.clear(); cur.update(cin[s])
                wi = cur["wi"]; bwi = cur["bwi"]
                T.op("act", lambda e: e.activation(out=absw[:], in_=wi[:], func=AF.Abs, scale=isc),
                     reads=[bwi], writes=[babsw])
                T.op("dve", lambda e: e.tensor_scalar(out=sgn[:], in0=wi[:], scalar1=0.0, scalar2=2.0,
                                                      op0=ALU.is_ge, op1=ALU.mult), reads=[bwi], writes=[bsgn])
                T.op("dve", lambda e: e.tensor_scalar(out=sgn[:], in0=sgn[:], scalar1=-1.0, scalar2=None,
                                                      op0=ALU.add), reads=[bsgn], writes=[bsgn])
                for step in range(LIM_QB + 2):
                    qi = step
                    qs = step - 1
                    qa = step - 2
                    iu = []
                    if qi < LIM_QB:
                        n = (qi + 1) * 128
                        iu = [(c, j) for c in range((n + 511) // 512) for j in range(8)]
                        make_diag(qi)
                    if 0 <= qs < LIM_QB:
                        select(qs)
                    au = list(range(qa + 1)) if qa >= 0 else []
                    na, ni = len(au), len(iu)
                    ai = 0
                    ii_ = 0
                    pend = None
                    total = max(na, 1)
                    while ai < na or ii_ < ni:
                        tgt = ni if ai >= na else (ni * (ai + 1)) // total
                        while ii_ < tgt:
                            idx_unit(s, qi, *iu[ii_])
                            ii_ += 1
                        if ai < na:
                            lp = att_logits(s, qa, au[ai])
                            if pend is not None:
                                att_pv(s, qa, *pend)
                            pend = (au[ai], lp)
                            ai += 1
                    if pend is not None:
                        att_pv(s, qa, *pend)
                    flush_acc()
                    if qa >= 0:
                        epilogue(s, qa)
                        bg_step(1)
            T.barrier()

    def phase_D(l, prefetch=()):
        prefetch = list(prefetch)
        bg_flush(l)
        with ExitStack() as es:
            Wo = salloc(es, "Wo", [128, 8, D], BF16); bWo = Buf("Wo")
            wl = wb_out[l].rearrange("(c p) n -> p c n", p=128)
            for c in range(8):
                T.dma("sp", Wo[:, c, :], wl[:, c, :], bWo, reads=[B_wb_out[l]], writes=[bWo])
            gb, bgb = ga_bcast(es, l, 16, "ga1")
            oc = [salloc(es, f"oc{i}", [128, D], BF16) for i in range(2)]
            boc = [Buf(f"oc{i}") for i in range(2)]
            oT = [salloc(es, f"oT{i}", [128, 8, 128], BF16) for i in range(2)]
            boT = [Buf(f"oT{i}") for i in range(2)]
            xts = [salloc(es, f"xd{i}", [128, D], F32) for i in range(2)]
            bxts = [Buf(f"xd{i}") for i in range(2)]
            tmp = [salloc(es, f"tm{i}", [128, D], F32) for i in range(2)]
            btmp = [Buf(f"tm{i}") for i in range(2)]
            src = x_in if l == 0 else xres
            ti = 0
            for s in range(NSEQ):
                for tt in range(NT):
                    t0 = tt * 128
                    k2 = ti % 2
                    ti += 1
                    T.dma("sp", oc[k2][:], ocat[s, t0:t0 + 128, :], boc[k2], writes=[boc[k2]])
                    T.dma("sp", xts[k2][:], src[s, t0:t0 + 128, :], bxts[k2], writes=[bxts[k2]])
                    if prefetch:
                        prefetch.pop(0)()
                    pv = pbf(k2).rearrange("p (c t) -> p c t", c=8)
                    for c in range(8):
                        T.op("pe", lambda e: e.transpose(pv[:, c, :], oc[k2][:, c * 128:(c + 1) * 128], ident_b[:]),
                             reads=[boc[k2], b_identb], writes=[PB[k2]])
                    T.op("act", lambda e: e.activation(out=oT[k2][:].rearrange("p c t -> p (c t)"), in_=pbf(k2)[:, 0:1024], func=AF.Copy),
                         reads=[PB[k2]], writes=[boT[k2]])
                    for half in range(2):
                        bank = 2 + k2 * 2 + half
                        for c in range(8):
                            T.op("pe", lambda e: e.matmul(pb[bank][:], lhsT=oT[k2][:, c, :], rhs=Wo[:, c, half * 512:(half + 1) * 512],
                                                          start=(c == 0), stop=(c == 7)),
                                 reads=[boT[k2], bWo], writes=[PB[bank]])
                        T.op("dve", lambda e: e.tensor_tensor(out=tmp[k2][:, half * 512:(half + 1) * 512], in0=pb[bank][:],
                                                              in1=gb[:, s, half * 512:(half + 1) * 512], op=ALU.mult),
                             reads=[PB[bank], bgb], writes=[btmp[k2]])
                    T.op("pool", lambda e: e.tensor_tensor(out=tmp[k2][:], in0=tmp[k2][:], in1=xts[k2][:], op=ALU.add),
                         reads=[btmp[k2], bxts[k2]], writes=[btmp[k2]])
                    T.dma("pool", xres[s, t0:t0 + 128, :], tmp[k2][:], btmp[k2], reads=[btmp[k2]])
            while prefetch:
                prefetch.pop(0)()
            T.barrier()

    def phase_DE(l, last):
        with ExitStack() as esw:
            Wu = salloc(esw, "Wu", [128, 8, DFF], BF16); bWu = Buf("Wu")
            Wd = salloc(esw, "Wd", [128, 32, D], BF16); bWd = Buf("Wd")
            wul = wb_up[l].rearrange("(c p) n -> p c n", p=128)
            wdl = wb_dn[l].rearrange("(c p) n -> p c n", p=128)
            pf = []
            for c in range(8):
                for hh in range(2):
                    pf.append(lambda c=c, hh=hh: T.dma(
                        "sp", Wu[:, c, hh * 2048:(hh + 1) * 2048], wul[:, c, hh * 2048:(hh + 1) * 2048], bWu,
                        reads=[B_wb_up[l]], writes=[bWu]))
            for c4 in range(8):
                pf.append(lambda c4=c4: T.dma(
                    "sp", Wd[:, c4 * 4:(c4 + 1) * 4, :], wdl[:, c4 * 4:(c4 + 1) * 4, :], bWd,
                    reads=[B_wb_dn[l]], writes=[bWd]))
            phase_D(l, prefetch=pf)
            phase_E(l, last, Wu, bWu, Wd, bWd)

    def phase_E(l, last, Wu, bWu, Wd, bWd):
        with ExitStack() as es:
            gb, bgb = ga_bcast(es, l, 40, "ga2")
            if last:
                gf = salloc(es, "gf", [128, D], F32); bgf = Buf("gf")
                T.dma("sp", gf[:], gfin_bc, bgf, writes=[bgf])
            TG = 256
            xts = [salloc(es, f"xe{i}", [128, D], F32) for i in range(4)]
            bxts = [Buf(f"xe{i}") for i in range(4)]
            xns = [salloc(es, f"xne{i}", [128, D], BF16) for i in range(2)]
            bxns = [Buf(f"xne{i}") for i in range(2)]
            hTs = [salloc(es, f"hTe{i}", [128, 8, TG], BF16) for i in range(2)]
            bhTs = [Buf(f"hTe{i}") for i in range(2)]
            aT = salloc(es, "aT", [128, 32, TG], BF16); baT = [Buf(f"aT{i}") for i in range(32)]
            rl = [salloc(es, f"rl{i}", [128, TG], BF16) for i in range(2)]
            brl = [Buf(f"rl{i}") for i in range(2)]
            ti = 0
            gi = 0
            ui = 0
            for s in range(NSEQ):
                for g in range(S // TG):
                    hT = hTs[gi % 2]; bhT = bhTs[gi % 2]
                    gi += 1
                    tiles = []
                    for j in range(TG // 128):
                        tt = g * (TG // 128) + j
                        t0 = tt * 128
                        xt = xts[ti % 4]; bxt = bxts[ti % 4]
                        xn = xns[ti % 2]; bxn = bxns[ti % 2]
                        tb = ti % 2
                        ti += 1
                        tiles.append((t0, xt, bxt))
                        T.dma("sp", xt[:], xres[s, t0:t0 + 128, :], bxt, writes=[bxt])
                        norm_to_T(xt[:], bxt,
                                  lambda c: gm2T[:, l, c, s:s + 1],
                                  lambda c: modT[:, l, 24 + c, s:s + 1],
                                  [b_gm2, b_modT_], xn, bxn, tb,
                                  lambda c: hT[:, c, j * 128:(j + 1) * 128], bhT)
                    for f in range(32):
                        bank = 2 + (ui % 2)
                        sl = ui % 2
                        ui += 1
                        for c in range(8):
                            T.op("pe", lambda e: e.matmul(pb[bank][:, 0:TG], lhsT=Wu[:, c, f * 128:(f + 1) * 128], rhs=hT[:, c, :],
                                                          start=(c == 0), stop=(c == 7)),
                                 reads=[bWu, bhT], writes=[PB[bank]])
                        T.op("act", lambda e: e.activation(out=rl[sl][:], in_=pb[bank][:, 0:TG], func=AF.Relu),
                             reads=[PB[bank]], writes=[brl[sl]])
                        T.op("pool" if f % 2 else "dve", lambda e: e.tensor_tensor(out=aT[:, f, :], in0=rl[sl][:], in1=rl[sl][:], op=ALU.mult),
                             reads=[brl[sl]], writes=[baT[f]])
                    for j, (t0, xt, bxt) in enumerate(tiles):
                        for half in range(2):
                            bank = 4 + (j % 2) * 2 + half
                            for f in range(32):
                                T.op("pe", lambda e: e.matmul(pb[bank][:], lhsT=aT[:, f, j * 128:(j + 1) * 128],
                                                              rhs=Wd[:, f, half * 512:(half + 1) * 512], start=(f == 0), stop=(f == 31)),
                                     reads=[baT[f], bWd], writes=[PB[bank]])
                            T.op("dve", lambda e: e.tensor_tensor(out=tmpE[j % 2][:, half * 512:(half + 1) * 512], in0=pb[bank][:],
                                                                  in1=gb[:, s, half * 512:(half + 1) * 512], op=ALU.mult),
                                 reads=[PB[bank], bgb], writes=[btmpE[j % 2]])
                        T.op("pool", lambda e: e.tensor_tensor(out=xt[:], in0=tmpE[j % 2][:], in1=xt[:], op=ALU.add),
                             reads=[btmpE[j % 2], bxt], writes=[bxt])
                        if not last:
                            T.dma("pool", xres[s, t0:t0 + 128, :], xt[:], bxt, reads=[bxt])
                        else:
                            rs, brs = rstd_from(xt[:], [bxt], D)
                            T.op("dve", lambda e: e.scalar_tensor_tensor(out=tmpE[j % 2][:], in0=xt[:], scalar=rs, in1=gf[:],
                                                                         op0=ALU.mult, op1=ALU.mult),
                                 reads=[bxt, brs, bgf], writes=[btmpE[j % 2]])
                            T.dma("pool", out_d[s, t0:t0 + 128, :], tmpE[j % 2][:], btmpE[j % 2], reads=[btmpE[j % 2]])
            T.barrier()

    tmpE = []
    btmpE = [Buf("tmpE0"), Buf("tmpE1")]

    tmpE.append(salloc(ges, "tmpE0", [128, D], F32))
    tmpE.append(salloc(ges, "tmpE1", [128, D], F32))

    convert_weights()
    esA0 = ExitStack()
    Wf0 = salloc(esA0, "Wf0", [128, 8, 2688], BF16); bWf0 = Buf("Wf0")
    Wt0 = salloc(esA0, "Wt0", [128, 8, 648], BF16); bWt0 = Buf("Wt0")
    preA = (Wf0, bWf0, Wt0, bWt0)
    prologue(preA)
    for l in range(nlayers):
        if "A" in phases:
            phase_A(l, pre=preA if l == 0 else None)
        if l == 0:
            esA0.close()
        if "B" in phases:
            phase_B(l)
        if "C" in phases:
            phase_C(l)
        if "D" in phases and "E" in phases:
            phase_DE(l, last=(l == nlayers - 1))
        elif "D" in phases:
            phase_D(l)
    T.barrier()
    ges.close()
    return nc, T


def _t5_bucket(rel):
    nb = 16
    max_exact = 8
    base = np.where(rel > 0, nb, 0)
    n = np.abs(rel)
    nf = np.maximum(n, max_exact).astype(np.float32)
    large = max_exact + (np.log(nf / np.float32(max_exact)) / np.float32(math.log(128 / max_exact))
                         * np.float32(nb - max_exact)).astype(np.int32)
    large = np.minimum(large, nb - 1)
    return base + np.where(n < max_exact, n, large)


def _consts():
    p = np.arange(128)
    c = {}
    c["c_ident"] = np.eye(128, dtype=np.float32)
    c["c_tri8"] = np.where(p[:, None] >= p[None, :], -8.0, 0.0).astype(np.float32)
    c["c_neg8"] = np.full((128, 128), -8.0, np.float32)
    cm = (p[:, None] < p[None, :]).astype(np.float32)
    c["c_cmask"] = np.ascontiguousarray(np.broadcast_to(cm[:, None, :], (128, 8, 128)))
    adm = ((p[:, None] // 64) <= (p[None, :] // 64)).astype(np.float32)
    c["c_adm"] = np.ascontiguousarray(np.broadcast_to(adm[:, None, :], (128, 8, 128)))
    c["c_admneg"] = np.ascontiguousarray(np.broadcast_to(np.where(adm > 0, 0.0, -1.0e5).astype(np.float32)[:, None, :], (128, 8, 128)))
    c["c_pow2"] = np.ascontiguousarray(np.broadcast_to(
        (0.5 ** np.arange(1, KBIS + 1)).astype(np.float32)[None, :], (128, KBIS)))
    c["c_ones"] = np.ones((128, 128), np.float32)
    return c


def _prep_inputs(inp, core):
    f = np.float32
    b0 = core * NSEQ
    bs = slice(b0, b0 + NSEQ)
    m = {}
    m["x"] = np.ascontiguousarray(inp["x"][bs], dtype=f)
    c = np.asarray(inp["c"], dtype=f)[bs]
    m["cT"] = np.ascontiguousarray(c.reshape(NSEQ, 8, 128).transpose(2, 1, 0))
    m["w_mod"] = np.ascontiguousarray(inp["w_mod"], dtype=f)
    bm = np.asarray(inp["b_mod"], dtype=f).reshape(2, 48, 128).transpose(2, 0, 1)
    m["b_modT"] = np.ascontiguousarray(np.broadcast_to(bm[..., None], (128, 2, 48, NSEQ)))
    ga = np.asarray(inp["g_attn"], dtype=f).reshape(2, 8, 128).transpose(2, 0, 1)
    m["g_attnT"] = np.ascontiguousarray(np.broadcast_to(ga[..., None], (128, 2, 8, NSEQ)))
    gm = np.asarray(inp["g_mlp"], dtype=f).reshape(2, 8, 128).transpose(2, 0, 1)
    m["g_mlpT"] = np.ascontiguousarray(np.broadcast_to(gm[..., None], (128, 2, 8, NSEQ)))
    m["w_in"] = np.ascontiguousarray(inp["w_in"], dtype=f)
    m["kvg_bc"] = np.ascontiguousarray(np.broadcast_to(np.asarray(inp["kv_norm_g"], dtype=f)[None], (128, 2, 128)))
    m["w_uv"] = np.ascontiguousarray(inp["w_uv"], dtype=f)
    m["goa_bc"] = np.ascontiguousarray(np.broadcast_to(np.asarray(inp["g_out_a"], dtype=f)[None], (128, 2, 512)))
    m["gob_bc"] = np.ascontiguousarray(np.broadcast_to(np.asarray(inp["g_out_b"], dtype=f)[None], (128, 2, 512)))
    m["w_out"] = np.ascontiguousarray(inp["w_out"], dtype=f)
    m["w_up"] = np.ascontiguousarray(inp["w_up"], dtype=f)
    m["w_down"] = np.ascontiguousarray(inp["w_down"], dtype=f)
    rb = np.asarray(inp["rel_bias"], dtype=f)
    p = np.arange(128)
    bn = np.empty((128, 2, 8, 128), f)
    for off in range(2):
        rel = (p[:, None] - off * 128) - p[None, :]
        bk = _t5_bucket(rel.astype(np.int32))
        bn[:, off] = rb[bk].transpose(0, 2, 1)
    m["biasn"] = bn
    far = rb[_t5_bucket(np.array([-1000], np.int32))[0]]
    m["bfar_bc"] = np.ascontiguousarray(np.broadcast_to(far[None, :, None], (128, 8, 128)))
    m["gfin_bc"] = np.ascontiguousarray(np.broadcast_to(np.asarray(inp["g_final"], dtype=f)[None], (128, D)))
    m.update(_consts())
    return m


_CACHE = {}


def kernel(**inputs):
    if "nc" not in _CACHE:
        _CACHE["nc"] = build_program()[0]
    nc = _CACHE["nc"]
    in_maps = [_prep_inputs(inputs, core) for core in range(8)]
    res = run_bass_kernel_spmd(nc, in_maps, core_ids=list(range(8)))
    out = np.concatenate([np.asarray(r["out"]) for r in res.results], axis=0)
    return out.astype(np.float32, copy=False)
```

```python
import math
from contextlib import ExitStack

import numpy as np
import concourse.bass as bass
import concourse.mybir as mybir
from concourse.bass_utils import run_bass_kernel_spmd

F32 = mybir.dt.float32
BF16 = mybir.dt.bfloat16
AF = mybir.ActivationFunctionType
ALU = mybir.AluOpType
AX = mybir.AxisListType

S = 2048
D = 1024
NSEQ = 2
NT = S // 128
DFF = 4096
DIN = 3272
EPS = 1e-6
KBIS = 16
TOPK = 256
EPOCH = 30000
LIM_SEQ = NSEQ
LIM_QB = NT
NO_POOL = False
ACC_DEPTH = 2
FLUSH_BG_BEFORE_C = False


class Buf:
    __slots__ = ("name", "last_w", "readers", "dsem", "bg")

    def __init__(self, name, bg=False):
        self.name = name
        self.last_w = None
        self.readers = []
        self.dsem = {}
        self.bg = bg


class Tracker:
    def __init__(self, nc):
        self.nc = nc
        self.eng = {"pe": nc.tensor, "act": nc.scalar, "dve": nc.vector,
                    "pool": nc.gpsimd, "sp": nc.sync}
        self.cnt = {e: 0 for e in self.eng}
        self.sems = {e: [] for e in self.eng}
        self.seen = {e: {} for e in self.eng}
        self.nsem = 0
        self.dma_bufs = []
        self.free_dsems = {"hw": [], "sw": []}
        self.nwaits = 0
        self.ninstr = 0

    def _newsem(self, name):
        self.nsem += 1
        return self.nc.alloc_semaphore(name=name)

    def _wait(self, e, tok):
        sem, val, src = tok
        key = id(sem)
        if self.seen[e].get(key, 0) >= val:
            return
        self.seen[e][key] = val
        self.eng[e].wait_ge(sem, val)
        self.nwaits += 1

    def _deps(self, e, reads, writes):
        for b in reads:
            if b.last_w is not None and not (b.last_w[2] == e and e == "pe"):
                self._wait(e, b.last_w)
        for b in writes:
            if b.last_w is not None and not (b.last_w[2] == e and e == "pe"):
                self._wait(e, b.last_w)
            for t in b.readers:
                if t[2] == e:
                    continue
                self._wait(e, t)

    def _commit(self, tok, reads, writes):
        for b in reads:
            b.readers.append(tok)
            if len(b.readers) > 12:
                d = {}
                for t in b.readers:
                    k = id(t[0])
                    if k not in d or d[k][1] < t[1]:
                        d[k] = t
                b.readers = list(d.values())
        for b in writes:
            b.last_w = tok
            b.readers = []

    def op(self, e, fn, reads=(), writes=()):
        self._deps(e, reads, writes)
        n = self.cnt[e]
        ep, v = divmod(n, EPOCH)
        while len(self.sems[e]) <= ep:
            self.sems[e].append(self._newsem(f"c_{e}_{len(self.sems[e])}"))
        sem = self.sems[e][ep]
        ins = fn(self.eng[e])
        ins.then_inc(sem, 1)
        self.cnt[e] = n + 1
        self.ninstr += 1
        tok = (sem, v + 1, e)
        self._commit(tok, reads, writes)
        return tok

    def dma(self, q, out, in_, sb, reads=(), writes=(), **kw):
        self._deps(q, reads, writes)
        kind = "sw" if q == "pool" else "hw"
        if kind not in sb.dsem:
            if self.free_dsems[kind]:
                sb.dsem[kind] = list(self.free_dsems[kind].pop())
            else:
                sb.dsem[kind] = [self._newsem(f"d{kind}_{sb.name}_{self.nsem}"), 0]
            self.dma_bufs.append((sb, kind))
        ent = sb.dsem[kind]
        ent[1] += 16
        ins = self.eng[q].dma_start(out=out, in_=in_, **kw)
        ins.then_inc(ent[0], 16)
        self.ninstr += 1
        tok = (ent[0], ent[1], "dma")
        self._commit(tok, reads, writes)
        return tok

    def barrier(self):
        toks = []
        for f in self.eng:
            n = self.cnt[f]
            if n == 0:
                continue
            ep, v = divmod(n - 1, EPOCH)
            toks.append((self.sems[f][ep], v + 1, f))
        for b, kind in self.dma_bufs:
            if not b.bg:
                toks.append((b.dsem[kind][0], b.dsem[kind][1], "dma"))
        for e in self.eng:
            for t in toks:
                if t[2] == e:
                    continue
                self._wait(e, t)
        keep = []
        for b, kind in self.dma_bufs:
            if b.bg:
                keep.append((b, kind))
            else:
                self.free_dsems[kind].append(tuple(b.dsem.pop(kind)))
        self.dma_bufs = keep


def build_program(nlayers=2, debug=False, phases="ABCDE"):
    nc = bass.Bass("TRN2", target_bir_lowering=False)
    T = Tracker(nc)
    uid = [0]

    def din(name, shape, dt=F32):
        return nc.dram_tensor(name, list(shape), dt, kind="ExternalInput").ap()

    def dscr(name, shape, dt):
        kind = "ExternalOutput" if debug else "Internal"
        return nc.dram_tensor(name, list(shape), dt, kind=kind).ap()

    x_in = din("x", [NSEQ, S, D])
    cT = din("cT", [128, 8, NSEQ])
    w_mod = din("w_mod", [2, D, 6 * D])
    b_modT = din("b_modT", [128, 2, 48, NSEQ])
    g_attnT = din("g_attnT", [128, 2, 8, NSEQ])
    g_mlpT = din("g_mlpT", [128, 2, 8, NSEQ])
    w_in = din("w_in", [2, D, DIN])
    kvg_bc = din("kvg_bc", [128, 2, 128])
    w_uv = din("w_uv", [2, 8, 128, 64])
    goa_bc = din("goa_bc", [128, 2, 512])
    gob_bc = din("gob_bc", [128, 2, 512])
    w_out = din("w_out", [2, D, D])
    w_up = din("w_up", [2, D, DFF])
    w_down = din("w_down", [2, DFF, D])
    biasn = din("biasn", [128, 2, 8, 128])
    bfar_bc = din("bfar_bc", [128, 8, 128])
    gfin_bc = din("gfin_bc", [128, D])
    c_ident = din("c_ident", [128, 128])
    c_tri8 = din("c_tri8", [128, 128])
    c_neg8 = din("c_neg8", [128, 128])
    c_cmask = din("c_cmask", [128, 8, 128])
    c_adm = din("c_adm", [128, 8, 128])
    c_admneg = din("c_admneg", [128, 8, 128])
    c_pow2 = din("c_pow2", [128, KBIS])
    c_ones = din("c_ones", [128, 128])
    out_d = nc.dram_tensor("out", [NSEQ, S, D], F32, kind="ExternalOutput").ap()

    xres = dscr("xres", [NSEQ, S, D], F32)
    featT = dscr("featT", [NSEQ, 21, 128, S], BF16)
    v_scr = dscr("v_scr", [NSEQ, S, 512], BF16)
    kv_tok = dscr("kv_tok", [NSEQ, S, 128], BF16)
    kvT_scr = dscr("kvT_scr", [NSEQ, 128, S], BF16)
    widx = dscr("widx", [NSEQ, S, 8], F32)
    ocat = dscr("ocat", [NSEQ, S, D], BF16)

    wb_in = [nc.dram_tensor(f"wb_in{l}", [D, DIN], BF16, kind="Internal").ap() for l in range(2)]
    wb_out = [nc.dram_tensor(f"wb_out{l}", [D, D], BF16, kind="Internal").ap() for l in range(2)]
    wb_up = [nc.dram_tensor(f"wb_up{l}", [D, DFF], BF16, kind="Internal").ap() for l in range(2)]
    wb_dn = [nc.dram_tensor(f"wb_dn{l}", [DFF, D], BF16, kind="Internal").ap() for l in range(2)]
    B_wb_in = [Buf(f"wb_in{l}", bg=True) for l in range(2)]
    B_wb_out = [Buf(f"wb_out{l}", bg=True) for l in range(2)]
    B_wb_up = [Buf(f"wb_up{l}", bg=True) for l in range(2)]
    B_wb_dn = [Buf(f"wb_dn{l}", bg=True) for l in range(2)]

    bgq = []

    def convert_weights():
        for l in range(nlayers):
            for (dst, src, bb, rows, step) in ((wb_in[l], w_in[l], B_wb_in[l], D, 256), (wb_out[l], w_out[l], B_wb_out[l], D, 512),
                                               (wb_up[l], w_up[l], B_wb_up[l], D, 256), (wb_dn[l], w_down[l], B_wb_dn[l], DFF, 1024)):
                for r0 in range(0, rows, step):
                    f = (lambda dst=dst, src=src, bb=bb, r0=r0, step=step:
                         T.dma("pool", dst[r0:r0 + step, :], src[r0:r0 + step, :], bb, writes=[bb]))
                    if l == 0 and dst is wb_in[0]:
                        f()
                    else:
                        bgq.append((l, f))

    def bg_step(n=1):
        for _ in range(n):
            if bgq:
                bgq.pop(0)[1]()

    def bg_flush(layer):
        while bgq and bgq[0][0] <= layer:
            bgq.pop(0)[1]()

    def salloc(es, name, shape, dt):
        uid[0] += 1
        return es.enter_context(nc.sbuf_tensor(f"{name}_{uid[0]}", list(shape), dt))

    ges = ExitStack()
    pb2 = [ges.enter_context(nc.psum_tensor(f"pbp{i}", [128, 1024], F32)) for i in range(4)]
    pb = [pb2[i // 2][:, (i % 2) * 512:(i % 2 + 1) * 512] for i in range(8)]
    PB = [Buf(f"pb{i}") for i in range(8)]

    def pbf(i):
        return pb[i].bitcast(BF16)

    ident_f = salloc(ges, "identf", [128, 128], F32); b_identf = Buf("identf")
    ident_b = salloc(ges, "identb", [128, 128], BF16); b_identb = Buf("identb")
    ones_f = salloc(ges, "onesf", [128, 128], F32); b_onesf = Buf("onesf")
    ones_b = salloc(ges, "onesb", [128, 128], BF16); b_onesb = Buf("onesb")
    modT = salloc(ges, "modT", [128, 2, 48, NSEQ], F32); b_modT_ = Buf("modT")
    gm1T = salloc(ges, "gm1T", [128, 2, 8, NSEQ], F32); b_gm1 = Buf("gm1T")
    gm2T = salloc(ges, "gm2T", [128, 2, 8, NSEQ], F32); b_gm2 = Buf("gm2T")
    stat = salloc(ges, "stat", [128, 8, 4], F32)
    STB = [Buf(f"stat{i}") for i in range(8)]
    junk = salloc(ges, "junk", [128, 2048], BF16); b_junk = Buf("junk")
    stat_i = [0]

    T.dma("sp", ident_f[:], c_ident, b_identf, writes=[b_identf])
    T.dma("pool", ident_b[:], c_ident, b_identb, writes=[b_identb])
    T.dma("sp", ones_f[:], c_ones, b_onesf, writes=[b_onesf])
    T.dma("pool", ones_b[:], c_ones, b_onesb, writes=[b_onesb])

    def prologue(preA=None):
        with ExitStack() as es:
            sT = salloc(es, "sT", [128, 8, NSEQ], F32); b_sT = Buf("sT")
            cTs = salloc(es, "cTs", [128, 8, NSEQ], F32); b_cTs = Buf("cTs")
            bm = salloc(es, "bm", [128, 2, 48, NSEQ], F32); b_bm = Buf("bm")
            ga = salloc(es, "ga", [128, 2, 8, NSEQ], F32); b_ga = Buf("ga")
            gmm = salloc(es, "gmm", [128, 2, 8, NSEQ], F32); b_gmm = Buf("gmm")
            NW = 4
            wms = [salloc(es, f"wm{i}", [128, 8, 512], F32) for i in range(NW)]
            b_wms = [Buf(f"wm{i}") for i in range(NW)]
            modrow = salloc(es, "modrow", [NSEQ, 6 * D], F32); b_mrow = Buf("modrow")
            T.dma("sp", cTs[:], cT, b_cTs, writes=[b_cTs])
            T.dma("sp", bm[:], b_modT, b_bm, writes=[b_bm])
            T.dma("sp", ga[:], g_attnT, b_ga, writes=[b_ga])
            T.dma("sp", gmm[:], g_mlpT, b_gmm, writes=[b_gmm])
            T.op("act", lambda e: e.activation(out=sT[:], in_=cTs[:], func=AF.Silu),
                 reads=[b_cTs], writes=[b_sT])
            it = 0
            for l in range(nlayers):
                wl = w_mod[l].rearrange("(c p) n -> p c n", p=128)
                for ns in range(12):
                    sl = it % NW
                    bank = it % 2
                    it += 1
                    T.dma("sp", wms[sl][:], wl[:, :, ns * 512:(ns + 1) * 512], b_wms[sl],
                          writes=[b_wms[sl]])
                    for c in range(8):
                        T.op("pe", lambda e: e.matmul(
                            pb[bank][0:NSEQ, 0:512], lhsT=sT[:, c, :], rhs=wms[sl][:, c, :],
                            start=(c == 0), stop=(c == 7)),
                            reads=[b_wms[sl], b_sT], writes=[PB[bank]])
                    T.op("act", lambda e: e.activation(out=modrow[:, ns * 512:(ns + 1) * 512],
                                                       in_=pb[bank][0:NSEQ, 0:512], func=AF.Copy),
                         reads=[PB[bank]], writes=[b_mrow])
                for j in range(48):
                    T.op("pe", lambda e: e.transpose(pb[2][:, j * NSEQ:(j + 1) * NSEQ],
                                                     modrow[0:NSEQ, j * 128:(j + 1) * 128],
                                                     ident_f[0:NSEQ, 0:NSEQ]),
                         reads=[b_mrow, b_identf], writes=[PB[2]])
                T.op("dve", lambda e: e.tensor_tensor(
                    out=modT[:, l, :, :],
                    in0=pb[2][:, 0:48 * NSEQ].rearrange("p (j b) -> p j b", b=NSEQ),
                    in1=bm[:, l, :, :], op=ALU.add),
                    reads=[PB[2], b_bm], writes=[b_modT_])
                T.op("dve", lambda e: e.scalar_tensor_tensor(
                    out=gm1T[:, l], in0=modT[:, l, 8:16, :], scalar=1.0, in1=ga[:, l],
                    op0=ALU.add, op1=ALU.mult), reads=[b_modT_, b_ga], writes=[b_gm1])
                T.op("dve", lambda e: e.scalar_tensor_tensor(
                    out=gm2T[:, l], in0=modT[:, l, 32:40, :], scalar=1.0, in1=gmm[:, l],
                    op0=ALU.add, op1=ALU.mult), reads=[b_modT_, b_gmm], writes=[b_gm2])
            if preA is not None:
                load_A_weights(0, *preA)
            T.barrier()

    def next_stat():
        i = stat_i[0] % 8
        stat_i[0] += 1
        return stat[:, i, :], STB[i]

    def rstd_from(src_ap, src_bufs, n, from_psum=False):
        st, sbuf_ = next_stat()
        T.op("act", lambda e: e.activation(out=junk[:, 0:n], in_=src_ap, func=AF.Square,
                                           accum_out=st[:, 0:1]),
             reads=src_bufs, writes=[b_junk, sbuf_])
        T.op("dve", lambda e: e.tensor_scalar(out=st[:, 1:2], in0=st[:, 0:1], scalar1=1.0 / n,
                                              scalar2=EPS, op0=ALU.mult, op1=ALU.add),
             reads=[sbuf_], writes=[sbuf_])
        T.op("act", lambda e: e.activation(out=st[:, 2:3], in_=st[:, 1:2], func=AF.Sqrt),
             reads=[sbuf_], writes=[sbuf_])
        T.op("dve", lambda e: e.reciprocal(out=st[:, 3:4], in_=st[:, 2:3]),
             reads=[sbuf_], writes=[sbuf_])
        return st[:, 3:4], sbuf_

    def norm_to_T(xt_ap, bx, gmT_ap, shT_ap, bmods, xn, bxn, tbank, hT_dst, bhT):
        rstd, brs = rstd_from(xt_ap, [bx], D)
        T.op("dve", lambda e: e.tensor_scalar(out=xn[:], in0=xt_ap, scalar1=rstd, scalar2=None,
                                              op0=ALU.mult), reads=[bx, brs], writes=[bxn])
        pv = pbf(tbank).rearrange("p (c t) -> p c t", c=8)
        for c in range(8):
            T.op("pe", lambda e: e.transpose(pv[:, c, :], xn[:, c * 128:(c + 1) * 128], ident_b[:]),
                 reads=[bxn, b_identb], writes=[PB[tbank]])
        for c in range(8):
            if c % 2 == 0:
                T.op("act", lambda e: e.activation(out=hT_dst(c), in_=pv[:, c, :], func=AF.Identity,
                                                   scale=gmT_ap(c), bias=shT_ap(c)),
                     reads=[PB[tbank]] + bmods, writes=[bhT])
            else:
                T.op("dve", lambda e: e.tensor_scalar(out=hT_dst(c), in0=pv[:, c, :],
                                                      scalar1=gmT_ap(c), scalar2=shT_ap(c),
                                                      op0=ALU.mult, op1=ALU.add),
                     reads=[PB[tbank]] + bmods, writes=[bhT])

    def ga_bcast(es, l, chunk0, name):
        gb = salloc(es, name, [128, NSEQ, D], F32); bgb = Buf(name)
        dg = salloc(es, name + "dg", [128, 2, 128], F32); bdg = [Buf(name + "dg0"), Buf(name + "dg1")]
        k = 0
        for b in range(NSEQ):
            for half in range(2):
                bank = 6 + half
                for cc in range(4):
                    c = half * 4 + cc
                    sl = k % 2
                    k += 1
                    T.op("dve", lambda e: e.tensor_scalar(
                        out=dg[:, sl, :], in0=ident_f[:], scalar1=modT[:, l, chunk0 + c, b:b + 1],
                        scalar2=None, op0=ALU.mult), reads=[b_identf, b_modT_], writes=[bdg[sl]])
                    T.op("pe", lambda e: e.matmul(pb[bank][:, cc * 128:(cc + 1) * 128], lhsT=ones_f[:],
                                                  rhs=dg[:, sl, :], start=(cc == 0), stop=True,
                                                  skip_group_check=True),
                         reads=[b_onesf, bdg[sl]], writes=[PB[bank]])
                T.op("act", lambda e: e.activation(out=gb[:, b, half * 512:(half + 1) * 512],
                                                   in_=pb[bank][:], func=AF.Copy),
                     reads=[PB[bank]], writes=[bgb])
        return gb, bgb

    def load_A_weights(l, Wf, bWf, Wt, bWt):
        wl = wb_in[l].rearrange("(c p) n -> p c n", p=128)
        for (a, b, o) in [(0, 1024, 0), (1536, 2560, 1024), (2688, 3200, 2048),
                          (3200, 3264, 2560), (3200, 3264, 2624)]:
            for c in range(8):
                T.dma("sp", Wf[:, c, o:o + (b - a)], wl[:, c, a:b], bWf, reads=[B_wb_in[l]], writes=[bWf])
        for (a, b, o) in [(1024, 1536, 0), (2560, 2688, 512), (3264, 3272, 640)]:
            T.dma("sp", Wt[:, :, o:o + (b - a)], wl[:, :, a:b], bWt, reads=[B_wb_in[l]], writes=[bWt])

    def phase_A(l, pre=None):
        with ExitStack() as es:
            if pre is None:
                bg_flush(l)
                Wf = salloc(es, "Wf", [128, 8, 2688], BF16); bWf = Buf("Wf")
                Wt = salloc(es, "Wt", [128, 8, 648], BF16); bWt = Buf("Wt")
                load_A_weights(l, Wf, bWf, Wt, bWt)
            else:
                Wf, bWf, Wt, bWt = pre
            kvg = salloc(es, "kvg", [128, 128], F32); bkvg = Buf("kvg")
            T.dma("sp", kvg[:], kvg_bc[:, l, :], bkvg, writes=[bkvg])
            xts = [salloc(es, f"xt{i}", [128, D], F32) for i in range(3)]
            bxts = [Buf(f"xt{i}") for i in range(3)]
            xns = [salloc(es, f"xn{i}", [128, D], BF16) for i in range(2)]
            bxns = [Buf(f"xn{i}") for i in range(2)]
            hTs = [salloc(es, f"hT{i}", [128, 8, 512], BF16) for i in range(2)]
            bhTs = [Buf(f"hT{i}") for i in range(2)]
            vts = [salloc(es, f"vt{i}", [128, 512], BF16) for i in range(2)]
            bvts = [Buf(f"vt{i}") for i in range(2)]
            kvn = [salloc(es, f"kvn{i}", [128, 128], BF16) for i in range(2)]
            bkvn = [Buf(f"kvn{i}") for i in range(2)]
            kvTt = [salloc(es, f"kvTt{i}", [128, 128], BF16) for i in range(2)]
            bkvTt = [Buf(f"kvTt{i}") for i in range(2)]
            wis = [salloc(es, f"wis{i}", [128, 8], F32) for i in range(2)]
            bwis = [Buf(f"wis{i}") for i in range(2)]
            fos = [salloc(es, f"fo{i}", [128, 512], BF16) for i in range(3)]
            bfos = [Buf(f"fo{i}") for i in range(3)]
            src = x_in if l == 0 else xres
            cnt = {"ti": 0, "fi": 0}
            groups = [(s, g) for s in range(NSEQ) for g in range(4)]

            def prep_a(gi, j):
                s, g = groups[gi]
                hT = hTs[gi % 2]; bhT = bhTs[gi % 2]
                tt = g * 4 + j
                t0 = tt * 128
                ti = cnt["ti"]
                cnt["ti"] += 1
                xt = xts[ti % 3]; bxt = bxts[ti % 3]
                xn = xns[ti % 2]; bxn = bxns[ti % 2]
                T.dma("sp", xt[:], src[s, t0:t0 + 128, :], bxt, writes=[bxt])
                norm_to_T(xt[:], bxt,
                          lambda c: gm1T[:, l, c, s:s + 1],
                          lambda c: modT[:, l, 0 + c, s:s + 1],
                          [b_gm1, b_modT_], xn, bxn, ti % 2,
                          lambda c: hT[:, c, j * 128:(j + 1) * 128], bhT)
                return ti

            def prep_b(gi, j, ti):
                s, g = groups[gi]
                hT = hTs[gi % 2]; bhT = bhTs[gi % 2]
                t0 = (g * 4 + j) * 128
                k2 = ti % 2
                for c in range(8):
                    T.op("pe", lambda e: e.matmul(pb[2][:, 0:512], lhsT=hT[:, c, j * 128:(j + 1) * 128],
                                                  rhs=Wt[:, c, 0:512], start=(c == 0), stop=(c == 7)),
                         reads=[bhT, bWt], writes=[PB[2]])
                for c in range(8):
                    T.op("pe", lambda e: e.matmul(pb[3][:, 0:136], lhsT=hT[:, c, j * 128:(j + 1) * 128],
                                                  rhs=Wt[:, c, 512:648], start=(c == 0), stop=(c == 7)),
                         reads=[bhT, bWt], writes=[PB[3]])
                T.op("act", lambda e: e.activation(out=vts[k2][:], in_=pb[2][:, 0:512], func=AF.Copy),
                     reads=[PB[2]], writes=[bvts[k2]])
                T.dma("pool", v_scr[s, t0:t0 + 128, :], vts[k2][:], bvts[k2], reads=[bvts[k2]])
                rs2, brs2 = rstd_from(pb[3][:, 0:128], [PB[3]], 128)
                T.op("dve", lambda e: e.scalar_tensor_tensor(
                    out=kvn[k2][:], in0=pb[3][:, 0:128], scalar=rs2, in1=kvg[:],
                    op0=ALU.mult, op1=ALU.mult), reads=[PB[3], brs2, bkvg], writes=[bkvn[k2]])
                T.op("dve", lambda e: e.tensor_copy(out=wis[k2][:], in_=pb[3][:, 128:136]),
                     reads=[PB[3]], writes=[bwis[k2]])
                T.dma("pool", kv_tok[s, t0:t0 + 128, :], kvn[k2][:], bkvn[k2], reads=[bkvn[k2]])
                T.dma("pool", widx[s, t0:t0 + 128, :], wis[k2][:], bwis[k2], reads=[bwis[k2]])
                T.op("pe", lambda e: e.transpose(pbf(4)[:, 0:128], kvn[k2][:], ident_b[:]),
                     reads=[bkvn[k2], b_identb], writes=[PB[4]])
                T.op("act", lambda e: e.activation(out=kvTt[k2][:], in_=pbf(4)[:, 0:128], func=AF.Copy),
                     reads=[PB[4]], writes=[bkvTt[k2]])
                T.dma("pool", kvT_scr[s, :, t0:t0 + 128], kvTt[k2][:], bkvTt[k2], reads=[bkvTt[k2]])

            def fm_chunk(gi, ch):
                s, g = groups[gi]
                hT = hTs[gi % 2]; bhT = bhTs[gi % 2]
                fi = cnt["fi"]
                cnt["fi"] += 1
                bank = 5 + (fi % 3)
                fo = fos[fi % 3]; bfo = bfos[fi % 3]
                for c in range(8):
                    T.op("pe", lambda e: e.matmul(pb[bank][:, 0:512], lhsT=Wf[:, c, ch * 128:(ch + 1) * 128],
                                                  rhs=hT[:, c, :], start=(c == 0), stop=(c == 7)),
                         reads=[bhT, bWf], writes=[PB[bank]])
                if fi % 2 == 0:
                    T.op("act", lambda e: e.activation(out=fo[:], in_=pb[bank][:, 0:512], func=AF.Copy),
                         reads=[PB[bank]], writes=[bfo])
                else:
                    T.op("dve", lambda e: e.tensor_copy(out=fo[:], in_=pb[bank][:, 0:512]),
                         reads=[PB[bank]], writes=[bfo])
                T.dma("pool", featT[s, ch, :, g * 512:(g + 1) * 512], fo[:], bfo, reads=[bfo])

            for j in range(4):
                ti0 = prep_a(0, j)
                prep_b(0, j, ti0)
            for gi in range(len(groups)):
                pend_b = None
                for ch in range(21):
                    fm_chunk(gi, ch)
                    if gi + 1 < len(groups):
                        if ch in (1, 6, 11, 16):
                            j = (1, 6, 11, 16).index(ch)
                            pend_b = (j, prep_a(gi + 1, j))
                        if ch in (4, 9, 14, 19) and pend_b is not None:
                            prep_b(gi + 1, pend_b[0], pend_b[1])
                            pend_b = None
            T.barrier()

    def phase_B(l):
        with ExitStack() as es:
            qT = salloc(es, "qT", [128, 4, S], BF16); bqT = Buf("qT")
            kT = salloc(es, "kT", [128, 4, S], BF16); bkT = Buf("kT")
            vv = salloc(es, "vv", [128, NT, 512], BF16); bvv = Buf("vv")
            tri8 = salloc(es, "tri8", [128, 128], BF16); btri = Buf("tri8")
            neg8 = salloc(es, "neg8", [128, 128], BF16); bneg = Buf("neg8")
            cm = salloc(es, "cm", [128, 512], BF16); bcm = Buf("cm")
            goa = salloc(es, "goa", [128, 512], F32); bgoa = Buf("goa")
            T.dma("pool", tri8[:], c_tri8, btri, writes=[btri])
            T.dma("pool", neg8[:], c_neg8, bneg, writes=[bneg])
            T.dma("pool", cm[:], c_cmask[:, 0:4, :].rearrange("p h t -> p (h t)"), bcm, writes=[bcm])
            T.dma("sp", goa[:], goa_bc[:, l, :], bgoa, writes=[bgoa])
            e32a = [salloc(es, f"e32a{i}", [128, 1024], F32) for i in range(2)]
            spba = [salloc(es, f"spba{i}", [128, 1024], BF16) for i in range(2)]
            wba = [salloc(es, f"wba{i}", [128, 1024], BF16) for i in range(2)]
            e32 = [[e32a[i][:, g * 512:(g + 1) * 512] for i in range(2)] for g in range(2)]
            spb = [[spba[i][:, g * 512:(g + 1) * 512] for i in range(2)] for g in range(2)]
            wb = [[wba[i][:, g * 512:(g + 1) * 512] for i in range(2)] for g in range(2)]
            be32 = [[Buf(f"e32{g}{i}") for i in range(2)] for g in range(2)]
            bspb = [[Buf(f"spb{g}{i}") for i in range(2)] for g in range(2)]
            bwb = [[Buf(f"wb{g}{i}") for i in range(2)] for g in range(2)]
            sps = [salloc(es, f"sps{g}", [128, 512], F32) for g in range(2)]
            bsps = [Buf(f"sps{g}") for g in range(2)]
            spsb = [[salloc(es, f"spsb{g}{i}", [128, 512], BF16) for i in range(2)] for g in range(2)]
            bspsb = [[Buf(f"spsb{g}{i}") for i in range(2)] for g in range(2)]
            oan = [salloc(es, f"oan{i}", [128, 512], BF16) for i in range(2)]
            boan = [Buf(f"oan{i}") for i in range(2)]
            ZB = [[0, 2], [1, 3]]
            AB = [4, 5]
            OB = [6, 7]
            carry_slot = [0, 0]

            def zmm(bank, hg, qb, kb, start_first):
                for i in range(4):
                    h = 2 * i + hg
                    ch = h // 2
                    r0 = (h % 2) * 64
                    T.op("pe", lambda e: e.matmul(
                        pb[bank][:, i * 128:(i + 1) * 128],
                        lhsT=kT[r0:r0 + 64, ch, kb * 128:(kb + 1) * 128],
                        rhs=qT[r0:r0 + 64, ch, qb * 128:(qb + 1) * 128],
                        start=(start_first and i == 0), stop=(i == 3),
                        skip_group_check=True),
                        reads=[bkT, bqT], writes=[PB[bank]])

            def stage_Z(n, qb, kb):
                sl = n % 2
                for hg in range(2):
                    zmm(ZB[hg][sl], hg, qb, kb, True)
                T.op("act", lambda e: e.activation(out=e32a[sl][:], in_=pb2[sl][:], func=AF.Exp, scale=0.125),
                     reads=[PB[ZB[0][sl]], PB[ZB[1][sl]]], writes=[be32[0][sl], be32[1][sl]])
                T.op("act", lambda e: e.activation(out=spba[sl][:], in_=e32a[sl][:], func=AF.Ln, bias=1.0),
                     reads=[be32[0][sl], be32[1][sl]], writes=[bspb[0][sl], bspb[1][sl]])
                for hg in range(2):
                    if kb == qb:
                        T.op("dve", lambda e: e.tensor_tensor(out=spb[hg][sl][:], in0=spb[hg][sl][:], in1=cm[:], op=ALU.mult),
                             reads=[bspb[hg][sl], bcm], writes=[bspb[hg][sl]])

            def stage_A(n, qb, kb):
                sl = n % 2
                for hg in range(2):
                    ab = AB[hg]
                    T.op("pe", lambda e: e.matmul(pb[ab][:], lhsT=tri8[:], rhs=spb[hg][sl][:], start=True, stop=False,
                                                  skip_group_check=True),
                         reads=[btri, bspb[hg][sl]], writes=[PB[ab]])
                    if kb < qb:
                        cs = carry_slot[hg]
                        T.op("pe", lambda e: e.matmul(pb[ab][:], lhsT=neg8[:], rhs=spsb[hg][cs][:], start=False, stop=False,
                                                      skip_group_check=True),
                             reads=[bneg, bspsb[hg][cs]], writes=[PB[ab]])
                for hg in range(2):
                    zmm(AB[hg], hg, qb, kb, False)
                T.op("act", lambda e: e.activation(out=wba[sl][:], in_=pb2[2][:], func=AF.Exp, scale=0.125),
                     reads=[PB[AB[0]], PB[AB[1]]], writes=[bwb[0][sl], bwb[1][sl]])
                for hg in range(2):
                    ab = AB[hg]
                    if kb == qb:
                        T.op("dve", lambda e: e.tensor_tensor(out=wb[hg][sl][:], in0=wb[hg][sl][:], in1=cm[:], op=ALU.mult),
                             reads=[bwb[hg][sl], bcm], writes=[bwb[hg][sl]])
                    if kb > 0:
                        if kb == qb:
                            T.op("dve", lambda e: e.tensor_copy(out=sps[hg][:], in_=spb[hg][sl][:]),
                                 reads=[bspb[hg][sl]], writes=[bsps[hg]])
                        else:
                            T.op("dve", lambda e: e.tensor_tensor(out=sps[hg][:], in0=sps[hg][:], in1=spb[hg][sl][:], op=ALU.add),
                                 reads=[bsps[hg], bspb[hg][sl]], writes=[bsps[hg]])
                        carry_slot[hg] ^= 1
                        cs = carry_slot[hg]
                        T.op("dve", lambda e: e.tensor_copy(out=spsb[hg][cs][:], in_=sps[hg][:]),
                             reads=[bsps[hg]], writes=[bspsb[hg][cs]])

            def stage_PV(n, s, qb, kb):
                sl = n % 2
                ob = OB[qb % 2]
                for hg in range(2):
                    for i in range(4):
                        h = 2 * i + hg
                        T.op("pe", lambda e: e.matmul(
                            pb[ob][:, h * 64:(h + 1) * 64], lhsT=wb[hg][sl][:, i * 128:(i + 1) * 128],
                            rhs=vv[:, kb, h * 64:(h + 1) * 64], start=(kb == qb and hg == 0 and i == 0), stop=False,
                            skip_group_check=True),
                            reads=[bwb[hg][sl], bvv], writes=[PB[ob]])
                if kb == 0:
                    osl = qb % 2
                    rs, brs = rstd_from(pb[ob][:], [PB[ob]], 512)
                    T.op("dve", lambda e: e.scalar_tensor_tensor(out=oan[osl][:], in0=pb[ob][:], scalar=rs, in1=goa[:],
                                                                 op0=ALU.mult, op1=ALU.mult),
                         reads=[PB[ob], brs, bgoa], writes=[boan[osl]])
                    T.dma("pool", ocat[s, qb * 128:(qb + 1) * 128, 0:512], oan[osl][:], boan[osl], reads=[boan[osl]])
                    bg_step(1)

            for s in range(LIM_SEQ):
                T.dma("sp", qT[:], featT[s, 0:4].rearrange("c p t -> p c t"), bqT, writes=[bqT])
                T.dma("sp", kT[:], featT[s, 4:8].rearrange("c p t -> p c t"), bkT, writes=[bkT])
                T.dma("sp", vv[:], v_scr[s].rearrange("(n p) f -> p n f", p=128), bvv, writes=[bvv])
                its = [(qb, kb) for qb in range(LIM_QB) for kb in range(qb, -1, -1)]
                N = len(its)
                for t in range(N + 2):
                    if t < N:
                        stage_Z(t, *its[t])
                    if 0 <= t - 1 < N:
                        stage_A(t - 1, *its[t - 1])
                    if 0 <= t - 2 < N:
                        stage_PV(t - 2, s, *its[t - 2])
            T.barrier()

    def phase_C(l):
        if FLUSH_BG_BEFORE_C:
            bg_flush(99)
        with ExitStack() as es:
            dqT = salloc(es, "dqT", [128, 8, S], BF16); bdq = Buf("dqT")
            iqT = salloc(es, "iqT", [128, 4, S], BF16); biq = Buf("iqT")
            ikT = salloc(es, "ikT", [128, S], BF16); bik = Buf("ikT")
            kvT = salloc(es, "kvT", [128, S], BF16); bkvT = Buf("kvT")
            kvt = salloc(es, "kvt", [128, NT, 128], BF16); bkvt = Buf("kvt")
            wi = salloc(es, "wi", [128, NT, 8], F32); bwi = Buf("wi")
            wuv = salloc(es, "wuv", [128, 8, 64], BF16); bwuv = Buf("wuv")
            gob = salloc(es, "gob", [128, 512], F32); bgob = Buf("gob")
            BN = salloc(es, "BN", [128, 2, 1024], BF16); bBN = Buf("BN")
            id4 = salloc(es, "id4", [128, 512], BF16); bid4 = Buf("id4")
            pw2 = salloc(es, "pw2", [128, KBIS], F32); bpw2 = Buf("pw2")
            T.dma("pool", wuv[:], w_uv[l].rearrange("h r d -> r h d"), bwuv, writes=[bwuv])
            T.dma("sp", gob[:], gob_bc[:, l, :], bgob, writes=[bgob])
            T.dma("sp", pw2[:], c_pow2, bpw2, writes=[bpw2])
            for i in range(4):
                T.dma("pool", id4[:, i * 128:(i + 1) * 128], c_ident, bid4, writes=[bid4])
            score = [salloc(es, f"score{i}", [128, S], F32) for i in range(2)]
            bsc = [Buf(f"score{i}") for i in range(2)]
            nmask = [salloc(es, f"nmask{i}", [128, S], BF16) for i in range(2)]
            bnm = [Buf(f"nmask{i}") for i in range(2)]
            PP = [salloc(es, f"PP{i}", [128, 1024], BF16) for i in range(2)]
            bPP = [Buf(f"PP{i}") for i in range(2)]
            oTs = salloc(es, "oTs", [128, 1024], BF16); boTs = Buf("oTs")
            bis = salloc(es, "bis", [128, 8 + 2 * KBIS], F32); bbis = Buf("bis")
            rden = salloc(es, "rden", [128, 8], F32); brden = Buf("rden")
            obf = salloc(es, "obf", [128, 512], F32); bobf = Buf("obf")
            obn = [salloc(es, f"obn{i}", [128, 512], BF16) for i in range(2)]
            bobn = [Buf(f"obn{i}") for i in range(2)]
            lsc = 128 ** -0.5
            isc = (64 ** -0.5) * (8 ** -0.5)
            with ExitStack() as es2:
                bn = salloc(es2, "bn", [128, 2, 1024], F32); bbn = Buf("bn")
                bf = salloc(es2, "bf", [128, 1024], F32); bbf = Buf("bf")
                adn = salloc(es2, "adn", [128, 1024], F32); badn = Buf("adn")
                T.dma("sp", bn[:], biasn.rearrange("p o h t -> p o (h t)"), bbn, writes=[bbn])
                T.dma("sp", bf[:], bfar_bc.rearrange("p h t -> p (h t)"), bbf, writes=[bbf])
                T.dma("sp", adn[:], c_admneg.rearrange("p h t -> p (h t)"), badn, writes=[badn])
                for o in range(2):
                    T.op("dve", lambda e: e.tensor_tensor(out=bn[:, o, :], in0=bn[:, o, :], in1=bf[:], op=ALU.subtract),
                         reads=[bbn, bbf], writes=[bbn])
                    if o == 0:
                        T.op("dve", lambda e: e.scalar_tensor_tensor(out=BN[:, o, :], in0=bn[:, o, :], scalar=1.0 / lsc, in1=adn[:],
                                                                     op0=ALU.mult, op1=ALU.add),
                             reads=[bbn, badn], writes=[bBN])
                    else:
                        T.op("dve", lambda e: e.tensor_scalar(out=BN[:, o, :], in0=bn[:, o, :], scalar1=1.0 / lsc, scalar2=None,
                                                              op0=ALU.mult), reads=[bbn], writes=[bBN])
                T.barrier()
            cnt_i = {"ii": 0, "pi": 0, "oi": 0, "lp": 0, "ib": 0}
            IBS = (2, 3)
            SB = 4
            NRB = 2 * ACC_DEPTH + 2
            Rb = [salloc(es, f"Rb{i}", [128, 512], BF16) for i in range(NRB)]
            bRb = [Buf(f"Rb{i}") for i in range(NRB)]
            dg = [salloc(es, f"dg{i}", [128, 8, 128], BF16) for i in range(2)]
            bdg = [Buf(f"dg{i}") for i in range(2)]
            absw = salloc(es, "absw", [128, NT, 8], F32); babsw = Buf("absw")
            sgn = salloc(es, "sgn", [128, NT, 8], F32); bsgn = Buf("sgn")
            OTB = (5, 6)
            DB = 7

            pend_acc = []

            def flush_acc(keep=0):
                while len(pend_acc) > keep:
                    (qb, c0, w, j, rsl, dsl) = pend_acc.pop(0)
                    sc_ = score[qb % 2]; bsc_ = bsc[qb % 2]
                    T.op("pe", lambda e: e.matmul(pb[SB][:, 0:w], lhsT=dg[dsl][:, j, :], rhs=Rb[rsl][:, 0:w],
                                                  start=(j == 0), stop=(j == 7), skip_group_check=True),
                         reads=[bdg[dsl], bRb[rsl]], writes=[PB[SB]])
                    if j == 7:
                        T.op("act", lambda e: e.activation(out=sc_[:, c0:c0 + w], in_=pb[SB][:, 0:w], func=AF.Copy),
                             reads=[PB[SB]], writes=[bsc_])

            def make_diag(qb):
                dsl = qb % 2
                for j in range(8):
                    T.op("act", lambda e: e.activation(out=dg[dsl][:, j, :], in_=ident_b[:], func=AF.Identity,
                                                       scale=sgn[:, qb, j:j + 1]),
                         reads=[b_identb, bsgn], writes=[bdg[dsl]])

            def idx_unit(s, qb, c, j):
                n = (qb + 1) * 128
                q0 = qb * 128
                c0 = c * 512
                w = min(512, n - c0)
                rsl = cnt_i["ii"] % NRB
                cnt_i["ii"] += 1
                ch = j // 2
                r0 = (j % 2) * 64
                IB = IBS[cnt_i["ib"] % 2]
                cnt_i["ib"] += 1
                T.op("pe", lambda e: e.matmul(pb[IB][:, 0:w], lhsT=iqT[r0:r0 + 64, ch, q0:q0 + 128],
                                              rhs=ikT[r0:r0 + 64, c0:c0 + w], start=True, stop=True),
                     reads=[biq, bik], writes=[PB[IB]])
                T.op("act", lambda e: e.activation(out=Rb[rsl][:, 0:w], in_=pb[IB][:, 0:w], func=AF.Relu,
                                                   scale=absw[:, qb, j:j + 1]),
                     reads=[PB[IB], babsw], writes=[bRb[rsl]])
                flush_acc(keep=ACC_DEPTH - 1)
                pend_acc.append((qb, c0, w, j, rsl, qb % 2))

            def select(qb):
                n = (qb + 1) * 128
                nm = nmask[qb % 2]; bn_ = bnm[qb % 2]
                score_ = score[qb % 2]; bsc_ = bsc[qb % 2]
                T.op("dve", lambda e: e.memset(score_[0:64, n - 64:n], -1.0e30), reads=[bsc_], writes=[bsc_])
                if qb >= 2:
                    hi = bis[:, 0:1]; lo = bis[:, 1:2]; w0 = bis[:, 2:3]; mid = bis[:, 3:4]
                    cnt = bis[:, 4:5]; sv = bis[:, 5:6]; thr = bis[:, 6:7]
                    H = bis[:, 8:8 + KBIS]; H2 = bis[:, 8 + KBIS:8 + 2 * KBIS]
                    T.op("dve", lambda e: e.tensor_reduce(out=hi, in_=score_[:, 0:n], axis=AX.X, op=ALU.max),
                         reads=[bsc_], writes=[bbis])
                    T.op("dve", lambda e: e.tensor_reduce(out=lo, in_=score_[:, 0:n - 64], axis=AX.X, op=ALU.min),
                         reads=[bsc_], writes=[bbis])
                    T.op("dve", lambda e: e.tensor_tensor(out=w0, in0=hi, in1=lo, op=ALU.subtract),
                         reads=[bbis], writes=[bbis])
                    T.op("dve", lambda e: e.tensor_scalar(out=H, in0=pw2[:], scalar1=w0, scalar2=None, op0=ALU.mult),
                         reads=[bbis, bpw2], writes=[bbis])
                    T.op("dve", lambda e: e.tensor_scalar(out=H2, in0=H, scalar1=2.0, scalar2=None, op0=ALU.mult),
                         reads=[bbis], writes=[bbis])
                    T.op("dve", lambda e: e.tensor_tensor(out=mid, in0=lo, in1=bis[:, 8:9], op=ALU.add),
                         reads=[bbis], writes=[bbis])
                    for k in range(KBIS):
                        T.op("dve", lambda e: e.tensor_scalar(out=junk[:, 0:n], in0=score_[:, 0:n], scalar1=mid, scalar2=None,
                                                              op0=ALU.is_ge, op1=ALU.add, accum_out=cnt),
                             reads=[bsc_, bbis], writes=[b_junk, bbis])
                        if k < KBIS - 1:
                            T.op("dve", lambda e: e.tensor_scalar(out=sv, in0=cnt, scalar1=float(TOPK),
                                                                  scalar2=bis[:, 8 + KBIS + k + 1:8 + KBIS + k + 2],
                                                                  op0=ALU.is_ge, op1=ALU.mult),
                                 reads=[bbis], writes=[bbis])
                            T.op("dve", lambda e: e.scalar_tensor_tensor(out=mid, in0=sv, scalar=bis[:, 8 + k + 1:8 + k + 2],
                                                                         in1=mid, op0=ALU.subtract, op1=ALU.add),
                                 reads=[bbis], writes=[bbis])
                        else:
                            T.op("dve", lambda e: e.tensor_scalar(out=sv, in0=cnt, scalar1=float(TOPK),
                                                                  scalar2=bis[:, 8 + k:8 + k + 1],
                                                                  op0=ALU.is_ge, op1=ALU.mult),
                                 reads=[bbis], writes=[bbis])
                            T.op("dve", lambda e: e.scalar_tensor_tensor(out=thr, in0=sv, scalar=bis[:, 8 + k:8 + k + 1],
                                                                         in1=mid, op0=ALU.subtract, op1=ALU.add),
                                 reads=[bbis], writes=[bbis])
                    T.op("dve", lambda e: e.tensor_scalar(out=nm[:, 0:n], in0=score_[:, 0:n], scalar1=thr, scalar2=-1.0e5,
                                                          op0=ALU.is_lt, op1=ALU.mult), reads=[bsc_, bbis], writes=[bn_])
                else:
                    T.op("dve", lambda e: e.tensor_scalar(out=nm[:, 0:n], in0=score_[:, 0:n], scalar1=-1.0e29, scalar2=-1.0e5,
                                                          op0=ALU.is_lt, op1=ALU.mult), reads=[bsc_], writes=[bn_])

            def att_logits(s, qb, kb):
                q0 = qb * 128
                nm = nmask[qb % 2]; bn_ = bnm[qb % 2]
                lp = cnt_i["lp"] % 2
                cnt_i["lp"] += 1
                P = PP[lp]; bP = bPP[lp]
                off = qb - kb
                for (bank, h0) in ((0, 0), (1, 4)):
                    T.op("pe", lambda e: e.matmul(
                        pb[bank][:].rearrange("p (h t) -> p h t", h=4),
                        lhsT=kvT[:, kb * 128:(kb + 1) * 128], rhs=dqT[:, h0:h0 + 4, q0:q0 + 128],
                        start=True, stop=False, skip_group_check=True), reads=[bkvT, bdq], writes=[PB[bank]])
                    T.op("pe", lambda e: e.matmul(pb[bank][:], lhsT=nm[:, kb * 128:(kb + 1) * 128], rhs=id4[:],
                                                  start=False, stop=(off >= 2), skip_group_check=True),
                         reads=[bn_, bid4], writes=[PB[bank]])
                    if off < 2:
                        T.op("pe", lambda e: e.matmul(pb[bank][:], lhsT=ident_b[:], rhs=BN[:, off, h0 * 128:(h0 + 4) * 128],
                                                      start=False, stop=True, skip_group_check=True),
                             reads=[b_identb, bBN], writes=[PB[bank]])
                    T.op("act", lambda e: e.activation(out=P[:, h0 * 128:(h0 + 4) * 128], in_=pb[bank][:], func=AF.Exp, scale=lsc),
                         reads=[PB[bank]], writes=[bP])
                return lp

            def att_pv(s, qb, kb, lp):
                P = PP[lp]; bP = bPP[lp]
                for (bank, h0) in ((OTB[0], 0), (OTB[1], 4)):
                    T.op("pe", lambda e: e.matmul(pb[bank][:], lhsT=kvt[:, kb, :], rhs=P[:, h0 * 128:(h0 + 4) * 128],
                                                  start=(kb == 0), stop=(kb == qb)),
                         reads=[bkvt, bP], writes=[PB[bank]])
                for h in range(8):
                    T.op("pe", lambda e: e.matmul(pb[DB][:, h:h + 1], lhsT=P[:, h * 128:(h + 1) * 128], rhs=ones_b[:, 0:1],
                                                  start=(kb == 0 and h == 0), stop=(kb == qb), skip_group_check=True),
                         reads=[bP, b_onesb], writes=[PB[DB]])

            def epilogue(s, qb):
                q0 = qb * 128
                T.op("act", lambda e: e.activation(out=oTs[:, 0:512], in_=pb[OTB[0]][:], func=AF.Copy), reads=[PB[OTB[0]]], writes=[boTs])
                T.op("act", lambda e: e.activation(out=oTs[:, 512:1024], in_=pb[OTB[1]][:], func=AF.Copy), reads=[PB[OTB[1]]], writes=[boTs])
                T.op("dve", lambda e: e.reciprocal(out=rden[:], in_=pb[DB][:, 0:8]), reads=[PB[DB]], writes=[brden])
                eb = IBS[cnt_i["ib"] % 2]
                cnt_i["ib"] += 1
                for h in range(8):
                    T.op("pe", lambda e: e.matmul(pb[eb][:, h * 64:(h + 1) * 64], lhsT=oTs[:, h * 128:(h + 1) * 128],
                                                  rhs=wuv[:, h, :], start=(h == 0), stop=True, skip_group_check=True),
                         reads=[boTs, bwuv], writes=[PB[eb]])
                for h in range(8):
                    T.op("dve", lambda e: e.tensor_scalar(out=obf[:, h * 64:(h + 1) * 64], in0=pb[eb][:, h * 64:(h + 1) * 64],
                                                          scalar1=rden[:, h:h + 1], scalar2=None, op0=ALU.mult),
                         reads=[PB[eb], brden], writes=[bobf])
                rs, brs = rstd_from(obf[:], [bobf], 512)
                osl = cnt_i["oi"] % 2
                cnt_i["oi"] += 1
                T.op("dve", lambda e: e.scalar_tensor_tensor(out=obn[osl][:], in0=obf[:], scalar=rs, in1=gob[:],
                                                             op0=ALU.mult, op1=ALU.mult),
                     reads=[bobf, brs, bgob], writes=[bobn[osl]])
                T.dma("pool", ocat[s, q0:q0 + 128, 512:1024], obn[osl][:], bobn[osl], reads=[bobn[osl]])

            for s in range(LIM_SEQ):
                T.dma("sp", dqT[:], featT[s, 8:16].rearrange("c p t -> p c t"), bdq, writes=[bdq])
                T.dma("sp", iqT[:], featT[s, 16:20].rearrange("c p t -> p c t"), biq, writes=[biq])
                T.dma("sp", ikT[:], featT[s, 20], bik, writes=[bik])
                T.dma("sp", kvT[:], kvT_scr[s], bkvT, writes=[bkvT])
                T.dma("sp", kvt[:], kv_tok[s].rearrange("(n p) r -> p n r", p=128), bkvt, writes=[bkvt])
                T.dma("sp", wi[:], widx[s].rearrange("(n p) j -> p n j", p=128), bwi, writes=[bwi])
                T.op("act", lambda e: e.activation(out=absw[:], in_=wi[:], func=AF.Abs, scale=isc),
                     reads=[bwi], writes=[babsw])
                T.op("dve", lambda e: e.tensor_scalar(out=sgn[:], in0=wi[:], scalar1=0.0, scalar2=2.0,
                                                      op0=ALU.is_ge, op1=ALU.mult), reads=[bwi], writes=[bsgn])
                T.op("dve", lambda e: e.tensor_scalar(out=sgn[:], in0=sgn[:], scalar1=-1.0, scalar2=None,
                                                      op0=ALU.add), reads=[bsgn], writes=[bsgn])
                for step in range(LIM_QB + 2):
                    qi = step
                    qs = step - 1
                    qa = step - 2
                    iu = []
                    if qi < LIM_QB:
                        n = (qi + 1) * 128
                        iu = [(c, j) for c in range((n + 511) // 512) for j in range(8)]
                        make_diag(qi)
                    if 0 <= qs < LIM_QB:
                        select(qs)
                    au = list(range(qa + 1)) if qa >= 0 else []
                    na, ni = len(au), len(iu)
                    ai = 0
                    ii_ = 0
                    pend = None
                    total = max(na, 1)
                    while ai < na or ii_ < ni:
                        tgt = ni if ai >= na else (ni * (ai + 1)) // total
                        while ii_ < tgt:
                            idx_unit(s, qi, *iu[ii_])
                            ii_ += 1
                        if ai < na:
                            lp = att_logits(s, qa, au[ai])
                            if pend is not None:
                                att_pv(s, qa, *pend)
                            pend = (au[ai], lp)
                            ai += 1
                    if pend is not None:
                        att_pv(s, qa, *pend)
                    flush_acc()
                    if qa >= 0:
                        epilogue(s, qa)
                        bg_step(1)
            T.barrier()

    def phase_D(l, prefetch=()):
        prefetch = list(prefetch)
        bg_flush(l)
        with ExitStack() as es:
            Wo = salloc(es, "Wo", [128, 8, D], BF16); bWo = Buf("Wo")
            wl = wb_out[l].rearrange("(c p) n -> p c n", p=128)
            for c in range(8):
                T.dma("sp", Wo[:, c, :], wl[:, c, :], bWo, reads=[B_wb_out[l]], writes=[bWo])
            gb, bgb = ga_bcast(es, l, 16, "ga1")
            oc = [salloc(es, f"oc{i}", [128, D], BF16) for i in range(2)]
            boc = [Buf(f"oc{i}") for i in range(2)]
            oT = [salloc(es, f"oT{i}", [128, 8, 128], BF16) for i in range(2)]
            boT = [Buf(f"oT{i}") for i in range(2)]
            xts = [salloc(es, f"xd{i}", [128, D], F32) for i in range(2)]
            bxts = [Buf(f"xd{i}") for i in range(2)]
            tmp = [salloc(es, f"tm{i}", [128, D], F32) for i in range(2)]
            btmp = [Buf(f"tm{i}") for i in range(2)]
            src = x_in if l == 0 else xres
            ti = 0
            for s in range(NSEQ):
                for tt in range(NT):
                    t0 = tt * 128
                    k2 = ti % 2
                    ti += 1
                    T.dma("sp", oc[k2][:], ocat[s, t0:t0 + 128, :], boc[k2], writes=[boc[k2]])
                    T.dma("sp", xts[k2][:], src[s, t0:t0 + 128, :], bxts[k2], writes=[bxts[k2]])
                    if prefetch:
                        prefetch.pop(0)()
                    pv = pbf(k2).rearrange("p (c t) -> p c t", c=8)
                    for c in range(8):
                        T.op("pe", lambda e: e.transpose(pv[:, c, :], oc[k2][:, c * 128:(c + 1) * 128], ident_b[:]),
                             reads=[boc[k2], b_identb], writes=[PB[k2]])
                    T.op("act", lambda e: e.activation(out=oT[k2][:].rearrange("p c t -> p (c t)"), in_=pbf(k2)[:, 0:1024], func=AF.Copy),
                         reads=[PB[k2]], writes=[boT[k2]])
                    for half in range(2):
                        bank = 2 + k2 * 2 + half
                        for c in range(8):
                            T.op("pe", lambda e: e.matmul(pb[bank][:], lhsT=oT[k2][:, c, :], rhs=Wo[:, c, half * 512:(half + 1) * 512],
                                                          start=(c == 0), stop=(c == 7)),
                                 reads=[boT[k2], bWo], writes=[PB[bank]])
                        T.op("dve", lambda e: e.tensor_tensor(out=tmp[k2][:, half * 512:(half + 1) * 512], in0=pb[bank][:],
                                                              in1=gb[:, s, half * 512:(half + 1) * 512], op=ALU.mult),
                             reads=[PB[bank], bgb], writes=[btmp[k2]])
                    T.op("pool", lambda e: e.tensor_tensor(out=tmp[k2][:], in0=tmp[k2][:], in1=xts[k2][:], op=ALU.add),
                         reads=[btmp[k2], bxts[k2]], writes=[btmp[k2]])
                    T.dma("pool", xres[s, t0:t0 + 128, :], tmp[k2][:], btmp[k2], reads=[btmp[k2]])
            while prefetch:
                prefetch.pop(0)()
            T.barrier()

    def phase_DE(l, last):
        with ExitStack() as esw:
            Wu = salloc(esw, "Wu", [128, 8, DFF], BF16); bWu = Buf("Wu")
            Wd = salloc(esw, "Wd", [128, 32, D], BF16); bWd = Buf("Wd")
            wul = wb_up[l].rearrange("(c p) n -> p c n", p=128)
            wdl = wb_dn[l].rearrange("(c p) n -> p c n", p=128)
            pf = []
            for c in range(8):
                for hh in range(2):
                    pf.append(lambda c=c, hh=hh: T.dma(
                        "sp", Wu[:, c, hh * 2048:(hh + 1) * 2048], wul[:, c, hh * 2048:(hh + 1) * 2048], bWu,
                        reads=[B_wb_up[l]], writes=[bWu]))
            for c4 in range(8):
                pf.append(lambda c4=c4: T.dma(
                    "sp", Wd[:, c4 * 4:(c4 + 1) * 4, :], wdl[:, c4 * 4:(c4 + 1) * 4, :], bWd,
                    reads=[B_wb_dn[l]], writes=[bWd]))
            phase_D(l, prefetch=pf)
            phase_E(l, last, Wu, bWu, Wd, bWd)

    def phase_E(l, last, Wu, bWu, Wd, bWd):
        with ExitStack() as es:
            gb, bgb = ga_bcast(es, l, 40, "ga2")
            if last:
                gf = salloc(es, "gf", [128, D], F32); bgf = Buf("gf")
                T.dma("sp", gf[:], gfin_bc, bgf, writes=[bgf])
            TG = 256
            xts = [salloc(es, f"xe{i}", [128, D], F32) for i in range(4)]
            bxts = [Buf(f"xe{i}") for i in range(4)]
            xns = [salloc(es, f"xne{i}", [128, D], BF16) for i in range(2)]
            bxns = [Buf(f"xne{i}") for i in range(2)]
            hTs = [salloc(es, f"hTe{i}", [128, 8, TG], BF16) for i in range(2)]
            bhTs = [Buf(f"hTe{i}") for i in range(2)]
            aT = salloc(es, "aT", [128, 32, TG], BF16); baT = [Buf(f"aT{i}") for i in range(32)]
            rl = [salloc(es, f"rl{i}", [128, TG], BF16) for i in range(2)]
            brl = [Buf(f"rl{i}") for i in range(2)]
            ti = 0
            gi = 0
            ui = 0
            for s in range(NSEQ):
                for g in range(S // TG):
                    hT = hTs[gi % 2]; bhT = bhTs[gi % 2]
                    gi += 1
                    tiles = []
                    for j in range(TG // 128):
                        tt = g * (TG // 128) + j
                        t0 = tt * 128
                        xt = xts[ti % 4]; bxt = bxts[ti % 4]
                        xn = xns[ti % 2]; bxn = bxns[ti % 2]
                        tb = ti % 2
                        ti += 1
                        tiles.append((t0, xt, bxt))
                        T.dma("sp", xt[:], xres[s, t0:t0 + 128, :], bxt, writes=[bxt])
                        norm_to_T(xt[:], bxt,
                                  lambda c: gm2T[:, l, c, s:s + 1],
                                  lambda c: modT[:, l, 24 + c, s:s + 1],
                                  [b_gm2, b_modT_], xn, bxn, tb,
                                  lambda c: hT[:, c, j * 128:(j + 1) * 128], bhT)
                    for f in range(32):
                        bank = 2 + (ui % 2)
                        sl = ui % 2
                        ui += 1
                        for c in range(8):
                            T.op("pe", lambda e: e.matmul(pb[bank][:, 0:TG], lhsT=Wu[:, c, f * 128:(f + 1) * 128], rhs=hT[:, c, :],
                                                          start=(c == 0), stop=(c == 7)),
                                 reads=[bWu, bhT], writes=[PB[bank]])
                        T.op("act", lambda e: e.activation(out=rl[sl][:], in_=pb[bank][:, 0:TG], func=AF.Relu),
                             reads=[PB[bank]], writes=[brl[sl]])
                        T.op("pool" if f % 2 else "dve", lambda e: e.tensor_tensor(out=aT[:, f, :], in0=rl[sl][:], in1=rl[sl][:], op=ALU.mult),
                             reads=[brl[sl]], writes=[baT[f]])
                    for j, (t0, xt, bxt) in enumerate(tiles):
                        for half in range(2):
                            bank = 4 + (j % 2) * 2 + half
                            for f in range(32):
                                T.op("pe", lambda e: e.matmul(pb[bank][:], lhsT=aT[:, f, j * 128:(j + 1) * 128],
                                                              rhs=Wd[:, f, half * 512:(half + 1) * 512], start=(f == 0), stop=(f == 31)),
                                     reads=[baT[f], bWd], writes=[PB[bank]])
                            T.op("dve", lambda e: e.tensor_tensor(out=tmpE[j % 2][:, half * 512:(half + 1) * 512], in0=pb[bank][:],
                                                                  in1=gb[:, s, half * 512:(half + 1) * 512], op=ALU.mult),
                                 reads=[PB[bank], bgb], writes=[btmpE[j % 2]])
                        T.op("pool", lambda e: e.tensor_tensor(out=xt[:], in0=tmpE[j % 2][:], in1=xt[:], op=ALU.add),
                             reads=[btmpE[j % 2], bxt], writes=[bxt])
                        if not last:
                            T.dma("pool", xres[s, t0:t0 + 128, :], xt[:], bxt, reads=[bxt])
                        else:
                            rs, brs = rstd_from(xt[:], [bxt], D)
                            T.op("dve", lambda e: e.scalar_tensor_tensor(out=tmpE[j % 2][:], in0=xt[:], scalar=rs, in1=gf[:],
                                                                         op0=ALU.mult, op1=ALU.mult),
                                 reads=[bxt, brs, bgf], writes=[btmpE[j % 2]])
                            T.dma("pool", out_d[s, t0:t0 + 128, :], tmpE[j % 2][:], btmpE[j % 2], reads=[btmpE[j % 2]])
            T.barrier()

    tmpE = []
    btmpE = [Buf("tmpE0"), Buf("tmpE1")]

    tmpE.append(salloc(ges, "tmpE0", [128, D], F32))
    tmpE.append(salloc(ges, "tmpE1", [128, D], F32))

    convert_weights()
    esA0 = ExitStack()
    Wf0 = salloc(esA0, "Wf0", [128, 8, 2688], BF16); bWf0 = Buf("Wf0")
    Wt0 = salloc(esA0, "Wt0", [128, 8, 648], BF16); bWt0 = Buf("Wt0")
    preA = (Wf0, bWf0, Wt0, bWt0)
    prologue(preA)
    for l in range(nlayers):
        if "A" in phases:
            phase_A(l, pre=preA if l == 0 else None)
        if l == 0:
            esA0.close()
        if "B" in phases:
            phase_B(l)
        if "C" in phases:
            phase_C(l)
        if "D" in phases and "E" in phases:
            phase_DE(l, last=(l == nlayers - 1))
        elif "D" in phases:
            phase_D(l)
    T.barrier()
    ges.close()
    return nc, T


def _t5_bucket(rel):
    nb = 16
    max_exact = 8
    base = np.where(rel > 0, nb, 0)
    n = np.abs(rel)
    nf = np.maximum(n, max_exact).astype(np.float32)
    large = max_exact + (np.log(nf / np.float32(max_exact)) / np.float32(math.log(128 / max_exact))
                         * np.float32(nb - max_exact)).astype(np.int32)
    large = np.minimum(large, nb - 1)
    return base + np.where(n < max_exact, n, large)


def _consts():
    p = np.arange(128)
    c = {}
    c["c_ident"] = np.eye(128, dtype=np.float32)
    c["c_tri8"] = np.where(p[:, None] >= p[None, :], -8.0, 0.0).astype(np.float32)
    c["c_neg8"] = np.full((128, 128), -8.0, np.float32)
    cm = (p[:, None] < p[None, :]).astype(np.float32)
    c["c_cmask"] = np.ascontiguousarray(np.broadcast_to(cm[:, None, :], (128, 8, 128)))
    adm = ((p[:, None] // 64) <= (p[None, :] // 64)).astype(np.float32)
    c["c_adm"] = np.ascontiguousarray(np.broadcast_to(adm[:, None, :], (128, 8, 128)))
    c["c_admneg"] = np.ascontiguousarray(np.broadcast_to(np.where(adm > 0, 0.0, -1.0e5).astype(np.float32)[:, None, :], (128, 8, 128)))
    c["c_pow2"] = np.ascontiguousarray(np.broadcast_to(
        (0.5 ** np.arange(1, KBIS + 1)).astype(np.float32)[None, :], (128, KBIS)))
    c["c_ones"] = np.ones((128, 128), np.float32)
    return c


def _prep_inputs(inp, core):
    f = np.float32
    b0 = core * NSEQ
    bs = slice(b0, b0 + NSEQ)
    m = {}
    m["x"] = np.ascontiguousarray(inp["x"][bs], dtype=f)
    c = np.asarray(inp["c"], dtype=f)[bs]
    m["cT"] = np.ascontiguousarray(c.reshape(NSEQ, 8, 128).transpose(2, 1, 0))
    m["w_mod"] = np.ascontiguousarray(inp["w_mod"], dtype=f)
    bm = np.asarray(inp["b_mod"], dtype=f).reshape(2, 48, 128).transpose(2, 0, 1)
    m["b_modT"] = np.ascontiguousarray(np.broadcast_to(bm[..., None], (128, 2, 48, NSEQ)))
    ga = np.asarray(inp["g_attn"], dtype=f).reshape(2, 8, 128).transpose(2, 0, 1)
    m["g_attnT"] = np.ascontiguousarray(np.broadcast_to(ga[..., None], (128, 2, 8, NSEQ)))
    gm = np.asarray(inp["g_mlp"], dtype=f).reshape(2, 8, 128).transpose(2, 0, 1)
    m["g_mlpT"] = np.ascontiguousarray(np.broadcast_to(gm[..., None], (128, 2, 8, NSEQ)))
    m["w_in"] = np.ascontiguousarray(inp["w_in"], dtype=f)
    m["kvg_bc"] = np.ascontiguousarray(np.broadcast_to(np.asarray(inp["kv_norm_g"], dtype=f)[None], (128, 2, 128)))
    m["w_uv"] = np.ascontiguousarray(inp["w_uv"], dtype=f)
    m["goa_bc"] = np.ascontiguousarray(np.broadcast_to(np.asarray(inp["g_out_a"], dtype=f)[None], (128, 2, 512)))
    m["gob_bc"] = np.ascontiguousarray(np.broadcast_to(np.asarray(inp["g_out_b"], dtype=f)[None], (128, 2, 512)))
    m["w_out"] = np.ascontiguousarray(inp["w_out"], dtype=f)
    m["w_up"] = np.ascontiguousarray(inp["w_up"], dtype=f)
    m["w_down"] = np.ascontiguousarray(inp["w_down"], dtype=f)
    rb = np.asarray(inp["rel_bias"], dtype=f)
    p = np.arange(128)
    bn = np.empty((128, 2, 8, 128), f)
    for off in range(2):
        rel = (p[:, None] - off * 128) - p[None, :]
        bk = _t5_bucket(rel.astype(np.int32))
        bn[:, off] = rb[bk].transpose(0, 2, 1)
    m["biasn"] = bn
    far = rb[_t5_bucket(np.array([-1000], np.int32))[0]]
    m["bfar_bc"] = np.ascontiguousarray(np.broadcast_to(far[None, :, None], (128, 8, 128)))
    m["gfin_bc"] = np.ascontiguousarray(np.broadcast_to(np.asarray(inp["g_final"], dtype=f)[None], (128, D)))
    m.update(_consts())
    return m


_CACHE = {}


def kernel(**inputs):
    if "nc" not in _CACHE:
        _CACHE["nc"] = build_program()[0]
    nc = _CACHE["nc"]
    in_maps = [_prep_inputs(inputs, core) for core in range(8)]
    res = run_bass_kernel_spmd(nc, in_maps, core_ids=list(range(8)))
    out = np.concatenate([np.asarray(r["out"]) for r in res.results], axis=0)
    return out.astype(np.float32, copy=False)
```

```python
import math
from contextlib import ExitStack

import numpy as np
import concourse.bass as bass
import concourse.mybir as mybir
from concourse.bass_utils import run_bass_kernel_spmd

F32 = mybir.dt.float32
BF16 = mybir.dt.bfloat16
AF = mybir.ActivationFunctionType
ALU = mybir.AluOpType
AX = mybir.AxisListType

S = 2048
D = 1024
NSEQ = 2
NT = S // 128
DFF = 4096
DIN = 3272
EPS = 1e-6
KBIS = 16
TOPK = 256
EPOCH = 30000
LIM_SEQ = NSEQ
LIM_QB = NT
NO_POOL = False
ACC_DEPTH = 2
FLUSH_BG_BEFORE_C = False


class Buf:
    __slots__ = ("name", "last_w", "readers", "dsem", "bg")

    def __init__(self, name, bg=False):
        self.name = name
        self.last_w = None
        self.readers = []
        self.dsem = {}
        self.bg = bg


class Tracker:
    def __init__(self, nc):
        self.nc = nc
        self.eng = {"pe": nc.tensor, "act": nc.scalar, "dve": nc.vector,
                    "pool": nc.gpsimd, "sp": nc.sync}
        self.cnt = {e: 0 for e in self.eng}
        self.sems = {e: [] for e in self.eng}
        self.seen = {e: {} for e in self.eng}
        self.nsem = 0
        self.dma_bufs = []
        self.free_dsems = {"hw": [], "sw": []}
        self.nwaits = 0
        self.ninstr = 0

    def _newsem(self, name):
        self.nsem += 1
        return self.nc.alloc_semaphore(name=name)

    def _wait(self, e, tok):
        sem, val, src = tok
        key = id(sem)
        if self.seen[e].get(key, 0) >= val:
            return
        self.seen[e][key] = val
        self.eng[e].wait_ge(sem, val)
        self.nwaits += 1

    def _deps(self, e, reads, writes):
        for b in reads:
            if b.last_w is not None and not (b.last_w[2] == e and e == "pe"):
                self._wait(e, b.last_w)
        for b in writes:
            if b.last_w is not None and not (b.last_w[2] == e and e == "pe"):
                self._wait(e, b.last_w)
            for t in b.readers:
                if t[2] == e:
                    continue
                self._wait(e, t)

    def _commit(self, tok, reads, writes):
        for b in reads:
            b.readers.append(tok)
            if len(b.readers) > 12:
                d = {}
                for t in b.readers:
                    k = id(t[0])
                    if k not in d or d[k][1] < t[1]:
                        d[k] = t
                b.readers = list(d.values())
        for b in writes:
            b.last_w = tok
            b.readers = []

    def op(self, e, fn, reads=(), writes=()):
        self._deps(e, reads, writes)
        n = self.cnt[e]
        ep, v = divmod(n, EPOCH)
        while len(self.sems[e]) <= ep:
            self.sems[e].append(self._newsem(f"c_{e}_{len(self.sems[e])}"))
        sem = self.sems[e][ep]
        ins = fn(self.eng[e])
        ins.then_inc(sem, 1)
        self.cnt[e] = n + 1
        self.ninstr += 1
        tok = (sem, v + 1, e)
        self._commit(tok, reads, writes)
        return tok

    def dma(self, q, out, in_, sb, reads=(), writes=(), **kw):
        self._deps(q, reads, writes)
        kind = "sw" if q == "pool" else "hw"
        if kind not in sb.dsem:
            if self.free_dsems[kind]:
                sb.dsem[kind] = list(self.free_dsems[kind].pop())
            else:
                sb.dsem[kind] = [self._newsem(f"d{kind}_{sb.name}_{self.nsem}"), 0]
            self.dma_bufs.append((sb, kind))
        ent = sb.dsem[kind]
        ent[1] += 16
        ins = self.eng[q].dma_start(out=out, in_=in_, **kw)
        ins.then_inc(ent[0], 16)
        self.ninstr += 1
        tok = (ent[0], ent[1], "dma")
        self._commit(tok, reads, writes)
        return tok

    def barrier(self):
        toks = []
        for f in self.eng:
            n = self.cnt[f]
            if n == 0:
                continue
            ep, v = divmod(n - 1, EPOCH)
            toks.append((self.sems[f][ep], v + 1, f))
        for b, kind in self.dma_bufs:
            if not b.bg:
                toks.append((b.dsem[kind][0], b.dsem[kind][1], "dma"))
        for e in self.eng:
            for t in toks:
                if t[2] == e:
                    continue
                self._wait(e, t)
        keep = []
        for b, kind in self.dma_bufs:
            if b.bg:
                keep.append((b, kind))
            else:
                self.free_dsems[kind].append(tuple(b.dsem.pop(kind)))
        self.dma_bufs = keep


def build_program(nlayers=2, debug=False, phases="ABCDE"):
    nc = bass.Bass("TRN2", target_bir_lowering=False)
    T = Tracker(nc)
    uid = [0]

    def din(name, shape, dt=F32):
        return nc.dram_tensor(name, list(shape), dt, kind="ExternalInput").ap()

    def dscr(name, shape, dt):
        kind = "ExternalOutput" if debug else "Internal"
        return nc.dram_tensor(name, list(shape), dt, kind=kind).ap()

    x_in = din("x", [NSEQ, S, D])
    cT = din("cT", [128, 8, NSEQ])
    w_mod = din("w_mod", [2, D, 6 * D])
    b_modT = din("b_modT", [128, 2, 48, NSEQ])
    g_attnT = din("g_attnT", [128, 2, 8, NSEQ])
    g_mlpT = din("g_mlpT", [128, 2, 8, NSEQ])
    w_in = din("w_in", [2, D, DIN])
    kvg_bc = din("kvg_bc", [128, 2, 128])
    w_uv = din("w_uv", [2, 8, 128, 64])
    goa_bc = din("goa_bc", [128, 2, 512])
    gob_bc = din("gob_bc", [128, 2, 512])
    w_out = din("w_out", [2, D, D])
    w_up = din("w_up", [2, D, DFF])
    w_down = din("w_down", [2, DFF, D])
    biasn = din("biasn", [128, 2, 8, 128])
    bfar_bc = din("bfar_bc", [128, 8, 128])
    gfin_bc = din("gfin_bc", [128, D])
    c_ident = din("c_ident", [128, 128])
    c_tri8 = din("c_tri8", [128, 128])
    c_neg8 = din("c_neg8", [128, 128])
    c_cmask = din("c_cmask", [128, 8, 128])
    c_adm = din("c_adm", [128, 8, 128])
    c_admneg = din("c_admneg", [128, 8, 128])
    c_pow2 = din("c_pow2", [128, KBIS])
    c_ones = din("c_ones", [128, 128])
    out_d = nc.dram_tensor("out", [NSEQ, S, D], F32, kind="ExternalOutput").ap()

    xres = dscr("xres", [NSEQ, S, D], F32)
    featT = dscr("featT", [NSEQ, 21, 128, S], BF16)
    v_scr = dscr("v_scr", [NSEQ, S, 512], BF16)
    kv_tok = dscr("kv_tok", [NSEQ, S, 128], BF16)
    kvT_scr = dscr("kvT_scr", [NSEQ, 128, S], BF16)
    widx = dscr("widx", [NSEQ, S, 8], F32)
    ocat = dscr("ocat", [NSEQ, S, D], BF16)

    wb_in = [nc.dram_tensor(f"wb_in{l}", [D, DIN], BF16, kind="Internal").ap() for l in range(2)]
    wb_out = [nc.dram_tensor(f"wb_out{l}", [D, D], BF16, kind="Internal").ap() for l in range(2)]
    wb_up = [nc.dram_tensor(f"wb_up{l}", [D, DFF], BF16, kind="Internal").ap() for l in range(2)]
    wb_dn = [nc.dram_tensor(f"wb_dn{l}", [DFF, D], BF16, kind="Internal").ap() for l in range(2)]
    B_wb_in = [Buf(f"wb_in{l}", bg=True) for l in range(2)]
    B_wb_out = [Buf(f"wb_out{l}", bg=True) for l in range(2)]
    B_wb_up = [Buf(f"wb_up{l}", bg=True) for l in range(2)]
    B_wb_dn = [Buf(f"wb_dn{l}", bg=True) for l in range(2)]

    bgq = []

    def convert_weights():
        for l in range(nlayers):
            for (dst, src, bb, rows, step) in ((wb_in[l], w_in[l], B_wb_in[l], D, 256), (wb_out[l], w_out[l], B_wb_out[l], D, 512),
                                               (wb_up[l], w_up[l], B_wb_up[l], D, 256), (wb_dn[l], w_down[l], B_wb_dn[l], DFF, 1024)):
                for r0 in range(0, rows, step):
                    f = (lambda dst=dst, src=src, bb=bb, r0=r0, step=step:
                         T.dma("pool", dst[r0:r0 + step, :], src[r0:r0 + step, :], bb, writes=[bb]))
                    if l == 0 and dst is wb_in[0]:
                        f()
                    else:
                        bgq.append((l, f))

    def bg_step(n=1):
        for _ in range(n):
            if bgq:
                bgq.pop(0)[1]()

    def bg_flush(layer):
        while bgq and bgq[0][0] <= layer:
            bgq.pop(0)[1]()

    def salloc(es, name, shape, dt):
        uid[0] += 1
        return es.enter_context(nc.sbuf_tensor(f"{name}_{uid[0]}", list(shape), dt))

    ges = ExitStack()
    pb2 = [ges.enter_context(nc.psum_tensor(f"pbp{i}", [128, 1024], F32)) for i in range(4)]
    pb = [pb2[i // 2][:, (i % 2) * 512:(i % 2 + 1) * 512] for i in range(8)]
    PB = [Buf(f"pb{i}") for i in range(8)]

    def pbf(i):
        return pb[i].bitcast(BF16)

    ident_f = salloc(ges, "identf", [128, 128], F32); b_identf = Buf("identf")
    ident_b = salloc(ges, "identb", [128, 128], BF16); b_identb = Buf("identb")
    ones_f = salloc(ges, "onesf", [128, 128], F32); b_onesf = Buf("onesf")
    ones_b = salloc(ges, "onesb", [128, 128], BF16); b_onesb = Buf("onesb")
    modT = salloc(ges, "modT", [128, 2, 48, NSEQ], F32); b_modT_ = Buf("modT")
    gm1T = salloc(ges, "gm1T", [128, 2, 8, NSEQ], F32); b_gm1 = Buf("gm1T")
    gm2T = salloc(ges, "gm2T", [128, 2, 8, NSEQ], F32); b_gm2 = Buf("gm2T")
    stat = salloc(ges, "stat", [128, 8, 4], F32)
    STB = [Buf(f"stat{i}") for i in range(8)]
    junk = salloc(ges, "junk", [128, 2048], BF16); b_junk = Buf("junk")
    stat_i = [0]

    T.dma("sp", ident_f[:], c_ident, b_identf, writes=[b_identf])
    T.dma("pool", ident_b[:], c_ident, b_identb, writes=[b_identb])
    T.dma("sp", ones_f[:], c_ones, b_onesf, writes=[b_onesf])
    T.dma("pool", ones_b[:], c_ones, b_onesb, writes=[b_onesb])

    def prologue(preA=None):
        with ExitStack() as es:
            sT = salloc(es, "sT", [128, 8, NSEQ], F32); b_sT = Buf("sT")
            cTs = salloc(es, "cTs", [128, 8, NSEQ], F32); b_cTs = Buf("cTs")
            bm = salloc(es, "bm", [128, 2, 48, NSEQ], F32); b_bm = Buf("bm")
            ga = salloc(es, "ga", [128, 2, 8, NSEQ], F32); b_ga = Buf("ga")
            gmm = salloc(es, "gmm", [128, 2, 8, NSEQ], F32); b_gmm = Buf("gmm")
            NW = 4
            wms = [salloc(es, f"wm{i}", [128, 8, 512], F32) for i in range(NW)]
            b_wms = [Buf(f"wm{i}") for i in range(NW)]
            modrow = salloc(es, "modrow", [NSEQ, 6 * D], F32); b_mrow = Buf("modrow")
            T.dma("sp", cTs[:], cT, b_cTs, writes=[b_cTs])
            T.dma("sp", bm[:], b_modT, b_bm, writes=[b_bm])
            T.dma("sp", ga[:], g_attnT, b_ga, writes=[b_ga])
            T.dma("sp", gmm[:], g_mlpT, b_gmm, writes=[b_gmm])
            T.op("act", lambda e: e.activation(out=sT[:], in_=cTs[:], func=AF.Silu),
                 reads=[b_cTs], writes=[b_sT])
            it = 0
            for l in range(nlayers):
                wl = w_mod[l].rearrange("(c p) n -> p c n", p=128)
                for ns in range(12):
                    sl = it % NW
                    bank = it % 2
                    it += 1
                    T.dma("sp", wms[sl][:], wl[:, :, ns * 512:(ns + 1) * 512], b_wms[sl],
                          writes=[b_wms[sl]])
                    for c in range(8):
                        T.op("pe", lambda e: e.matmul(
                            pb[bank][0:NSEQ, 0:512], lhsT=sT[:, c, :], rhs=wms[sl][:, c, :],
                            start=(c == 0), stop=(c == 7)),
                            reads=[b_wms[sl], b_sT], writes=[PB[bank]])
                    T.op("act", lambda e: e.activation(out=modrow[:, ns * 512:(ns + 1) * 512],
                                                       in_=pb[bank][0:NSEQ, 0:512], func=AF.Copy),
                         reads=[PB[bank]], writes=[b_mrow])
                for j in range(48):
                    T.op("pe", lambda e: e.transpose(pb[2][:, j * NSEQ:(j + 1) * NSEQ],
                                                     modrow[0:NSEQ, j * 128:(j + 1) * 128],
                                                     ident_f[0:NSEQ, 0:NSEQ]),
                         reads=[b_mrow, b_identf], writes=[PB[2]])
                T.op("dve", lambda e: e.tensor_tensor(
                    out=modT[:, l, :, :],
                    in0=pb[2][:, 0:48 * NSEQ].rearrange("p (j b) -> p j b", b=NSEQ),
                    in1=bm[:, l, :, :], op=ALU.add),
                    reads=[PB[2], b_bm], writes=[b_modT_])
                T.op("dve", lambda e: e.scalar_tensor_tensor(
                    out=gm1T[:, l], in0=modT[:, l, 8:16, :], scalar=1.0, in1=ga[:, l],
                    op0=ALU.add, op1=ALU.mult), reads=[b_modT_, b_ga], writes=[b_gm1])
                T.op("dve", lambda e: e.scalar_tensor_tensor(
                    out=gm2T[:, l], in0=modT[:, l, 32:40, :], scalar=1.0, in1=gmm[:, l],
                    op0=ALU.add, op1=ALU.mult), reads=[b_modT_, b_gmm], writes=[b_gm2])
            if preA is not None:
                load_A_weights(0, *preA)
            T.barrier()

    def next_stat():
        i = stat_i[0] % 8
        stat_i[0] += 1
        return stat[:, i, :], STB[i]

    def rstd_from(src_ap, src_bufs, n, from_psum=False):
        st, sbuf_ = next_stat()
        T.op("act", lambda e: e.activation(out=junk[:, 0:n], in_=src_ap, func=AF.Square,
                                           accum_out=st[:, 0:1]),
             reads=src_bufs, writes=[b_junk, sbuf_])
        T.op("dve", lambda e: e.tensor_scalar(out=st[:, 1:2], in0=st[:, 0:1], scalar1=1.0 / n,
                                              scalar2=EPS, op0=ALU.mult, op1=ALU.add),
             reads=[sbuf_], writes=[sbuf_])
        T.op("act", lambda e: e.activation(out=st[:, 2:3], in_=st[:, 1:2], func=AF.Sqrt),
             reads=[sbuf_], writes=[sbuf_])
        T.op("dve", lambda e: e.reciprocal(out=st[:, 3:4], in_=st[:, 2:3]),
             reads=[sbuf_], writes=[sbuf_])
        return st[:, 3:4], sbuf_

    def norm_to_T(xt_ap, bx, gmT_ap, shT_ap, bmods, xn, bxn, tbank, hT_dst, bhT):
        rstd, brs = rstd_from(xt_ap, [bx], D)
        T.op("dve", lambda e: e.tensor_scalar(out=xn[:], in0=xt_ap, scalar1=rstd, scalar2=None,
                                              op0=ALU.mult), reads=[bx, brs], writes=[bxn])
        pv = pbf(tbank).rearrange("p (c t) -> p c t", c=8)
        for c in range(8):
            T.op("pe", lambda e: e.transpose(pv[:, c, :], xn[:, c * 128:(c + 1) * 128], ident_b[:]),
                 reads=[bxn, b_identb], writes=[PB[tbank]])
        for c in range(8):
            if c % 2 == 0:
                T.op("act", lambda e: e.activation(out=hT_dst(c), in_=pv[:, c, :], func=AF.Identity,
                                                   scale=gmT_ap(c), bias=shT_ap(c)),
                     reads=[PB[tbank]] + bmods, writes=[bhT])
            else:
                T.op("dve", lambda e: e.tensor_scalar(out=hT_dst(c), in0=pv[:, c, :],
                                                      scalar1=gmT_ap(c), scalar2=shT_ap(c),
                                                      op0=ALU.mult, op1=ALU.add),
                     reads=[PB[tbank]] + bmods, writes=[bhT])

    def ga_bcast(es, l, chunk0, name):
        gb = salloc(es, name, [128, NSEQ, D], F32); bgb = Buf(name)
        dg = salloc(es, name + "dg", [128, 2, 128], F32); bdg = [Buf(name + "dg0"), Buf(name + "dg1")]
        k = 0
        for b in range(NSEQ):
            for half in range(2):
                bank = 6 + half
                for cc in range(4):
                    c = half * 4 + cc
                    sl = k % 2
                    k += 1
                    T.op("dve", lambda e: e.tensor_scalar(
                        out=dg[:, sl, :], in0=ident_f[:], scalar1=modT[:, l, chunk0 + c, b:b + 1],
                        scalar2=None, op0=ALU.mult), reads=[b_identf, b_modT_], writes=[bdg[sl]])
                    T.op("pe", lambda e: e.matmul(pb[bank][:, cc * 128:(cc + 1) * 128], lhsT=ones_f[:],
                                                  rhs=dg[:, sl, :], start=(cc == 0), stop=True,
                                                  skip_group_check=True),
                         reads=[b_onesf, bdg[sl]], writes=[PB[bank]])
                T.op("act", lambda e: e.activation(out=gb[:, b, half * 512:(half + 1) * 512],
                                                   in_=pb[bank][:], func=AF.Copy),
                     reads=[PB[bank]], writes=[bgb])
        return gb, bgb

    def load_A_weights(l, Wf, bWf, Wt, bWt):
        wl = wb_in[l].rearrange("(c p) n -> p c n", p=128)
        for (a, b, o) in [(0, 1024, 0), (1536, 2560, 1024), (2688, 3200, 2048),
                          (3200, 3264, 2560), (3200, 3264, 2624)]:
            for c in range(8):
                T.dma("sp", Wf[:, c, o:o + (b - a)], wl[:, c, a:b], bWf, reads=[B_wb_in[l]], writes=[bWf])
        for (a, b, o) in [(1024, 1536, 0), (2560, 2688, 512), (3264, 3272, 640)]:
            T.dma("sp", Wt[:, :, o:o + (b - a)], wl[:, :, a:b], bWt, reads=[B_wb_in[l]], writes=[bWt])

    def phase_A(l, pre=None):
        with ExitStack() as es:
            if pre is None:
                bg_flush(l)
                Wf = salloc(es, "Wf", [128, 8, 2688], BF16); bWf = Buf("Wf")
                Wt = salloc(es, "Wt", [128, 8, 648], BF16); bWt = Buf("Wt")
                load_A_weights(l, Wf, bWf, Wt, bWt)
            else:
                Wf, bWf, Wt, bWt = pre
            kvg = salloc(es, "kvg", [128, 128], F32); bkvg = Buf("kvg")
            T.dma("sp", kvg[:], kvg_bc[:, l, :], bkvg, writes=[bkvg])
            xts = [salloc(es, f"xt{i}", [128, D], F32) for i in range(3)]
            bxts = [Buf(f"xt{i}") for i in range(3)]
            xns = [salloc(es, f"xn{i}", [128, D], BF16) for i in range(2)]
            bxns = [Buf(f"xn{i}") for i in range(2)]
            hTs = [salloc(es, f"hT{i}", [128, 8, 512], BF16) for i in range(2)]
            bhTs = [Buf(f"hT{i}") for i in range(2)]
            vts = [salloc(es, f"vt{i}", [128, 512], BF16) for i in range(2)]
            bvts = [Buf(f"vt{i}") for i in range(2)]
            kvn = [salloc(es, f"kvn{i}", [128, 128], BF16) for i in range(2)]
            bkvn = [Buf(f"kvn{i}") for i in range(2)]
            kvTt = [salloc(es, f"kvTt{i}", [128, 128], BF16) for i in range(2)]
            bkvTt = [Buf(f"kvTt{i}") for i in range(2)]
            wis = [salloc(es, f"wis{i}", [128, 8], F32) for i in range(2)]
            bwis = [Buf(f"wis{i}") for i in range(2)]
            fos = [salloc(es, f"fo{i}", [128, 512], BF16) for i in range(3)]
            bfos = [Buf(f"fo{i}") for i in range(3)]
            src = x_in if l == 0 else xres
            cnt = {"ti": 0, "fi": 0}
            groups = [(s, g) for s in range(NSEQ) for g in range(4)]

            def prep_a(gi, j):
                s, g = groups[gi]
                hT = hTs[gi % 2]; bhT = bhTs[gi % 2]
                tt = g * 4 + j
                t0 = tt * 128
                ti = cnt["ti"]
                cnt["ti"] += 1
                xt = xts[ti % 3]; bxt = bxts[ti % 3]
                xn = xns[ti % 2]; bxn = bxns[ti % 2]
                T.dma("sp", xt[:], src[s, t0:t0 + 128, :], bxt, writes=[bxt])
                norm_to_T(xt[:], bxt,
                          lambda c: gm1T[:, l, c, s:s + 1],
                          lambda c: modT[:, l, 0 + c, s:s + 1],
                          [b_gm1, b_modT_], xn, bxn, ti % 2,
                          lambda c: hT[:, c, j * 128:(j + 1) * 128], bhT)
                return ti

            def prep_b(gi, j, ti):
                s, g = groups[gi]
                hT = hTs[gi % 2]; bhT = bhTs[gi % 2]
                t0 = (g * 4 + j) * 128
                k2 = ti % 2
                for c in range(8):
                    T.op("pe", lambda e: e.matmul(pb[2][:, 0:512], lhsT=hT[:, c, j * 128:(j + 1) * 128],
                                                  rhs=Wt[:, c, 0:512], start=(c == 0), stop=(c == 7)),
                         reads=[bhT, bWt], writes=[PB[2]])
                for c in range(8):
                    T.op("pe", lambda e: e.matmul(pb[3][:, 0:136], lhsT=hT[:, c, j * 128:(j + 1) * 128],
                                                  rhs=Wt[:, c, 512:648], start=(c == 0), stop=(c == 7)),
                         reads=[bhT, bWt], writes=[PB[3]])
                T.op("act", lambda e: e.activation(out=vts[k2][:], in_=pb[2][:, 0:512], func=AF.Copy),
                     reads=[PB[2]], writes=[bvts[k2]])
                T.dma("pool", v_scr[s, t0:t0 + 128, :], vts[k2][:], bvts[k2], reads=[bvts[k2]])
                rs2, brs2 = rstd_from(pb[3][:, 0:128], [PB[3]], 128)
                T.op("dve", lambda e: e.scalar_tensor_tensor(
                    out=kvn[k2][:], in0=pb[3][:, 0:128], scalar=rs2, in1=kvg[:],
                    op0=ALU.mult, op1=ALU.mult), reads=[PB[3], brs2, bkvg], writes=[bkvn[k2]])
                T.op("dve", lambda e: e.tensor_copy(out=wis[k2][:], in_=pb[3][:, 128:136]),
                     reads=[PB[3]], writes=[bwis[k2]])
                T.dma("pool", kv_tok[s, t0:t0 + 128, :], kvn[k2][:], bkvn[k2], reads=[bkvn[k2]])
                T.dma("pool", widx[s, t0:t0 + 128, :], wis[k2][:], bwis[k2], reads=[bwis[k2]])
                T.op("pe", lambda e: e.transpose(pbf(4)[:, 0:128], kvn[k2][:], ident_b[:]),
                     reads=[bkvn[k2], b_identb], writes=[PB[4]])
                T.op("act", lambda e: e.activation(out=kvTt[k2][:], in_=pbf(4)[:, 0:128], func=AF.Copy),
                     reads=[PB[4]], writes=[bkvTt[k2]])
                T.dma("pool", kvT_scr[s, :, t0:t0 + 128], kvTt[k2][:], bkvTt[k2], reads=[bkvTt[k2]])

            def fm_chunk(gi, ch):
                s, g = groups[gi]
                hT = hTs[gi % 2]; bhT = bhTs[gi % 2]
                fi = cnt["fi"]
                cnt["fi"] += 1
                bank = 5 + (fi % 3)
                fo = fos[fi % 3]; bfo = bfos[fi % 3]
                for c in range(8):
                    T.op("pe", lambda e: e.matmul(pb[bank][:, 0:512], lhsT=Wf[:, c, ch * 128:(ch + 1) * 128],
                                                  rhs=hT[:, c, :], start=(c == 0), stop=(c == 7)),
                         reads=[bhT, bWf], writes=[PB[bank]])
                if fi % 2 == 0:
                    T.op("act", lambda e: e.activation(out=fo[:], in_=pb[bank][:, 0:512], func=AF.Copy),
                         reads=[PB[bank]], writes=[bfo])
                else:
                    T.op("dve", lambda e: e.tensor_copy(out=fo[:], in_=pb[bank][:, 0:512]),
                         reads=[PB[bank]], writes=[bfo])
                T.dma("pool", featT[s, ch, :, g * 512:(g + 1) * 512], fo[:], bfo, reads=[bfo])

            for j in range(4):
                ti0 = prep_a(0, j)
                prep_b(0, j, ti0)
            for gi in range(len(groups)):
                pend_b = None
                for ch in range(21):
                    fm_chunk(gi, ch)
                    if gi + 1 < len(groups):
                        if ch in (1, 6, 11, 16):
                            j = (1, 6, 11, 16).index(ch)
                            pend_b = (j, prep_a(gi + 1, j))
                        if ch in (4, 9, 14, 19) and pend_b is not None:
                            prep_b(gi + 1, pend_b[0], pend_b[1])
                            pend_b = None
            T.barrier()

    def phase_B(l):
        with ExitStack() as es:
            qT = salloc(es, "qT", [128, 4, S], BF16); bqT = Buf("qT")
            kT = salloc(es, "kT", [128, 4, S], BF16); bkT = Buf("kT")
            vv = salloc(es, "vv", [128, NT, 512], BF16); bvv = Buf("vv")
            tri8 = salloc(es, "tri8", [128, 128], BF16); btri = Buf("tri8")
            neg8 = salloc(es, "neg8", [128, 128], BF16); bneg = Buf("neg8")
            cm = salloc(es, "cm", [128, 512], BF16); bcm = Buf("cm")
            goa = salloc(es, "goa", [128, 512], F32); bgoa = Buf("goa")
            T.dma("pool", tri8[:], c_tri8, btri, writes=[btri])
            T.dma("pool", neg8[:], c_neg8, bneg, writes=[bneg])
            T.dma("pool", cm[:], c_cmask[:, 0:4, :].rearrange("p h t -> p (h t)"), bcm, writes=[bcm])
            T.dma("sp", goa[:], goa_bc[:, l, :], bgoa, writes=[bgoa])
            e32a = [salloc(es, f"e32a{i}", [128, 1024], F32) for i in range(2)]
            spba = [salloc(es, f"spba{i}", [128, 1024], BF16) for i in range(2)]
            wba = [salloc(es, f"wba{i}", [128, 1024], BF16) for i in range(2)]
            e32 = [[e32a[i][:, g * 512:(g + 1) * 512] for i in range(2)] for g in range(2)]
            spb = [[spba[i][:, g * 512:(g + 1) * 512] for i in range(2)] for g in range(2)]
            wb = [[wba[i][:, g * 512:(g + 1) * 512] for i in range(2)] for g in range(2)]
            be32 = [[Buf(f"e32{g}{i}") for i in range(2)] for g in range(2)]
            bspb = [[Buf(f"spb{g}{i}") for i in range(2)] for g in range(2)]
            bwb = [[Buf(f"wb{g}{i}") for i in range(2)] for g in range(2)]
            sps = [salloc(es, f"sps{g}", [128, 512], F32) for g in range(2)]
            bsps = [Buf(f"sps{g}") for g in range(2)]
            spsb = [[salloc(es, f"spsb{g}{i}", [128, 512], BF16) for i in range(2)] for g in range(2)]
            bspsb = [[Buf(f"spsb{g}{i}") for i in range(2)] for g in range(2)]
            oan = [salloc(es, f"oan{i}", [128, 512], BF16) for i in range(2)]
            boan = [Buf(f"oan{i}") for i in range(2)]
            ZB = [[0, 2], [1, 3]]
            AB = [4, 5]
            OB = [6, 7]
            carry_slot = [0, 0]

            def zmm(bank, hg, qb, kb, start_first):
                for i in range(4):
                    h = 2 * i + hg
                    ch = h // 2
                    r0 = (h % 2) * 64
                    T.op("pe", lambda e: e.matmul(
                        pb[bank][:, i * 128:(i + 1) * 128],
                        lhsT=kT[r0:r0 + 64, ch, kb * 128:(kb + 1) * 128],
                        rhs=qT[r0:r0 + 64, ch, qb * 128:(qb + 1) * 128],
                        start=(start_first and i == 0), stop=(i == 3),
                        skip_group_check=True),
                        reads=[bkT, bqT], writes=[PB[bank]])

            def stage_Z(n, qb, kb):
                sl = n % 2
                for hg in range(2):
                    zmm(ZB[hg][sl], hg, qb, kb, True)
                T.op("act", lambda e: e.activation(out=e32a[sl][:], in_=pb2[sl][:], func=AF.Exp, scale=0.125),
                     reads=[PB[ZB[0][sl]], PB[ZB[1][sl]]], writes=[be32[0][sl], be32[1][sl]])
                T.op("act", lambda e: e.activation(out=spba[sl][:], in_=e32a[sl][:], func=AF.Ln, bias=1.0),
                     reads=[be32[0][sl], be32[1][sl]], writes=[bspb[0][sl], bspb[1][sl]])
                for hg in range(2):
                    if kb == qb:
                        T.op("dve", lambda e: e.tensor_tensor(out=spb[hg][sl][:], in0=spb[hg][sl][:], in1=cm[:], op=ALU.mult),
                             reads=[bspb[hg][sl], bcm], writes=[bspb[hg][sl]])

            def stage_A(n, qb, kb):
                sl = n % 2
                for hg in range(2):
                    ab = AB[hg]
                    T.op("pe", lambda e: e.matmul(pb[ab][:], lhsT=tri8[:], rhs=spb[hg][sl][:], start=True, stop=False,
                                                  skip_group_check=True),
                         reads=[btri, bspb[hg][sl]], writes=[PB[ab]])
                    if kb < qb:
                        cs = carry_slot[hg]
                        T.op("pe", lambda e: e.matmul(pb[ab][:], lhsT=neg8[:], rhs=spsb[hg][cs][:], start=False, stop=False,
                                                      skip_group_check=True),
                             reads=[bneg, bspsb[hg][cs]], writes=[PB[ab]])
                for hg in range(2):
                    zmm(AB[hg], hg, qb, kb, False)
                T.op("act", lambda e: e.activation(out=wba[sl][:], in_=pb2[2][:], func=AF.Exp, scale=0.125),
                     reads=[PB[AB[0]], PB[AB[1]]], writes=[bwb[0][sl], bwb[1][sl]])
                for hg in range(2):
                    ab = AB[hg]
                    if kb == qb:
                        T.op("dve", lambda e: e.tensor_tensor(out=wb[hg][sl][:], in0=wb[hg][sl][:], in1=cm[:], op=ALU.mult),
                             reads=[bwb[hg][sl], bcm], writes=[bwb[hg][sl]])
                    if kb > 0:
                        if kb == qb:
                            T.op("dve", lambda e: e.tensor_copy(out=sps[hg][:], in_=spb[hg][sl][:]),
                                 reads=[bspb[hg][sl]], writes=[bsps[hg]])
                        else:
                            T.op("dve", lambda e: e.tensor_tensor(out=sps[hg][:], in0=sps[hg][:], in1=spb[hg][sl][:], op=ALU.add),
                                 reads=[bsps[hg], bspb[hg][sl]], writes=[bsps[hg]])
                        carry_slot[hg] ^= 1
                        cs = carry_slot[hg]
                        T.op("dve", lambda e: e.tensor_copy(out=spsb[hg][cs][:], in_=sps[hg][:]),
                             reads=[bsps[hg]], writes=[bspsb[hg][cs]])

            def stage_PV(n, s, qb, kb):
                sl = n % 2
                ob = OB[qb % 2]
                for hg in range(2):
                    for i in range(4):
                        h = 2 * i + hg
                        T.op("pe", lambda e: e.matmul(
                            pb[ob][:, h * 64:(h + 1) * 64], lhsT=wb[hg][sl][:, i * 128:(i + 1) * 128],
                            rhs=vv[:, kb, h * 64:(h + 1) * 64], start=(kb == qb and hg == 0 and i == 0), stop=False,
                            skip_group_check=True),
                            reads=[bwb[hg][sl], bvv], writes=[PB[ob]])
                if kb == 0:
                    osl = qb % 2
                    rs, brs = rstd_from(pb[ob][:], [PB[ob]], 512)
                    T.op("dve", lambda e: e.scalar_tensor_tensor(out=oan[osl][:], in0=pb[ob][:], scalar=rs, in1=goa[:],
                                                                 op0=ALU.mult, op1=ALU.mult),
                         reads=[PB[ob], brs, bgoa], writes=[boan[osl]])
                    T.dma("pool", ocat[s, qb * 128:(qb + 1) * 128, 0:512], oan[osl][:], boan[osl], reads=[boan[osl]])
                    bg_step(1)

            for s in range(LIM_SEQ):
                T.dma("sp", qT[:], featT[s, 0:4].rearrange("c p t -> p c t"), bqT, writes=[bqT])
                T.dma("sp", kT[:], featT[s, 4:8].rearrange("c p t -> p c t"), bkT, writes=[bkT])
                T.dma("sp", vv[:], v_scr[s].rearrange("(n p) f -> p n f", p=128), bvv, writes=[bvv])
                its = [(qb, kb) for qb in range(LIM_QB) for kb in range(qb, -1, -1)]
                N = len(its)
                for t in range(N + 2):
                    if t < N:
                        stage_Z(t, *its[t])
                    if 0 <= t - 1 < N:
                        stage_A(t - 1, *its[t - 1])
                    if 0 <= t - 2 < N:
                        stage_PV(t - 2, s, *its[t - 2])
            T.barrier()

    def phase_C(l):
        if FLUSH_BG_BEFORE_C:
            bg_flush(99)
        with ExitStack() as es:
            dqT = salloc(es, "dqT", [128, 8, S], BF16); bdq = Buf("dqT")
            iqT = salloc(es, "iqT", [128, 4, S], BF16); biq = Buf("iqT")
            ikT = salloc(es, "ikT", [128, S], BF16); bik = Buf("ikT")
            kvT = salloc(es, "kvT", [128, S], BF16); bkvT = Buf("kvT")
            kvt = salloc(es, "kvt", [128, NT, 128], BF16); bkvt = Buf("kvt")
            wi = salloc(es, "wi", [128, NT, 8], F32); bwi = Buf("wi")
            wuv = salloc(es, "wuv", [128, 8, 64], BF16); bwuv = Buf("wuv")
            gob = salloc(es, "gob", [128, 512], F32); bgob = Buf("gob")
            BN = salloc(es, "BN", [128, 2, 1024], BF16); bBN = Buf("BN")
            id4 = salloc(es, "id4", [128, 512], BF16); bid4 = Buf("id4")
            pw2 = salloc(es, "pw2", [128, KBIS], F32); bpw2 = Buf("pw2")
            T.dma("pool", wuv[:], w_uv[l].rearrange("h r d -> r h d"), bwuv, writes=[bwuv])
            T.dma("sp", gob[:], gob_bc[:, l, :], bgob, writes=[bgob])
            T.dma("sp", pw2[:], c_pow2, bpw2, writes=[bpw2])
            for i in range(4):
                T.dma("pool", id4[:, i * 128:(i + 1) * 128], c_ident, bid4, writes=[bid4])
            score = [salloc(es, f"score{i}", [128, S], F32) for i in range(2)]
            bsc = [Buf(f"score{i}") for i in range(2)]
            nmask = [salloc(es, f"nmask{i}", [128, S], BF16) for i in range(2)]
            bnm = [Buf(f"nmask{i}") for i in range(2)]
            PP = [salloc(es, f"PP{i}", [128, 1024], BF16) for i in range(2)]
            bPP = [Buf(f"PP{i}") for i in range(2)]
            oTs = salloc(es, "oTs", [128, 1024], BF16); boTs = Buf("oTs")
            bis = salloc(es, "bis", [128, 8 + 2 * KBIS], F32); bbis = Buf("bis")
            rden = salloc(es, "rden", [128, 8], F32); brden = Buf("rden")
            obf = salloc(es, "obf", [128, 512], F32); bobf = Buf("obf")
            obn = [salloc(es, f"obn{i}", [128, 512], BF16) for i in range(2)]
            bobn = [Buf(f"obn{i}") for i in range(2)]
            lsc = 128 ** -0.5
            isc = (64 ** -0.5) * (8 ** -0.5)
            with ExitStack() as es2:
                bn = salloc(es2, "bn", [128, 2, 1024], F32); bbn = Buf("bn")
                bf = salloc(es2, "bf", [128, 1024], F32); bbf = Buf("bf")
                adn = salloc(es2, "adn", [128, 1024], F32); badn = Buf("adn")
                T.dma("sp", bn[:], biasn.rearrange("p o h t -> p o (h t)"), bbn, writes=[bbn])
                T.dma("sp", bf[:], bfar_bc.rearrange("p h t -> p (h t)"), bbf, writes=[bbf])
                T.dma("sp", adn[:], c_admneg.rearrange("p h t -> p (h t)"), badn, writes=[badn])
                for o in range(2):
                    T.op("dve", lambda e: e.tensor_tensor(out=bn[:, o, :], in0=bn[:, o, :], in1=bf[:], op=ALU.subtract),
                         reads=[bbn, bbf], writes=[bbn])
                    if o == 0:
                        T.op("dve", lambda e: e.scalar_tensor_tensor(out=BN[:, o, :], in0=bn[:, o, :], scalar=1.0 / lsc, in1=adn[:],
                                                                     op0=ALU.mult, op1=ALU.add),
                             reads=[bbn, badn], writes=[bBN])
                    else:
                        T.op("dve", lambda e: e.tensor_scalar(out=BN[:, o, :], in0=bn[:, o, :], scalar1=1.0 / lsc, scalar2=None,
                                                              op0=ALU.mult), reads=[bbn], writes=[bBN])
                T.barrier()
            cnt_i = {"ii": 0, "pi": 0, "oi": 0, "lp": 0, "ib": 0}
            IBS = (2, 3)
            SB = 4
            NRB = 2 * ACC_DEPTH + 2
            Rb = [salloc(es, f"Rb{i}", [128, 512], BF16) for i in range(NRB)]
            bRb = [Buf(f"Rb{i}") for i in range(NRB)]
            dg = [salloc(es, f"dg{i}", [128, 8, 128], BF16) for i in range(2)]
            bdg = [Buf(f"dg{i}") for i in range(2)]
            absw = salloc(es, "absw", [128, NT, 8], F32); babsw = Buf("absw")
            sgn = salloc(es, "sgn", [128, NT, 8], F32); bsgn = Buf("sgn")
            OTB = (5, 6)
            DB = 7

            pend_acc = []

            def flush_acc(keep=0):
                while len(pend_acc) > keep:
                    (qb, c0, w, j, rsl, dsl) = pend_acc.pop(0)
                    sc_ = score[qb % 2]; bsc_ = bsc[qb % 2]
                    T.op("pe", lambda e: e.matmul(pb[SB][:, 0:w], lhsT=dg[dsl][:, j, :], rhs=Rb[rsl][:, 0:w],
                                                  start=(j == 0), stop=(j == 7), skip_group_check=True),
                         reads=[bdg[dsl], bRb[rsl]], writes=[PB[SB]])
                    if j == 7:
                        T.op("act", lambda e: e.activation(out=sc_[:, c0:c0 + w], in_=pb[SB][:, 0:w], func=AF.Copy),
                             reads=[PB[SB]], writes=[bsc_])

            def make_diag(qb):
                dsl = qb % 2
                for j in range(8):
                    T.op("act", lambda e: e.activation(out=dg[dsl][:, j, :], in_=ident_b[:], func=AF.Identity,
                                                       scale=sgn[:, qb, j:j + 1]),
                         reads=[b_identb, bsgn], writes=[bdg[dsl]])

            def idx_unit(s, qb, c, j):
                n = (qb + 1) * 128
                q0 = qb * 128
                c0 = c * 512
                w = min(512, n - c0)
                rsl = cnt_i["ii"] % NRB
                cnt_i["ii"] += 1
                ch = j // 2
                r0 = (j % 2) * 64
                IB = IBS[cnt_i["ib"] % 2]
                cnt_i["ib"] += 1
                T.op("pe", lambda e: e.matmul(pb[IB][:, 0:w], lhsT=iqT[r0:r0 + 64, ch, q0:q0 + 128],
                                              rhs=ikT[r0:r0 + 64, c0:c0 + w], start=True, stop=True),
                     reads=[biq, bik], writes=[PB[IB]])
                T.op("act", lambda e: e.activation(out=Rb[rsl][:, 0:w], in_=pb[IB][:, 0:w], func=AF.Relu,
                                                   scale=absw[:, qb, j:j + 1]),
                     reads=[PB[IB], babsw], writes=[bRb[rsl]])
                flush_acc(keep=ACC_DEPTH - 1)
                pend_acc.append((qb, c0, w, j, rsl, qb % 2))

            def select(qb):
                n = (qb + 1) * 128
                nm = nmask[qb % 2]; bn_ = bnm[qb % 2]
                score_ = score[qb % 2]; bsc_ = bsc[qb % 2]
                T.op("dve", lambda e: e.memset(score_[0:64, n - 64:n], -1.0e30), reads=[bsc_], writes=[bsc_])
                if qb >= 2:
                    hi = bis[:, 0:1]; lo = bis[:, 1:2]; w0 = bis[:, 2:3]; mid = bis[:, 3:4]
                    cnt = bis[:, 4:5]; sv = bis[:, 5:6]; thr = bis[:, 6:7]
                    H = bis[:, 8:8 + KBIS]; H2 = bis[:, 8 + KBIS:8 + 2 * KBIS]
                    T.op("dve", lambda e: e.tensor_reduce(out=hi, in_=score_[:, 0:n], axis=AX.X, op=ALU.max),
                         reads=[bsc_], writes=[bbis])
                    T.op("dve", lambda e: e.tensor_reduce(out=lo, in_=score_[:, 0:n - 64], axis=AX.X, op=ALU.min),
                         reads=[bsc_], writes=[bbis])
                    T.op("dve", lambda e: e.tensor_tensor(out=w0, in0=hi, in1=lo, op=ALU.subtract),
                         reads=[bbis], writes=[bbis])
                    T.op("dve", lambda e: e.tensor_scalar(out=H, in0=pw2[:], scalar1=w0, scalar2=None, op0=ALU.mult),
                         reads=[bbis, bpw2], writes=[bbis])
                    T.op("dve", lambda e: e.tensor_scalar(out=H2, in0=H, scalar1=2.0, scalar2=None, op0=ALU.mult),
                         reads=[bbis], writes=[bbis])
                    T.op("dve", lambda e: e.tensor_tensor(out=mid, in0=lo, in1=bis[:, 8:9], op=ALU.add),
                         reads=[bbis], writes=[bbis])
                    for k in range(KBIS):
                        T.op("dve", lambda e: e.tensor_scalar(out=junk[:, 0:n], in0=score_[:, 0:n], scalar1=mid, scalar2=None,
                                                              op0=ALU.is_ge, op1=ALU.add, accum_out=cnt),
                             reads=[bsc_, bbis], writes=[b_junk, bbis])
                        if k < KBIS - 1:
                            T.op("dve", lambda e: e.tensor_scalar(out=sv, in0=cnt, scalar1=float(TOPK),
                                                                  scalar2=bis[:, 8 + KBIS + k + 1:8 + KBIS + k + 2],
                                                                  op0=ALU.is_ge, op1=ALU.mult),
                                 reads=[bbis], writes=[bbis])
                            T.op("dve", lambda e: e.scalar_tensor_tensor(out=mid, in0=sv, scalar=bis[:, 8 + k + 1:8 + k + 2],
                                                                         in1=mid, op0=ALU.subtract, op1=ALU.add),
                                 reads=[bbis], writes=[bbis])
                        else:
                            T.op("dve", lambda e: e.tensor_scalar(out=sv, in0=cnt, scalar1=float(TOPK),
                                                                  scalar2=bis[:, 8 + k:8 + k + 1],
                                                                  op0=ALU.is_ge, op1=ALU.mult),
                                 reads=[bbis], writes=[bbis])
                            T.op("dve", lambda e: e.scalar_tensor_tensor(out=thr, in0=sv, scalar=bis[:, 8 + k:8 + k + 1],
                                                                         in1=mid, op0=ALU.subtract, op1=ALU.add),
                                 reads=[bbis], writes=[bbis])
                    T.op("dve", lambda e: e.tensor_scalar(out=nm[:, 0:n], in0=score_[:, 0:n], scalar1=thr, scalar2=-1.0e5,
                                                          op0=ALU.is_lt, op1=ALU.mult), reads=[bsc_, bbis], writes=[bn_])
                else:
                    T.op("dve", lambda e: e.tensor_scalar(out=nm[:, 0:n], in0=score_[:, 0:n], scalar1=-1.0e29, scalar2=-1.0e5,
                                                          op0=ALU.is_lt, op1=ALU.mult), reads=[bsc_], writes=[bn_])

            def att_logits(s, qb, kb):
                q0 = qb * 128
                nm = nmask[qb % 2]; bn_ = bnm[qb % 2]
                lp = cnt_i["lp"] % 2
                cnt_i["lp"] += 1
                P = PP[lp]; bP = bPP[lp]
                off = qb - kb
                for (bank, h0) in ((0, 0), (1, 4)):
                    T.op("pe", lambda e: e.matmul(
                        pb[bank][:].rearrange("p (h t) -> p h t", h=4),
                        lhsT=kvT[:, kb * 128:(kb + 1) * 128], rhs=dqT[:, h0:h0 + 4, q0:q0 + 128],
                        start=True, stop=False, skip_group_check=True), reads=[bkvT, bdq], writes=[PB[bank]])
                    T.op("pe", lambda e: e.matmul(pb[bank][:], lhsT=nm[:, kb * 128:(kb + 1) * 128], rhs=id4[:],
                                                  start=False, stop=(off >= 2), skip_group_check=True),
                         reads=[bn_, bid4], writes=[PB[bank]])
                    if off < 2:
                        T.op("pe", lambda e: e.matmul(pb[bank][:], lhsT=ident_b[:], rhs=BN[:, off, h0 * 128:(h0 + 4) * 128],
                                                      start=False, stop=True, skip_group_check=True),
                             reads=[b_identb, bBN], writes=[PB[bank]])
                T.op("act", lambda e: e.activation(out=P[:], in_=pb2[0][:], func=AF.Exp, scale=lsc),
                     reads=[PB[0], PB[1]], writes=[bP])
                return lp

            def att_pv(s, qb, kb, lp):
                P = PP[lp]; bP = bPP[lp]
                for (bank, h0) in ((OTB[0], 0), (OTB[1], 4)):
                    T.op("pe", lambda e: e.matmul(pb[bank][:], lhsT=kvt[:, kb, :], rhs=P[:, h0 * 128:(h0 + 4) * 128],
                                                  start=(kb == 0), stop=(kb == qb)),
                         reads=[bkvt, bP], writes=[PB[bank]])
                for h in range(8):
                    T.op("pe", lambda e: e.matmul(pb[DB][:, h:h + 1], lhsT=P[:, h * 128:(h + 1) * 128], rhs=ones_b[:, 0:1],
                                                  start=(kb == 0 and h == 0), stop=(kb == qb), skip_group_check=True),
                         reads=[bP, b_onesb], writes=[PB[DB]])

            def epilogue(s, qb):
                q0 = qb * 128
                T.op("act", lambda e: e.activation(out=oTs[:, 0:512], in_=pb[OTB[0]][:], func=AF.Copy), reads=[PB[OTB[0]]], writes=[boTs])
                T.op("act", lambda e: e.activation(out=oTs[:, 512:1024], in_=pb[OTB[1]][:], func=AF.Copy), reads=[PB[OTB[1]]], writes=[boTs])
                T.op("dve", lambda e: e.reciprocal(out=rden[:], in_=pb[DB][:, 0:8]), reads=[PB[DB]], writes=[brden])
                eb = IBS[cnt_i["ib"] % 2]
                cnt_i["ib"] += 1
                for h in range(8):
                    T.op("pe", lambda e: e.matmul(pb[eb][:, h * 64:(h + 1) * 64], lhsT=oTs[:, h * 128:(h + 1) * 128],
                                                  rhs=wuv[:, h, :], start=(h == 0), stop=True, skip_group_check=True),
                         reads=[boTs, bwuv], writes=[PB[eb]])
                for h in range(8):
                    T.op("dve", lambda e: e.tensor_scalar(out=obf[:, h * 64:(h + 1) * 64], in0=pb[eb][:, h * 64:(h + 1) * 64],
                                                          scalar1=rden[:, h:h + 1], scalar2=None, op0=ALU.mult),
                         reads=[PB[eb], brden], writes=[bobf])
                rs, brs = rstd_from(obf[:], [bobf], 512)
                osl = cnt_i["oi"] % 2
                cnt_i["oi"] += 1
                T.op("dve", lambda e: e.scalar_tensor_tensor(out=obn[osl][:], in0=obf[:], scalar=rs, in1=gob[:],
                                                             op0=ALU.mult, op1=ALU.mult),
                     reads=[bobf, brs, bgob], writes=[bobn[osl]])
                T.dma("pool", ocat[s, q0:q0 + 128, 512:1024], obn[osl][:], bobn[osl], reads=[bobn[osl]])

            for s in range(LIM_SEQ):
                T.dma("sp", dqT[:], featT[s, 8:16].rearrange("c p t -> p c t"), bdq, writes=[bdq])
                T.dma("sp", iqT[:], featT[s, 16:20].rearrange("c p t -> p c t"), biq, writes=[biq])
                T.dma("sp", ikT[:], featT[s, 20], bik, writes=[bik])
                T.dma("sp", kvT[:], kvT_scr[s], bkvT, writes=[bkvT])
                T.dma("sp", kvt[:], kv_tok[s].rearrange("(n p) r -> p n r", p=128), bkvt, writes=[bkvt])
                T.dma("sp", wi[:], widx[s].rearrange("(n p) j -> p n j", p=128), bwi, writes=[bwi])
                T.op("act", lambda e: e.activation(out=absw[:], in_=wi[:], func=AF.Abs, scale=isc),
                     reads=[bwi], writes=[babsw])
                T.op("dve", lambda e: e.tensor_scalar(out=sgn[:], in0=wi[:], scalar1=0.0, scalar2=2.0,
                                                      op0=ALU.is_ge, op1=ALU.mult), reads=[bwi], writes=[bsgn])
                T.op("dve", lambda e: e.tensor_scalar(out=sgn[:], in0=sgn[:], scalar1=-1.0, scalar2=None,
                                                      op0=ALU.add), reads=[bsgn], writes=[bsgn])
                for step in range(LIM_QB + 2):
                    qi = step
                    qs = step - 1
                    qa = step - 2
                    iu = []
                    if qi < LIM_QB:
                        n = (qi + 1) * 128
                        iu = [(c, j) for c in range((n + 511) // 512) for j in range(8)]
                        make_diag(qi)
                    if 0 <= qs < LIM_QB:
                        select(qs)
                    au = list(range(qa + 1)) if qa >= 0 else []
                    na, ni = len(au), len(iu)
                    ai = 0
                    ii_ = 0
                    pend = None
                    total = max(na, 1)
                    while ai < na or ii_ < ni:
                        tgt = ni if ai >= na else (ni * (ai + 1)) // total
                        while ii_ < tgt:
                            idx_unit(s, qi, *iu[ii_])
                            ii_ += 1
                        if ai < na:
                            lp = att_logits(s, qa, au[ai])
                            if pend is not None:
                                att_pv(s, qa, *pend)
                            pend = (au[ai], lp)
                            ai += 1
                    if pend is not None:
                        att_pv(s, qa, *pend)
                    flush_acc()
                    if qa >= 0:
                        epilogue(s, qa)
                        bg_step(1)
            T.barrier()

    def phase_D(l, prefetch=()):
        prefetch = list(prefetch)
        bg_flush(l)
        with ExitStack() as es:
            Wo = salloc(es, "Wo", [128, 8, D], BF16); bWo = Buf("Wo")
            wl = wb_out[l].rearrange("(c p) n -> p c n", p=128)
            for c in range(8):
                T.dma("sp", Wo[:, c, :], wl[:, c, :], bWo, reads=[B_wb_out[l]], writes=[bWo])
            gb, bgb = ga_bcast(es, l, 16, "ga1")
            oc = [salloc(es, f"oc{i}", [128, D], BF16) for i in range(2)]
            boc = [Buf(f"oc{i}") for i in range(2)]
            oT = [salloc(es, f"oT{i}", [128, 8, 128], BF16) for i in range(2)]
            boT = [Buf(f"oT{i}") for i in range(2)]
            xts = [salloc(es, f"xd{i}", [128, D], F32) for i in range(2)]
            bxts = [Buf(f"xd{i}") for i in range(2)]
            tmp = [salloc(es, f"tm{i}", [128, D], F32) for i in range(2)]
            btmp = [Buf(f"tm{i}") for i in range(2)]
            src = x_in if l == 0 else xres
            ti = 0
            for s in range(NSEQ):
                for tt in range(NT):
                    t0 = tt * 128
                    k2 = ti % 2
                    ti += 1
                    T.dma("sp", oc[k2][:], ocat[s, t0:t0 + 128, :], boc[k2], writes=[boc[k2]])
                    T.dma("sp", xts[k2][:], src[s, t0:t0 + 128, :], bxts[k2], writes=[bxts[k2]])
                    if prefetch:
                        prefetch.pop(0)()
                    pv = pbf(k2).rearrange("p (c t) -> p c t", c=8)
                    for c in range(8):
                        T.op("pe", lambda e: e.transpose(pv[:, c, :], oc[k2][:, c * 128:(c + 1) * 128], ident_b[:]),
                             reads=[boc[k2], b_identb], writes=[PB[k2]])
                    T.op("act", lambda e: e.activation(out=oT[k2][:].rearrange("p c t -> p (c t)"), in_=pbf(k2)[:, 0:1024], func=AF.Copy),
                         reads=[PB[k2]], writes=[boT[k2]])
                    for half in range(2):
                        bank = 2 + k2 * 2 + half
                        for c in range(8):
                            T.op("pe", lambda e: e.matmul(pb[bank][:], lhsT=oT[k2][:, c, :], rhs=Wo[:, c, half * 512:(half + 1) * 512],
                                                          start=(c == 0), stop=(c == 7)),
                                 reads=[boT[k2], bWo], writes=[PB[bank]])
                        T.op("dve", lambda e: e.tensor_tensor(out=tmp[k2][:, half * 512:(half + 1) * 512], in0=pb[bank][:],
                                                              in1=gb[:, s, half * 512:(half + 1) * 512], op=ALU.mult),
                             reads=[PB[bank], bgb], writes=[btmp[k2]])
                    T.op("dve", lambda e: e.tensor_tensor(out=tmp[k2][:], in0=tmp[k2][:], in1=xts[k2][:], op=ALU.add),
                         reads=[btmp[k2], bxts[k2]], writes=[btmp[k2]])
                    T.dma("pool", xres[s, t0:t0 + 128, :], tmp[k2][:], btmp[k2], reads=[btmp[k2]])
            while prefetch:
                prefetch.pop(0)()
            T.barrier()

    def phase_DE(l, last):
        with ExitStack() as esw:
            Wu = salloc(esw, "Wu", [128, 8, DFF], BF16); bWu = Buf("Wu")
            Wd = salloc(esw, "Wd", [128, 32, D], BF16); bWd = Buf("Wd")
            wul = wb_up[l].rearrange("(c p) n -> p c n", p=128)
            wdl = wb_dn[l].rearrange("(c p) n -> p c n", p=128)
            pf = []
            for c in range(8):
                for hh in range(2):
                    pf.append(lambda c=c, hh=hh: T.dma(
                        "sp", Wu[:, c, hh * 2048:(hh + 1) * 2048], wul[:, c, hh * 2048:(hh + 1) * 2048], bWu,
                        reads=[B_wb_up[l]], writes=[bWu]))
            for c4 in range(8):
                pf.append(lambda c4=c4: T.dma(
                    "sp", Wd[:, c4 * 4:(c4 + 1) * 4, :], wdl[:, c4 * 4:(c4 + 1) * 4, :], bWd,
                    reads=[B_wb_dn[l]], writes=[bWd]))
            phase_D(l, prefetch=pf)
            phase_E(l, last, Wu, bWu, Wd, bWd)

    def phase_E(l, last, Wu, bWu, Wd, bWd):
        with ExitStack() as es:
            gb, bgb = ga_bcast(es, l, 40, "ga2")
            if last:
                gf = salloc(es, "gf", [128, D], F32); bgf = Buf("gf")
                T.dma("sp", gf[:], gfin_bc, bgf, writes=[bgf])
            TG = 256
            xts = [salloc(es, f"xe{i}", [128, D], F32) for i in range(4)]
            bxts = [Buf(f"xe{i}") for i in range(4)]
            xns = [salloc(es, f"xne{i}", [128, D], BF16) for i in range(2)]
            bxns = [Buf(f"xne{i}") for i in range(2)]
            hTs = [salloc(es, f"hTe{i}", [128, 8, TG], BF16) for i in range(2)]
            bhTs = [Buf(f"hTe{i}") for i in range(2)]
            aT = salloc(es, "aT", [128, 32, TG], BF16); baT = [Buf(f"aT{i}") for i in range(32)]
            rl = [salloc(es, f"rl{i}", [128, TG], BF16) for i in range(2)]
            brl = [Buf(f"rl{i}") for i in range(2)]
            ti = 0
            gi = 0
            ui = 0
            for s in range(NSEQ):
                for g in range(S // TG):
                    hT = hTs[gi % 2]; bhT = bhTs[gi % 2]
                    gi += 1
                    tiles = []
                    for j in range(TG // 128):
                        tt = g * (TG // 128) + j
                        t0 = tt * 128
                        xt = xts[ti % 4]; bxt = bxts[ti % 4]
                        xn = xns[ti % 2]; bxn = bxns[ti % 2]
                        tb = ti % 2
                        ti += 1
                        tiles.append((t0, xt, bxt))
                        T.dma("sp", xt[:], xres[s, t0:t0 + 128, :], bxt, writes=[bxt])
                        norm_to_T(xt[:], bxt,
                                  lambda c: gm2T[:, l, c, s:s + 1],
                                  lambda c: modT[:, l, 24 + c, s:s + 1],
                                  [b_gm2, b_modT_], xn, bxn, tb,
                                  lambda c: hT[:, c, j * 128:(j + 1) * 128], bhT)
                    for f in range(32):
                        bank = 2 + (ui % 2)
                        sl = ui % 2
                        ui += 1
                        for c in range(8):
                            T.op("pe", lambda e: e.matmul(pb[bank][:, 0:TG], lhsT=Wu[:, c, f * 128:(f + 1) * 128], rhs=hT[:, c, :],
                                                          start=(c == 0), stop=(c == 7)),
                                 reads=[bWu, bhT], writes=[PB[bank]])
                        T.op("act", lambda e: e.activation(out=rl[sl][:], in_=pb[bank][:, 0:TG], func=AF.Relu),
                             reads=[PB[bank]], writes=[brl[sl]])
                        T.op("dve", lambda e: e.tensor_tensor(out=aT[:, f, :], in0=rl[sl][:], in1=rl[sl][:], op=ALU.mult),
                             reads=[brl[sl]], writes=[baT[f]])
                    for j, (t0, xt, bxt) in enumerate(tiles):
                        for half in range(2):
                            bank = 4 + (j % 2) * 2 + half
                            for f in range(32):
                                T.op("pe", lambda e: e.matmul(pb[bank][:], lhsT=aT[:, f, j * 128:(j + 1) * 128],
                                                              rhs=Wd[:, f, half * 512:(half + 1) * 512], start=(f == 0), stop=(f == 31)),
                                     reads=[baT[f], bWd], writes=[PB[bank]])
                            T.op("dve", lambda e: e.tensor_tensor(out=tmpE[j % 2][:, half * 512:(half + 1) * 512], in0=pb[bank][:],
                                                                  in1=gb[:, s, half * 512:(half + 1) * 512], op=ALU.mult),
                                 reads=[PB[bank], bgb], writes=[btmpE[j % 2]])
                        T.op("dve", lambda e: e.tensor_tensor(out=xt[:], in0=tmpE[j % 2][:], in1=xt[:], op=ALU.add),
                             reads=[btmpE[j % 2], bxt], writes=[bxt])
                        if not last:
                            T.dma("pool", xres[s, t0:t0 + 128, :], xt[:], bxt, reads=[bxt])
                        else:
                            rs, brs = rstd_from(xt[:], [bxt], D)
                            T.op("dve", lambda e: e.scalar_tensor_tensor(out=tmpE[j % 2][:], in0=xt[:], scalar=rs, in1=gf[:],
                                                                         op0=ALU.mult, op1=ALU.mult),
                                 reads=[bxt, brs, bgf], writes=[btmpE[j % 2]])
                            T.dma("pool", out_d[s, t0:t0 + 128, :], tmpE[j % 2][:], btmpE[j % 2], reads=[btmpE[j % 2]])
            T.barrier()

    tmpE = []
    btmpE = [Buf("tmpE0"), Buf("tmpE1")]

    tmpE.append(salloc(ges, "tmpE0", [128, D], F32))
    tmpE.append(salloc(ges, "tmpE1", [128, D], F32))

    convert_weights()
    esA0 = ExitStack()
    Wf0 = salloc(esA0, "Wf0", [128, 8, 2688], BF16); bWf0 = Buf("Wf0")
    Wt0 = salloc(esA0, "Wt0", [128, 8, 648], BF16); bWt0 = Buf("Wt0")
    preA = (Wf0, bWf0, Wt0, bWt0)
    prologue(preA)
    for l in range(nlayers):
        if "A" in phases:
            phase_A(l, pre=preA if l == 0 else None)
        if l == 0:
            esA0.close()
        if "B" in phases:
            phase_B(l)
        if "C" in phases:
            phase_C(l)
        if "D" in phases and "E" in phases:
            phase_DE(l, last=(l == nlayers - 1))
        elif "D" in phases:
            phase_D(l)
    T.barrier()
    ges.close()
    return nc, T


def _t5_bucket(rel):
    nb = 16
    max_exact = 8
    base = np.where(rel > 0, nb, 0)
    n = np.abs(rel)
    nf = np.maximum(n, max_exact).astype(np.float32)
    large = max_exact + (np.log(nf / np.float32(max_exact)) / np.float32(math.log(128 / max_exact))
                         * np.float32(nb - max_exact)).astype(np.int32)
    large = np.minimum(large, nb - 1)
    return base + np.where(n < max_exact, n, large)


def _consts():
    p = np.arange(128)
    c = {}
    c["c_ident"] = np.eye(128, dtype=np.float32)
    c["c_tri8"] = np.where(p[:, None] >= p[None, :], -8.0, 0.0).astype(np.float32)
    c["c_neg8"] = np.full((128, 128), -8.0, np.float32)
    cm = (p[:, None] < p[None, :]).astype(np.float32)
    c["c_cmask"] = np.ascontiguousarray(np.broadcast_to(cm[:, None, :], (128, 8, 128)))
    adm = ((p[:, None] // 64) <= (p[None, :] // 64)).astype(np.float32)
    c["c_adm"] = np.ascontiguousarray(np.broadcast_to(adm[:, None, :], (128, 8, 128)))
    c["c_admneg"] = np.ascontiguousarray(np.broadcast_to(np.where(adm > 0, 0.0, -1.0e5).astype(np.float32)[:, None, :], (128, 8, 128)))
    c["c_pow2"] = np.ascontiguousarray(np.broadcast_to(
        (0.5 ** np.arange(1, KBIS + 1)).astype(np.float32)[None, :], (128, KBIS)))
    c["c_ones"] = np.ones((128, 128), np.float32)
    return c


def _prep_inputs(inp, core):
    f = np.float32
    b0 = core * NSEQ
    bs = slice(b0, b0 + NSEQ)
    m = {}
    m["x"] = np.ascontiguousarray(inp["x"][bs], dtype=f)
    c = np.asarray(inp["c"], dtype=f)[bs]
    m["cT"] = np.ascontiguousarray(c.reshape(NSEQ, 8, 128).transpose(2, 1, 0))
    m["w_mod"] = np.ascontiguousarray(inp["w_mod"], dtype=f)
    bm = np.asarray(inp["b_mod"], dtype=f).reshape(2, 48, 128).transpose(2, 0, 1)
    m["b_modT"] = np.ascontiguousarray(np.broadcast_to(bm[..., None], (128, 2, 48, NSEQ)))
    ga = np.asarray(inp["g_attn"], dtype=f).reshape(2, 8, 128).transpose(2, 0, 1)
    m["g_attnT"] = np.ascontiguousarray(np.broadcast_to(ga[..., None], (128, 2, 8, NSEQ)))
    gm = np.asarray(inp["g_mlp"], dtype=f).reshape(2, 8, 128).transpose(2, 0, 1)
    m["g_mlpT"] = np.ascontiguousarray(np.broadcast_to(gm[..., None], (128, 2, 8, NSEQ)))
    m["w_in"] = np.ascontiguousarray(inp["w_in"], dtype=f)
    m["kvg_bc"] = np.ascontiguousarray(np.broadcast_to(np.asarray(inp["kv_norm_g"], dtype=f)[None], (128, 2, 128)))
    m["w_uv"] = np.ascontiguousarray(inp["w_uv"], dtype=f)
    m["goa_bc"] = np.ascontiguousarray(np.broadcast_to(np.asarray(inp["g_out_a"], dtype=f)[None], (128, 2, 512)))
    m["gob_bc"] = np.ascontiguousarray(np.broadcast_to(np.asarray(inp["g_out_b"], dtype=f)[None], (128, 2, 512)))
    m["w_out"] = np.ascontiguousarray(inp["w_out"], dtype=f)
    m["w_up"] = np.ascontiguousarray(inp["w_up"], dtype=f)
    m["w_down"] = np.ascontiguousarray(inp["w_down"], dtype=f)
    rb = np.asarray(inp["rel_bias"], dtype=f)
    p = np.arange(128)
    bn = np.empty((128, 2, 8, 128), f)
    for off in range(2):
        rel = (p[:, None] - off * 128) - p[None, :]
        bk = _t5_bucket(rel.astype(np.int32))
        bn[:, off] = rb[bk].transpose(0, 2, 1)
    m["biasn"] = bn
    far = rb[_t5_bucket(np.array([-1000], np.int32))[0]]
    m["bfar_bc"] = np.ascontiguousarray(np.broadcast_to(far[None, :, None], (128, 8, 128)))
    m["gfin_bc"] = np.ascontiguousarray(np.broadcast_to(np.asarray(inp["g_final"], dtype=f)[None], (128, D)))
    m.update(_consts())
    return m


_CACHE = {}


def kernel(**inputs):
    if "nc" not in _CACHE:
        _CACHE["nc"] = build_program()[0]
    nc = _CACHE["nc"]
    in_maps = [_prep_inputs(inputs, core) for core in range(8)]
    res = run_bass_kernel_spmd(nc, in_maps, core_ids=list(range(8)))
    out = np.concatenate([np.asarray(r["out"]) for r in res.results], axis=0)
    return out.astype(np.float32, copy=False)
```

```python
import math
from contextlib import ExitStack

import numpy as np
import concourse.bass as bass
import concourse.mybir as mybir
from concourse.bass_utils import run_bass_kernel_spmd

F32 = mybir.dt.float32
BF16 = mybir.dt.bfloat16
AF = mybir.ActivationFunctionType
ALU = mybir.AluOpType
AX = mybir.AxisListType

S = 2048
D = 1024
NSEQ = 2
NT = S // 128
DFF = 4096
DIN = 3272
EPS = 1e-6
KBIS = 16
TOPK = 256
EPOCH = 30000
LIM_SEQ = NSEQ
LIM_QB = NT
NO_POOL = False
ACC_DEPTH = 2
FLUSH_BG_BEFORE_C = False


class Buf:
    __slots__ = ("name", "last_w", "readers", "dsem", "bg")

    def __init__(self, name, bg=False):
        self.name = name
        self.last_w = None
        self.readers = []
        self.dsem = {}
        self.bg = bg


class Tracker:
    def __init__(self, nc):
        self.nc = nc
        self.eng = {"pe": nc.tensor, "act": nc.scalar, "dve": nc.vector,
                    "pool": nc.gpsimd, "sp": nc.sync}
        self.cnt = {e: 0 for e in self.eng}
        self.sems = {e: [] for e in self.eng}
        self.seen = {e: {} for e in self.eng}
        self.nsem = 0
        self.dma_bufs = []
        self.free_dsems = {"hw": [], "sw": []}
        self.nwaits = 0
        self.ninstr = 0

    def _newsem(self, name):
        self.nsem += 1
        return self.nc.alloc_semaphore(name=name)

    def _wait(self, e, tok):
        sem, val, src = tok
        key = id(sem)
        if self.seen[e].get(key, 0) >= val:
            return
        self.seen[e][key] = val
        self.eng[e].wait_ge(sem, val)
        self.nwaits += 1

    def _deps(self, e, reads, writes):
        for b in reads:
            if b.last_w is not None and not (b.last_w[2] == e and e == "pe"):
                self._wait(e, b.last_w)
        for b in writes:
            if b.last_w is not None and not (b.last_w[2] == e and e == "pe"):
                self._wait(e, b.last_w)
            for t in b.readers:
                if t[2] == e:
                    continue
                self._wait(e, t)

    def _commit(self, tok, reads, writes):
        for b in reads:
            b.readers.append(tok)
            if len(b.readers) > 12:
                d = {}
                for t in b.readers:
                    k = id(t[0])
                    if k not in d or d[k][1] < t[1]:
                        d[k] = t
                b.readers = list(d.values())
        for b in writes:
            b.last_w = tok
            b.readers = []

    def op(self, e, fn, reads=(), writes=()):
        self._deps(e, reads, writes)
        n = self.cnt[e]
        ep, v = divmod(n, EPOCH)
        while len(self.sems[e]) <= ep:
            self.sems[e].append(self._newsem(f"c_{e}_{len(self.sems[e])}"))
        sem = self.sems[e][ep]
        ins = fn(self.eng[e])
        ins.then_inc(sem, 1)
        self.cnt[e] = n + 1
        self.ninstr += 1
        tok = (sem, v + 1, e)
        self._commit(tok, reads, writes)
        return tok

    def dma(self, q, out, in_, sb, reads=(), writes=(), **kw):
        self._deps(q, reads, writes)
        kind = "sw" if q == "pool" else "hw"
        if kind not in sb.dsem:
            if self.free_dsems[kind]:
                sb.dsem[kind] = list(self.free_dsems[kind].pop())
            else:
                sb.dsem[kind] = [self._newsem(f"d{kind}_{sb.name}_{self.nsem}"), 0]
            self.dma_bufs.append((sb, kind))
        ent = sb.dsem[kind]
        ent[1] += 16
        ins = self.eng[q].dma_start(out=out, in_=in_, **kw)
        ins.then_inc(ent[0], 16)
        self.ninstr += 1
        tok = (ent[0], ent[1], "dma")
        self._commit(tok, reads, writes)
        return tok

    def barrier(self):
        toks = []
        for f in self.eng:
            n = self.cnt[f]
            if n == 0:
                continue
            ep, v = divmod(n - 1, EPOCH)
            toks.append((self.sems[f][ep], v + 1, f))
        for b, kind in self.dma_bufs:
            if not b.bg:
                toks.append((b.dsem[kind][0], b.dsem[kind][1], "dma"))
        for e in self.eng:
            for t in toks:
                if t[2] == e:
                    continue
                self._wait(e, t)
        keep = []
        for b, kind in self.dma_bufs:
            if b.bg:
                keep.append((b, kind))
            else:
                self.free_dsems[kind].append(tuple(b.dsem.pop(kind)))
        self.dma_bufs = keep


def build_program(nlayers=2, debug=False, phases="ABCDE"):
    nc = bass.Bass("TRN2", target_bir_lowering=False)
    T = Tracker(nc)
    uid = [0]

    def din(name, shape, dt=F32):
        return nc.dram_tensor(name, list(shape), dt, kind="ExternalInput").ap()

    def dscr(name, shape, dt):
        kind = "ExternalOutput" if debug else "Internal"
        return nc.dram_tensor(name, list(shape), dt, kind=kind).ap()

    x_in = din("x", [NSEQ, S, D])
    cT = din("cT", [128, 8, NSEQ])
    w_mod = din("w_mod", [2, D, 6 * D])
    b_modT = din("b_modT", [128, 2, 48, NSEQ])
    g_attnT = din("g_attnT", [128, 2, 8, NSEQ])
    g_mlpT = din("g_mlpT", [128, 2, 8, NSEQ])
    w_in = din("w_in", [2, D, DIN])
    kvg_bc = din("kvg_bc", [128, 2, 128])
    w_uv = din("w_uv", [2, 8, 128, 64])
    goa_bc = din("goa_bc", [128, 2, 512])
    gob_bc = din("gob_bc", [128, 2, 512])
    w_out = din("w_out", [2, D, D])
    w_up = din("w_up", [2, D, DFF])
    w_down = din("w_down", [2, DFF, D])
    biasn = din("biasn", [128, 2, 8, 128])
    bfar_bc = din("bfar_bc", [128, 8, 128])
    gfin_bc = din("gfin_bc", [128, D])
    c_ident = din("c_ident", [128, 128])
    c_tri8 = din("c_tri8", [128, 128])
    c_neg8 = din("c_neg8", [128, 128])
    c_cmask = din("c_cmask", [128, 8, 128])
    c_adm = din("c_adm", [128, 8, 128])
    c_admneg = din("c_admneg", [128, 8, 128])
    c_pow2 = din("c_pow2", [128, KBIS])
    c_ones = din("c_ones", [128, 128])
    out_d = nc.dram_tensor("out", [NSEQ, S, D], F32, kind="ExternalOutput").ap()

    xres = dscr("xres", [NSEQ, S, D], F32)
    featT = dscr("featT", [NSEQ, 21, 128, S], BF16)
    v_scr = dscr("v_scr", [NSEQ, S, 512], BF16)
    kv_tok = dscr("kv_tok", [NSEQ, S, 128], BF16)
    kvT_scr = dscr("kvT_scr", [NSEQ, 128, S], BF16)
    widx = dscr("widx", [NSEQ, S, 8], F32)
    ocat = dscr("ocat", [NSEQ, S, D], BF16)

    wb_in = [nc.dram_tensor(f"wb_in{l}", [D, DIN], BF16, kind="Internal").ap() for l in range(2)]
    wb_out = [nc.dram_tensor(f"wb_out{l}", [D, D], BF16, kind="Internal").ap() for l in range(2)]
    wb_up = [nc.dram_tensor(f"wb_up{l}", [D, DFF], BF16, kind="Internal").ap() for l in range(2)]
    wb_dn = [nc.dram_tensor(f"wb_dn{l}", [DFF, D], BF16, kind="Internal").ap() for l in range(2)]
    B_wb_in = [Buf(f"wb_in{l}", bg=True) for l in range(2)]
    B_wb_out = [Buf(f"wb_out{l}", bg=True) for l in range(2)]
    B_wb_up = [Buf(f"wb_up{l}", bg=True) for l in range(2)]
    B_wb_dn = [Buf(f"wb_dn{l}", bg=True) for l in range(2)]

    bgq = []

    def convert_weights():
        for l in range(nlayers):
            for (dst, src, bb, rows, step) in ((wb_in[l], w_in[l], B_wb_in[l], D, 256), (wb_out[l], w_out[l], B_wb_out[l], D, 512),
                                               (wb_up[l], w_up[l], B_wb_up[l], D, 256), (wb_dn[l], w_down[l], B_wb_dn[l], DFF, 1024)):
                for r0 in range(0, rows, step):
                    f = (lambda dst=dst, src=src, bb=bb, r0=r0, step=step:
                         T.dma("pool", dst[r0:r0 + step, :], src[r0:r0 + step, :], bb, writes=[bb]))
                    if l == 0 and dst is wb_in[0]:
                        f()
                    else:
                        bgq.append((l, f))

    def bg_step(n=1):
        for _ in range(n):
            if bgq:
                bgq.pop(0)[1]()

    def bg_flush(layer):
        while bgq and bgq[0][0] <= layer:
            bgq.pop(0)[1]()

    def salloc(es, name, shape, dt):
        uid[0] += 1
        return es.enter_context(nc.sbuf_tensor(f"{name}_{uid[0]}", list(shape), dt))

    ges = ExitStack()
    pb2 = [ges.enter_context(nc.psum_tensor(f"pbp{i}", [128, 1024], F32)) for i in range(4)]
    pb = [pb2[i // 2][:, (i % 2) * 512:(i % 2 + 1) * 512] for i in range(8)]
    PB = [Buf(f"pb{i}") for i in range(8)]

    def pbf(i):
        return pb[i].bitcast(BF16)

    ident_f = salloc(ges, "identf", [128, 128], F32); b_identf = Buf("identf")
    ident_b = salloc(ges, "identb", [128, 128], BF16); b_identb = Buf("identb")
    ones_f = salloc(ges, "onesf", [128, 128], F32); b_onesf = Buf("onesf")
    ones_b = salloc(ges, "onesb", [128, 128], BF16); b_onesb = Buf("onesb")
    modT = salloc(ges, "modT", [128, 2, 48, NSEQ], F32); b_modT_ = Buf("modT")
    gm1T = salloc(ges, "gm1T", [128, 2, 8, NSEQ], F32); b_gm1 = Buf("gm1T")
    gm2T = salloc(ges, "gm2T", [128, 2, 8, NSEQ], F32); b_gm2 = Buf("gm2T")
    stat = salloc(ges, "stat", [128, 8, 4], F32)
    STB = [Buf(f"stat{i}") for i in range(8)]
    junk = salloc(ges, "junk", [128, 2048], BF16); b_junk = Buf("junk")
    stat_i = [0]

    T.dma("sp", ident_f[:], c_ident, b_identf, writes=[b_identf])
    T.dma("pool", ident_b[:], c_ident, b_identb, writes=[b_identb])
    T.dma("sp", ones_f[:], c_ones, b_onesf, writes=[b_onesf])
    T.dma("pool", ones_b[:], c_ones, b_onesb, writes=[b_onesb])

    def prologue(preA=None):
        with ExitStack() as es:
            sT = salloc(es, "sT", [128, 8, NSEQ], F32); b_sT = Buf("sT")
            cTs = salloc(es, "cTs", [128, 8, NSEQ], F32); b_cTs = Buf("cTs")
            bm = salloc(es, "bm", [128, 2, 48, NSEQ], F32); b_bm = Buf("bm")
            ga = salloc(es, "ga", [128, 2, 8, NSEQ], F32); b_ga = Buf("ga")
            gmm = salloc(es, "gmm", [128, 2, 8, NSEQ], F32); b_gmm = Buf("gmm")
            NW = 4
            wms = [salloc(es, f"wm{i}", [128, 8, 512], F32) for i in range(NW)]
            b_wms = [Buf(f"wm{i}") for i in range(NW)]
            modrow = salloc(es, "modrow", [NSEQ, 6 * D], F32); b_mrow = Buf("modrow")
            T.dma("sp", cTs[:], cT, b_cTs, writes=[b_cTs])
            T.dma("sp", bm[:], b_modT, b_bm, writes=[b_bm])
            T.dma("sp", ga[:], g_attnT, b_ga, writes=[b_ga])
            T.dma("sp", gmm[:], g_mlpT, b_gmm, writes=[b_gmm])
            T.op("act", lambda e: e.activation(out=sT[:], in_=cTs[:], func=AF.Silu),
                 reads=[b_cTs], writes=[b_sT])
            it = 0
            for l in range(nlayers):
                wl = w_mod[l].rearrange("(c p) n -> p c n", p=128)
                for ns in range(12):
                    sl = it % NW
                    bank = it % 2
                    it += 1
                    T.dma("sp", wms[sl][:], wl[:, :, ns * 512:(ns + 1) * 512], b_wms[sl],
                          writes=[b_wms[sl]])
                    for c in range(8):
                        T.op("pe", lambda e: e.matmul(
                            pb[bank][0:NSEQ, 0:512], lhsT=sT[:, c, :], rhs=wms[sl][:, c, :],
                            start=(c == 0), stop=(c == 7)),
                            reads=[b_wms[sl], b_sT], writes=[PB[bank]])
                    T.op("act", lambda e: e.activation(out=modrow[:, ns * 512:(ns + 1) * 512],
                                                       in_=pb[bank][0:NSEQ, 0:512], func=AF.Copy),
                         reads=[PB[bank]], writes=[b_mrow])
                for j in range(48):
                    T.op("pe", lambda e: e.transpose(pb[2][:, j * NSEQ:(j + 1) * NSEQ],
                                                     modrow[0:NSEQ, j * 128:(j + 1) * 128],
                                                     ident_f[0:NSEQ, 0:NSEQ]),
                         reads=[b_mrow, b_identf], writes=[PB[2]])
                T.op("dve", lambda e: e.tensor_tensor(
                    out=modT[:, l, :, :],
                    in0=pb[2][:, 0:48 * NSEQ].rearrange("p (j b) -> p j b", b=NSEQ),
                    in1=bm[:, l, :, :], op=ALU.add),
                    reads=[PB[2], b_bm], writes=[b_modT_])
                T.op("dve", lambda e: e.scalar_tensor_tensor(
                    out=gm1T[:, l], in0=modT[:, l, 8:16, :], scalar=1.0, in1=ga[:, l],
                    op0=ALU.add, op1=ALU.mult), reads=[b_modT_, b_ga], writes=[b_gm1])
                T.op("dve", lambda e: e.scalar_tensor_tensor(
                    out=gm2T[:, l], in0=modT[:, l, 32:40, :], scalar=1.0, in1=gmm[:, l],
                    op0=ALU.add, op1=ALU.mult), reads=[b_modT_, b_gmm], writes=[b_gm2])
            if preA is not None:
                load_A_weights(0, *preA)
            T.barrier()

    def next_stat():
        i = stat_i[0] % 8
        stat_i[0] += 1
        return stat[:, i, :], STB[i]

    def rstd_from(src_ap, src_bufs, n, from_psum=False):
        st, sbuf_ = next_stat()
        T.op("act", lambda e: e.activation(out=junk[:, 0:n], in_=src_ap, func=AF.Square,
                                           accum_out=st[:, 0:1]),
             reads=src_bufs, writes=[b_junk, sbuf_])
        T.op("dve", lambda e: e.tensor_scalar(out=st[:, 1:2], in0=st[:, 0:1], scalar1=1.0 / n,
                                              scalar2=EPS, op0=ALU.mult, op1=ALU.add),
             reads=[sbuf_], writes=[sbuf_])
        T.op("act", lambda e: e.activation(out=st[:, 2:3], in_=st[:, 1:2], func=AF.Sqrt),
             reads=[sbuf_], writes=[sbuf_])
        T.op("dve", lambda e: e.reciprocal(out=st[:, 3:4], in_=st[:, 2:3]),
             reads=[sbuf_], writes=[sbuf_])
        return st[:, 3:4], sbuf_

    def norm_to_T(xt_ap, bx, gmT_ap, shT_ap, bmods, xn, bxn, tbank, hT_dst, bhT):
        rstd, brs = rstd_from(xt_ap, [bx], D)
        T.op("dve", lambda e: e.tensor_scalar(out=xn[:], in0=xt_ap, scalar1=rstd, scalar2=None,
                                              op0=ALU.mult), reads=[bx, brs], writes=[bxn])
        pv = pbf(tbank).rearrange("p (c t) -> p c t", c=8)
        for c in range(8):
            T.op("pe", lambda e: e.transpose(pv[:, c, :], xn[:, c * 128:(c + 1) * 128], ident_b[:]),
                 reads=[bxn, b_identb], writes=[PB[tbank]])
        for c in range(8):
            if c % 2 == 0:
                T.op("act", lambda e: e.activation(out=hT_dst(c), in_=pv[:, c, :], func=AF.Identity,
                                                   scale=gmT_ap(c), bias=shT_ap(c)),
                     reads=[PB[tbank]] + bmods, writes=[bhT])
            else:
                T.op("dve", lambda e: e.tensor_scalar(out=hT_dst(c), in0=pv[:, c, :],
                                                      scalar1=gmT_ap(c), scalar2=shT_ap(c),
                                                      op0=ALU.mult, op1=ALU.add),
                     reads=[PB[tbank]] + bmods, writes=[bhT])

    def ga_bcast(es, l, chunk0, name):
        gb = salloc(es, name, [128, NSEQ, D], F32); bgb = Buf(name)
        dg = salloc(es, name + "dg", [128, 2, 128], F32); bdg = [Buf(name + "dg0"), Buf(name + "dg1")]
        k = 0
        for b in range(NSEQ):
            for half in range(2):
                bank = 6 + half
                for cc in range(4):
                    c = half * 4 + cc
                    sl = k % 2
                    k += 1
                    T.op("dve", lambda e: e.tensor_scalar(
                        out=dg[:, sl, :], in0=ident_f[:], scalar1=modT[:, l, chunk0 + c, b:b + 1],
                        scalar2=None, op0=ALU.mult), reads=[b_identf, b_modT_], writes=[bdg[sl]])
                    T.op("pe", lambda e: e.matmul(pb[bank][:, cc * 128:(cc + 1) * 128], lhsT=ones_f[:],
                                                  rhs=dg[:, sl, :], start=(cc == 0), stop=True,
                                                  skip_group_check=True),
                         reads=[b_onesf, bdg[sl]], writes=[PB[bank]])
                T.op("act", lambda e: e.activation(out=gb[:, b, half * 512:(half + 1) * 512],
                                                   in_=pb[bank][:], func=AF.Copy),
                     reads=[PB[bank]], writes=[bgb])
        return gb, bgb

    def load_A_weights(l, Wf, bWf, Wt, bWt):
        wl = wb_in[l].rearrange("(c p) n -> p c n", p=128)
        for (a, b, o) in [(0, 1024, 0), (1536, 2560, 1024), (2688, 3200, 2048),
                          (3200, 3264, 2560), (3200, 3264, 2624)]:
            for c in range(8):
                T.dma("sp", Wf[:, c, o:o + (b - a)], wl[:, c, a:b], bWf, reads=[B_wb_in[l]], writes=[bWf])
        for (a, b, o) in [(1024, 1536, 0), (2560, 2688, 512), (3264, 3272, 640)]:
            T.dma("sp", Wt[:, :, o:o + (b - a)], wl[:, :, a:b], bWt, reads=[B_wb_in[l]], writes=[bWt])

    def phase_A(l, pre=None):
        with ExitStack() as es:
            if pre is None:
                bg_flush(l)
                Wf = salloc(es, "Wf", [128, 8, 2688], BF16); bWf = Buf("Wf")
                Wt = salloc(es, "Wt", [128, 8, 648], BF16); bWt = Buf("Wt")
                load_A_weights(l, Wf, bWf, Wt, bWt)
            else:
                Wf, bWf, Wt, bWt = pre
            kvg = salloc(es, "kvg", [128, 128], F32); bkvg = Buf("kvg")
            T.dma("sp", kvg[:], kvg_bc[:, l, :], bkvg, writes=[bkvg])
            xts = [salloc(es, f"xt{i}", [128, D], F32) for i in range(3)]
            bxts = [Buf(f"xt{i}") for i in range(3)]
            xns = [salloc(es, f"xn{i}", [128, D], BF16) for i in range(2)]
            bxns = [Buf(f"xn{i}") for i in range(2)]
            hTs = [salloc(es, f"hT{i}", [128, 8, 512], BF16) for i in range(2)]
            bhTs = [Buf(f"hT{i}") for i in range(2)]
            vts = [salloc(es, f"vt{i}", [128, 512], BF16) for i in range(2)]
            bvts = [Buf(f"vt{i}") for i in range(2)]
            kvn = [salloc(es, f"kvn{i}", [128, 128], BF16) for i in range(2)]
            bkvn = [Buf(f"kvn{i}") for i in range(2)]
            kvTt = [salloc(es, f"kvTt{i}", [128, 128], BF16) for i in range(2)]
            bkvTt = [Buf(f"kvTt{i}") for i in range(2)]
            wis = [salloc(es, f"wis{i}", [128, 8], F32) for i in range(2)]
            bwis = [Buf(f"wis{i}") for i in range(2)]
            fos = [salloc(es, f"fo{i}", [128, 512], BF16) for i in range(3)]
            bfos = [Buf(f"fo{i}") for i in range(3)]
            src = x_in if l == 0 else xres
            cnt = {"ti": 0, "fi": 0}
            groups = [(s, g) for s in range(NSEQ) for g in range(4)]

            def prep_a(gi, j):
                s, g = groups[gi]
                hT = hTs[gi % 2]; bhT = bhTs[gi % 2]
                tt = g * 4 + j
                t0 = tt * 128
                ti = cnt["ti"]
                cnt["ti"] += 1
                xt = xts[ti % 3]; bxt = bxts[ti % 3]
                xn = xns[ti % 2]; bxn = bxns[ti % 2]
                T.dma("sp", xt[:], src[s, t0:t0 + 128, :], bxt, writes=[bxt])
                norm_to_T(xt[:], bxt,
                          lambda c: gm1T[:, l, c, s:s + 1],
                          lambda c: modT[:, l, 0 + c, s:s + 1],
                          [b_gm1, b_modT_], xn, bxn, ti % 2,
                          lambda c: hT[:, c, j * 128:(j + 1) * 128], bhT)
                return ti

            def prep_b(gi, j, ti):
                s, g = groups[gi]
                hT = hTs[gi % 2]; bhT = bhTs[gi % 2]
                t0 = (g * 4 + j) * 128
                k2 = ti % 2
                for c in range(8):
                    T.op("pe", lambda e: e.matmul(pb[2][:, 0:512], lhsT=hT[:, c, j * 128:(j + 1) * 128],
                                                  rhs=Wt[:, c, 0:512], start=(c == 0), stop=(c == 7)),
                         reads=[bhT, bWt], writes=[PB[2]])
                for c in range(8):
                    T.op("pe", lambda e: e.matmul(pb[3][:, 0:136], lhsT=hT[:, c, j * 128:(j + 1) * 128],
                                                  rhs=Wt[:, c, 512:648], start=(c == 0), stop=(c == 7)),
                         reads=[bhT, bWt], writes=[PB[3]])
                T.op("act", lambda e: e.activation(out=vts[k2][:], in_=pb[2][:, 0:512], func=AF.Copy),
                     reads=[PB[2]], writes=[bvts[k2]])
                T.dma("pool", v_scr[s, t0:t0 + 128, :], vts[k2][:], bvts[k2], reads=[bvts[k2]])
                rs2, brs2 = rstd_from(pb[3][:, 0:128], [PB[3]], 128)
                T.op("dve", lambda e: e.scalar_tensor_tensor(
                    out=kvn[k2][:], in0=pb[3][:, 0:128], scalar=rs2, in1=kvg[:],
                    op0=ALU.mult, op1=ALU.mult), reads=[PB[3], brs2, bkvg], writes=[bkvn[k2]])
                T.op("dve", lambda e: e.tensor_copy(out=wis[k2][:], in_=pb[3][:, 128:136]),
                     reads=[PB[3]], writes=[bwis[k2]])
                T.dma("pool", kv_tok[s, t0:t0 + 128, :], kvn[k2][:], bkvn[k2], reads=[bkvn[k2]])
                T.dma("pool", widx[s, t0:t0 + 128, :], wis[k2][:], bwis[k2], reads=[bwis[k2]])
                T.op("pe", lambda e: e.transpose(pbf(4)[:, 0:128], kvn[k2][:], ident_b[:]),
                     reads=[bkvn[k2], b_identb], writes=[PB[4]])
                T.op("act", lambda e: e.activation(out=kvTt[k2][:], in_=pbf(4)[:, 0:128], func=AF.Copy),
                     reads=[PB[4]], writes=[bkvTt[k2]])
                T.dma("pool", kvT_scr[s, :, t0:t0 + 128], kvTt[k2][:], bkvTt[k2], reads=[bkvTt[k2]])

            def fm_chunk(gi, ch):
                s, g = groups[gi]
                hT = hTs[gi % 2]; bhT = bhTs[gi % 2]
                fi = cnt["fi"]
                cnt["fi"] += 1
                bank = 5 + (fi % 3)
                fo = fos[fi % 3]; bfo = bfos[fi % 3]
                for c in range(8):
                    T.op("pe", lambda e: e.matmul(pb[bank][:, 0:512], lhsT=Wf[:, c, ch * 128:(ch + 1) * 128],
                                                  rhs=hT[:, c, :], start=(c == 0), stop=(c == 7)),
                         reads=[bhT, bWf], writes=[PB[bank]])
                if fi % 2 == 0:
                    T.op("act", lambda e: e.activation(out=fo[:], in_=pb[bank][:, 0:512], func=AF.Copy),
                         reads=[PB[bank]], writes=[bfo])
                else:
                    T.op("dve", lambda e: e.tensor_copy(out=fo[:], in_=pb[bank][:, 0:512]),
                         reads=[PB[bank]], writes=[bfo])
                T.dma("pool", featT[s, ch, :, g * 512:(g + 1) * 512], fo[:], bfo, reads=[bfo])

            for j in range(4):
                ti0 = prep_a(0, j)
                prep_b(0, j, ti0)
            for gi in range(len(groups)):
                pend_b = None
                for ch in range(21):
                    fm_chunk(gi, ch)
                    if gi + 1 < len(groups):
                        if ch in (1, 6, 11, 16):
                            j = (1, 6, 11, 16).index(ch)
                            pend_b = (j, prep_a(gi + 1, j))
                        if ch in (4, 9, 14, 19) and pend_b is not None:
                            prep_b(gi + 1, pend_b[0], pend_b[1])
                            pend_b = None
            T.barrier()

    def phase_B(l):
        with ExitStack() as es:
            qT = salloc(es, "qT", [128, 4, S], BF16); bqT = Buf("qT")
            kT = salloc(es, "kT", [128, 4, S], BF16); bkT = Buf("kT")
            vv = salloc(es, "vv", [128, NT, 512], BF16); bvv = Buf("vv")
            tri8 = salloc(es, "tri8", [128, 128], BF16); btri = Buf("tri8")
            neg8 = salloc(es, "neg8", [128, 128], BF16); bneg = Buf("neg8")
            cm = salloc(es, "cm", [128, 512], BF16); bcm = Buf("cm")
            goa = salloc(es, "goa", [128, 512], F32); bgoa = Buf("goa")
            T.dma("pool", tri8[:], c_tri8, btri, writes=[btri])
            T.dma("pool", neg8[:], c_neg8, bneg, writes=[bneg])
            T.dma("pool", cm[:], c_cmask[:, 0:4, :].rearrange("p h t -> p (h t)"), bcm, writes=[bcm])
            T.dma("sp", goa[:], goa_bc[:, l, :], bgoa, writes=[bgoa])
            e32a = [salloc(es, f"e32a{i}", [128, 1024], F32) for i in range(2)]
            spba = [salloc(es, f"spba{i}", [128, 1024], BF16) for i in range(2)]
            wba = [salloc(es, f"wba{i}", [128, 1024], BF16) for i in range(2)]
            e32 = [[e32a[i][:, g * 512:(g + 1) * 512] for i in range(2)] for g in range(2)]
            spb = [[spba[i][:, g * 512:(g + 1) * 512] for i in range(2)] for g in range(2)]
            wb = [[wba[i][:, g * 512:(g + 1) * 512] for i in range(2)] for g in range(2)]
            be32 = [[Buf(f"e32{g}{i}") for i in range(2)] for g in range(2)]
            bspb = [[Buf(f"spb{g}{i}") for i in range(2)] for g in range(2)]
            bwb = [[Buf(f"wb{g}{i}") for i in range(2)] for g in range(2)]
            sps = [salloc(es, f"sps{g}", [128, 512], F32) for g in range(2)]
            bsps = [Buf(f"sps{g}") for g in range(2)]
            spsb = [[salloc(es, f"spsb{g}{i}", [128, 512], BF16) for i in range(2)] for g in range(2)]
            bspsb = [[Buf(f"spsb{g}{i}") for i in range(2)] for g in range(2)]
            oan = [salloc(es, f"oan{i}", [128, 512], BF16) for i in range(2)]
            boan = [Buf(f"oan{i}") for i in range(2)]
            ZB = [[0, 2], [1, 3]]
            AB = [4, 5]
            OB = [6, 7]
            carry_slot = [0, 0]

            def zmm(bank, hg, qb, kb, start_first):
                for i in range(4):
                    h = 2 * i + hg
                    ch = h // 2
                    r0 = (h % 2) * 64
                    T.op("pe", lambda e: e.matmul(
                        pb[bank][:, i * 128:(i + 1) * 128],
                        lhsT=kT[r0:r0 + 64, ch, kb * 128:(kb + 1) * 128],
                        rhs=qT[r0:r0 + 64, ch, qb * 128:(qb + 1) * 128],
                        start=(start_first and i == 0), stop=(i == 3),
                        skip_group_check=True),
                        reads=[bkT, bqT], writes=[PB[bank]])

            def stage_Z(n, qb, kb):
                sl = n % 2
                for hg in range(2):
                    zmm(ZB[hg][sl], hg, qb, kb, True)
                T.op("act", lambda e: e.activation(out=e32a[sl][:], in_=pb2[sl][:], func=AF.Exp, scale=0.125),
                     reads=[PB[ZB[0][sl]], PB[ZB[1][sl]]], writes=[be32[0][sl], be32[1][sl]])
                T.op("act", lambda e: e.activation(out=spba[sl][:], in_=e32a[sl][:], func=AF.Ln, bias=1.0),
                     reads=[be32[0][sl], be32[1][sl]], writes=[bspb[0][sl], bspb[1][sl]])
                for hg in range(2):
                    if kb == qb:
                        T.op("dve", lambda e: e.tensor_tensor(out=spb[hg][sl][:], in0=spb[hg][sl][:], in1=cm[:], op=ALU.mult),
                             reads=[bspb[hg][sl], bcm], writes=[bspb[hg][sl]])

            def stage_A(n, qb, kb):
                sl = n % 2
                for hg in range(2):
                    ab = AB[hg]
                    T.op("pe", lambda e: e.matmul(pb[ab][:], lhsT=tri8[:], rhs=spb[hg][sl][:], start=True, stop=False,
                                                  skip_group_check=True),
                         reads=[btri, bspb[hg][sl]], writes=[PB[ab]])
                    if kb < qb:
                        cs = carry_slot[hg]
                        T.op("pe", lambda e: e.matmul(pb[ab][:], lhsT=neg8[:], rhs=spsb[hg][cs][:], start=False, stop=False,
                                                      skip_group_check=True),
                             reads=[bneg, bspsb[hg][cs]], writes=[PB[ab]])
                for hg in range(2):
                    zmm(AB[hg], hg, qb, kb, False)
                T.op("act", lambda e: e.activation(out=wba[sl][:], in_=pb2[2][:], func=AF.Exp, scale=0.125),
                     reads=[PB[AB[0]], PB[AB[1]]], writes=[bwb[0][sl], bwb[1][sl]])
                for hg in range(2):
                    ab = AB[hg]
                    if kb == qb:
                        T.op("dve", lambda e: e.tensor_tensor(out=wb[hg][sl][:], in0=wb[hg][sl][:], in1=cm[:], op=ALU.mult),
                             reads=[bwb[hg][sl], bcm], writes=[bwb[hg][sl]])
                    if kb > 0:
                        if kb == qb:
                            T.op("dve", lambda e: e.tensor_copy(out=sps[hg][:], in_=spb[hg][sl][:]),
                                 reads=[bspb[hg][sl]], writes=[bsps[hg]])
                        else:
                            T.op("dve", lambda e: e.tensor_tensor(out=sps[hg][:], in0=sps[hg][:], in1=spb[hg][sl][:], op=ALU.add),
                                 reads=[bsps[hg], bspb[hg][sl]], writes=[bsps[hg]])
                        carry_slot[hg] ^= 1
                        cs = carry_slot[hg]
                        T.op("dve", lambda e: e.tensor_copy(out=spsb[hg][cs][:], in_=sps[hg][:]),
                             reads=[bsps[hg]], writes=[bspsb[hg][cs]])

            def stage_PV(n, s, qb, kb):
                sl = n % 2
                ob = OB[qb % 2]
                for hg in range(2):
                    for i in range(4):
                        h = 2 * i + hg
                        T.op("pe", lambda e: e.matmul(
                            pb[ob][:, h * 64:(h + 1) * 64], lhsT=wb[hg][sl][:, i * 128:(i + 1) * 128],
                            rhs=vv[:, kb, h * 64:(h + 1) * 64], start=(kb == qb and hg == 0 and i == 0), stop=False,
                            skip_group_check=True),
                            reads=[bwb[hg][sl], bvv], writes=[PB[ob]])
                if kb == 0:
                    osl = qb % 2
                    rs, brs = rstd_from(pb[ob][:], [PB[ob]], 512)
                    T.op("dve", lambda e: e.scalar_tensor_tensor(out=oan[osl][:], in0=pb[ob][:], scalar=rs, in1=goa[:],
                                                                 op0=ALU.mult, op1=ALU.mult),
                         reads=[PB[ob], brs, bgoa], writes=[boan[osl]])
                    T.dma("pool", ocat[s, qb * 128:(qb + 1) * 128, 0:512], oan[osl][:], boan[osl], reads=[boan[osl]])
                    bg_step(1)

            for s in range(LIM_SEQ):
                T.dma("sp", qT[:], featT[s, 0:4].rearrange("c p t -> p c t"), bqT, writes=[bqT])
                T.dma("sp", kT[:], featT[s, 4:8].rearrange("c p t -> p c t"), bkT, writes=[bkT])
                T.dma("sp", vv[:], v_scr[s].rearrange("(n p) f -> p n f", p=128), bvv, writes=[bvv])
                its = [(qb, kb) for qb in range(LIM_QB) for kb in range(qb, -1, -1)]
                N = len(its)
                for t in range(N + 2):
                    if t < N:
                        stage_Z(t, *its[t])
                    if 0 <= t - 1 < N:
                        stage_A(t - 1, *its[t - 1])
                    if 0 <= t - 2 < N:
                        stage_PV(t - 2, s, *its[t - 2])
            T.barrier()

    def phase_C(l):
        if FLUSH_BG_BEFORE_C:
            bg_flush(99)
        with ExitStack() as es:
            cin = []
            for s_ in range(NSEQ):
                d_ = dict(
                    dqT=salloc(es, f"dqT{s_}", [128, 8, S], BF16), bdq=Buf(f"dqT{s_}"),
                    iqT=salloc(es, f"iqT{s_}", [128, 4, S], BF16), biq=Buf(f"iqT{s_}"),
                    ikT=salloc(es, f"ikT{s_}", [128, S], BF16), bik=Buf(f"ikT{s_}"),
                    kvT=salloc(es, f"kvT{s_}", [128, S], BF16), bkvT=Buf(f"kvT{s_}"),
                    kvt=salloc(es, f"kvt{s_}", [128, NT, 128], BF16), bkvt=Buf(f"kvt{s_}"),
                    wi=salloc(es, f"wi{s_}", [128, NT, 8], F32), bwi=Buf(f"wi{s_}"))
                cin.append(d_)
            cur = {}

            def load_seq_C(s_, gate=()):
                d_ = cin[s_]
                g = list(gate)
                T.dma("sp", d_["iqT"][:], featT[s_, 16:20].rearrange("c p t -> p c t"), d_["biq"], reads=g, writes=[d_["biq"]])
                T.dma("sp", d_["ikT"][:], featT[s_, 20], d_["bik"], reads=g, writes=[d_["bik"]])
                T.dma("sp", d_["wi"][:], widx[s_].rearrange("(n p) j -> p n j", p=128), d_["bwi"], reads=g, writes=[d_["bwi"]])
                T.dma("sp", d_["kvT"][:], kvT_scr[s_], d_["bkvT"], reads=g, writes=[d_["bkvT"]])
                T.dma("sp", d_["kvt"][:], kv_tok[s_].rearrange("(n p) r -> p n r", p=128), d_["bkvt"], reads=g, writes=[d_["bkvt"]])
                T.dma("sp", d_["dqT"][:], featT[s_, 8:16].rearrange("c p t -> p c t"), d_["bdq"], reads=g, writes=[d_["bdq"]])
            wuv = salloc(es, "wuv", [128, 8, 64], BF16); bwuv = Buf("wuv")
            gob = salloc(es, "gob", [128, 512], F32); bgob = Buf("gob")
            BN = salloc(es, "BN", [128, 2, 1024], BF16); bBN = Buf("BN")
            id4 = salloc(es, "id4", [128, 512], BF16); bid4 = Buf("id4")
            pw2 = salloc(es, "pw2", [128, KBIS], F32); bpw2 = Buf("pw2")
            T.dma("pool", wuv[:], w_uv[l].rearrange("h r d -> r h d"), bwuv, writes=[bwuv])
            T.dma("sp", gob[:], gob_bc[:, l, :], bgob, writes=[bgob])
            T.dma("sp", pw2[:], c_pow2, bpw2, writes=[bpw2])
            for i in range(4):
                T.dma("pool", id4[:, i * 128:(i + 1) * 128], c_ident, bid4, writes=[bid4])
            score = [salloc(es, f"score{i}", [128, S], F32) for i in range(2)]
            bsc = [Buf(f"score{i}") for i in range(2)]
            nmask = [salloc(es, f"nmask{i}", [128, S], BF16) for i in range(2)]
            bnm = [Buf(f"nmask{i}") for i in range(2)]
            PP = [salloc(es, f"PP{i}", [128, 1024], BF16) for i in range(2)]
            bPP = [Buf(f"PP{i}") for i in range(2)]
            oTs = salloc(es, "oTs", [128, 1024], BF16); boTs = Buf("oTs")
            bis = salloc(es, "bis", [128, 8 + 2 * KBIS], F32); bbis = Buf("bis")
            rden = salloc(es, "rden", [128, 8], F32); brden = Buf("rden")
            obf = salloc(es, "obf", [128, 512], F32); bobf = Buf("obf")
            obn = [salloc(es, f"obn{i}", [128, 512], BF16) for i in range(2)]
            bobn = [Buf(f"obn{i}") for i in range(2)]
            lsc = 128 ** -0.5
            isc = (64 ** -0.5) * (8 ** -0.5)
            with ExitStack() as es2:
                bn = salloc(es2, "bn", [128, 2, 1024], F32); bbn = Buf("bn")
                bf = salloc(es2, "bf", [128, 1024], F32); bbf = Buf("bf")
                adn = salloc(es2, "adn", [128, 1024], F32); badn = Buf("adn")
                T.dma("sp", bn[:], biasn.rearrange("p o h t -> p o (h t)"), bbn, writes=[bbn])
                T.dma("sp", bf[:], bfar_bc.rearrange("p h t -> p (h t)"), bbf, writes=[bbf])
                T.dma("sp", adn[:], c_admneg.rearrange("p h t -> p (h t)"), badn, writes=[badn])
                for o in range(2):
                    T.op("dve", lambda e: e.tensor_tensor(out=bn[:, o, :], in0=bn[:, o, :], in1=bf[:], op=ALU.subtract),
                         reads=[bbn, bbf], writes=[bbn])
                    if o == 0:
                        T.op("dve", lambda e: e.scalar_tensor_tensor(out=BN[:, o, :], in0=bn[:, o, :], scalar=1.0 / lsc, in1=adn[:],
                                                                     op0=ALU.mult, op1=ALU.add),
                             reads=[bbn, badn], writes=[bBN])
                    else:
                        T.op("dve", lambda e: e.tensor_scalar(out=BN[:, o, :], in0=bn[:, o, :], scalar1=1.0 / lsc, scalar2=None,
                                                              op0=ALU.mult), reads=[bbn], writes=[bBN])
                T.barrier()
            cnt_i = {"ii": 0, "pi": 0, "oi": 0, "lp": 0, "ib": 0}
            IBS = (2, 3)
            SB = 4
            NRB = 2 * ACC_DEPTH + 2
            Rb = [salloc(es, f"Rb{i}", [128, 512], BF16) for i in range(NRB)]
            bRb = [Buf(f"Rb{i}") for i in range(NRB)]
            dg = [salloc(es, f"dg{i}", [128, 8, 128], BF16) for i in range(2)]
            bdg = [Buf(f"dg{i}") for i in range(2)]
            absw = salloc(es, "absw", [128, NT, 8], F32); babsw = Buf("absw")
            sgn = salloc(es, "sgn", [128, NT, 8], F32); bsgn = Buf("sgn")
            OTB = (5, 6)
            DB = 7

            pend_acc = []

            def flush_acc(keep=0):
                while len(pend_acc) > keep:
                    (qb, c0, w, j, rsl, dsl) = pend_acc.pop(0)
                    sc_ = score[qb % 2]; bsc_ = bsc[qb % 2]
                    T.op("pe", lambda e: e.matmul(pb[SB][:, 0:w], lhsT=dg[dsl][:, j, :], rhs=Rb[rsl][:, 0:w],
                                                  start=(j == 0), stop=(j == 7), skip_group_check=True),
                         reads=[bdg[dsl], bRb[rsl]], writes=[PB[SB]])
                    if j == 7:
                        T.op("act", lambda e: e.activation(out=sc_[:, c0:c0 + w], in_=pb[SB][:, 0:w], func=AF.Copy),
                             reads=[PB[SB]], writes=[bsc_])

            def make_diag(qb):
                dsl = qb % 2
                for j in range(8):
                    T.op("act", lambda e: e.activation(out=dg[dsl][:, j, :], in_=ident_b[:], func=AF.Identity,
                                                       scale=sgn[:, qb, j:j + 1]),
                         reads=[b_identb, bsgn], writes=[bdg[dsl]])

            def idx_unit(s, qb, c, j):
                n = (qb + 1) * 128
                q0 = qb * 128
                c0 = c * 512
                w = min(512, n - c0)
                rsl = cnt_i["ii"] % NRB
                cnt_i["ii"] += 1
                ch = j // 2
                r0 = (j % 2) * 64
                IB = IBS[cnt_i["ib"] % 2]
                cnt_i["ib"] += 1
                T.op("pe", lambda e: e.matmul(pb[IB][:, 0:w], lhsT=cur['iqT'][r0:r0 + 64, ch, q0:q0 + 128],
                                              rhs=cur['ikT'][r0:r0 + 64, c0:c0 + w], start=True, stop=True),
                     reads=[cur['biq'], cur['bik']], writes=[PB[IB]])
                T.op("act", lambda e: e.activation(out=Rb[rsl][:, 0:w], in_=pb[IB][:, 0:w], func=AF.Relu,
                                                   scale=absw[:, qb, j:j + 1]),
                     reads=[PB[IB], babsw], writes=[bRb[rsl]])
                flush_acc(keep=ACC_DEPTH - 1)
                pend_acc.append((qb, c0, w, j, rsl, qb % 2))

            def select(qb):
                n = (qb + 1) * 128
                nm = nmask[qb % 2]; bn_ = bnm[qb % 2]
                score_ = score[qb % 2]; bsc_ = bsc[qb % 2]
                T.op("dve", lambda e: e.memset(score_[0:64, n - 64:n], -1.0e30), reads=[bsc_], writes=[bsc_])
                if qb >= 2:
                    hi = bis[:, 0:1]; lo = bis[:, 1:2]; w0 = bis[:, 2:3]; mid = bis[:, 3:4]
                    cnt = bis[:, 4:5]; sv = bis[:, 5:6]; thr = bis[:, 6:7]
                    H = bis[:, 8:8 + KBIS]; H2 = bis[:, 8 + KBIS:8 + 2 * KBIS]
                    T.op("dve", lambda e: e.tensor_reduce(out=hi, in_=score_[:, 0:n], axis=AX.X, op=ALU.max),
                         reads=[bsc_], writes=[bbis])
                    T.op("dve", lambda e: e.tensor_reduce(out=lo, in_=score_[:, 0:n - 64], axis=AX.X, op=ALU.min),
                         reads=[bsc_], writes=[bbis])
                    T.op("dve", lambda e: e.tensor_tensor(out=w0, in0=hi, in1=lo, op=ALU.subtract),
                         reads=[bbis], writes=[bbis])
                    T.op("dve", lambda e: e.tensor_scalar(out=H, in0=pw2[:], scalar1=w0, scalar2=None, op0=ALU.mult),
                         reads=[bbis, bpw2], writes=[bbis])
                    T.op("dve", lambda e: e.tensor_scalar(out=H2, in0=H, scalar1=2.0, scalar2=None, op0=ALU.mult),
                         reads=[bbis], writes=[bbis])
                    T.op("dve", lambda e: e.tensor_tensor(out=mid, in0=lo, in1=bis[:, 8:9], op=ALU.add),
                         reads=[bbis], writes=[bbis])
                    for k in range(KBIS):
                        T.op("dve", lambda e: e.tensor_scalar(out=junk[:, 0:n], in0=score_[:, 0:n], scalar1=mid, scalar2=None,
                                                              op0=ALU.is_ge, op1=ALU.add, accum_out=cnt),
                             reads=[bsc_, bbis], writes=[b_junk, bbis])
                        if k < KBIS - 1:
                            T.op("dve", lambda e: e.tensor_scalar(out=sv, in0=cnt, scalar1=float(TOPK),
                                                                  scalar2=bis[:, 8 + KBIS + k + 1:8 + KBIS + k + 2],
                                                                  op0=ALU.is_ge, op1=ALU.mult),
                                 reads=[bbis], writes=[bbis])
                            T.op("dve", lambda e: e.scalar_tensor_tensor(out=mid, in0=sv, scalar=bis[:, 8 + k + 1:8 + k + 2],
                                                                         in1=mid, op0=ALU.subtract, op1=ALU.add),
                                 reads=[bbis], writes=[bbis])
                        else:
                            T.op("dve", lambda e: e.tensor_scalar(out=sv, in0=cnt, scalar1=float(TOPK),
                                                                  scalar2=bis[:, 8 + k:8 + k + 1],
                                                                  op0=ALU.is_ge, op1=ALU.mult),
                                 reads=[bbis], writes=[bbis])
                            T.op("dve", lambda e: e.scalar_tensor_tensor(out=thr, in0=sv, scalar=bis[:, 8 + k:8 + k + 1],
                                                                         in1=mid, op0=ALU.subtract, op1=ALU.add),
                                 reads=[bbis], writes=[bbis])
                    T.op("dve", lambda e: e.tensor_scalar(out=nm[:, 0:n], in0=score_[:, 0:n], scalar1=thr, scalar2=-1.0e5,
                                                          op0=ALU.is_lt, op1=ALU.mult), reads=[bsc_, bbis], writes=[bn_])
                else:
                    T.op("dve", lambda e: e.tensor_scalar(out=nm[:, 0:n], in0=score_[:, 0:n], scalar1=-1.0e29, scalar2=-1.0e5,
                                                          op0=ALU.is_lt, op1=ALU.mult), reads=[bsc_], writes=[bn_])

            def att_logits(s, qb, kb):
                q0 = qb * 128
                nm = nmask[qb % 2]; bn_ = bnm[qb % 2]
                lp = cnt_i["lp"] % 2
                cnt_i["lp"] += 1
                P = PP[lp]; bP = bPP[lp]
                off = qb - kb
                for (bank, h0) in ((0, 0), (1, 4)):
                    T.op("pe", lambda e: e.matmul(
                        pb[bank][:].rearrange("p (h t) -> p h t", h=4),
                        lhsT=cur['kvT'][:, kb * 128:(kb + 1) * 128], rhs=cur['dqT'][:, h0:h0 + 4, q0:q0 + 128],
                        start=True, stop=False, skip_group_check=True), reads=[cur['bkvT'], cur['bdq']], writes=[PB[bank]])
                    T.op("pe", lambda e: e.matmul(pb[bank][:], lhsT=nm[:, kb * 128:(kb + 1) * 128], rhs=id4[:],
                                                  start=False, stop=(off >= 2), skip_group_check=True),
                         reads=[bn_, bid4], writes=[PB[bank]])
                    if off < 2:
                        T.op("pe", lambda e: e.matmul(pb[bank][:], lhsT=ident_b[:], rhs=BN[:, off, h0 * 128:(h0 + 4) * 128],
                                                      start=False, stop=True, skip_group_check=True),
                             reads=[b_identb, bBN], writes=[PB[bank]])
                T.op("act", lambda e: e.activation(out=P[:], in_=pb2[0][:], func=AF.Exp, scale=lsc),
                     reads=[PB[0], PB[1]], writes=[bP])
                return lp

            def att_pv(s, qb, kb, lp):
                P = PP[lp]; bP = bPP[lp]
                for (bank, h0) in ((OTB[0], 0), (OTB[1], 4)):
                    T.op("pe", lambda e: e.matmul(pb[bank][:], lhsT=cur['kvt'][:, kb, :], rhs=P[:, h0 * 128:(h0 + 4) * 128],
                                                  start=(kb == 0), stop=(kb == qb)),
                         reads=[cur['bkvt'], bP], writes=[PB[bank]])
                for h in range(8):
                    T.op("pe", lambda e: e.matmul(pb[DB][:, h:h + 1], lhsT=P[:, h * 128:(h + 1) * 128], rhs=ones_b[:, 0:1],
                                                  start=(kb == 0 and h == 0), stop=(kb == qb), skip_group_check=True),
                         reads=[bP, b_onesb], writes=[PB[DB]])

            def epilogue(s, qb):
                q0 = qb * 128
                T.op("act", lambda e: e.activation(out=oTs[:, 0:512], in_=pb[OTB[0]][:], func=AF.Copy), reads=[PB[OTB[0]]], writes=[boTs])
                T.op("act", lambda e: e.activation(out=oTs[:, 512:1024], in_=pb[OTB[1]][:], func=AF.Copy), reads=[PB[OTB[1]]], writes=[boTs])
                T.op("dve", lambda e: e.reciprocal(out=rden[:], in_=pb[DB][:, 0:8]), reads=[PB[DB]], writes=[brden])
                eb = IBS[cnt_i["ib"] % 2]
                cnt_i["ib"] += 1
                for h in range(8):
                    T.op("pe", lambda e: e.matmul(pb[eb][:, h * 64:(h + 1) * 64], lhsT=oTs[:, h * 128:(h + 1) * 128],
                                                  rhs=wuv[:, h, :], start=(h == 0), stop=True, skip_group_check=True),
                         reads=[boTs, bwuv], writes=[PB[eb]])
                for h in range(8):
                    T.op("dve", lambda e: e.tensor_scalar(out=obf[:, h * 64:(h + 1) * 64], in0=pb[eb][:, h * 64:(h + 1) * 64],
                                                          scalar1=rden[:, h:h + 1], scalar2=None, op0=ALU.mult),
                         reads=[PB[eb], brden], writes=[bobf])
                rs, brs = rstd_from(obf[:], [bobf], 512)
                osl = cnt_i["oi"] % 2
                cnt_i["oi"] += 1
                T.op("dve", lambda e: e.scalar_tensor_tensor(out=obn[osl][:], in0=obf[:], scalar=rs, in1=gob[:],
                                                             op0=ALU.mult, op1=ALU.mult),
                     reads=[bobf, brs, bgob], writes=[bobn[osl]])
                T.dma("pool", ocat[s, q0:q0 + 128, 512:1024], obn[osl][:], bobn[osl], reads=[bobn[osl]])
                if s == 0 and LIM_SEQ > 1 and qb == min(8, LIM_QB - 1):
                    load_seq_C(1, gate=[bobn[osl]])

            for s in range(LIM_SEQ):
                if s == 0:
                    load_seq_C(0)
                cur.clear(); cur.update(cin[s])
                wi = cur["wi"]; bwi = cur["bwi"]
                T.op("act", lambda e: e.activation(out=absw[:], in_=wi[:], func=AF.Abs, scale=isc),
                     reads=[bwi], writes=[babsw])
                T.op("dve", lambda e: e.tensor_scalar(out=sgn[:], in0=wi[:], scalar1=0.0, scalar2=2.0,
                                                      op0=ALU.is_ge, op1=ALU.mult), reads=[bwi], writes=[bsgn])
                T.op("dve", lambda e: e.tensor_scalar(out=sgn[:], in0=sgn[:], scalar1=-1.0, scalar2=None,
                                                      op0=ALU.add), reads=[bsgn], writes=[bsgn])
                for step in range(LIM_QB + 2):
                    qi = step
                    qs = step - 1
                    qa = step - 2
                    iu = []
                    if qi < LIM_QB:
                        n = (qi + 1) * 128
                        iu = [(c, j) for c in range((n + 511) // 512) for j in range(8)]
                        make_diag(qi)
                    if 0 <= qs < LIM_QB:
                        select(qs)
                    au = list(range(qa + 1)) if qa >= 0 else []
                    na, ni = len(au), len(iu)
                    ai = 0
                    ii_ = 0
                    pend = None
                    total = max(na, 1)
                    while ai < na or ii_ < ni:
                        tgt = ni if ai >= na else (ni * (ai + 1)) // total
                        while ii_ < tgt:
                            idx_unit(s, qi, *iu[ii_])
                            ii_ += 1
                        if ai < na:
                            lp = att_logits(s, qa, au[ai])
                            if pend is not None:
                                att_pv(s, qa, *pend)
                            pend = (au[ai], lp)
                            ai += 1
                    if pend is not None:
                        att_pv(s, qa, *pend)
                    flush_acc()
                    if qa >= 0:
                        epilogue(s, qa)
                        bg_step(1)
            T.barrier()

    def phase_D(l, prefetch=()):
        prefetch = list(prefetch)
        bg_flush(l)
        with ExitStack() as es:
            Wo = salloc(es, "Wo", [128, 8, D], BF16); bWo = Buf("Wo")
            wl = wb_out[l].rearrange("(c p) n -> p c n", p=128)
            for c in range(8):
                T.dma("sp", Wo[:, c, :], wl[:, c, :], bWo, reads=[B_wb_out[l]], writes=[bWo])
            gb, bgb = ga_bcast(es, l, 16, "ga1")
            oc = [salloc(es, f"oc{i}", [128, D], BF16) for i in range(2)]
            boc = [Buf(f"oc{i}") for i in range(2)]
            oT = [salloc(es, f"oT{i}", [128, 8, 128], BF16) for i in range(2)]
            boT = [Buf(f"oT{i}") for i in range(2)]
            xts = [salloc(es, f"xd{i}", [128, D], F32) for i in range(2)]
            bxts = [Buf(f"xd{i}") for i in range(2)]
            tmp = [salloc(es, f"tm{i}", [128, D], F32) for i in range(2)]
            btmp = [Buf(f"tm{i}") for i in range(2)]
            src = x_in if l == 0 else xres
            ti = 0
            for s in range(NSEQ):
                for tt in range(NT):
                    t0 = tt * 128
                    k2 = ti % 2
                    ti += 1
                    T.dma("sp", oc[k2][:], ocat[s, t0:t0 + 128, :], boc[k2], writes=[boc[k2]])
                    T.dma("sp", xts[k2][:], src[s, t0:t0 + 128, :], bxts[k2], writes=[bxts[k2]])
                    if prefetch:
                        prefetch.pop(0)()
                    pv = pbf(k2).rearrange("p (c t) -> p c t", c=8)
                    for c in range(8):
                        T.op("pe", lambda e: e.transpose(pv[:, c, :], oc[k2][:, c * 128:(c + 1) * 128], ident_b[:]),
                             reads=[boc[k2], b_identb], writes=[PB[k2]])
                    T.op("act", lambda e: e.activation(out=oT[k2][:].rearrange("p c t -> p (c t)"), in_=pbf(k2)[:, 0:1024], func=AF.Copy),
                         reads=[PB[k2]], writes=[boT[k2]])
                    for half in range(2):
                        bank = 2 + k2 * 2 + half
                        for c in range(8):
                            T.op("pe", lambda e: e.matmul(pb[bank][:], lhsT=oT[k2][:, c, :], rhs=Wo[:, c, half * 512:(half + 1) * 512],
                                                          start=(c == 0), stop=(c == 7)),
                                 reads=[boT[k2], bWo], writes=[PB[bank]])
                        T.op("dve", lambda e: e.tensor_tensor(out=tmp[k2][:, half * 512:(half + 1) * 512], in0=pb[bank][:],
                                                              in1=gb[:, s, half * 512:(half + 1) * 512], op=ALU.mult),
                             reads=[PB[bank], bgb], writes=[btmp[k2]])
                    T.op("dve", lambda e: e.tensor_tensor(out=tmp[k2][:], in0=tmp[k2][:], in1=xts[k2][:], op=ALU.add),
                         reads=[btmp[k2], bxts[k2]], writes=[btmp[k2]])
                    T.dma("pool", xres[s, t0:t0 + 128, :], tmp[k2][:], btmp[k2], reads=[btmp[k2]])
            while prefetch:
                prefetch.pop(0)()
            T.barrier()

    def phase_DE(l, last):
        with ExitStack() as esw:
            Wu = salloc(esw, "Wu", [128, 8, DFF], BF16); bWu = Buf("Wu")
            Wd = salloc(esw, "Wd", [128, 32, D], BF16); bWd = Buf("Wd")
            wul = wb_up[l].rearrange("(c p) n -> p c n", p=128)
            wdl = wb_dn[l].rearrange("(c p) n -> p c n", p=128)
            pf = []
            for c in range(8):
                for hh in range(2):
                    pf.append(lambda c=c, hh=hh: T.dma(
                        "sp", Wu[:, c, hh * 2048:(hh + 1) * 2048], wul[:, c, hh * 2048:(hh + 1) * 2048], bWu,
                        reads=[B_wb_up[l]], writes=[bWu]))
            for c4 in range(8):
                pf.append(lambda c4=c4: T.dma(
                    "sp", Wd[:, c4 * 4:(c4 + 1) * 4, :], wdl[:, c4 * 4:(c4 + 1) * 4, :], bWd,
                    reads=[B_wb_dn[l]], writes=[bWd]))
            phase_D(l, prefetch=pf)
            phase_E(l, last, Wu, bWu, Wd, bWd)

    def phase_E(l, last, Wu, bWu, Wd, bWd):
        with ExitStack() as es:
            gb, bgb = ga_bcast(es, l, 40, "ga2")
            if last:
                gf = salloc(es, "gf", [128, D], F32); bgf = Buf("gf")
                T.dma("sp", gf[:], gfin_bc, bgf, writes=[bgf])
            TG = 256
            xts = [salloc(es, f"xe{i}", [128, D], F32) for i in range(4)]
            bxts = [Buf(f"xe{i}") for i in range(4)]
            xns = [salloc(es, f"xne{i}", [128, D], BF16) for i in range(2)]
            bxns = [Buf(f"xne{i}") for i in range(2)]
            hTs = [salloc(es, f"hTe{i}", [128, 8, TG], BF16) for i in range(2)]
            bhTs = [Buf(f"hTe{i}") for i in range(2)]
            aT = salloc(es, "aT", [128, 32, TG], BF16); baT = [Buf(f"aT{i}") for i in range(32)]
            rl = [salloc(es, f"rl{i}", [128, TG], BF16) for i in range(2)]
            brl = [Buf(f"rl{i}") for i in range(2)]
            ti = 0
            gi = 0
            ui = 0
            for s in range(NSEQ):
                for g in range(S // TG):
                    hT = hTs[gi % 2]; bhT = bhTs[gi % 2]
                    gi += 1
                    tiles = []
                    for j in range(TG // 128):
                        tt = g * (TG // 128) + j
                        t0 = tt * 128
                        xt = xts[ti % 4]; bxt = bxts[ti % 4]
                        xn = xns[ti % 2]; bxn = bxns[ti % 2]
                        tb = ti % 2
                        ti += 1
                        tiles.append((t0, xt, bxt))
                        T.dma("sp", xt[:], xres[s, t0:t0 + 128, :], bxt, writes=[bxt])
                        norm_to_T(xt[:], bxt,
                                  lambda c: gm2T[:, l, c, s:s + 1],
                                  lambda c: modT[:, l, 24 + c, s:s + 1],
                                  [b_gm2, b_modT_], xn, bxn, tb,
                                  lambda c: hT[:, c, j * 128:(j + 1) * 128], bhT)
                    for f in range(32):
                        bank = 2 + (ui % 2)
                        sl = ui % 2
                        ui += 1
                        for c in range(8):
                            T.op("pe", lambda e: e.matmul(pb[bank][:, 0:TG], lhsT=Wu[:, c, f * 128:(f + 1) * 128], rhs=hT[:, c, :],
                                                          start=(c == 0), stop=(c == 7)),
                                 reads=[bWu, bhT], writes=[PB[bank]])
                        T.op("act", lambda e: e.activation(out=rl[sl][:], in_=pb[bank][:, 0:TG], func=AF.Relu),
                             reads=[PB[bank]], writes=[brl[sl]])
                        T.op("dve", lambda e: e.tensor_tensor(out=aT[:, f, :], in0=rl[sl][:], in1=rl[sl][:], op=ALU.mult),
                             reads=[brl[sl]], writes=[baT[f]])
                    for j, (t0, xt, bxt) in enumerate(tiles):
                        for half in range(2):
                            bank = 4 + (j % 2) * 2 + half
                            for f in range(32):
                                T.op("pe", lambda e: e.matmul(pb[bank][:], lhsT=aT[:, f, j * 128:(j + 1) * 128],
                                                              rhs=Wd[:, f, half * 512:(half + 1) * 512], start=(f == 0), stop=(f == 31)),
                                     reads=[baT[f], bWd], writes=[PB[bank]])
                            T.op("dve", lambda e: e.tensor_tensor(out=tmpE[j % 2][:, half * 512:(half + 1) * 512], in0=pb[bank][:],
                                                                  in1=gb[:, s, half * 512:(half + 1) * 512], op=ALU.mult),
                                 reads=[PB[bank], bgb], writes=[btmpE[j % 2]])
                        T.op("dve", lambda e: e.tensor_tensor(out=xt[:], in0=tmpE[j % 2][:], in1=xt[:], op=ALU.add),
                             reads=[btmpE[j % 2], bxt], writes=[bxt])
                        if not last:
                            T.dma("pool", xres[s, t0:t0 + 128, :], xt[:], bxt, reads=[bxt])
                        else:
                            rs, brs = rstd_from(xt[:], [bxt], D)
                            T.op("dve", lambda e: e.scalar_tensor_tensor(out=tmpE[j % 2][:], in0=xt[:], scalar=rs, in1=gf[:],
                                                                         op0=ALU.mult, op1=ALU.mult),
                                 reads=[bxt, brs, bgf], writes=[btmpE[j % 2]])
                            T.dma("pool", out_d[s, t0:t0 + 128, :], tmpE[j % 2][:], btmpE[j % 2], reads=[btmpE[j % 2]])
            T.barrier()

    tmpE = []
    btmpE = [Buf("tmpE0"), Buf("tmpE1")]

    tmpE.append(salloc(ges, "tmpE0", [128, D], F32))
    tmpE.append(salloc(ges, "tmpE1", [128, D], F32))

    convert_weights()
    esA0 = ExitStack()
    Wf0 = salloc(esA0, "Wf0", [128, 8, 2688], BF16); bWf0 = Buf("Wf0")
    Wt0 = salloc(esA0, "Wt0", [128, 8, 648], BF16); bWt0 = Buf("Wt0")
    preA = (Wf0, bWf0, Wt0, bWt0)
    prologue(preA)
    for l in range(nlayers):
        if "A" in phases:
            phase_A(l, pre=preA if l == 0 else None)
        if l == 0:
            esA0.close()
        if "B" in phases:
            phase_B(l)
        if "C" in phases:
            phase_C(l)
        if "D" in phases and "E" in phases:
            phase_DE(l, last=(l == nlayers - 1))
        elif "D" in phases:
            phase_D(l)
    T.barrier()
    ges.close()
    return nc, T


def _t5_bucket(rel):
    nb = 16
    max_exact = 8
    base = np.where(rel > 0, nb, 0)
    n = np.abs(rel)
    nf = np.maximum(n, max_exact).astype(np.float32)
    large = max_exact + (np.log(nf / np.float32(max_exact)) / np.float32(math.log(128 / max_exact))
                         * np.float32(nb - max_exact)).astype(np.int32)
    large = np.minimum(large, nb - 1)
    return base + np.where(n < max_exact, n, large)


def _consts():
    p = np.arange(128)
    c = {}
    c["c_ident"] = np.eye(128, dtype=np.float32)
    c["c_tri8"] = np.where(p[:, None] >= p[None, :], -8.0, 0.0).astype(np.float32)
    c["c_neg8"] = np.full((128, 128), -8.0, np.float32)
    cm = (p[:, None] < p[None, :]).astype(np.float32)
    c["c_cmask"] = np.ascontiguousarray(np.broadcast_to(cm[:, None, :], (128, 8, 128)))
    adm = ((p[:, None] // 64) <= (p[None, :] // 64)).astype(np.float32)
    c["c_adm"] = np.ascontiguousarray(np.broadcast_to(adm[:, None, :], (128, 8, 128)))
    c["c_admneg"] = np.ascontiguousarray(np.broadcast_to(np.where(adm > 0, 0.0, -1.0e5).astype(np.float32)[:, None, :], (128, 8, 128)))
    c["c_pow2"] = np.ascontiguousarray(np.broadcast_to(
        (0.5 ** np.arange(1, KBIS + 1)).astype(np.float32)[None, :], (128, KBIS)))
    c["c_ones"] = np.ones((128, 128), np.float32)
    return c


def _prep_inputs(inp, core):
    f = np.float32
    b0 = core * NSEQ
    bs = slice(b0, b0 + NSEQ)
    m = {}
    m["x"] = np.ascontiguousarray(inp["x"][bs], dtype=f)
    c = np.asarray(inp["c"], dtype=f)[bs]
    m["cT"] = np.ascontiguousarray(c.reshape(NSEQ, 8, 128).transpose(2, 1, 0))
    m["w_mod"] = np.ascontiguousarray(inp["w_mod"], dtype=f)
    bm = np.asarray(inp["b_mod"], dtype=f).reshape(2, 48, 128).transpose(2, 0, 1)
    m["b_modT"] = np.ascontiguousarray(np.broadcast_to(bm[..., None], (128, 2, 48, NSEQ)))
    ga = np.asarray(inp["g_attn"], dtype=f).reshape(2, 8, 128).transpose(2, 0, 1)
    m["g_attnT"] = np.ascontiguousarray(np.broadcast_to(ga[..., None], (128, 2, 8, NSEQ)))
    gm = np.asarray(inp["g_mlp"], dtype=f).reshape(2, 8, 128).transpose(2, 0, 1)
    m["g_mlpT"] = np.ascontiguousarray(np.broadcast_to(gm[..., None], (128, 2, 8, NSEQ)))
    m["w_in"] = np.ascontiguousarray(inp["w_in"], dtype=f)
    m["kvg_bc"] = np.ascontiguousarray(np.broadcast_to(np.asarray(inp["kv_norm_g"], dtype=f)[None], (128, 2, 128)))
    m["w_uv"] = np.ascontiguousarray(inp["w_uv"], dtype=f)
    m["goa_bc"] = np.ascontiguousarray(np.broadcast_to(np.asarray(inp["g_out_a"], dtype=f)[None], (128, 2, 512)))
    m["gob_bc"] = np.ascontiguousarray(np.broadcast_to(np.asarray(inp["g_out_b"], dtype=f)[None], (128, 2, 512)))
    m["w_out"] = np.ascontiguousarray(inp["w_out"], dtype=f)
    m["w_up"] = np.ascontiguousarray(inp["w_up"], dtype=f)
    m["w_down"] = np.ascontiguousarray(inp["w_down"], dtype=f)
    rb = np.asarray(inp["rel_bias"], dtype=f)
    p = np.arange(128)
    bn = np.empty((128, 2, 8, 128), f)
    for off in range(2):
        rel = (p[:, None] - off * 128) - p[None, :]
        bk = _t5_bucket(rel.astype(np.int32))
        bn[:, off] = rb[bk].transpose(0, 2, 1)
    m["biasn"] = bn
    far = rb[_t5_bucket(np.array([-1000], np.int32))[0]]
    m["bfar_bc"] = np.ascontiguousarray(np.broadcast_to(far[None, :, None], (128, 8, 128)))
    m["gfin_bc"] = np.ascontiguousarray(np.broadcast_to(np.asarray(inp["g_final"], dtype=f)[None], (128, D)))
    m.update(_consts())
    return m


_CACHE = {}


def kernel(**inputs):
    if "nc" not in _CACHE:
        _CACHE["nc"] = build_program()[0]
    nc = _CACHE["nc"]
    in_maps = [_prep_inputs(inputs, core) for core in range(8)]
    res = run_bass_kernel_spmd(nc, in_maps, core_ids=list(range(8)))
    out = np.concatenate([np.asarray(r["out"]) for r in res.results], axis=0)
    return out.astype(np.float32, copy=False)
```

```python
import math
from contextlib import ExitStack

import numpy as np
import concourse.bass as bass
import concourse.mybir as mybir
from concourse.bass_utils import run_bass_kernel_spmd

F32 = mybir.dt.float32
BF16 = mybir.dt.bfloat16
AF = mybir.ActivationFunctionType
ALU = mybir.AluOpType
AX = mybir.AxisListType

S = 2048
D = 1024
NSEQ = 2
NT = S // 128
DFF = 4096
DIN = 3272
EPS = 1e-6
KBIS = 16
TOPK = 256
EPOCH = 30000
LIM_SEQ = NSEQ
LIM_QB = NT
NO_POOL = False
ACC_DEPTH = 2
FLUSH_BG_BEFORE_C = False


class Buf:
    __slots__ = ("name", "last_w", "readers", "dsem", "bg")

    def __init__(self, name, bg=False):
        self.name = name
        self.last_w = None
        self.readers = []
        self.dsem = {}
        self.bg = bg


class Tracker:
    def __init__(self, nc):
        self.nc = nc
        self.eng = {"pe": nc.tensor, "act": nc.scalar, "dve": nc.vector,
                    "pool": nc.gpsimd, "sp": nc.sync}
        self.cnt = {e: 0 for e in self.eng}
        self.sems = {e: [] for e in self.eng}
        self.seen = {e: {} for e in self.eng}
        self.nsem = 0
        self.dma_bufs = []
        self.free_dsems = {"hw": [], "sw": []}
        self.nwaits = 0
        self.ninstr = 0

    def _newsem(self, name):
        self.nsem += 1
        return self.nc.alloc_semaphore(name=name)

    def _wait(self, e, tok):
        sem, val, src = tok
        key = id(sem)
        if self.seen[e].get(key, 0) >= val:
            return
        self.seen[e][key] = val
        self.eng[e].wait_ge(sem, val)
        self.nwaits += 1

    def _deps(self, e, reads, writes):
        for b in reads:
            if b.last_w is not None and not (b.last_w[2] == e and e == "pe"):
                self._wait(e, b.last_w)
        for b in writes:
            if b.last_w is not None and not (b.last_w[2] == e and e == "pe"):
                self._wait(e, b.last_w)
            for t in b.readers:
                if t[2] == e:
                    continue
                self._wait(e, t)

    def _commit(self, tok, reads, writes):
        for b in reads:
            b.readers.append(tok)
            if len(b.readers) > 12:
                d = {}
                for t in b.readers:
                    k = id(t[0])
                    if k not in d or d[k][1] < t[1]:
                        d[k] = t
                b.readers = list(d.values())
        for b in writes:
            b.last_w = tok
            b.readers = []

    def op(self, e, fn, reads=(), writes=()):
        self._deps(e, reads, writes)
        n = self.cnt[e]
        ep, v = divmod(n, EPOCH)
        while len(self.sems[e]) <= ep:
            self.sems[e].append(self._newsem(f"c_{e}_{len(self.sems[e])}"))
        sem = self.sems[e][ep]
        ins = fn(self.eng[e])
        ins.then_inc(sem, 1)
        self.cnt[e] = n + 1
        self.ninstr += 1
        tok = (sem, v + 1, e)
        self._commit(tok, reads, writes)
        return tok

    def dma(self, q, out, in_, sb, reads=(), writes=(), **kw):
        self._deps(q, reads, writes)
        kind = "sw" if q == "pool" else "hw"
        if kind not in sb.dsem:
            if self.free_dsems[kind]:
                sb.dsem[kind] = list(self.free_dsems[kind].pop())
            else:
                sb.dsem[kind] = [self._newsem(f"d{kind}_{sb.name}_{self.nsem}"), 0]
            self.dma_bufs.append((sb, kind))
        ent = sb.dsem[kind]
        ent[1] += 16
        ins = self.eng[q].dma_start(out=out, in_=in_, **kw)
        ins.then_inc(ent[0], 16)
        self.ninstr += 1
        tok = (ent[0], ent[1], "dma")
        self._commit(tok, reads, writes)
        return tok

    def barrier(self):
        toks = []
        for f in self.eng:
            n = self.cnt[f]
            if n == 0:
                continue
            ep, v = divmod(n - 1, EPOCH)
            toks.append((self.sems[f][ep], v + 1, f))
        for b, kind in self.dma_bufs:
            if not b.bg:
                toks.append((b.dsem[kind][0], b.dsem[kind][1], "dma"))
        for e in self.eng:
            for t in toks:
                if t[2] == e:
                    continue
                self._wait(e, t)
        keep = []
        for b, kind in self.dma_bufs:
            if b.bg:
                keep.append((b, kind))
            else:
                self.free_dsems[kind].append(tuple(b.dsem.pop(kind)))
        self.dma_bufs = keep


def build_program(nlayers=2, debug=False, phases="ABCDE"):
    nc = bass.Bass("TRN2", target_bir_lowering=False)
    T = Tracker(nc)
    uid = [0]

    def din(name, shape, dt=F32):
        return nc.dram_tensor(name, list(shape), dt, kind="ExternalInput").ap()

    def dscr(name, shape, dt):
        kind = "ExternalOutput" if debug else "Internal"
        return nc.dram_tensor(name, list(shape), dt, kind=kind).ap()

    x_in = din("x", [NSEQ, S, D])
    cT = din("cT", [128, 8, NSEQ])
    w_mod = din("w_mod", [2, D, 6 * D])
    b_modT = din("b_modT", [128, 2, 48, NSEQ])
    g_attnT = din("g_attnT", [128, 2, 8, NSEQ])
    g_mlpT = din("g_mlpT", [128, 2, 8, NSEQ])
    w_in = din("w_in", [2, D, DIN])
    kvg_bc = din("kvg_bc", [128, 2, 128])
    w_uv = din("w_uv", [2, 8, 128, 64])
    goa_bc = din("goa_bc", [128, 2, 512])
    gob_bc = din("gob_bc", [128, 2, 512])
    w_out = din("w_out", [2, D, D])
    w_up = din("w_up", [2, D, DFF])
    w_down = din("w_down", [2, DFF, D])
    biasn = din("biasn", [128, 2, 8, 128])
    bfar_bc = din("bfar_bc", [128, 8, 128])
    gfin_bc = din("gfin_bc", [128, D])
    c_ident = din("c_ident", [128, 128])
    c_tri8 = din("c_tri8", [128, 128])
    c_neg8 = din("c_neg8", [128, 128])
    c_cmask = din("c_cmask", [128, 8, 128])
    c_adm = din("c_adm", [128, 8, 128])
    c_admneg = din("c_admneg", [128, 8, 128])
    c_pow2 = din("c_pow2", [128, KBIS])
    c_ones = din("c_ones", [128, 128])
    out_d = nc.dram_tensor("out", [NSEQ, S, D], F32, kind="ExternalOutput").ap()

    xres = dscr("xres", [NSEQ, S, D], F32)
    featT = dscr("featT", [NSEQ, 21, 128, S], BF16)
    v_scr = dscr("v_scr", [NSEQ, S, 512], BF16)
    kv_tok = dscr("kv_tok", [NSEQ, S, 128], BF16)
    kvT_scr = dscr("kvT_scr", [NSEQ, 128, S], BF16)
    widx = dscr("widx", [NSEQ, S, 8], F32)
    ocat = dscr("ocat", [NSEQ, S, D], BF16)

    wb_in = [nc.dram_tensor(f"wb_in{l}", [D, DIN], BF16, kind="Internal").ap() for l in range(2)]
    wb_out = [nc.dram_tensor(f"wb_out{l}", [D, D], BF16, kind="Internal").ap() for l in range(2)]
    wb_up = [nc.dram_tensor(f"wb_up{l}", [D, DFF], BF16, kind="Internal").ap() for l in range(2)]
    wb_dn = [nc.dram_tensor(f"wb_dn{l}", [DFF, D], BF16, kind="Internal").ap() for l in range(2)]
    B_wb_in = [Buf(f"wb_in{l}", bg=True) for l in range(2)]
    B_wb_out = [Buf(f"wb_out{l}", bg=True) for l in range(2)]
    B_wb_up = [Buf(f"wb_up{l}", bg=True) for l in range(2)]
    B_wb_dn = [Buf(f"wb_dn{l}", bg=True) for l in range(2)]

    bgq = []

    def convert_weights():
        for l in range(nlayers):
            for (dst, src, bb, rows, step) in ((wb_in[l], w_in[l], B_wb_in[l], D, 256), (wb_out[l], w_out[l], B_wb_out[l], D, 512),
                                               (wb_up[l], w_up[l], B_wb_up[l], D, 256), (wb_dn[l], w_down[l], B_wb_dn[l], DFF, 1024)):
                for r0 in range(0, rows, step):
                    f = (lambda dst=dst, src=src, bb=bb, r0=r0, step=step:
                         T.dma("pool", dst[r0:r0 + step, :], src[r0:r0 + step, :], bb, writes=[bb]))
                    if l == 0 and dst is wb_in[0]:
                        f()
                    else:
                        bgq.append((l, f))

    def bg_step(n=1):
        for _ in range(n):
            if bgq:
                bgq.pop(0)[1]()

    def bg_flush(layer):
        while bgq and bgq[0][0] <= layer:
            bgq.pop(0)[1]()

    def salloc(es, name, shape, dt):
        uid[0] += 1
        return es.enter_context(nc.sbuf_tensor(f"{name}_{uid[0]}", list(shape), dt))

    ges = ExitStack()
    pb2 = [ges.enter_context(nc.psum_tensor(f"pbp{i}", [128, 1024], F32)) for i in range(4)]
    pb = [pb2[i // 2][:, (i % 2) * 512:(i % 2 + 1) * 512] for i in range(8)]
    PB = [Buf(f"pb{i}") for i in range(8)]

    def pbf(i):
        return pb[i].bitcast(BF16)

    ident_f = salloc(ges, "identf", [128, 128], F32); b_identf = Buf("identf")
    ident_b = salloc(ges, "identb", [128, 128], BF16); b_identb = Buf("identb")
    ones_f = salloc(ges, "onesf", [128, 128], F32); b_onesf = Buf("onesf")
    ones_b = salloc(ges, "onesb", [128, 128], BF16); b_onesb = Buf("onesb")
    modT = salloc(ges, "modT", [128, 2, 48, NSEQ], F32); b_modT_ = Buf("modT")
    gm1T = salloc(ges, "gm1T", [128, 2, 8, NSEQ], F32); b_gm1 = Buf("gm1T")
    gm2T = salloc(ges, "gm2T", [128, 2, 8, NSEQ], F32); b_gm2 = Buf("gm2T")
    stat = salloc(ges, "stat", [128, 8, 4], F32)
    STB = [Buf(f"stat{i}") for i in range(8)]
    junk = salloc(ges, "junk", [128, 2048], BF16); b_junk = Buf("junk")
    stat_i = [0]

    T.dma("sp", ident_f[:], c_ident, b_identf, writes=[b_identf])
    T.dma("pool", ident_b[:], c_ident, b_identb, writes=[b_identb])
    T.dma("sp", ones_f[:], c_ones, b_onesf, writes=[b_onesf])
    T.dma("pool", ones_b[:], c_ones, b_onesb, writes=[b_onesb])

    def prologue(preA=None):
        with ExitStack() as es:
            sT = salloc(es, "sT", [128, 8, NSEQ], F32); b_sT = Buf("sT")
            cTs = salloc(es, "cTs", [128, 8, NSEQ], F32); b_cTs = Buf("cTs")
            bm = salloc(es, "bm", [128, 2, 48, NSEQ], F32); b_bm = Buf("bm")
            ga = salloc(es, "ga", [128, 2, 8, NSEQ], F32); b_ga = Buf("ga")
            gmm = salloc(es, "gmm", [128, 2, 8, NSEQ], F32); b_gmm = Buf("gmm")
            NW = 4
            wms = [salloc(es, f"wm{i}", [128, 8, 512], F32) for i in range(NW)]
            b_wms = [Buf(f"wm{i}") for i in range(NW)]
            modrow = salloc(es, "modrow", [NSEQ, 6 * D], F32); b_mrow = Buf("modrow")
            T.dma("sp", cTs[:], cT, b_cTs, writes=[b_cTs])
            T.dma("sp", bm[:], b_modT, b_bm, writes=[b_bm])
            T.dma("sp", ga[:], g_attnT, b_ga, writes=[b_ga])
            T.dma("sp", gmm[:], g_mlpT, b_gmm, writes=[b_gmm])
            T.op("act", lambda e: e.activation(out=sT[:], in_=cTs[:], func=AF.Silu),
                 reads=[b_cTs], writes=[b_sT])
            it = 0
            for l in range(nlayers):
                wl = w_mod[l].rearrange("(c p) n -> p c n", p=128)
                for ns in range(12):
                    sl = it % NW
                    bank = it % 2
                    it += 1
                    T.dma("sp", wms[sl][:], wl[:, :, ns * 512:(ns + 1) * 512], b_wms[sl],
                          writes=[b_wms[sl]])
                    for c in range(8):
                        T.op("pe", lambda e: e.matmul(
                            pb[bank][0:NSEQ, 0:512], lhsT=sT[:, c, :], rhs=wms[sl][:, c, :],
                            start=(c == 0), stop=(c == 7)),
                            reads=[b_wms[sl], b_sT], writes=[PB[bank]])
                    T.op("act", lambda e: e.activation(out=modrow[:, ns * 512:(ns + 1) * 512],
                                                       in_=pb[bank][0:NSEQ, 0:512], func=AF.Copy),
                         reads=[PB[bank]], writes=[b_mrow])
                for j in range(48):
                    T.op("pe", lambda e: e.transpose(pb[2][:, j * NSEQ:(j + 1) * NSEQ],
                                                     modrow[0:NSEQ, j * 128:(j + 1) * 128],
                                                     ident_f[0:NSEQ, 0:NSEQ]),
                         reads=[b_mrow, b_identf], writes=[PB[2]])
                T.op("dve", lambda e: e.tensor_tensor(
                    out=modT[:, l, :, :],
                    in0=pb[2][:, 0:48 * NSEQ].rearrange("p (j b) -> p j b", b=NSEQ),
                    in1=bm[:, l, :, :], op=ALU.add),
                    reads=[PB[2], b_bm], writes=[b_modT_])
                T.op("dve", lambda e: e.scalar_tensor_tensor(
                    out=gm1T[:, l], in0=modT[:, l, 8:16, :], scalar=1.0, in1=ga[:, l],
                    op0=ALU.add, op1=ALU.mult), reads=[b_modT_, b_ga], writes=[b_gm1])
                T.op("dve", lambda e: e.scalar_tensor_tensor(
                    out=gm2T[:, l], in0=modT[:, l, 32:40, :], scalar=1.0, in1=gmm[:, l],
                    op0=ALU.add, op1=ALU.mult), reads=[b_modT_, b_gmm], writes=[b_gm2])
            if preA is not None:
                load_A_weights(0, *preA)
            T.barrier()

    def next_stat():
        i = stat_i[0] % 8
        stat_i[0] += 1
        return stat[:, i, :], STB[i]

    def rstd_from(src_ap, src_bufs, n, from_psum=False):
        st, sbuf_ = next_stat()
        T.op("act", lambda e: e.activation(out=junk[:, 0:n], in_=src_ap, func=AF.Square,
                                           accum_out=st[:, 0:1]),
             reads=src_bufs, writes=[b_junk, sbuf_])
        T.op("dve", lambda e: e.tensor_scalar(out=st[:, 1:2], in0=st[:, 0:1], scalar1=1.0 / n,
                                              scalar2=EPS, op0=ALU.mult, op1=ALU.add),
             reads=[sbuf_], writes=[sbuf_])
        T.op("act", lambda e: e.activation(out=st[:, 2:3], in_=st[:, 1:2], func=AF.Sqrt),
             reads=[sbuf_], writes=[sbuf_])
        T.op("dve", lambda e: e.reciprocal(out=st[:, 3:4], in_=st[:, 2:3]),
             reads=[sbuf_], writes=[sbuf_])
        return st[:, 3:4], sbuf_

    def norm_to_T(xt_ap, bx, gmT_ap, shT_ap, bmods, xn, bxn, tbank, hT_dst, bhT):
        rstd, brs = rstd_from(xt_ap, [bx], D)
        T.op("dve", lambda e: e.tensor_scalar(out=xn[:], in0=xt_ap, scalar1=rstd, scalar2=None,
                                              op0=ALU.mult), reads=[bx, brs], writes=[bxn])
        pv = pbf(tbank).rearrange("p (c t) -> p c t", c=8)
        for c in range(8):
            T.op("pe", lambda e: e.transpose(pv[:, c, :], xn[:, c * 128:(c + 1) * 128], ident_b[:]),
                 reads=[bxn, b_identb], writes=[PB[tbank]])
        for c in range(8):
            if c % 2 == 0:
                T.op("act", lambda e: e.activation(out=hT_dst(c), in_=pv[:, c, :], func=AF.Identity,
                                                   scale=gmT_ap(c), bias=shT_ap(c)),
                     reads=[PB[tbank]] + bmods, writes=[bhT])
            else:
                T.op("dve", lambda e: e.tensor_scalar(out=hT_dst(c), in0=pv[:, c, :],
                                                      scalar1=gmT_ap(c), scalar2=shT_ap(c),
                                                      op0=ALU.mult, op1=ALU.add),
                     reads=[PB[tbank]] + bmods, writes=[bhT])

    def ga_bcast(es, l, chunk0, name):
        gb = salloc(es, name, [128, NSEQ, D], F32); bgb = Buf(name)
        dg = salloc(es, name + "dg", [128, 2, 128], F32); bdg = [Buf(name + "dg0"), Buf(name + "dg1")]
        k = 0
        for b in range(NSEQ):
            for half in range(2):
                bank = 6 + half
                for cc in range(4):
                    c = half * 4 + cc
                    sl = k % 2
                    k += 1
                    T.op("dve", lambda e: e.tensor_scalar(
                        out=dg[:, sl, :], in0=ident_f[:], scalar1=modT[:, l, chunk0 + c, b:b + 1],
                        scalar2=None, op0=ALU.mult), reads=[b_identf, b_modT_], writes=[bdg[sl]])
                    T.op("pe", lambda e: e.matmul(pb[bank][:, cc * 128:(cc + 1) * 128], lhsT=ones_f[:],
                                                  rhs=dg[:, sl, :], start=(cc == 0), stop=True,
                                                  skip_group_check=True),
                         reads=[b_onesf, bdg[sl]], writes=[PB[bank]])
                T.op("act", lambda e: e.activation(out=gb[:, b, half * 512:(half + 1) * 512],
                                                   in_=pb[bank][:], func=AF.Copy),
                     reads=[PB[bank]], writes=[bgb])
        return gb, bgb

    def load_A_weights(l, Wf, bWf, Wt, bWt):
        wl = wb_in[l].rearrange("(c p) n -> p c n", p=128)
        for (a, b, o) in [(1024, 1536, 0), (2560, 2688, 512), (3264, 3272, 640)]:
            T.dma("sp", Wt[:, :, o:o + (b - a)], wl[:, :, a:b], bWt, reads=[B_wb_in[l]], writes=[bWt])
        for (a, b, o, sg) in [(0, 1024, 0, 0), (1536, 2560, 1024, 1), (2688, 3200, 2048, 2),
                              (3200, 3264, 2560, 3), (3200, 3264, 2624, 3)]:
            for c in range(8):
                T.dma("sp", Wf[:, c, o:o + (b - a)], wl[:, c, a:b], bWf[sg], reads=[B_wb_in[l]], writes=[bWf[sg]])

    def phase_A(l, pre=None):
        with ExitStack() as es:
            if pre is None:
                bg_flush(l)
                Wf = salloc(es, "Wf", [128, 8, 2688], BF16); bWf = [Buf(f"Wf{i}") for i in range(4)]
                Wt = salloc(es, "Wt", [128, 8, 648], BF16); bWt = Buf("Wt")
                load_A_weights(l, Wf, bWf, Wt, bWt)
            else:
                Wf, bWf, Wt, bWt = pre
            kvg = salloc(es, "kvg", [128, 128], F32); bkvg = Buf("kvg")
            T.dma("sp", kvg[:], kvg_bc[:, l, :], bkvg, writes=[bkvg])
            xts = [salloc(es, f"xt{i}", [128, D], F32) for i in range(3)]
            bxts = [Buf(f"xt{i}") for i in range(3)]
            xns = [salloc(es, f"xn{i}", [128, D], BF16) for i in range(2)]
            bxns = [Buf(f"xn{i}") for i in range(2)]
            hTs = [salloc(es, f"hT{i}", [128, 8, 512], BF16) for i in range(2)]
            bhTs = [Buf(f"hT{i}") for i in range(2)]
            vts = [salloc(es, f"vt{i}", [128, 512], BF16) for i in range(2)]
            bvts = [Buf(f"vt{i}") for i in range(2)]
            kvn = [salloc(es, f"kvn{i}", [128, 128], BF16) for i in range(2)]
            bkvn = [Buf(f"kvn{i}") for i in range(2)]
            kvTt = [salloc(es, f"kvTt{i}", [128, 128], BF16) for i in range(2)]
            bkvTt = [Buf(f"kvTt{i}") for i in range(2)]
            wis = [salloc(es, f"wis{i}", [128, 8], F32) for i in range(2)]
            bwis = [Buf(f"wis{i}") for i in range(2)]
            fos = [salloc(es, f"fo{i}", [128, 512], BF16) for i in range(3)]
            bfos = [Buf(f"fo{i}") for i in range(3)]
            src = x_in if l == 0 else xres
            cnt = {"ti": 0, "fi": 0}
            groups = [(s, g) for s in range(NSEQ) for g in range(4)]

            def prep_a(gi, j):
                s, g = groups[gi]
                hT = hTs[gi % 2]; bhT = bhTs[gi % 2]
                tt = g * 4 + j
                t0 = tt * 128
                ti = cnt["ti"]
                cnt["ti"] += 1
                xt = xts[ti % 3]; bxt = bxts[ti % 3]
                xn = xns[ti % 2]; bxn = bxns[ti % 2]
                T.dma("sp", xt[:], src[s, t0:t0 + 128, :], bxt, writes=[bxt])
                norm_to_T(xt[:], bxt,
                          lambda c: gm1T[:, l, c, s:s + 1],
                          lambda c: modT[:, l, 0 + c, s:s + 1],
                          [b_gm1, b_modT_], xn, bxn, ti % 2,
                          lambda c: hT[:, c, j * 128:(j + 1) * 128], bhT)
                return ti

            def prep_b(gi, j, ti):
                s, g = groups[gi]
                hT = hTs[gi % 2]; bhT = bhTs[gi % 2]
                t0 = (g * 4 + j) * 128
                k2 = ti % 2
                for c in range(8):
                    T.op("pe", lambda e: e.matmul(pb[2][:, 0:512], lhsT=hT[:, c, j * 128:(j + 1) * 128],
                                                  rhs=Wt[:, c, 0:512], start=(c == 0), stop=(c == 7)),
                         reads=[bhT, bWt], writes=[PB[2]])
                for c in range(8):
                    T.op("pe", lambda e: e.matmul(pb[3][:, 0:136], lhsT=hT[:, c, j * 128:(j + 1) * 128],
                                                  rhs=Wt[:, c, 512:648], start=(c == 0), stop=(c == 7)),
                         reads=[bhT, bWt], writes=[PB[3]])
                T.op("act", lambda e: e.activation(out=vts[k2][:], in_=pb[2][:, 0:512], func=AF.Copy),
                     reads=[PB[2]], writes=[bvts[k2]])
                T.dma("pool", v_scr[s, t0:t0 + 128, :], vts[k2][:], bvts[k2], reads=[bvts[k2]])
                rs2, brs2 = rstd_from(pb[3][:, 0:128], [PB[3]], 128)
                T.op("dve", lambda e: e.scalar_tensor_tensor(
                    out=kvn[k2][:], in0=pb[3][:, 0:128], scalar=rs2, in1=kvg[:],
                    op0=ALU.mult, op1=ALU.mult), reads=[PB[3], brs2, bkvg], writes=[bkvn[k2]])
                T.op("dve", lambda e: e.tensor_copy(out=wis[k2][:], in_=pb[3][:, 128:136]),
                     reads=[PB[3]], writes=[bwis[k2]])
                T.dma("pool", kv_tok[s, t0:t0 + 128, :], kvn[k2][:], bkvn[k2], reads=[bkvn[k2]])
                T.dma("pool", widx[s, t0:t0 + 128, :], wis[k2][:], bwis[k2], reads=[bwis[k2]])
                T.op("pe", lambda e: e.transpose(pbf(4)[:, 0:128], kvn[k2][:], ident_b[:]),
                     reads=[bkvn[k2], b_identb], writes=[PB[4]])
                T.op("act", lambda e: e.activation(out=kvTt[k2][:], in_=pbf(4)[:, 0:128], func=AF.Copy),
                     reads=[PB[4]], writes=[bkvTt[k2]])
                T.dma("pool", kvT_scr[s, :, t0:t0 + 128], kvTt[k2][:], bkvTt[k2], reads=[bkvTt[k2]])

            def fm_chunk(gi, ch):
                s, g = groups[gi]
                hT = hTs[gi % 2]; bhT = bhTs[gi % 2]
                fi = cnt["fi"]
                cnt["fi"] += 1
                bank = 5 + (fi % 3)
                fo = fos[fi % 3]; bfo = bfos[fi % 3]
                for c in range(8):
                    T.op("pe", lambda e: e.matmul(pb[bank][:, 0:512], lhsT=Wf[:, c, ch * 128:(ch + 1) * 128],
                                                  rhs=hT[:, c, :], start=(c == 0), stop=(c == 7)),
                         reads=[bhT, bWf[0 if ch < 8 else (1 if ch < 16 else (2 if ch < 20 else 3))]], writes=[PB[bank]])
                if fi % 2 == 0:
                    T.op("act", lambda e: e.activation(out=fo[:], in_=pb[bank][:, 0:512], func=AF.Copy),
                         reads=[PB[bank]], writes=[bfo])
                else:
                    T.op("dve", lambda e: e.tensor_copy(out=fo[:], in_=pb[bank][:, 0:512]),
                         reads=[PB[bank]], writes=[bfo])
                T.dma("pool", featT[s, ch, :, g * 512:(g + 1) * 512], fo[:], bfo, reads=[bfo])

            for j in range(4):
                ti0 = prep_a(0, j)
                prep_b(0, j, ti0)
            for gi in range(len(groups)):
                pend_b = None
                for ch in range(21):
                    fm_chunk(gi, ch)
                    if gi + 1 < len(groups):
                        if ch in (1, 6, 11, 16):
                            j = (1, 6, 11, 16).index(ch)
                            pend_b = (j, prep_a(gi + 1, j))
                        if ch in (4, 9, 14, 19) and pend_b is not None:
                            prep_b(gi + 1, pend_b[0], pend_b[1])
                            pend_b = None
            T.barrier()

    def phase_B(l):
        with ExitStack() as es:
            qT = salloc(es, "qT", [128, 4, S], BF16); bqT = Buf("qT")
            kT = salloc(es, "kT", [128, 4, S], BF16); bkT = Buf("kT")
            vv = salloc(es, "vv", [128, NT, 512], BF16); bvv = Buf("vv")
            tri8 = salloc(es, "tri8", [128, 128], BF16); btri = Buf("tri8")
            neg8 = salloc(es, "neg8", [128, 128], BF16); bneg = Buf("neg8")
            cm = salloc(es, "cm", [128, 512], BF16); bcm = Buf("cm")
            goa = salloc(es, "goa", [128, 512], F32); bgoa = Buf("goa")
            T.dma("pool", tri8[:], c_tri8, btri, writes=[btri])
            T.dma("pool", neg8[:], c_neg8, bneg, writes=[bneg])
            T.dma("pool", cm[:], c_cmask[:, 0:4, :].rearrange("p h t -> p (h t)"), bcm, writes=[bcm])
            T.dma("sp", goa[:], goa_bc[:, l, :], bgoa, writes=[bgoa])
            e32a = [salloc(es, f"e32a{i}", [128, 1024], F32) for i in range(2)]
            spba = [salloc(es, f"spba{i}", [128, 1024], BF16) for i in range(2)]
            wba = [salloc(es, f"wba{i}", [128, 1024], BF16) for i in range(2)]
            e32 = [[e32a[i][:, g * 512:(g + 1) * 512] for i in range(2)] for g in range(2)]
            spb = [[spba[i][:, g * 512:(g + 1) * 512] for i in range(2)] for g in range(2)]
            wb = [[wba[i][:, g * 512:(g + 1) * 512] for i in range(2)] for g in range(2)]
            be32 = [[Buf(f"e32{g}{i}") for i in range(2)] for g in range(2)]
            bspb = [[Buf(f"spb{g}{i}") for i in range(2)] for g in range(2)]
            bwb = [[Buf(f"wb{g}{i}") for i in range(2)] for g in range(2)]
            sps = [salloc(es, f"sps{g}", [128, 512], F32) for g in range(2)]
            bsps = [Buf(f"sps{g}") for g in range(2)]
            spsb = [[salloc(es, f"spsb{g}{i}", [128, 512], BF16) for i in range(2)] for g in range(2)]
            bspsb = [[Buf(f"spsb{g}{i}") for i in range(2)] for g in range(2)]
            oan = [salloc(es, f"oan{i}", [128, 512], BF16) for i in range(2)]
            boan = [Buf(f"oan{i}") for i in range(2)]
            ZB = [[0, 2], [1, 3]]
            AB = [4, 5]
            OB = [6, 7]
            carry_slot = [0, 0]

            def zmm(bank, hg, qb, kb, start_first):
                for i in range(4):
                    h = 2 * i + hg
                    ch = h // 2
                    r0 = (h % 2) * 64
                    T.op("pe", lambda e: e.matmul(
                        pb[bank][:, i * 128:(i + 1) * 128],
                        lhsT=kT[r0:r0 + 64, ch, kb * 128:(kb + 1) * 128],
                        rhs=qT[r0:r0 + 64, ch, qb * 128:(qb + 1) * 128],
                        start=(start_first and i == 0), stop=(i == 3),
                        skip_group_check=True),
                        reads=[bkT, bqT], writes=[PB[bank]])

            def stage_Z(n, qb, kb):
                sl = n % 2
                for hg in range(2):
                    zmm(ZB[hg][sl], hg, qb, kb, True)
                T.op("act", lambda e: e.activation(out=e32a[sl][:], in_=pb2[sl][:], func=AF.Exp, scale=0.125),
                     reads=[PB[ZB[0][sl]], PB[ZB[1][sl]]], writes=[be32[0][sl], be32[1][sl]])
                T.op("act", lambda e: e.activation(out=spba[sl][:], in_=e32a[sl][:], func=AF.Ln, bias=1.0),
                     reads=[be32[0][sl], be32[1][sl]], writes=[bspb[0][sl], bspb[1][sl]])
                for hg in range(2):
                    if kb == qb:
                        T.op("dve", lambda e: e.tensor_tensor(out=spb[hg][sl][:], in0=spb[hg][sl][:], in1=cm[:], op=ALU.mult),
                             reads=[bspb[hg][sl], bcm], writes=[bspb[hg][sl]])

            def stage_A(n, qb, kb):
                sl = n % 2
                for hg in range(2):
                    ab = AB[hg]
                    T.op("pe", lambda e: e.matmul(pb[ab][:], lhsT=tri8[:], rhs=spb[hg][sl][:], start=True, stop=False,
                                                  skip_group_check=True),
                         reads=[btri, bspb[hg][sl]], writes=[PB[ab]])
                    if kb < qb:
                        cs = carry_slot[hg]
                        T.op("pe", lambda e: e.matmul(pb[ab][:], lhsT=neg8[:], rhs=spsb[hg][cs][:], start=False, stop=False,
                                                      skip_group_check=True),
                             reads=[bneg, bspsb[hg][cs]], writes=[PB[ab]])
                for hg in range(2):
                    zmm(AB[hg], hg, qb, kb, False)
                T.op("act", lambda e: e.activation(out=wba[sl][:], in_=pb2[2][:], func=AF.Exp, scale=0.125),
                     reads=[PB[AB[0]], PB[AB[1]]], writes=[bwb[0][sl], bwb[1][sl]])
                for hg in range(2):
                    ab = AB[hg]
                    if kb == qb:
                        T.op("dve", lambda e: e.tensor_tensor(out=wb[hg][sl][:], in0=wb[hg][sl][:], in1=cm[:], op=ALU.mult),
                             reads=[bwb[hg][sl], bcm], writes=[bwb[hg][sl]])
                    if kb > 0:
                        if kb == qb:
                            T.op("dve", lambda e: e.tensor_copy(out=sps[hg][:], in_=spb[hg][sl][:]),
                                 reads=[bspb[hg][sl]], writes=[bsps[hg]])
                        else:
                            T.op("dve", lambda e: e.tensor_tensor(out=sps[hg][:], in0=sps[hg][:], in1=spb[hg][sl][:], op=ALU.add),
                                 reads=[bsps[hg], bspb[hg][sl]], writes=[bsps[hg]])
                        carry_slot[hg] ^= 1
                        cs = carry_slot[hg]
                        T.op("dve", lambda e: e.tensor_copy(out=spsb[hg][cs][:], in_=sps[hg][:]),
                             reads=[bsps[hg]], writes=[bspsb[hg][cs]])

            def stage_PV(n, s, qb, kb):
                sl = n % 2
                ob = OB[qb % 2]
                for hg in range(2):
                    for i in range(4):
                        h = 2 * i + hg
                        T.op("pe", lambda e: e.matmul(
                            pb[ob][:, h * 64:(h + 1) * 64], lhsT=wb[hg][sl][:, i * 128:(i + 1) * 128],
                            rhs=vv[:, kb, h * 64:(h + 1) * 64], start=(kb == qb and hg == 0 and i == 0), stop=False,
                            skip_group_check=True),
                            reads=[bwb[hg][sl], bvv], writes=[PB[ob]])
                if kb == 0:
                    osl = qb % 2
                    rs, brs = rstd_from(pb[ob][:], [PB[ob]], 512)
                    T.op("dve", lambda e: e.scalar_tensor_tensor(out=oan[osl][:], in0=pb[ob][:], scalar=rs, in1=goa[:],
                                                                 op0=ALU.mult, op1=ALU.mult),
                         reads=[PB[ob], brs, bgoa], writes=[boan[osl]])
                    T.dma("pool", ocat[s, qb * 128:(qb + 1) * 128, 0:512], oan[osl][:], boan[osl], reads=[boan[osl]])
                    bg_step(1)

            for s in range(LIM_SEQ):
                T.dma("sp", qT[:], featT[s, 0:4].rearrange("c p t -> p c t"), bqT, writes=[bqT])
                T.dma("sp", kT[:], featT[s, 4:8].rearrange("c p t -> p c t"), bkT, writes=[bkT])
                T.dma("sp", vv[:], v_scr[s].rearrange("(n p) f -> p n f", p=128), bvv, writes=[bvv])
                its = [(qb, kb) for qb in range(LIM_QB) for kb in range(qb, -1, -1)]
                N = len(its)
                for t in range(N + 2):
                    if t < N:
                        stage_Z(t, *its[t])
                    if 0 <= t - 1 < N:
                        stage_A(t - 1, *its[t - 1])
                    if 0 <= t - 2 < N:
                        stage_PV(t - 2, s, *its[t - 2])
            T.barrier()

    def phase_C(l):
        if FLUSH_BG_BEFORE_C:
            bg_flush(99)
        with ExitStack() as es:
            cin = []
            for s_ in range(NSEQ):
                d_ = dict(
                    dqT=salloc(es, f"dqT{s_}", [128, 8, S], BF16), bdq=Buf(f"dqT{s_}"),
                    iqT=salloc(es, f"iqT{s_}", [128, 4, S], BF16), biq=Buf(f"iqT{s_}"),
                    ikT=salloc(es, f"ikT{s_}", [128, S], BF16), bik=Buf(f"ikT{s_}"),
                    kvT=salloc(es, f"kvT{s_}", [128, S], BF16), bkvT=Buf(f"kvT{s_}"),
                    kvt=salloc(es, f"kvt{s_}", [128, NT, 128], BF16), bkvt=Buf(f"kvt{s_}"),
                    wi=salloc(es, f"wi{s_}", [128, NT, 8], F32), bwi=Buf(f"wi{s_}"))
                cin.append(d_)
            cur = {}

            def load_seq_C(s_, gate=()):
                d_ = cin[s_]
                g = list(gate)
                T.dma("sp", d_["iqT"][:], featT[s_, 16:20].rearrange("c p t -> p c t"), d_["biq"], reads=g, writes=[d_["biq"]])
                T.dma("sp", d_["ikT"][:], featT[s_, 20], d_["bik"], reads=g, writes=[d_["bik"]])
                T.dma("sp", d_["wi"][:], widx[s_].rearrange("(n p) j -> p n j", p=128), d_["bwi"], reads=g, writes=[d_["bwi"]])
                T.dma("sp", d_["kvT"][:], kvT_scr[s_], d_["bkvT"], reads=g, writes=[d_["bkvT"]])
                T.dma("sp", d_["kvt"][:], kv_tok[s_].rearrange("(n p) r -> p n r", p=128), d_["bkvt"], reads=g, writes=[d_["bkvt"]])
                T.dma("sp", d_["dqT"][:], featT[s_, 8:16].rearrange("c p t -> p c t"), d_["bdq"], reads=g, writes=[d_["bdq"]])
            wuv = salloc(es, "wuv", [128, 8, 64], BF16); bwuv = Buf("wuv")
            gob = salloc(es, "gob", [128, 512], F32); bgob = Buf("gob")
            BN = salloc(es, "BN", [128, 2, 1024], BF16); bBN = Buf("BN")
            id4 = salloc(es, "id4", [128, 512], BF16); bid4 = Buf("id4")
            pw2 = salloc(es, "pw2", [128, KBIS], F32); bpw2 = Buf("pw2")
            T.dma("pool", wuv[:], w_uv[l].rearrange("h r d -> r h d"), bwuv, writes=[bwuv])
            T.dma("sp", gob[:], gob_bc[:, l, :], bgob, writes=[bgob])
            T.dma("sp", pw2[:], c_pow2, bpw2, writes=[bpw2])
            for i in range(4):
                T.dma("pool", id4[:, i * 128:(i + 1) * 128], c_ident, bid4, writes=[bid4])
            score = [salloc(es, f"score{i}", [128, S], F32) for i in range(2)]
            bsc = [Buf(f"score{i}") for i in range(2)]
            nmask = [salloc(es, f"nmask{i}", [128, S], BF16) for i in range(2)]
            bnm = [Buf(f"nmask{i}") for i in range(2)]
            PP = [salloc(es, f"PP{i}", [128, 1024], BF16) for i in range(2)]
            bPP = [Buf(f"PP{i}") for i in range(2)]
            oTs = salloc(es, "oTs", [128, 1024], BF16); boTs = Buf("oTs")
            bis = salloc(es, "bis", [128, 8 + 2 * KBIS], F32); bbis = Buf("bis")
            rden = salloc(es, "rden", [128, 8], F32); brden = Buf("rden")
            obf = salloc(es, "obf", [128, 512], F32); bobf = Buf("obf")
            obn = [salloc(es, f"obn{i}", [128, 512], BF16) for i in range(2)]
            bobn = [Buf(f"obn{i}") for i in range(2)]
            lsc = 128 ** -0.5
            isc = (64 ** -0.5) * (8 ** -0.5)
            with ExitStack() as es2:
                bn = salloc(es2, "bn", [128, 2, 1024], F32); bbn = Buf("bn")
                bf = salloc(es2, "bf", [128, 1024], F32); bbf = Buf("bf")
                adn = salloc(es2, "adn", [128, 1024], F32); badn = Buf("adn")
                T.dma("sp", bn[:], biasn.rearrange("p o h t -> p o (h t)"), bbn, writes=[bbn])
                T.dma("sp", bf[:], bfar_bc.rearrange("p h t -> p (h t)"), bbf, writes=[bbf])
                T.dma("sp", adn[:], c_admneg.rearrange("p h t -> p (h t)"), badn, writes=[badn])
                for o in range(2):
                    T.op("dve", lambda e: e.tensor_tensor(out=bn[:, o, :], in0=bn[:, o, :], in1=bf[:], op=ALU.subtract),
                         reads=[bbn, bbf], writes=[bbn])
                    if o == 0:
                        T.op("dve", lambda e: e.scalar_tensor_tensor(out=BN[:, o, :], in0=bn[:, o, :], scalar=1.0 / lsc, in1=adn[:],
                                                                     op0=ALU.mult, op1=ALU.add),
                             reads=[bbn, badn], writes=[bBN])
                    else:
                        T.op("dve", lambda e: e.tensor_scalar(out=BN[:, o, :], in0=bn[:, o, :], scalar1=1.0 / lsc, scalar2=None,
                                                              op0=ALU.mult), reads=[bbn], writes=[bBN])
                T.barrier()
            cnt_i = {"ii": 0, "pi": 0, "oi": 0, "lp": 0, "ib": 0}
            IBS = (2, 3)
            SB = 4
            NRB = 2 * ACC_DEPTH + 2
            Rb = [salloc(es, f"Rb{i}", [128, 512], BF16) for i in range(NRB)]
            bRb = [Buf(f"Rb{i}") for i in range(NRB)]
            dg = [salloc(es, f"dg{i}", [128, 8, 128], BF16) for i in range(2)]
            bdg = [Buf(f"dg{i}") for i in range(2)]
            absw = salloc(es, "absw", [128, NT, 8], F32); babsw = Buf("absw")
            sgn = salloc(es, "sgn", [128, NT, 8], F32); bsgn = Buf("sgn")
            OTB = (5, 6)
            DB = 7

            pend_acc = []

            def flush_acc(keep=0):
                while len(pend_acc) > keep:
                    (qb, c0, w, j, rsl, dsl) = pend_acc.pop(0)
                    sc_ = score[qb % 2]; bsc_ = bsc[qb % 2]
                    T.op("pe", lambda e: e.matmul(pb[SB][:, 0:w], lhsT=dg[dsl][:, j, :], rhs=Rb[rsl][:, 0:w],
                                                  start=(j == 0), stop=(j == 7), skip_group_check=True),
                         reads=[bdg[dsl], bRb[rsl]], writes=[PB[SB]])
                    if j == 7:
                        T.op("act", lambda e: e.activation(out=sc_[:, c0:c0 + w], in_=pb[SB][:, 0:w], func=AF.Copy),
                             reads=[PB[SB]], writes=[bsc_])

            def make_diag(qb):
                dsl = qb % 2
                for j in range(8):
                    T.op("act", lambda e: e.activation(out=dg[dsl][:, j, :], in_=ident_b[:], func=AF.Identity,
                                                       scale=sgn[:, qb, j:j + 1]),
                         reads=[b_identb, bsgn], writes=[bdg[dsl]])

            def idx_unit(s, qb, c, j):
                n = (qb + 1) * 128
                q0 = qb * 128
                c0 = c * 512
                w = min(512, n - c0)
                rsl = cnt_i["ii"] % NRB
                cnt_i["ii"] += 1
                ch = j // 2
                r0 = (j % 2) * 64
                IB = IBS[cnt_i["ib"] % 2]
                cnt_i["ib"] += 1
                T.op("pe", lambda e: e.matmul(pb[IB][:, 0:w], lhsT=cur['iqT'][r0:r0 + 64, ch, q0:q0 + 128],
                                              rhs=cur['ikT'][r0:r0 + 64, c0:c0 + w], start=True, stop=True),
                     reads=[cur['biq'], cur['bik']], writes=[PB[IB]])
                T.op("act", lambda e: e.activation(out=Rb[rsl][:, 0:w], in_=pb[IB][:, 0:w], func=AF.Relu,
                                                   scale=absw[:, qb, j:j + 1]),
                     reads=[PB[IB], babsw], writes=[bRb[rsl]])
                flush_acc(keep=ACC_DEPTH - 1)
                pend_acc.append((qb, c0, w, j, rsl, qb % 2))

            def select(qb):
                n = (qb + 1) * 128
                nm = nmask[qb % 2]; bn_ = bnm[qb % 2]
                score_ = score[qb % 2]; bsc_ = bsc[qb % 2]
                T.op("dve", lambda e: e.memset(score_[0:64, n - 64:n], -1.0e30), reads=[bsc_], writes=[bsc_])
                if qb >= 2:
                    hi = bis[:, 0:1]; lo = bis[:, 1:2]; w0 = bis[:, 2:3]; mid = bis[:, 3:4]
                    cnt = bis[:, 4:5]; sv = bis[:, 5:6]; thr = bis[:, 6:7]
                    H = bis[:, 8:8 + KBIS]; H2 = bis[:, 8 + KBIS:8 + 2 * KBIS]
                    T.op("dve", lambda e: e.tensor_reduce(out=hi, in_=score_[:, 0:n], axis=AX.X, op=ALU.max),
                         reads=[bsc_], writes=[bbis])
                    T.op("dve", lambda e: e.tensor_reduce(out=lo, in_=score_[:, 0:n - 64], axis=AX.X, op=ALU.min),
                         reads=[bsc_], writes=[bbis])
                    T.op("dve", lambda e: e.tensor_tensor(out=w0, in0=hi, in1=lo, op=ALU.subtract),
                         reads=[bbis], writes=[bbis])
                    T.op("dve", lambda e: e.tensor_scalar(out=H, in0=pw2[:], scalar1=w0, scalar2=None, op0=ALU.mult),
                         reads=[bbis, bpw2], writes=[bbis])
                    T.op("dve", lambda e: e.tensor_scalar(out=H2, in0=H, scalar1=2.0, scalar2=None, op0=ALU.mult),
                         reads=[bbis], writes=[bbis])
                    T.op("dve", lambda e: e.tensor_tensor(out=mid, in0=lo, in1=bis[:, 8:9], op=ALU.add),
                         reads=[bbis], writes=[bbis])
                    for k in range(KBIS):
                        T.op("dve", lambda e: e.tensor_scalar(out=junk[:, 0:n], in0=score_[:, 0:n], scalar1=mid, scalar2=None,
                                                              op0=ALU.is_ge, op1=ALU.add, accum_out=cnt),
                             reads=[bsc_, bbis], writes=[b_junk, bbis])
                        if k < KBIS - 1:
                            T.op("dve", lambda e: e.tensor_scalar(out=sv, in0=cnt, scalar1=float(TOPK),
                                                                  scalar2=bis[:, 8 + KBIS + k + 1:8 + KBIS + k + 2],
                                                                  op0=ALU.is_ge, op1=ALU.mult),
                                 reads=[bbis], writes=[bbis])
                            T.op("dve", lambda e: e.scalar_tensor_tensor(out=mid, in0=sv, scalar=bis[:, 8 + k + 1:8 + k + 2],
                                                                         in1=mid, op0=ALU.subtract, op1=ALU.add),
                                 reads=[bbis], writes=[bbis])
                        else:
                            T.op("dve", lambda e: e.tensor_scalar(out=sv, in0=cnt, scalar1=float(TOPK),
                                                                  scalar2=bis[:, 8 + k:8 + k + 1],
                                                                  op0=ALU.is_ge, op1=ALU.mult),
                                 reads=[bbis], writes=[bbis])
                            T.op("dve", lambda e: e.scalar_tensor_tensor(out=thr, in0=sv, scalar=bis[:, 8 + k:8 + k + 1],
                                                                         in1=mid, op0=ALU.subtract, op1=ALU.add),
                                 reads=[bbis], writes=[bbis])
                    T.op("dve", lambda e: e.tensor_scalar(out=nm[:, 0:n], in0=score_[:, 0:n], scalar1=thr, scalar2=-1.0e5,
                                                          op0=ALU.is_lt, op1=ALU.mult), reads=[bsc_, bbis], writes=[bn_])
                else:
                    T.op("dve", lambda e: e.tensor_scalar(out=nm[:, 0:n], in0=score_[:, 0:n], scalar1=-1.0e29, scalar2=-1.0e5,
                                                          op0=ALU.is_lt, op1=ALU.mult), reads=[bsc_], writes=[bn_])

            def att_logits(s, qb, kb):
                q0 = qb * 128
                nm = nmask[qb % 2]; bn_ = bnm[qb % 2]
                lp = cnt_i["lp"] % 2
                cnt_i["lp"] += 1
                P = PP[lp]; bP = bPP[lp]
                off = qb - kb
                for (bank, h0) in ((0, 0), (1, 4)):
                    T.op("pe", lambda e: e.matmul(
                        pb[bank][:].rearrange("p (h t) -> p h t", h=4),
                        lhsT=cur['kvT'][:, kb * 128:(kb + 1) * 128], rhs=cur['dqT'][:, h0:h0 + 4, q0:q0 + 128],
                        start=True, stop=False, skip_group_check=True), reads=[cur['bkvT'], cur['bdq']], writes=[PB[bank]])
                    T.op("pe", lambda e: e.matmul(pb[bank][:], lhsT=nm[:, kb * 128:(kb + 1) * 128], rhs=id4[:],
                                                  start=False, stop=(off >= 2), skip_group_check=True),
                         reads=[bn_, bid4], writes=[PB[bank]])
                    if off < 2:
                        T.op("pe", lambda e: e.matmul(pb[bank][:], lhsT=ident_b[:], rhs=BN[:, off, h0 * 128:(h0 + 4) * 128],
                                                      start=False, stop=True, skip_group_check=True),
                             reads=[b_identb, bBN], writes=[PB[bank]])
                T.op("act", lambda e: e.activation(out=P[:], in_=pb2[0][:], func=AF.Exp, scale=lsc),
                     reads=[PB[0], PB[1]], writes=[bP])
                return lp

            def att_pv(s, qb, kb, lp):
                P = PP[lp]; bP = bPP[lp]
                for (bank, h0) in ((OTB[0], 0), (OTB[1], 4)):
                    T.op("pe", lambda e: e.matmul(pb[bank][:], lhsT=cur['kvt'][:, kb, :], rhs=P[:, h0 * 128:(h0 + 4) * 128],
                                                  start=(kb == 0), stop=(kb == qb)),
                         reads=[cur['bkvt'], bP], writes=[PB[bank]])
                for h in range(8):
                    T.op("pe", lambda e: e.matmul(pb[DB][:, h:h + 1], lhsT=P[:, h * 128:(h + 1) * 128], rhs=ones_b[:, 0:1],
                                                  start=(kb == 0 and h == 0), stop=(kb == qb), skip_group_check=True),
                         reads=[bP, b_onesb], writes=[PB[DB]])

            def epilogue(s, qb):
                q0 = qb * 128
                T.op("act", lambda e: e.activation(out=oTs[:, 0:512], in_=pb[OTB[0]][:], func=AF.Copy), reads=[PB[OTB[0]]], writes=[boTs])
                T.op("act", lambda e: e.activation(out=oTs[:, 512:1024], in_=pb[OTB[1]][:], func=AF.Copy), reads=[PB[OTB[1]]], writes=[boTs])
                T.op("dve", lambda e: e.reciprocal(out=rden[:], in_=pb[DB][:, 0:8]), reads=[PB[DB]], writes=[brden])
                eb = IBS[cnt_i["ib"] % 2]
                cnt_i["ib"] += 1
                for h in range(8):
                    T.op("pe", lambda e: e.matmul(pb[eb][:, h * 64:(h + 1) * 64], lhsT=oTs[:, h * 128:(h + 1) * 128],
                                                  rhs=wuv[:, h, :], start=(h == 0), stop=True, skip_group_check=True),
                         reads=[boTs, bwuv], writes=[PB[eb]])
                for h in range(8):
                    T.op("dve", lambda e: e.tensor_scalar(out=obf[:, h * 64:(h + 1) * 64], in0=pb[eb][:, h * 64:(h + 1) * 64],
                                                          scalar1=rden[:, h:h + 1], scalar2=None, op0=ALU.mult),
                         reads=[PB[eb], brden], writes=[bobf])
                rs, brs = rstd_from(obf[:], [bobf], 512)
                osl = cnt_i["oi"] % 2
                cnt_i["oi"] += 1
                T.op("dve", lambda e: e.scalar_tensor_tensor(out=obn[osl][:], in0=obf[:], scalar=rs, in1=gob[:],
                                                             op0=ALU.mult, op1=ALU.mult),
                     reads=[bobf, brs, bgob], writes=[bobn[osl]])
                T.dma("pool", ocat[s, q0:q0 + 128, 512:1024], obn[osl][:], bobn[osl], reads=[bobn[osl]])
                if s == 0 and LIM_SEQ > 1 and qb == min(8, LIM_QB - 1):
                    load_seq_C(1, gate=[bobn[osl]])

            for s in range(LIM_SEQ):
                if s == 0:
                    load_seq_C(0)
                cur.clear(); cur.update(cin[s])
                wi = cur["wi"]; bwi = cur["bwi"]
                T.op("act", lambda e: e.activation(out=absw[:], in_=wi[:], func=AF.Abs, scale=isc),
                     reads=[bwi], writes=[babsw])
                T.op("dve", lambda e: e.tensor_scalar(out=sgn[:], in0=wi[:], scalar1=0.0, scalar2=2.0,
                                                      op0=ALU.is_ge, op1=ALU.mult), reads=[bwi], writes=[bsgn])
                T.op("dve", lambda e: e.tensor_scalar(out=sgn[:], in0=sgn[:], scalar1=-1.0, scalar2=None,
                                                      op0=ALU.add), reads=[bsgn], writes=[bsgn])
                for step in range(LIM_QB + 2):
                    qi = step
                    qs = step - 1
                    qa = step - 2
                    iu = []
                    if qi < LIM_QB:
                        n = (qi + 1) * 128
                        iu = [(c, j) for c in range((n + 511) // 512) for j in range(8)]
                        make_diag(qi)
                    if 0 <= qs < LIM_QB:
                        select(qs)
                    au = list(range(qa + 1)) if qa >= 0 else []
                    na, ni = len(au), len(iu)
                    ai = 0
                    ii_ = 0
                    pend = None
                    total = max(na, 1)
                    while ai < na or ii_ < ni:
                        tgt = ni if ai >= na else (ni * (ai + 1)) // total
                        while ii_ < tgt:
                            idx_unit(s, qi, *iu[ii_])
                            ii_ += 1
                        if ai < na:
                            lp = att_logits(s, qa, au[ai])
                            if pend is not None:
                                att_pv(s, qa, *pend)
                            pend = (au[ai], lp)
                            ai += 1
                    if pend is not None:
                        att_pv(s, qa, *pend)
                    flush_acc()
                    if qa >= 0:
                        epilogue(s, qa)
                        bg_step(1)
            T.barrier()

    def phase_D(l, prefetch=()):
        prefetch = list(prefetch)
        bg_flush(l)
        with ExitStack() as es:
            Wo = salloc(es, "Wo", [128, 8, D], BF16); bWo = Buf("Wo")
            wl = wb_out[l].rearrange("(c p) n -> p c n", p=128)
            for c in range(8):
                T.dma("sp", Wo[:, c, :], wl[:, c, :], bWo, reads=[B_wb_out[l]], writes=[bWo])
            gb, bgb = ga_bcast(es, l, 16, "ga1")
            oc = [salloc(es, f"oc{i}", [128, D], BF16) for i in range(2)]
            boc = [Buf(f"oc{i}") for i in range(2)]
            oT = [salloc(es, f"oT{i}", [128, 8, 128], BF16) for i in range(2)]
            boT = [Buf(f"oT{i}") for i in range(2)]
            xts = [salloc(es, f"xd{i}", [128, D], F32) for i in range(2)]
            bxts = [Buf(f"xd{i}") for i in range(2)]
            tmp = [salloc(es, f"tm{i}", [128, D], F32) for i in range(2)]
            btmp = [Buf(f"tm{i}") for i in range(2)]
            src = x_in if l == 0 else xres
            ti = 0
            for s in range(NSEQ):
                for tt in range(NT):
                    t0 = tt * 128
                    k2 = ti % 2
                    ti += 1
                    T.dma("sp", oc[k2][:], ocat[s, t0:t0 + 128, :], boc[k2], writes=[boc[k2]])
                    T.dma("sp", xts[k2][:], src[s, t0:t0 + 128, :], bxts[k2], writes=[bxts[k2]])
                    if prefetch:
                        prefetch.pop(0)()
                    pv = pbf(k2).rearrange("p (c t) -> p c t", c=8)
                    for c in range(8):
                        T.op("pe", lambda e: e.transpose(pv[:, c, :], oc[k2][:, c * 128:(c + 1) * 128], ident_b[:]),
                             reads=[boc[k2], b_identb], writes=[PB[k2]])
                    T.op("act", lambda e: e.activation(out=oT[k2][:].rearrange("p c t -> p (c t)"), in_=pbf(k2)[:, 0:1024], func=AF.Copy),
                         reads=[PB[k2]], writes=[boT[k2]])
                    for half in range(2):
                        bank = 2 + k2 * 2 + half
                        for c in range(8):
                            T.op("pe", lambda e: e.matmul(pb[bank][:], lhsT=oT[k2][:, c, :], rhs=Wo[:, c, half * 512:(half + 1) * 512],
                                                          start=(c == 0), stop=(c == 7)),
                                 reads=[boT[k2], bWo], writes=[PB[bank]])
                        T.op("dve", lambda e: e.tensor_tensor(out=tmp[k2][:, half * 512:(half + 1) * 512], in0=pb[bank][:],
                                                              in1=gb[:, s, half * 512:(half + 1) * 512], op=ALU.mult),
                             reads=[PB[bank], bgb], writes=[btmp[k2]])
                    T.op("dve", lambda e: e.tensor_tensor(out=tmp[k2][:], in0=tmp[k2][:], in1=xts[k2][:], op=ALU.add),
                         reads=[btmp[k2], bxts[k2]], writes=[btmp[k2]])
                    T.dma("pool", xres[s, t0:t0 + 128, :], tmp[k2][:], btmp[k2], reads=[btmp[k2]])
            while prefetch:
                prefetch.pop(0)()
            T.barrier()

    def phase_DE(l, last):
        with ExitStack() as esw:
            Wu = salloc(esw, "Wu", [128, 8, DFF], BF16); bWu = Buf("Wu")
            Wd = salloc(esw, "Wd", [128, 32, D], BF16); bWd = Buf("Wd")
            wul = wb_up[l].rearrange("(c p) n -> p c n", p=128)
            wdl = wb_dn[l].rearrange("(c p) n -> p c n", p=128)
            pf = []
            for c in range(8):
                for hh in range(2):
                    pf.append(lambda c=c, hh=hh: T.dma(
                        "sp", Wu[:, c, hh * 2048:(hh + 1) * 2048], wul[:, c, hh * 2048:(hh + 1) * 2048], bWu,
                        reads=[B_wb_up[l]], writes=[bWu]))
            for c4 in range(8):
                pf.append(lambda c4=c4: T.dma(
                    "sp", Wd[:, c4 * 4:(c4 + 1) * 4, :], wdl[:, c4 * 4:(c4 + 1) * 4, :], bWd,
                    reads=[B_wb_dn[l]], writes=[bWd]))
            phase_D(l, prefetch=pf)
            phase_E(l, last, Wu, bWu, Wd, bWd)

    def phase_E(l, last, Wu, bWu, Wd, bWd):
        with ExitStack() as es:
            gb, bgb = ga_bcast(es, l, 40, "ga2")
            if last:
                gf = salloc(es, "gf", [128, D], F32); bgf = Buf("gf")
                T.dma("sp", gf[:], gfin_bc, bgf, writes=[bgf])
            TG = 256
            xts = [salloc(es, f"xe{i}", [128, D], F32) for i in range(4)]
            bxts = [Buf(f"xe{i}") for i in range(4)]
            xns = [salloc(es, f"xne{i}", [128, D], BF16) for i in range(2)]
            bxns = [Buf(f"xne{i}") for i in range(2)]
            hTs = [salloc(es, f"hTe{i}", [128, 8, TG], BF16) for i in range(2)]
            bhTs = [Buf(f"hTe{i}") for i in range(2)]
            aT = salloc(es, "aT", [128, 32, TG], BF16); baT = [Buf(f"aT{i}") for i in range(32)]
            rl = [salloc(es, f"rl{i}", [128, TG], BF16) for i in range(2)]
            brl = [Buf(f"rl{i}") for i in range(2)]
            ti = 0
            gi = 0
            ui = 0
            for s in range(NSEQ):
                for g in range(S // TG):
                    hT = hTs[gi % 2]; bhT = bhTs[gi % 2]
                    gi += 1
                    tiles = []
                    for j in range(TG // 128):
                        tt = g * (TG // 128) + j
                        t0 = tt * 128
                        xt = xts[ti % 4]; bxt = bxts[ti % 4]
                        xn = xns[ti % 2]; bxn = bxns[ti % 2]
                        tb = ti % 2
                        ti += 1
                        tiles.append((t0, xt, bxt))
                        T.dma("sp", xt[:], xres[s, t0:t0 + 128, :], bxt, writes=[bxt])
                        norm_to_T(xt[:], bxt,
                                  lambda c: gm2T[:, l, c, s:s + 1],
                                  lambda c: modT[:, l, 24 + c, s:s + 1],
                                  [b_gm2, b_modT_], xn, bxn, tb,
                                  lambda c: hT[:, c, j * 128:(j + 1) * 128], bhT)
                    for f in range(32):
                        bank = 2 + (ui % 2)
                        sl = ui % 2
                        ui += 1
                        for c in range(8):
                            T.op("pe", lambda e: e.matmul(pb[bank][:, 0:TG], lhsT=Wu[:, c, f * 128:(f + 1) * 128], rhs=hT[:, c, :],
                                                          start=(c == 0), stop=(c == 7)),
                                 reads=[bWu, bhT], writes=[PB[bank]])
                        T.op("act", lambda e: e.activation(out=rl[sl][:], in_=pb[bank][:, 0:TG], func=AF.Relu),
                             reads=[PB[bank]], writes=[brl[sl]])
                        T.op("dve", lambda e: e.tensor_tensor(out=aT[:, f, :], in0=rl[sl][:], in1=rl[sl][:], op=ALU.mult),
                             reads=[brl[sl]], writes=[baT[f]])
                    for j, (t0, xt, bxt) in enumerate(tiles):
                        for half in range(2):
                            bank = 4 + (j % 2) * 2 + half
                            for f in range(32):
                                T.op("pe", lambda e: e.matmul(pb[bank][:], lhsT=aT[:, f, j * 128:(j + 1) * 128],
                                                              rhs=Wd[:, f, half * 512:(half + 1) * 512], start=(f == 0), stop=(f == 31)),
                                     reads=[baT[f], bWd], writes=[PB[bank]])
                            T.op("dve", lambda e: e.tensor_tensor(out=tmpE[j % 2][:, half * 512:(half + 1) * 512], in0=pb[bank][:],
                                                                  in1=gb[:, s, half * 512:(half + 1) * 512], op=ALU.mult),
                                 reads=[PB[bank], bgb], writes=[btmpE[j % 2]])
                        T.op("dve", lambda e: e.tensor_tensor(out=xt[:], in0=tmpE[j % 2][:], in1=xt[:], op=ALU.add),
                             reads=[btmpE[j % 2], bxt], writes=[bxt])
                        if not last:
                            T.dma("pool", xres[s, t0:t0 + 128, :], xt[:], bxt, reads=[bxt])
                        else:
                            rs, brs = rstd_from(xt[:], [bxt], D)
                            T.op("dve", lambda e: e.scalar_tensor_tensor(out=tmpE[j % 2][:], in0=xt[:], scalar=rs, in1=gf[:],
                                                                         op0=ALU.mult, op1=ALU.mult),
                                 reads=[bxt, brs, bgf], writes=[btmpE[j % 2]])
                            T.dma("pool", out_d[s, t0:t0 + 128, :], tmpE[j % 2][:], btmpE[j % 2], reads=[btmpE[j % 2]])
            T.barrier()

    tmpE = []
    btmpE = [Buf("tmpE0"), Buf("tmpE1")]

    tmpE.append(salloc(ges, "tmpE0", [128, D], F32))
    tmpE.append(salloc(ges, "tmpE1", [128, D], F32))

    convert_weights()
    esA0 = ExitStack()
    Wf0 = salloc(esA0, "Wf0", [128, 8, 2688], BF16); bWf0 = [Buf(f"Wf0{i}") for i in range(4)]
    Wt0 = salloc(esA0, "Wt0", [128, 8, 648], BF16); bWt0 = Buf("Wt0")
    preA = (Wf0, bWf0, Wt0, bWt0)
    prologue(preA)
    for l in range(nlayers):
        if "A" in phases:
            phase_A(l, pre=preA if l == 0 else None)
        if l == 0:
            esA0.close()
        if "B" in phases:
            phase_B(l)
        if "C" in phases:
            phase_C(l)
        if "D" in phases and "E" in phases:
            phase_DE(l, last=(l == nlayers - 1))
        elif "D" in phases:
            phase_D(l)
    T.barrier()
    ges.close()
    return nc, T


def _t5_bucket(rel):
    nb = 16
    max_exact = 8
    base = np.where(rel > 0, nb, 0)
    n = np.abs(rel)
    nf = np.maximum(n, max_exact).astype(np.float32)
    large = max_exact + (np.log(nf / np.float32(max_exact)) / np.float32(math.log(128 / max_exact))
                         * np.float32(nb - max_exact)).astype(np.int32)
    large = np.minimum(large, nb - 1)
    return base + np.where(n < max_exact, n, large)


def _consts():
    p = np.arange(128)
    c = {}
    c["c_ident"] = np.eye(128, dtype=np.float32)
    c["c_tri8"] = np.where(p[:, None] >= p[None, :], -8.0, 0.0).astype(np.float32)
    c["c_neg8"] = np.full((128, 128), -8.0, np.float32)
    cm = (p[:, None] < p[None, :]).astype(np.float32)
    c["c_cmask"] = np.ascontiguousarray(np.broadcast_to(cm[:, None, :], (128, 8, 128)))
    adm = ((p[:, None] // 64) <= (p[None, :] // 64)).astype(np.float32)
    c["c_adm"] = np.ascontiguousarray(np.broadcast_to(adm[:, None, :], (128, 8, 128)))
    c["c_admneg"] = np.ascontiguousarray(np.broadcast_to(np.where(adm > 0, 0.0, -1.0e5).astype(np.float32)[:, None, :], (128, 8, 128)))
    c["c_pow2"] = np.ascontiguousarray(np.broadcast_to(
        (0.5 ** np.arange(1, KBIS + 1)).astype(np.float32)[None, :], (128, KBIS)))
    c["c_ones"] = np.ones((128, 128), np.float32)
    return c


def _prep_inputs(inp, core):
    f = np.float32
    b0 = core * NSEQ
    bs = slice(b0, b0 + NSEQ)
    m = {}
    m["x"] = np.ascontiguousarray(inp["x"][bs], dtype=f)
    c = np.asarray(inp["c"], dtype=f)[bs]
    m["cT"] = np.ascontiguousarray(c.reshape(NSEQ, 8, 128).transpose(2, 1, 0))
    m["w_mod"] = np.ascontiguousarray(inp["w_mod"], dtype=f)
    bm = np.asarray(inp["b_mod"], dtype=f).reshape(2, 48, 128).transpose(2, 0, 1)
    m["b_modT"] = np.ascontiguousarray(np.broadcast_to(bm[..., None], (128, 2, 48, NSEQ)))
    ga = np.asarray(inp["g_attn"], dtype=f).reshape(2, 8, 128).transpose(2, 0, 1)
    m["g_attnT"] = np.ascontiguousarray(np.broadcast_to(ga[..., None], (128, 2, 8, NSEQ)))
    gm = np.asarray(inp["g_mlp"], dtype=f).reshape(2, 8, 128).transpose(2, 0, 1)
    m["g_mlpT"] = np.ascontiguousarray(np.broadcast_to(gm[..., None], (128, 2, 8, NSEQ)))
    m["w_in"] = np.ascontiguousarray(inp["w_in"], dtype=f)
    m["kvg_bc"] = np.ascontiguousarray(np.broadcast_to(np.asarray(inp["kv_norm_g"], dtype=f)[None], (128, 2, 128)))
    m["w_uv"] = np.ascontiguousarray(inp["w_uv"], dtype=f)
    m["goa_bc"] = np.ascontiguousarray(np.broadcast_to(np.asarray(inp["g_out_a"], dtype=f)[None], (128, 2, 512)))
    m["gob_bc"] = np.ascontiguousarray(np.broadcast_to(np.asarray(inp["g_out_b"], dtype=f)[None], (128, 2, 512)))
    m["w_out"] = np.ascontiguousarray(inp["w_out"], dtype=f)
    m["w_up"] = np.ascontiguousarray(inp["w_up"], dtype=f)
    m["w_down"] = np.ascontiguousarray(inp["w_down"], dtype=f)
    rb = np.asarray(inp["rel_bias"], dtype=f)
    p = np.arange(128)
    bn = np.empty((128, 2, 8, 128), f)
    for off in range(2):
        rel = (p[:, None] - off * 128) - p[None, :]
        bk = _t5_bucket(rel.astype(np.int32))
        bn[:, off] = rb[bk].transpose(0, 2, 1)
    m["biasn"] = bn
    far = rb[_t5_bucket(np.array([-1000], np.int32))[0]]
    m["bfar_bc"] = np.ascontiguousarray(np.broadcast_to(far[None, :, None], (128, 8, 128)))
    m["gfin_bc"] = np.ascontiguousarray(np.broadcast_to(np.asarray(inp["g_final"], dtype=f)[None], (128, D)))
    m.update(_consts())
    return m


_CACHE = {}


def kernel(**inputs):
    if "nc" not in _CACHE:
        _CACHE["nc"] = build_program()[0]
    nc = _CACHE["nc"]
    in_maps = [_prep_inputs(inputs, core) for core in range(8)]
    res = run_bass_kernel_spmd(nc, in_maps, core_ids=list(range(8)))
    out = np.concatenate([np.asarray(r["out"]) for r in res.results], axis=0)
    return out.astype(np.float32, copy=False)
```

```python
import math
from contextlib import ExitStack

import numpy as np
import concourse.bass as bass
import concourse.mybir as mybir
from concourse.bass_utils import run_bass_kernel_spmd

F32 = mybir.dt.float32
BF16 = mybir.dt.bfloat16
AF = mybir.ActivationFunctionType
ALU = mybir.AluOpType
AX = mybir.AxisListType

S = 2048
D = 1024
NSEQ = 2
NT = S // 128
DFF = 4096
DIN = 3272
EPS = 1e-6
KBIS = 16
TOPK = 256
EPOCH = 30000
LIM_SEQ = NSEQ
LIM_QB = NT
NO_POOL = False
ACC_DEPTH = 2
FLUSH_BG_BEFORE_C = False


class Buf:
    __slots__ = ("name", "last_w", "readers", "dsem", "bg")

    def __init__(self, name, bg=False):
        self.name = name
        self.last_w = None
        self.readers = []
        self.dsem = {}
        self.bg = bg


class Tracker:
    def __init__(self, nc):
        self.nc = nc
        self.eng = {"pe": nc.tensor, "act": nc.scalar, "dve": nc.vector,
                    "pool": nc.gpsimd, "sp": nc.sync}
        self.cnt = {e: 0 for e in self.eng}
        self.sems = {e: [] for e in self.eng}
        self.seen = {e: {} for e in self.eng}
        self.nsem = 0
        self.dma_bufs = []
        self.free_dsems = {"hw": [], "sw": []}
        self.nwaits = 0
        self.ninstr = 0

    def _newsem(self, name):
        self.nsem += 1
        return self.nc.alloc_semaphore(name=name)

    def _wait(self, e, tok):
        sem, val, src = tok
        key = id(sem)
        if self.seen[e].get(key, 0) >= val:
            return
        self.seen[e][key] = val
        self.eng[e].wait_ge(sem, val)
        self.nwaits += 1

    def _deps(self, e, reads, writes):
        for b in reads:
            if b.last_w is not None and not (b.last_w[2] == e and e == "pe"):
                self._wait(e, b.last_w)
        for b in writes:
            if b.last_w is not None and not (b.last_w[2] == e and e == "pe"):
                self._wait(e, b.last_w)
            for t in b.readers:
                if t[2] == e:
                    continue
                self._wait(e, t)

    def _commit(self, tok, reads, writes):
        for b in reads:
            b.readers.append(tok)
            if len(b.readers) > 12:
                d = {}
                for t in b.readers:
                    k = id(t[0])
                    if k not in d or d[k][1] < t[1]:
                        d[k] = t
                b.readers = list(d.values())
        for b in writes:
            b.last_w = tok
            b.readers = []

    def op(self, e, fn, reads=(), writes=()):
        self._deps(e, reads, writes)
        n = self.cnt[e]
        ep, v = divmod(n, EPOCH)
        while len(self.sems[e]) <= ep:
            self.sems[e].append(self._newsem(f"c_{e}_{len(self.sems[e])}"))
        sem = self.sems[e][ep]
        ins = fn(self.eng[e])
        ins.then_inc(sem, 1)
        self.cnt[e] = n + 1
        self.ninstr += 1
        tok = (sem, v + 1, e)
        self._commit(tok, reads, writes)
        return tok

    def dma(self, q, out, in_, sb, reads=(), writes=(), **kw):
        self._deps(q, reads, writes)
        kind = "sw" if q == "pool" else "hw"
        if kind not in sb.dsem:
            if self.free_dsems[kind]:
                sb.dsem[kind] = list(self.free_dsems[kind].pop())
            else:
                sb.dsem[kind] = [self._newsem(f"d{kind}_{sb.name}_{self.nsem}"), 0]
            self.dma_bufs.append((sb, kind))
        ent = sb.dsem[kind]
        ent[1] += 16
        ins = self.eng[q].dma_start(out=out, in_=in_, **kw)
        ins.then_inc(ent[0], 16)
        self.ninstr += 1
        tok = (ent[0], ent[1], "dma")
        self._commit(tok, reads, writes)
        return tok

    def barrier(self):
        toks = []
        for f in self.eng:
            n = self.cnt[f]
            if n == 0:
                continue
            ep, v = divmod(n - 1, EPOCH)
            toks.append((self.sems[f][ep], v + 1, f))
        for b, kind in self.dma_bufs:
            if not b.bg:
                toks.append((b.dsem[kind][0], b.dsem[kind][1], "dma"))
        for e in self.eng:
            for t in toks:
                if t[2] == e:
                    continue
                self._wait(e, t)
        keep = []
        for b, kind in self.dma_bufs:
            if b.bg:
                keep.append((b, kind))
            else:
                self.free_dsems[kind].append(tuple(b.dsem.pop(kind)))
        self.dma_bufs = keep


def build_program(nlayers=2, debug=False, phases="ABCDE"):
    nc = bass.Bass("TRN2", target_bir_lowering=False)
    T = Tracker(nc)
    uid = [0]

    def din(name, shape, dt=F32):
        return nc.dram_tensor(name, list(shape), dt, kind="ExternalInput").ap()

    def dscr(name, shape, dt):
        kind = "ExternalOutput" if debug else "Internal"
        return nc.dram_tensor(name, list(shape), dt, kind=kind).ap()

    x_in = din("x", [NSEQ, S, D])
    cT = din("cT", [128, 8, NSEQ])
    w_mod = din("w_mod", [2, D, 6 * D])
    b_modT = din("b_modT", [128, 2, 48, NSEQ])
    g_attnT = din("g_attnT", [128, 2, 8, NSEQ])
    g_mlpT = din("g_mlpT", [128, 2, 8, NSEQ])
    w_in = din("w_in", [2, D, DIN])
    kvg_bc = din("kvg_bc", [128, 2, 128])
    w_uv = din("w_uv", [2, 8, 128, 64])
    goa_bc = din("goa_bc", [128, 2, 512])
    gob_bc = din("gob_bc", [128, 2, 512])
    w_out = din("w_out", [2, D, D])
    w_up = din("w_up", [2, D, DFF])
    w_down = din("w_down", [2, DFF, D])
    biasn = din("biasn", [128, 2, 8, 128])
    bfar_bc = din("bfar_bc", [128, 8, 128])
    gfin_bc = din("gfin_bc", [128, D])
    c_ident = din("c_ident", [128, 128])
    c_tri8 = din("c_tri8", [128, 128])
    c_neg8 = din("c_neg8", [128, 128])
    c_cmask = din("c_cmask", [128, 8, 128])
    c_adm = din("c_adm", [128, 8, 128])
    c_admneg = din("c_admneg", [128, 8, 128])
    c_pow2 = din("c_pow2", [128, KBIS])
    c_ones = din("c_ones", [128, 128])
    out_d = nc.dram_tensor("out", [NSEQ, S, D], F32, kind="ExternalOutput").ap()

    xres = dscr("xres", [NSEQ, S, D], F32)
    featT = dscr("featT", [NSEQ, 21, 128, S], BF16)
    v_scr = dscr("v_scr", [NSEQ, S, 512], BF16)
    kv_tok = dscr("kv_tok", [NSEQ, S, 128], BF16)
    kvT_scr = dscr("kvT_scr", [NSEQ, 128, S], BF16)
    widx = dscr("widx", [NSEQ, S, 8], F32)
    ocat = dscr("ocat", [NSEQ, S, D], BF16)

    wb_in = [nc.dram_tensor(f"wb_in{l}", [D, DIN], BF16, kind="Internal").ap() for l in range(2)]
    wb_out = [nc.dram_tensor(f"wb_out{l}", [D, D], BF16, kind="Internal").ap() for l in range(2)]
    wb_up = [nc.dram_tensor(f"wb_up{l}", [D, DFF], BF16, kind="Internal").ap() for l in range(2)]
    wb_dn = [nc.dram_tensor(f"wb_dn{l}", [DFF, D], BF16, kind="Internal").ap() for l in range(2)]
    B_wb_in = [Buf(f"wb_in{l}", bg=True) for l in range(2)]
    B_wb_out = [Buf(f"wb_out{l}", bg=True) for l in range(2)]
    B_wb_up = [Buf(f"wb_up{l}", bg=True) for l in range(2)]
    B_wb_dn = [Buf(f"wb_dn{l}", bg=True) for l in range(2)]

    bgq = []

    def convert_weights():
        for l in range(nlayers):
            for (dst, src, bb, rows, step) in ((wb_in[l], w_in[l], B_wb_in[l], D, 256), (wb_out[l], w_out[l], B_wb_out[l], D, 512),
                                               (wb_up[l], w_up[l], B_wb_up[l], D, 256), (wb_dn[l], w_down[l], B_wb_dn[l], DFF, 1024)):
                for r0 in range(0, rows, step):
                    f = (lambda dst=dst, src=src, bb=bb, r0=r0, step=step:
                         T.dma("pool", dst[r0:r0 + step, :], src[r0:r0 + step, :], bb, writes=[bb]))
                    if l == 0 and dst is wb_in[0]:
                        f()
                    else:
                        bgq.append((l, f))

    def bg_step(n=1):
        for _ in range(n):
            if bgq:
                bgq.pop(0)[1]()

    def bg_flush(layer):
        while bgq and bgq[0][0] <= layer:
            bgq.pop(0)[1]()

    def salloc(es, name, shape, dt):
        uid[0] += 1
        return es.enter_context(nc.sbuf_tensor(f"{name}_{uid[0]}", list(shape), dt))

    ges = ExitStack()
    pb2 = [ges.enter_context(nc.psum_tensor(f"pbp{i}", [128, 1024], F32)) for i in range(4)]
    pb = [pb2[i // 2][:, (i % 2) * 512:(i % 2 + 1) * 512] for i in range(8)]
    PB = [Buf(f"pb{i}") for i in range(8)]

    def pbf(i):
        return pb[i].bitcast(BF16)

    ident_f = salloc(ges, "identf", [128, 128], F32); b_identf = Buf("identf")
    ident_b = salloc(ges, "identb", [128, 128], BF16); b_identb = Buf("identb")
    ones_f = salloc(ges, "onesf", [128, 128], F32); b_onesf = Buf("onesf")
    ones_b = salloc(ges, "onesb", [128, 128], BF16); b_onesb = Buf("onesb")
    modT = salloc(ges, "modT", [128, 2, 48, NSEQ], F32); b_modT_ = Buf("modT")
    gm1T = salloc(ges, "gm1T", [128, 2, 8, NSEQ], F32); b_gm1 = Buf("gm1T")
    gm2T = salloc(ges, "gm2T", [128, 2, 8, NSEQ], F32); b_gm2 = Buf("gm2T")
    stat = salloc(ges, "stat", [128, 8, 4], F32)
    STB = [Buf(f"stat{i}") for i in range(8)]
    junk = salloc(ges, "junk", [128, 2048], BF16); b_junk = Buf("junk")
    stat_i = [0]

    T.dma("sp", ident_f[:], c_ident, b_identf, writes=[b_identf])
    T.dma("pool", ident_b[:], c_ident, b_identb, writes=[b_identb])
    T.dma("sp", ones_f[:], c_ones, b_onesf, writes=[b_onesf])
    T.dma("pool", ones_b[:], c_ones, b_onesb, writes=[b_onesb])

    def prologue(preA=None):
        with ExitStack() as es:
            sT = salloc(es, "sT", [128, 8, NSEQ], F32); b_sT = Buf("sT")
            cTs = salloc(es, "cTs", [128, 8, NSEQ], F32); b_cTs = Buf("cTs")
            bm = salloc(es, "bm", [128, 2, 48, NSEQ], F32); b_bm = Buf("bm")
            ga = salloc(es, "ga", [128, 2, 8, NSEQ], F32); b_ga = Buf("ga")
            gmm = salloc(es, "gmm", [128, 2, 8, NSEQ], F32); b_gmm = Buf("gmm")
            NW = 4
            wms = [salloc(es, f"wm{i}", [128, 8, 512], F32) for i in range(NW)]
            b_wms = [Buf(f"wm{i}") for i in range(NW)]
            modrow = salloc(es, "modrow", [NSEQ, 6 * D], F32); b_mrow = Buf("modrow")
            T.dma("sp", cTs[:], cT, b_cTs, writes=[b_cTs])
            T.dma("sp", bm[:], b_modT, b_bm, writes=[b_bm])
            T.dma("sp", ga[:], g_attnT, b_ga, writes=[b_ga])
            T.dma("sp", gmm[:], g_mlpT, b_gmm, writes=[b_gmm])
            T.op("act", lambda e: e.activation(out=sT[:], in_=cTs[:], func=AF.Silu),
                 reads=[b_cTs], writes=[b_sT])
            it = 0
            for l in range(nlayers):
                wl = w_mod[l].rearrange("(c p) n -> p c n", p=128)
                for ns in range(12):
                    sl = it % NW
                    bank = it % 2
                    it += 1
                    T.dma("sp", wms[sl][:], wl[:, :, ns * 512:(ns + 1) * 512], b_wms[sl],
                          writes=[b_wms[sl]])
                    for c in range(8):
                        T.op("pe", lambda e: e.matmul(
                            pb[bank][0:NSEQ, 0:512], lhsT=sT[:, c, :], rhs=wms[sl][:, c, :],
                            start=(c == 0), stop=(c == 7)),
                            reads=[b_wms[sl], b_sT], writes=[PB[bank]])
                    T.op("act", lambda e: e.activation(out=modrow[:, ns * 512:(ns + 1) * 512],
                                                       in_=pb[bank][0:NSEQ, 0:512], func=AF.Copy),
                         reads=[PB[bank]], writes=[b_mrow])
                for j in range(48):
                    T.op("pe", lambda e: e.transpose(pb[2][:, j * NSEQ:(j + 1) * NSEQ],
                                                     modrow[0:NSEQ, j * 128:(j + 1) * 128],
                                                     ident_f[0:NSEQ, 0:NSEQ]),
                         reads=[b_mrow, b_identf], writes=[PB[2]])
                T.op("dve", lambda e: e.tensor_tensor(
                    out=modT[:, l, :, :],
                    in0=pb[2][:, 0:48 * NSEQ].rearrange("p (j b) -> p j b", b=NSEQ),
                    in1=bm[:, l, :, :], op=ALU.add),
                    reads=[PB[2], b_bm], writes=[b_modT_])
                T.op("dve", lambda e: e.scalar_tensor_tensor(
                    out=gm1T[:, l], in0=modT[:, l, 8:16, :], scalar=1.0, in1=ga[:, l],
                    op0=ALU.add, op1=ALU.mult), reads=[b_modT_, b_ga], writes=[b_gm1])
                T.op("dve", lambda e: e.scalar_tensor_tensor(
                    out=gm2T[:, l], in0=modT[:, l, 32:40, :], scalar=1.0, in1=gmm[:, l],
                    op0=ALU.add, op1=ALU.mult), reads=[b_modT_, b_gmm], writes=[b_gm2])
            if preA is not None:
                load_A_weights(0, *preA)
            T.barrier()

    def next_stat():
        i = stat_i[0] % 8
        stat_i[0] += 1
        return stat[:, i, :], STB[i]

    def rstd_from(src_ap, src_bufs, n, from_psum=False):
        st, sbuf_ = next_stat()
        T.op("act", lambda e: e.activation(out=junk[:, 0:n], in_=src_ap, func=AF.Square,
                                           accum_out=st[:, 0:1]),
             reads=src_bufs, writes=[b_junk, sbuf_])
        T.op("dve", lambda e: e.tensor_scalar(out=st[:, 1:2], in0=st[:, 0:1], scalar1=1.0 / n,
                                              scalar2=EPS, op0=ALU.mult, op1=ALU.add),
             reads=[sbuf_], writes=[sbuf_])
        T.op("act", lambda e: e.activation(out=st[:, 2:3], in_=st[:, 1:2], func=AF.Sqrt),
             reads=[sbuf_], writes=[sbuf_])
        T.op("dve", lambda e: e.reciprocal(out=st[:, 3:4], in_=st[:, 2:3]),
             reads=[sbuf_], writes=[sbuf_])
        return st[:, 3:4], sbuf_

    def norm_to_T(xt_ap, bx, gmT_ap, shT_ap, bmods, xn, bxn, tbank, hT_dst, bhT):
        rstd, brs = rstd_from(xt_ap, [bx], D)
        T.op("dve", lambda e: e.tensor_scalar(out=xn[:], in0=xt_ap, scalar1=rstd, scalar2=None,
                                              op0=ALU.mult), reads=[bx, brs], writes=[bxn])
        pv = pbf(tbank).rearrange("p (c t) -> p c t", c=8)
        for c in range(8):
            T.op("pe", lambda e: e.transpose(pv[:, c, :], xn[:, c * 128:(c + 1) * 128], ident_b[:]),
                 reads=[bxn, b_identb], writes=[PB[tbank]])
        for c in range(8):
            if c % 2 == 0:
                T.op("act", lambda e: e.activation(out=hT_dst(c), in_=pv[:, c, :], func=AF.Identity,
                                                   scale=gmT_ap(c), bias=shT_ap(c)),
                     reads=[PB[tbank]] + bmods, writes=[bhT])
            else:
                T.op("dve", lambda e: e.tensor_scalar(out=hT_dst(c), in0=pv[:, c, :],
                                                      scalar1=gmT_ap(c), scalar2=shT_ap(c),
                                                      op0=ALU.mult, op1=ALU.add),
                     reads=[PB[tbank]] + bmods, writes=[bhT])

    def ga_bcast(es, l, chunk0, name):
        gb = salloc(es, name, [128, NSEQ, D], F32); bgb = Buf(name)
        dg = salloc(es, name + "dg", [128, 2, 128], F32); bdg = [Buf(name + "dg0"), Buf(name + "dg1")]
        k = 0
        for b in range(NSEQ):
            for half in range(2):
                bank = 6 + half
                for cc in range(4):
                    c = half * 4 + cc
                    sl = k % 2
                    k += 1
                    T.op("dve", lambda e: e.tensor_scalar(
                        out=dg[:, sl, :], in0=ident_f[:], scalar1=modT[:, l, chunk0 + c, b:b + 1],
                        scalar2=None, op0=ALU.mult), reads=[b_identf, b_modT_], writes=[bdg[sl]])
                    T.op("pe", lambda e: e.matmul(pb[bank][:, cc * 128:(cc + 1) * 128], lhsT=ones_f[:],
                                                  rhs=dg[:, sl, :], start=(cc == 0), stop=True,
                                                  skip_group_check=True),
                         reads=[b_onesf, bdg[sl]], writes=[PB[bank]])
                T.op("act", lambda e: e.activation(out=gb[:, b, half * 512:(half + 1) * 512],
                                                   in_=pb[bank][:], func=AF.Copy),
                     reads=[PB[bank]], writes=[bgb])
        return gb, bgb

    def load_A_weights(l, Wf, bWf, Wt, bWt):
        wl = wb_in[l].rearrange("(c p) n -> p c n", p=128)
        for (a, b, o) in [(0, 1024, 0), (1536, 2560, 1024), (2688, 3200, 2048),
                          (3200, 3264, 2560), (3200, 3264, 2624)]:
            for c in range(8):
                T.dma("sp", Wf[:, c, o:o + (b - a)], wl[:, c, a:b], bWf, reads=[B_wb_in[l]], writes=[bWf])
        for (a, b, o) in [(1024, 1536, 0), (2560, 2688, 512), (3264, 3272, 640)]:
            T.dma("sp", Wt[:, :, o:o + (b - a)], wl[:, :, a:b], bWt, reads=[B_wb_in[l]], writes=[bWt])

    def phase_A(l, pre=None):
        with ExitStack() as es:
            if pre is None:
                bg_flush(l)
                Wf = salloc(es, "Wf", [128, 8, 2688], BF16); bWf = Buf("Wf")
                Wt = salloc(es, "Wt", [128, 8, 648], BF16); bWt = Buf("Wt")
                load_A_weights(l, Wf, bWf, Wt, bWt)
            else:
                Wf, bWf, Wt, bWt = pre
            kvg = salloc(es, "kvg", [128, 128], F32); bkvg = Buf("kvg")
            T.dma("sp", kvg[:], kvg_bc[:, l, :], bkvg, writes=[bkvg])
            xts = [salloc(es, f"xt{i}", [128, D], F32) for i in range(3)]
            bxts = [Buf(f"xt{i}") for i in range(3)]
            xns = [salloc(es, f"xn{i}", [128, D], BF16) for i in range(2)]
            bxns = [Buf(f"xn{i}") for i in range(2)]
            hTs = [salloc(es, f"hT{i}", [128, 8, 512], BF16) for i in range(2)]
            bhTs = [Buf(f"hT{i}") for i in range(2)]
            vts = [salloc(es, f"vt{i}", [128, 512], BF16) for i in range(2)]
            bvts = [Buf(f"vt{i}") for i in range(2)]
            kvn = [salloc(es, f"kvn{i}", [128, 128], BF16) for i in range(2)]
            bkvn = [Buf(f"kvn{i}") for i in range(2)]
            kvTt = [salloc(es, f"kvTt{i}", [128, 128], BF16) for i in range(2)]
            bkvTt = [Buf(f"kvTt{i}") for i in range(2)]
            wis = [salloc(es, f"wis{i}", [128, 8], F32) for i in range(2)]
            bwis = [Buf(f"wis{i}") for i in range(2)]
            fos = [salloc(es, f"fo{i}", [128, 512], BF16) for i in range(3)]
            bfos = [Buf(f"fo{i}") for i in range(3)]
            src = x_in if l == 0 else xres
            cnt = {"ti": 0, "fi": 0}
            groups = [(s, g) for s in range(NSEQ) for g in range(4)]

            def prep_a(gi, j):
                s, g = groups[gi]
                hT = hTs[gi % 2]; bhT = bhTs[gi % 2]
                tt = g * 4 + j
                t0 = tt * 128
                ti = cnt["ti"]
                cnt["ti"] += 1
                xt = xts[ti % 3]; bxt = bxts[ti % 3]
                xn = xns[ti % 2]; bxn = bxns[ti % 2]
                T.dma("sp", xt[:], src[s, t0:t0 + 128, :], bxt, writes=[bxt])
                norm_to_T(xt[:], bxt,
                          lambda c: gm1T[:, l, c, s:s + 1],
                          lambda c: modT[:, l, 0 + c, s:s + 1],
                          [b_gm1, b_modT_], xn, bxn, ti % 2,
                          lambda c: hT[:, c, j * 128:(j + 1) * 128], bhT)
                return ti

            def prep_b(gi, j, ti):
                s, g = groups[gi]
                hT = hTs[gi % 2]; bhT = bhTs[gi % 2]
                t0 = (g * 4 + j) * 128
                k2 = ti % 2
                for c in range(8):
                    T.op("pe", lambda e: e.matmul(pb[2][:, 0:512], lhsT=hT[:, c, j * 128:(j + 1) * 128],
                                                  rhs=Wt[:, c, 0:512], start=(c == 0), stop=(c == 7)),
                         reads=[bhT, bWt], writes=[PB[2]])
                for c in range(8):
                    T.op("pe", lambda e: e.matmul(pb[3][:, 0:136], lhsT=hT[:, c, j * 128:(j + 1) * 128],
                                                  rhs=Wt[:, c, 512:648], start=(c == 0), stop=(c == 7)),
                         reads=[bhT, bWt], writes=[PB[3]])
                T.op("act", lambda e: e.activation(out=vts[k2][:], in_=pb[2][:, 0:512], func=AF.Copy),
                     reads=[PB[2]], writes=[bvts[k2]])
                T.dma("pool", v_scr[s, t0:t0 + 128, :], vts[k2][:], bvts[k2], reads=[bvts[k2]])
                rs2, brs2 = rstd_from(pb[3][:, 0:128], [PB[3]], 128)
                T.op("dve", lambda e: e.scalar_tensor_tensor(
                    out=kvn[k2][:], in0=pb[3][:, 0:128], scalar=rs2, in1=kvg[:],
                    op0=ALU.mult, op1=ALU.mult), reads=[PB[3], brs2, bkvg], writes=[bkvn[k2]])
                T.op("dve", lambda e: e.tensor_copy(out=wis[k2][:], in_=pb[3][:, 128:136]),
                     reads=[PB[3]], writes=[bwis[k2]])
                T.dma("pool", kv_tok[s, t0:t0 + 128, :], kvn[k2][:], bkvn[k2], reads=[bkvn[k2]])
                T.dma("pool", widx[s, t0:t0 + 128, :], wis[k2][:], bwis[k2], reads=[bwis[k2]])
                T.op("pe", lambda e: e.transpose(pbf(4)[:, 0:128], kvn[k2][:], ident_b[:]),
                     reads=[bkvn[k2], b_identb], writes=[PB[4]])
                T.op("act", lambda e: e.activation(out=kvTt[k2][:], in_=pbf(4)[:, 0:128], func=AF.Copy),
                     reads=[PB[4]], writes=[bkvTt[k2]])
                T.dma("pool", kvT_scr[s, :, t0:t0 + 128], kvTt[k2][:], bkvTt[k2], reads=[bkvTt[k2]])

            def fm_chunk(gi, ch):
                s, g = groups[gi]
                hT = hTs[gi % 2]; bhT = bhTs[gi % 2]
                fi = cnt["fi"]
                cnt["fi"] += 1
                bank = 5 + (fi % 3)
                fo = fos[fi % 3]; bfo = bfos[fi % 3]
                for c in range(8):
                    T.op("pe", lambda e: e.matmul(pb[bank][:, 0:512], lhsT=Wf[:, c, ch * 128:(ch + 1) * 128],
                                                  rhs=hT[:, c, :], start=(c == 0), stop=(c == 7)),
                         reads=[bhT, bWf], writes=[PB[bank]])
                if fi % 2 == 0:
                    T.op("act", lambda e: e.activation(out=fo[:], in_=pb[bank][:, 0:512], func=AF.Copy),
                         reads=[PB[bank]], writes=[bfo])
                else:
                    T.op("dve", lambda e: e.tensor_copy(out=fo[:], in_=pb[bank][:, 0:512]),
                         reads=[PB[bank]], writes=[bfo])
                T.dma("pool", featT[s, ch, :, g * 512:(g + 1) * 512], fo[:], bfo, reads=[bfo])

            for j in range(4):
                ti0 = prep_a(0, j)
                prep_b(0, j, ti0)
            for gi in range(len(groups)):
                pend_b = None
                for ch in range(21):
                    fm_chunk(gi, ch)
                    if gi + 1 < len(groups):
                        if ch in (1, 6, 11, 16):
                            j = (1, 6, 11, 16).index(ch)
                            pend_b = (j, prep_a(gi + 1, j))
                        if ch in (4, 9, 14, 19) and pend_b is not None:
                            prep_b(gi + 1, pend_b[0], pend_b[1])
                            pend_b = None
            T.barrier()

    def phase_B(l, cin0=None):
        with ExitStack() as es:
            qT = salloc(es, "qT", [128, 4, S], BF16); bqT = Buf("qT")
            kT = salloc(es, "kT", [128, 4, S], BF16); bkT = Buf("kT")
            vv = salloc(es, "vv", [128, NT, 512], BF16); bvv = Buf("vv")
            tri8 = salloc(es, "tri8", [128, 128], BF16); btri = Buf("tri8")
            neg8 = salloc(es, "neg8", [128, 128], BF16); bneg = Buf("neg8")
            cm = salloc(es, "cm", [128, 512], BF16); bcm = Buf("cm")
            goa = salloc(es, "goa", [128, 512], F32); bgoa = Buf("goa")
            T.dma("pool", tri8[:], c_tri8, btri, writes=[btri])
            T.dma("pool", neg8[:], c_neg8, bneg, writes=[bneg])
            T.dma("pool", cm[:], c_cmask[:, 0:4, :].rearrange("p h t -> p (h t)"), bcm, writes=[bcm])
            T.dma("sp", goa[:], goa_bc[:, l, :], bgoa, writes=[bgoa])
            e32a = [salloc(es, f"e32a{i}", [128, 1024], F32) for i in range(2)]
            spba = [salloc(es, f"spba{i}", [128, 1024], BF16) for i in range(2)]
            wba = [salloc(es, f"wba{i}", [128, 1024], BF16) for i in range(2)]
            e32 = [[e32a[i][:, g * 512:(g + 1) * 512] for i in range(2)] for g in range(2)]
            spb = [[spba[i][:, g * 512:(g + 1) * 512] for i in range(2)] for g in range(2)]
            wb = [[wba[i][:, g * 512:(g + 1) * 512] for i in range(2)] for g in range(2)]
            be32 = [[Buf(f"e32{g}{i}") for i in range(2)] for g in range(2)]
            bspb = [[Buf(f"spb{g}{i}") for i in range(2)] for g in range(2)]
            bwb = [[Buf(f"wb{g}{i}") for i in range(2)] for g in range(2)]
            sps = [salloc(es, f"sps{g}", [128, 512], F32) for g in range(2)]
            bsps = [Buf(f"sps{g}") for g in range(2)]
            spsb = [[salloc(es, f"spsb{g}{i}", [128, 512], BF16) for i in range(2)] for g in range(2)]
            bspsb = [[Buf(f"spsb{g}{i}") for i in range(2)] for g in range(2)]
            oan = [salloc(es, f"oan{i}", [128, 512], BF16) for i in range(2)]
            boan = [Buf(f"oan{i}") for i in range(2)]
            ZB = [[0, 2], [1, 3]]
            AB = [4, 5]
            OB = [6, 7]
            carry_slot = [0, 0]

            def zmm(bank, hg, qb, kb, start_first):
                for i in range(4):
                    h = 2 * i + hg
                    ch = h // 2
                    r0 = (h % 2) * 64
                    T.op("pe", lambda e: e.matmul(
                        pb[bank][:, i * 128:(i + 1) * 128],
                        lhsT=kT[r0:r0 + 64, ch, kb * 128:(kb + 1) * 128],
                        rhs=qT[r0:r0 + 64, ch, qb * 128:(qb + 1) * 128],
                        start=(start_first and i == 0), stop=(i == 3),
                        skip_group_check=True),
                        reads=[bkT, bqT], writes=[PB[bank]])

            def stage_Z(n, qb, kb):
                sl = n % 2
                for hg in range(2):
                    zmm(ZB[hg][sl], hg, qb, kb, True)
                T.op("act", lambda e: e.activation(out=e32a[sl][:], in_=pb2[sl][:], func=AF.Exp, scale=0.125),
                     reads=[PB[ZB[0][sl]], PB[ZB[1][sl]]], writes=[be32[0][sl], be32[1][sl]])
                T.op("act", lambda e: e.activation(out=spba[sl][:], in_=e32a[sl][:], func=AF.Ln, bias=1.0),
                     reads=[be32[0][sl], be32[1][sl]], writes=[bspb[0][sl], bspb[1][sl]])
                for hg in range(2):
                    if kb == qb:
                        T.op("dve", lambda e: e.tensor_tensor(out=spb[hg][sl][:], in0=spb[hg][sl][:], in1=cm[:], op=ALU.mult),
                             reads=[bspb[hg][sl], bcm], writes=[bspb[hg][sl]])

            def stage_A(n, qb, kb):
                sl = n % 2
                for hg in range(2):
                    ab = AB[hg]
                    T.op("pe", lambda e: e.matmul(pb[ab][:], lhsT=tri8[:], rhs=spb[hg][sl][:], start=True, stop=False,
                                                  skip_group_check=True),
                         reads=[btri, bspb[hg][sl]], writes=[PB[ab]])
                    if kb < qb:
                        cs = carry_slot[hg]
                        T.op("pe", lambda e: e.matmul(pb[ab][:], lhsT=neg8[:], rhs=spsb[hg][cs][:], start=False, stop=False,
                                                      skip_group_check=True),
                             reads=[bneg, bspsb[hg][cs]], writes=[PB[ab]])
                for hg in range(2):
                    zmm(AB[hg], hg, qb, kb, False)
                T.op("act", lambda e: e.activation(out=wba[sl][:], in_=pb2[2][:], func=AF.Exp, scale=0.125),
                     reads=[PB[AB[0]], PB[AB[1]]], writes=[bwb[0][sl], bwb[1][sl]])
                for hg in range(2):
                    ab = AB[hg]
                    if kb == qb:
                        T.op("dve", lambda e: e.tensor_tensor(out=wb[hg][sl][:], in0=wb[hg][sl][:], in1=cm[:], op=ALU.mult),
                             reads=[bwb[hg][sl], bcm], writes=[bwb[hg][sl]])
                    if kb > 0:
                        if kb == qb:
                            T.op("dve", lambda e: e.tensor_copy(out=sps[hg][:], in_=spb[hg][sl][:]),
                                 reads=[bspb[hg][sl]], writes=[bsps[hg]])
                        else:
                            T.op("dve", lambda e: e.tensor_tensor(out=sps[hg][:], in0=sps[hg][:], in1=spb[hg][sl][:], op=ALU.add),
                                 reads=[bsps[hg], bspb[hg][sl]], writes=[bsps[hg]])
                        carry_slot[hg] ^= 1
                        cs = carry_slot[hg]
                        T.op("dve", lambda e: e.tensor_copy(out=spsb[hg][cs][:], in_=sps[hg][:]),
                             reads=[bsps[hg]], writes=[bspsb[hg][cs]])

            def stage_PV(n, s, qb, kb):
                sl = n % 2
                ob = OB[qb % 2]
                for hg in range(2):
                    for i in range(4):
                        h = 2 * i + hg
                        T.op("pe", lambda e: e.matmul(
                            pb[ob][:, h * 64:(h + 1) * 64], lhsT=wb[hg][sl][:, i * 128:(i + 1) * 128],
                            rhs=vv[:, kb, h * 64:(h + 1) * 64], start=(kb == qb and hg == 0 and i == 0), stop=False,
                            skip_group_check=True),
                            reads=[bwb[hg][sl], bvv], writes=[PB[ob]])
                if kb == 0:
                    osl = qb % 2
                    rs, brs = rstd_from(pb[ob][:], [PB[ob]], 512)
                    T.op("dve", lambda e: e.scalar_tensor_tensor(out=oan[osl][:], in0=pb[ob][:], scalar=rs, in1=goa[:],
                                                                 op0=ALU.mult, op1=ALU.mult),
                         reads=[PB[ob], brs, bgoa], writes=[boan[osl]])
                    T.dma("pool", ocat[s, qb * 128:(qb + 1) * 128, 0:512], oan[osl][:], boan[osl], reads=[boan[osl]])
                    bg_step(1)
                    if cin0 is not None and s == LIM_SEQ - 1 and qb == min(9, LIM_QB - 1):
                        load_cin(cin0, 0, gate=[boan[osl]])

            for s in range(LIM_SEQ):
                T.dma("sp", qT[:], featT[s, 0:4].rearrange("c p t -> p c t"), bqT, writes=[bqT])
                T.dma("sp", kT[:], featT[s, 4:8].rearrange("c p t -> p c t"), bkT, writes=[bkT])
                T.dma("sp", vv[:], v_scr[s].rearrange("(n p) f -> p n f", p=128), bvv, writes=[bvv])
                its = [(qb, kb) for qb in range(LIM_QB) for kb in range(qb, -1, -1)]
                N = len(its)
                for t in range(N + 2):
                    if t < N:
                        stage_Z(t, *its[t])
                    if 0 <= t - 1 < N:
                        stage_A(t - 1, *its[t - 1])
                    if 0 <= t - 2 < N:
                        stage_PV(t - 2, s, *its[t - 2])
            T.barrier()

    def alloc_cin(es, s_):
        return dict(
            dqT=salloc(es, f"dqT{s_}", [128, 8, S], BF16), bdq=Buf(f"dqT{s_}"),
            iqT=salloc(es, f"iqT{s_}", [128, 4, S], BF16), biq=Buf(f"iqT{s_}"),
            ikT=salloc(es, f"ikT{s_}", [128, S], BF16), bik=Buf(f"ikT{s_}"),
            kvT=salloc(es, f"kvT{s_}", [128, S], BF16), bkvT=Buf(f"kvT{s_}"),
            kvt=salloc(es, f"kvt{s_}", [128, NT, 128], BF16), bkvt=Buf(f"kvt{s_}"),
            wi=salloc(es, f"wi{s_}", [128, NT, 8], F32), bwi=Buf(f"wi{s_}"))

    def load_cin(d_, s_, gate=()):
        g = list(gate)
        T.dma("sp", d_["iqT"][:], featT[s_, 16:20].rearrange("c p t -> p c t"), d_["biq"], reads=g, writes=[d_["biq"]])
        T.dma("sp", d_["ikT"][:], featT[s_, 20], d_["bik"], reads=g, writes=[d_["bik"]])
        T.dma("sp", d_["wi"][:], widx[s_].rearrange("(n p) j -> p n j", p=128), d_["bwi"], reads=g, writes=[d_["bwi"]])
        T.dma("sp", d_["kvT"][:], kvT_scr[s_], d_["bkvT"], reads=g, writes=[d_["bkvT"]])
        T.dma("sp", d_["kvt"][:], kv_tok[s_].rearrange("(n p) r -> p n r", p=128), d_["bkvt"], reads=g, writes=[d_["bkvt"]])
        T.dma("sp", d_["dqT"][:], featT[s_, 8:16].rearrange("c p t -> p c t"), d_["bdq"], reads=g, writes=[d_["bdq"]])

    def phase_C(l, cin0=None):
        if FLUSH_BG_BEFORE_C:
            bg_flush(99)
        with ExitStack() as es:
            cin = [cin0 if cin0 is not None else alloc_cin(es, 0)]
            for s_ in range(1, NSEQ):
                cin.append(alloc_cin(es, s_))
            cur = {}

            def load_seq_C(s_, gate=()):
                d_ = cin[s_]
                g = list(gate)
                T.dma("sp", d_["iqT"][:], featT[s_, 16:20].rearrange("c p t -> p c t"), d_["biq"], reads=g, writes=[d_["biq"]])
                T.dma("sp", d_["ikT"][:], featT[s_, 20], d_["bik"], reads=g, writes=[d_["bik"]])
                T.dma("sp", d_["wi"][:], widx[s_].rearrange("(n p) j -> p n j", p=128), d_["bwi"], reads=g, writes=[d_["bwi"]])
                T.dma("sp", d_["kvT"][:], kvT_scr[s_], d_["bkvT"], reads=g, writes=[d_["bkvT"]])
                T.dma("sp", d_["kvt"][:], kv_tok[s_].rearrange("(n p) r -> p n r", p=128), d_["bkvt"], reads=g, writes=[d_["bkvt"]])
                T.dma("sp", d_["dqT"][:], featT[s_, 8:16].rearrange("c p t -> p c t"), d_["bdq"], reads=g, writes=[d_["bdq"]])
            wuv = salloc(es, "wuv", [128, 8, 64], BF16); bwuv = Buf("wuv")
            gob = salloc(es, "gob", [128, 512], F32); bgob = Buf("gob")
            BN = salloc(es, "BN", [128, 2, 1024], BF16); bBN = Buf("BN")
            id4 = salloc(es, "id4", [128, 512], BF16); bid4 = Buf("id4")
            pw2 = salloc(es, "pw2", [128, KBIS], F32); bpw2 = Buf("pw2")
            T.dma("pool", wuv[:], w_uv[l].rearrange("h r d -> r h d"), bwuv, writes=[bwuv])
            T.dma("sp", gob[:], gob_bc[:, l, :], bgob, writes=[bgob])
            T.dma("sp", pw2[:], c_pow2, bpw2, writes=[bpw2])
            for i in range(4):
                T.dma("pool", id4[:, i * 128:(i + 1) * 128], c_ident, bid4, writes=[bid4])
            score = [salloc(es, f"score{i}", [128, S], F32) for i in range(2)]
            bsc = [Buf(f"score{i}") for i in range(2)]
            nmask = [salloc(es, f"nmask{i}", [128, S], BF16) for i in range(2)]
            bnm = [Buf(f"nmask{i}") for i in range(2)]
            PP = [salloc(es, f"PP{i}", [128, 1024], BF16) for i in range(2)]
            bPP = [Buf(f"PP{i}") for i in range(2)]
            oTs = salloc(es, "oTs", [128, 1024], BF16); boTs = Buf("oTs")
            bis = salloc(es, "bis", [128, 8 + 2 * KBIS], F32); bbis = Buf("bis")
            rden = salloc(es, "rden", [128, 8], F32); brden = Buf("rden")
            obf = salloc(es, "obf", [128, 512], F32); bobf = Buf("obf")
            obn = [salloc(es, f"obn{i}", [128, 512], BF16) for i in range(2)]
            bobn = [Buf(f"obn{i}") for i in range(2)]
            lsc = 128 ** -0.5
            isc = (64 ** -0.5) * (8 ** -0.5)
            with ExitStack() as es2:
                bn = salloc(es2, "bn", [128, 2, 1024], F32); bbn = Buf("bn")
                bf = salloc(es2, "bf", [128, 1024], F32); bbf = Buf("bf")
                adn = salloc(es2, "adn", [128, 1024], F32); badn = Buf("adn")
                T.dma("sp", bn[:], biasn.rearrange("p o h t -> p o (h t)"), bbn, writes=[bbn])
                T.dma("sp", bf[:], bfar_bc.rearrange("p h t -> p (h t)"), bbf, writes=[bbf])
                T.dma("sp", adn[:], c_admneg.rearrange("p h t -> p (h t)"), badn, writes=[badn])
                for o in range(2):
                    T.op("dve", lambda e: e.tensor_tensor(out=bn[:, o, :], in0=bn[:, o, :], in1=bf[:], op=ALU.subtract),
                         reads=[bbn, bbf], writes=[bbn])
                    if o == 0:
                        T.op("dve", lambda e: e.scalar_tensor_tensor(out=BN[:, o, :], in0=bn[:, o, :], scalar=1.0 / lsc, in1=adn[:],
                                                                     op0=ALU.mult, op1=ALU.add),
                             reads=[bbn, badn], writes=[bBN])
                    else:
                        T.op("dve", lambda e: e.tensor_scalar(out=BN[:, o, :], in0=bn[:, o, :], scalar1=1.0 / lsc, scalar2=None,
                                                              op0=ALU.mult), reads=[bbn], writes=[bBN])
                T.barrier()
            cnt_i = {"ii": 0, "pi": 0, "oi": 0, "lp": 0, "ib": 0}
            IBS = (2, 3)
            SB = 4
            NRB = 2 * ACC_DEPTH + 2
            Rb = [salloc(es, f"Rb{i}", [128, 512], BF16) for i in range(NRB)]
            bRb = [Buf(f"Rb{i}") for i in range(NRB)]
            dg = [salloc(es, f"dg{i}", [128, 8, 128], BF16) for i in range(2)]
            bdg = [Buf(f"dg{i}") for i in range(2)]
            absw = salloc(es, "absw", [128, NT, 8], F32); babsw = Buf("absw")
            sgn = salloc(es, "sgn", [128, NT, 8], F32); bsgn = Buf("sgn")
            OTB = (5, 6)
            DB = 7

            pend_acc = []

            def flush_acc(keep=0):
                while len(pend_acc) > keep:
                    (qb, c0, w, j, rsl, dsl) = pend_acc.pop(0)
                    sc_ = score[qb % 2]; bsc_ = bsc[qb % 2]
                    T.op("pe", lambda e: e.matmul(pb[SB][:, 0:w], lhsT=dg[dsl][:, j, :], rhs=Rb[rsl][:, 0:w],
                                                  start=(j == 0), stop=(j == 7), skip_group_check=True),
                         reads=[bdg[dsl], bRb[rsl]], writes=[PB[SB]])
                    if j == 7:
                        T.op("act", lambda e: e.activation(out=sc_[:, c0:c0 + w], in_=pb[SB][:, 0:w], func=AF.Copy),
                             reads=[PB[SB]], writes=[bsc_])

            def make_diag(qb):
                dsl = qb % 2
                for j in range(8):
                    T.op("act", lambda e: e.activation(out=dg[dsl][:, j, :], in_=ident_b[:], func=AF.Identity,
                                                       scale=sgn[:, qb, j:j + 1]),
                         reads=[b_identb, bsgn], writes=[bdg[dsl]])

            def idx_unit(s, qb, c, j):
                n = (qb + 1) * 128
                q0 = qb * 128
                c0 = c * 512
                w = min(512, n - c0)
                rsl = cnt_i["ii"] % NRB
                cnt_i["ii"] += 1
                ch = j // 2
                r0 = (j % 2) * 64
                IB = IBS[cnt_i["ib"] % 2]
                cnt_i["ib"] += 1
                T.op("pe", lambda e: e.matmul(pb[IB][:, 0:w], lhsT=cur['iqT'][r0:r0 + 64, ch, q0:q0 + 128],
                                              rhs=cur['ikT'][r0:r0 + 64, c0:c0 + w], start=True, stop=True),
                     reads=[cur['biq'], cur['bik']], writes=[PB[IB]])
                T.op("act", lambda e: e.activation(out=Rb[rsl][:, 0:w], in_=pb[IB][:, 0:w], func=AF.Relu,
                                                   scale=absw[:, qb, j:j + 1]),
                     reads=[PB[IB], babsw], writes=[bRb[rsl]])
                flush_acc(keep=ACC_DEPTH - 1)
                pend_acc.append((qb, c0, w, j, rsl, qb % 2))

            def select(qb):
                n = (qb + 1) * 128
                nm = nmask[qb % 2]; bn_ = bnm[qb % 2]
                score_ = score[qb % 2]; bsc_ = bsc[qb % 2]
                T.op("dve", lambda e: e.memset(score_[0:64, n - 64:n], -1.0e30), reads=[bsc_], writes=[bsc_])
                if qb >= 2:
                    hi = bis[:, 0:1]; lo = bis[:, 1:2]; w0 = bis[:, 2:3]; mid = bis[:, 3:4]
                    cnt = bis[:, 4:5]; sv = bis[:, 5:6]; thr = bis[:, 6:7]
                    H = bis[:, 8:8 + KBIS]; H2 = bis[:, 8 + KBIS:8 + 2 * KBIS]
                    T.op("dve", lambda e: e.tensor_reduce(out=hi, in_=score_[:, 0:n], axis=AX.X, op=ALU.max),
                         reads=[bsc_], writes=[bbis])
                    T.op("dve", lambda e: e.tensor_reduce(out=lo, in_=score_[:, 0:n - 64], axis=AX.X, op=ALU.min),
                         reads=[bsc_], writes=[bbis])
                    T.op("dve", lambda e: e.tensor_tensor(out=w0, in0=hi, in1=lo, op=ALU.subtract),
                         reads=[bbis], writes=[bbis])
                    T.op("dve", lambda e: e.tensor_scalar(out=H, in0=pw2[:], scalar1=w0, scalar2=None, op0=ALU.mult),
                         reads=[bbis, bpw2], writes=[bbis])
                    T.op("dve", lambda e: e.tensor_scalar(out=H2, in0=H, scalar1=2.0, scalar2=None, op0=ALU.mult),
                         reads=[bbis], writes=[bbis])
                    T.op("dve", lambda e: e.tensor_tensor(out=mid, in0=lo, in1=bis[:, 8:9], op=ALU.add),
                         reads=[bbis], writes=[bbis])
                    for k in range(KBIS):
                        T.op("dve", lambda e: e.tensor_scalar(out=junk[:, 0:n], in0=score_[:, 0:n], scalar1=mid, scalar2=None,
                                                              op0=ALU.is_ge, op1=ALU.add, accum_out=cnt),
                             reads=[bsc_, bbis], writes=[b_junk, bbis])
                        if k < KBIS - 1:
                            T.op("dve", lambda e: e.tensor_scalar(out=sv, in0=cnt, scalar1=float(TOPK),
                                                                  scalar2=bis[:, 8 + KBIS + k + 1:8 + KBIS + k + 2],
                                                                  op0=ALU.is_ge, op1=ALU.mult),
                                 reads=[bbis], writes=[bbis])
                            T.op("dve", lambda e: e.scalar_tensor_tensor(out=mid, in0=sv, scalar=bis[:, 8 + k + 1:8 + k + 2],
                                                                         in1=mid, op0=ALU.subtract, op1=ALU.add),
                                 reads=[bbis], writes=[bbis])
                        else:
                            T.op("dve", lambda e: e.tensor_scalar(out=sv, in0=cnt, scalar1=float(TOPK),
                                                                  scalar2=bis[:, 8 + k:8 + k + 1],
                                                                  op0=ALU.is_ge, op1=ALU.mult),
                                 reads=[bbis], writes=[bbis])
                            T.op("dve", lambda e: e.scalar_tensor_tensor(out=thr, in0=sv, scalar=bis[:, 8 + k:8 + k + 1],
                                                                         in1=mid, op0=ALU.subtract, op1=ALU.add),
                                 reads=[bbis], writes=[bbis])
                    T.op("dve", lambda e: e.tensor_scalar(out=nm[:, 0:n], in0=score_[:, 0:n], scalar1=thr, scalar2=-1.0e5,
                                                          op0=ALU.is_lt, op1=ALU.mult), reads=[bsc_, bbis], writes=[bn_])
                else:
                    T.op("dve", lambda e: e.tensor_scalar(out=nm[:, 0:n], in0=score_[:, 0:n], scalar1=-1.0e29, scalar2=-1.0e5,
                                                          op0=ALU.is_lt, op1=ALU.mult), reads=[bsc_], writes=[bn_])

            def att_logits(s, qb, kb):
                q0 = qb * 128
                nm = nmask[qb % 2]; bn_ = bnm[qb % 2]
                lp = cnt_i["lp"] % 2
                cnt_i["lp"] += 1
                P = PP[lp]; bP = bPP[lp]
                off = qb - kb
                for (bank, h0) in ((0, 0), (1, 4)):
                    T.op("pe", lambda e: e.matmul(
                        pb[bank][:].rearrange("p (h t) -> p h t", h=4),
                        lhsT=cur['kvT'][:, kb * 128:(kb + 1) * 128], rhs=cur['dqT'][:, h0:h0 + 4, q0:q0 + 128],
                        start=True, stop=False, skip_group_check=True), reads=[cur['bkvT'], cur['bdq']], writes=[PB[bank]])
                    T.op("pe", lambda e: e.matmul(pb[bank][:], lhsT=nm[:, kb * 128:(kb + 1) * 128], rhs=id4[:],
                                                  start=False, stop=(off >= 2), skip_group_check=True),
                         reads=[bn_, bid4], writes=[PB[bank]])
                    if off < 2:
                        T.op("pe", lambda e: e.matmul(pb[bank][:], lhsT=ident_b[:], rhs=BN[:, off, h0 * 128:(h0 + 4) * 128],
                                                      start=False, stop=True, skip_group_check=True),
                             reads=[b_identb, bBN], writes=[PB[bank]])
                T.op("act", lambda e: e.activation(out=P[:], in_=pb2[0][:], func=AF.Exp, scale=lsc),
                     reads=[PB[0], PB[1]], writes=[bP])
                return lp

            def att_pv(s, qb, kb, lp):
                P = PP[lp]; bP = bPP[lp]
                for (bank, h0) in ((OTB[0], 0), (OTB[1], 4)):
                    T.op("pe", lambda e: e.matmul(pb[bank][:], lhsT=cur['kvt'][:, kb, :], rhs=P[:, h0 * 128:(h0 + 4) * 128],
                                                  start=(kb == 0), stop=(kb == qb)),
                         reads=[cur['bkvt'], bP], writes=[PB[bank]])
                for h in range(8):
                    T.op("pe", lambda e: e.matmul(pb[DB][:, h:h + 1], lhsT=P[:, h * 128:(h + 1) * 128], rhs=ones_b[:, 0:1],
                                                  start=(kb == 0 and h == 0), stop=(kb == qb), skip_group_check=True),
                         reads=[bP, b_onesb], writes=[PB[DB]])

            def epilogue(s, qb):
                q0 = qb * 128
                T.op("act", lambda e: e.activation(out=oTs[:, 0:512], in_=pb[OTB[0]][:], func=AF.Copy), reads=[PB[OTB[0]]], writes=[boTs])
                T.op("act", lambda e: e.activation(out=oTs[:, 512:1024], in_=pb[OTB[1]][:], func=AF.Copy), reads=[PB[OTB[1]]], writes=[boTs])
                T.op("dve", lambda e: e.reciprocal(out=rden[:], in_=pb[DB][:, 0:8]), reads=[PB[DB]], writes=[brden])
                eb = IBS[cnt_i["ib"] % 2]
                cnt_i["ib"] += 1
                for h in range(8):
                    T.op("pe", lambda e: e.matmul(pb[eb][:, h * 64:(h + 1) * 64], lhsT=oTs[:, h * 128:(h + 1) * 128],
                                                  rhs=wuv[:, h, :], start=(h == 0), stop=True, skip_group_check=True),
                         reads=[boTs, bwuv], writes=[PB[eb]])
                for h in range(8):
                    T.op("dve", lambda e: e.tensor_scalar(out=obf[:, h * 64:(h + 1) * 64], in0=pb[eb][:, h * 64:(h + 1) * 64],
                                                          scalar1=rden[:, h:h + 1], scalar2=None, op0=ALU.mult),
                         reads=[PB[eb], brden], writes=[bobf])
                rs, brs = rstd_from(obf[:], [bobf], 512)
                osl = cnt_i["oi"] % 2
                cnt_i["oi"] += 1
                T.op("dve", lambda e: e.scalar_tensor_tensor(out=obn[osl][:], in0=obf[:], scalar=rs, in1=gob[:],
                                                             op0=ALU.mult, op1=ALU.mult),
                     reads=[bobf, brs, bgob], writes=[bobn[osl]])
                T.dma("pool", ocat[s, q0:q0 + 128, 512:1024], obn[osl][:], bobn[osl], reads=[bobn[osl]])
                if s == 0 and LIM_SEQ > 1 and qb == min(8, LIM_QB - 1):
                    load_seq_C(1, gate=[bobn[osl]])

            for s in range(LIM_SEQ):
                if s == 0 and cin0 is None:
                    load_seq_C(0)
                cur.clear(); cur.update(cin[s])
                wi = cur["wi"]; bwi = cur["bwi"]
                T.op("act", lambda e: e.activation(out=absw[:], in_=wi[:], func=AF.Abs, scale=isc),
                     reads=[bwi], writes=[babsw])
                T.op("dve", lambda e: e.tensor_scalar(out=sgn[:], in0=wi[:], scalar1=0.0, scalar2=2.0,
                                                      op0=ALU.is_ge, op1=ALU.mult), reads=[bwi], writes=[bsgn])
                T.op("dve", lambda e: e.tensor_scalar(out=sgn[:], in0=sgn[:], scalar1=-1.0, scalar2=None,
                                                      op0=ALU.add), reads=[bsgn], writes=[bsgn])
                for step in range(LIM_QB + 2):
                    qi = step
                    qs = step - 1
                    qa = step - 2
                    iu = []
                    if qi < LIM_QB:
                        n = (qi + 1) * 128
                        iu = [(c, j) for c in range((n + 511) // 512) for j in range(8)]
                        make_diag(qi)
                    if 0 <= qs < LIM_QB:
                        select(qs)
                    au = list(range(qa + 1)) if qa >= 0 else []
                    na, ni = len(au), len(iu)
                    ai = 0
                    ii_ = 0
                    pend = None
                    total = max(na, 1)
                    while ai < na or ii_ < ni:
                        tgt = ni if ai >= na else (ni * (ai + 1)) // total
                        while ii_ < tgt:
                            idx_unit(s, qi, *iu[ii_])
                            ii_ += 1
                        if ai < na:
                            lp = att_logits(s, qa, au[ai])
                            if pend is not None:
                                att_pv(s, qa, *pend)
                            pend = (au[ai], lp)
                            ai += 1
                    if pend is not None:
                        att_pv(s, qa, *pend)
                    flush_acc()
                    if qa >= 0:
                        epilogue(s, qa)
                        bg_step(1)
            T.barrier()

    def phase_D(l, prefetch=()):
        prefetch = list(prefetch)
        bg_flush(l)
        with ExitStack() as es:
            Wo = salloc(es, "Wo", [128, 8, D], BF16); bWo = Buf("Wo")
            wl = wb_out[l].rearrange("(c p) n -> p c n", p=128)
            for c in range(8):
                T.dma("sp", Wo[:, c, :], wl[:, c, :], bWo, reads=[B_wb_out[l]], writes=[bWo])
            gb, bgb = ga_bcast(es, l, 16, "ga1")
            oc = [salloc(es, f"oc{i}", [128, D], BF16) for i in range(2)]
            boc = [Buf(f"oc{i}") for i in range(2)]
            oT = [salloc(es, f"oT{i}", [128, 8, 128], BF16) for i in range(2)]
            boT = [Buf(f"oT{i}") for i in range(2)]
            xts = [salloc(es, f"xd{i}", [128, D], F32) for i in range(2)]
            bxts = [Buf(f"xd{i}") for i in range(2)]
            tmp = [salloc(es, f"tm{i}", [128, D], F32) for i in range(2)]
            btmp = [Buf(f"tm{i}") for i in range(2)]
            src = x_in if l == 0 else xres
            ti = 0
            for s in range(NSEQ):
                for tt in range(NT):
                    t0 = tt * 128
                    k2 = ti % 2
                    ti += 1
                    T.dma("sp", oc[k2][:], ocat[s, t0:t0 + 128, :], boc[k2], writes=[boc[k2]])
                    T.dma("sp", xts[k2][:], src[s, t0:t0 + 128, :], bxts[k2], writes=[bxts[k2]])
                    if prefetch:
                        prefetch.pop(0)()
                    pv = pbf(k2).rearrange("p (c t) -> p c t", c=8)
                    for c in range(8):
                        T.op("pe", lambda e: e.transpose(pv[:, c, :], oc[k2][:, c * 128:(c + 1) * 128], ident_b[:]),
                             reads=[boc[k2], b_identb], writes=[PB[k2]])
                    T.op("act", lambda e: e.activation(out=oT[k2][:].rearrange("p c t -> p (c t)"), in_=pbf(k2)[:, 0:1024], func=AF.Copy),
                         reads=[PB[k2]], writes=[boT[k2]])
                    for half in range(2):
                        bank = 2 + k2 * 2 + half
                        for c in range(8):
                            T.op("pe", lambda e: e.matmul(pb[bank][:], lhsT=oT[k2][:, c, :], rhs=Wo[:, c, half * 512:(half + 1) * 512],
                                                          start=(c == 0), stop=(c == 7)),
                                 reads=[boT[k2], bWo], writes=[PB[bank]])
                        T.op("dve", lambda e: e.tensor_tensor(out=tmp[k2][:, half * 512:(half + 1) * 512], in0=pb[bank][:],
                                                              in1=gb[:, s, half * 512:(half + 1) * 512], op=ALU.mult),
                             reads=[PB[bank], bgb], writes=[btmp[k2]])
                    T.op("dve", lambda e: e.tensor_tensor(out=tmp[k2][:], in0=tmp[k2][:], in1=xts[k2][:], op=ALU.add),
                         reads=[btmp[k2], bxts[k2]], writes=[btmp[k2]])
                    T.dma("pool", xres[s, t0:t0 + 128, :], tmp[k2][:], btmp[k2], reads=[btmp[k2]])
            while prefetch:
                prefetch.pop(0)()
            T.barrier()

    def phase_DE(l, last):
        with ExitStack() as esw:
            Wu = salloc(esw, "Wu", [128, 8, DFF], BF16); bWu = Buf("Wu")
            Wd = salloc(esw, "Wd", [128, 32, D], BF16); bWd = Buf("Wd")
            wul = wb_up[l].rearrange("(c p) n -> p c n", p=128)
            wdl = wb_dn[l].rearrange("(c p) n -> p c n", p=128)
            pf = []
            for c in range(8):
                for hh in range(2):
                    pf.append(lambda c=c, hh=hh: T.dma(
                        "sp", Wu[:, c, hh * 2048:(hh + 1) * 2048], wul[:, c, hh * 2048:(hh + 1) * 2048], bWu,
                        reads=[B_wb_up[l]], writes=[bWu]))
            for c4 in range(8):
                pf.append(lambda c4=c4: T.dma(
                    "sp", Wd[:, c4 * 4:(c4 + 1) * 4, :], wdl[:, c4 * 4:(c4 + 1) * 4, :], bWd,
                    reads=[B_wb_dn[l]], writes=[bWd]))
            phase_D(l, prefetch=pf)
            phase_E(l, last, Wu, bWu, Wd, bWd)

    def phase_E(l, last, Wu, bWu, Wd, bWd):
        with ExitStack() as es:
            gb, bgb = ga_bcast(es, l, 40, "ga2")
            if last:
                gf = salloc(es, "gf", [128, D], F32); bgf = Buf("gf")
                T.dma("sp", gf[:], gfin_bc, bgf, writes=[bgf])
            TG = 256
            xts = [salloc(es, f"xe{i}", [128, D], F32) for i in range(4)]
            bxts = [Buf(f"xe{i}") for i in range(4)]
            xns = [salloc(es, f"xne{i}", [128, D], BF16) for i in range(2)]
            bxns = [Buf(f"xne{i}") for i in range(2)]
            hTs = [salloc(es, f"hTe{i}", [128, 8, TG], BF16) for i in range(2)]
            bhTs = [Buf(f"hTe{i}") for i in range(2)]
            aT = salloc(es, "aT", [128, 32, TG], BF16); baT = [Buf(f"aT{i}") for i in range(32)]
            rl = [salloc(es, f"rl{i}", [128, TG], BF16) for i in range(2)]
            brl = [Buf(f"rl{i}") for i in range(2)]
            ti = 0
            gi = 0
            ui = 0
            for s in range(NSEQ):
                for g in range(S // TG):
                    hT = hTs[gi % 2]; bhT = bhTs[gi % 2]
                    gi += 1
                    tiles = []
                    for j in range(TG // 128):
                        tt = g * (TG // 128) + j
                        t0 = tt * 128
                        xt = xts[ti % 4]; bxt = bxts[ti % 4]
                        xn = xns[ti % 2]; bxn = bxns[ti % 2]
                        tb = ti % 2
                        ti += 1
                        tiles.append((t0, xt, bxt))
                        T.dma("sp", xt[:], xres[s, t0:t0 + 128, :], bxt, writes=[bxt])
                        norm_to_T(xt[:], bxt,
                                  lambda c: gm2T[:, l, c, s:s + 1],
                                  lambda c: modT[:, l, 24 + c, s:s + 1],
                                  [b_gm2, b_modT_], xn, bxn, tb,
                                  lambda c: hT[:, c, j * 128:(j + 1) * 128], bhT)
                    for f in range(32):
                        bank = 2 + (ui % 2)
                        sl = ui % 2
                        ui += 1
                        for c in range(8):
                            T.op("pe", lambda e: e.matmul(pb[bank][:, 0:TG], lhsT=Wu[:, c, f * 128:(f + 1) * 128], rhs=hT[:, c, :],
                                                          start=(c == 0), stop=(c == 7)),
                                 reads=[bWu, bhT], writes=[PB[bank]])
                        T.op("act", lambda e: e.activation(out=rl[sl][:], in_=pb[bank][:, 0:TG], func=AF.Relu),
                             reads=[PB[bank]], writes=[brl[sl]])
                        T.op("dve", lambda e: e.tensor_tensor(out=aT[:, f, :], in0=rl[sl][:], in1=rl[sl][:], op=ALU.mult),
                             reads=[brl[sl]], writes=[baT[f]])
                    for j, (t0, xt, bxt) in enumerate(tiles):
                        for half in range(2):
                            bank = 4 + (j % 2) * 2 + half
                            for f in range(32):
                                T.op("pe", lambda e: e.matmul(pb[bank][:], lhsT=aT[:, f, j * 128:(j + 1) * 128],
                                                              rhs=Wd[:, f, half * 512:(half + 1) * 512], start=(f == 0), stop=(f == 31)),
                                     reads=[baT[f], bWd], writes=[PB[bank]])
                            T.op("dve", lambda e: e.tensor_tensor(out=tmpE[j % 2][:, half * 512:(half + 1) * 512], in0=pb[bank][:],
                                                                  in1=gb[:, s, half * 512:(half + 1) * 512], op=ALU.mult),
                                 reads=[PB[bank], bgb], writes=[btmpE[j % 2]])
                        T.op("dve", lambda e: e.tensor_tensor(out=xt[:], in0=tmpE[j % 2][:], in1=xt[:], op=ALU.add),
                             reads=[btmpE[j % 2], bxt], writes=[bxt])
                        if not last:
                            T.dma("pool", xres[s, t0:t0 + 128, :], xt[:], bxt, reads=[bxt])
                        else:
                            rs, brs = rstd_from(xt[:], [bxt], D)
                            T.op("dve", lambda e: e.scalar_tensor_tensor(out=tmpE[j % 2][:], in0=xt[:], scalar=rs, in1=gf[:],
                                                                         op0=ALU.mult, op1=ALU.mult),
                                 reads=[bxt, brs, bgf], writes=[btmpE[j % 2]])
                            T.dma("pool", out_d[s, t0:t0 + 128, :], tmpE[j % 2][:], btmpE[j % 2], reads=[btmpE[j % 2]])
            T.barrier()

    tmpE = []
    btmpE = [Buf("tmpE0"), Buf("tmpE1")]

    tmpE.append(salloc(ges, "tmpE0", [128, D], F32))
    tmpE.append(salloc(ges, "tmpE1", [128, D], F32))

    convert_weights()
    esA0 = ExitStack()
    Wf0 = salloc(esA0, "Wf0", [128, 8, 2688], BF16); bWf0 = Buf("Wf0")
    Wt0 = salloc(esA0, "Wt0", [128, 8, 648], BF16); bWt0 = Buf("Wt0")
    preA = (Wf0, bWf0, Wt0, bWt0)
    prologue(preA)
    for l in range(nlayers):
        if "A" in phases:
            phase_A(l, pre=preA if l == 0 else None)
        if l == 0:
            esA0.close()
        if "B" in phases and "C" in phases:
            with ExitStack() as esC0:
                cin0 = alloc_cin(esC0, 0)
                phase_B(l, cin0)
                phase_C(l, cin0)
        else:
            if "B" in phases:
                phase_B(l)
            if "C" in phases:
                phase_C(l)
        if "D" in phases and "E" in phases:
            phase_DE(l, last=(l == nlayers - 1))
        elif "D" in phases:
            phase_D(l)
    T.barrier()
    ges.close()
    return nc, T


def _t5_bucket(rel):
    nb = 16
    max_exact = 8
    base = np.where(rel > 0, nb, 0)
    n = np.abs(rel)
    nf = np.maximum(n, max_exact).astype(np.float32)
    large = max_exact + (np.log(nf / np.float32(max_exact)) / np.float32(math.log(128 / max_exact))
                         * np.float32(nb - max_exact)).astype(np.int32)
    large = np.minimum(large, nb - 1)
    return base + np.where(n < max_exact, n, large)


def _consts():
    p = np.arange(128)
    c = {}
    c["c_ident"] = np.eye(128, dtype=np.float32)
    c["c_tri8"] = np.where(p[:, None] >= p[None, :], -8.0, 0.0).astype(np.float32)
    c["c_neg8"] = np.full((128, 128), -8.0, np.float32)
    cm = (p[:, None] < p[None, :]).astype(np.float32)
    c["c_cmask"] = np.ascontiguousarray(np.broadcast_to(cm[:, None, :], (128, 8, 128)))
    adm = ((p[:, None] // 64) <= (p[None, :] // 64)).astype(np.float32)
    c["c_adm"] = np.ascontiguousarray(np.broadcast_to(adm[:, None, :], (128, 8, 128)))
    c["c_admneg"] = np.ascontiguousarray(np.broadcast_to(np.where(adm > 0, 0.0, -1.0e5).astype(np.float32)[:, None, :], (128, 8, 128)))
    c["c_pow2"] = np.ascontiguousarray(np.broadcast_to(
        (0.5 ** np.arange(1, KBIS + 1)).astype(np.float32)[None, :], (128, KBIS)))
    c["c_ones"] = np.ones((128, 128), np.float32)
    return c


def _prep_inputs(inp, core):
    f = np.float32
    b0 = core * NSEQ
    bs = slice(b0, b0 + NSEQ)
    m = {}
    m["x"] = np.ascontiguousarray(inp["x"][bs], dtype=f)
    c = np.asarray(inp["c"], dtype=f)[bs]
    m["cT"] = np.ascontiguousarray(c.reshape(NSEQ, 8, 128).transpose(2, 1, 0))
    m["w_mod"] = np.ascontiguousarray(inp["w_mod"], dtype=f)
    bm = np.asarray(inp["b_mod"], dtype=f).reshape(2, 48, 128).transpose(2, 0, 1)
    m["b_modT"] = np.ascontiguousarray(np.broadcast_to(bm[..., None], (128, 2, 48, NSEQ)))
    ga = np.asarray(inp["g_attn"], dtype=f).reshape(2, 8, 128).transpose(2, 0, 1)
    m["g_attnT"] = np.ascontiguousarray(np.broadcast_to(ga[..., None], (128, 2, 8, NSEQ)))
    gm = np.asarray(inp["g_mlp"], dtype=f).reshape(2, 8, 128).transpose(2, 0, 1)
    m["g_mlpT"] = np.ascontiguousarray(np.broadcast_to(gm[..., None], (128, 2, 8, NSEQ)))
    m["w_in"] = np.ascontiguousarray(inp["w_in"], dtype=f)
    m["kvg_bc"] = np.ascontiguousarray(np.broadcast_to(np.asarray(inp["kv_norm_g"], dtype=f)[None], (128, 2, 128)))
    m["w_uv"] = np.ascontiguousarray(inp["w_uv"], dtype=f)
    m["goa_bc"] = np.ascontiguousarray(np.broadcast_to(np.asarray(inp["g_out_a"], dtype=f)[None], (128, 2, 512)))
    m["gob_bc"] = np.ascontiguousarray(np.broadcast_to(np.asarray(inp["g_out_b"], dtype=f)[None], (128, 2, 512)))
    m["w_out"] = np.ascontiguousarray(inp["w_out"], dtype=f)
    m["w_up"] = np.ascontiguousarray(inp["w_up"], dtype=f)
    m["w_down"] = np.ascontiguousarray(inp["w_down"], dtype=f)
    rb = np.asarray(inp["rel_bias"], dtype=f)
    p = np.arange(128)
    bn = np.empty((128, 2, 8, 128), f)
    for off in range(2):
        rel = (p[:, None] - off * 128) - p[None, :]
        bk = _t5_bucket(rel.astype(np.int32))
        bn[:, off] = rb[bk].transpose(0, 2, 1)
    m["biasn"] = bn
    far = rb[_t5_bucket(np.array([-1000], np.int32))[0]]
    m["bfar_bc"] = np.ascontiguousarray(np.broadcast_to(far[None, :, None], (128, 8, 128)))
    m["gfin_bc"] = np.ascontiguousarray(np.broadcast_to(np.asarray(inp["g_final"], dtype=f)[None], (128, D)))
    m.update(_consts())
    return m


_CACHE = {}


def kernel(**inputs):
    if "nc" not in _CACHE:
        _CACHE["nc"] = build_program()[0]
    nc = _CACHE["nc"]
    in_maps = [_prep_inputs(inputs, core) for core in range(8)]
    res = run_bass_kernel_spmd(nc, in_maps, core_ids=list(range(8)))
    out = np.concatenate([np.asarray(r["out"]) for r in res.results], axis=0)
    return out.astype(np.float32, copy=False)
```

```python
import math
from contextlib import ExitStack

import numpy as np
import concourse.bass as bass
import concourse.mybir as mybir
from concourse.bass_utils import run_bass_kernel_spmd

F32 = mybir.dt.float32
BF16 = mybir.dt.bfloat16
AF = mybir.ActivationFunctionType
ALU = mybir.AluOpType
AX = mybir.AxisListType

S = 2048
D = 1024
NSEQ = 2
NT = S // 128
DFF = 4096
DIN = 3272
EPS = 1e-6
KBIS = 16
TOPK = 256
EPOCH = 30000
LIM_SEQ = NSEQ
LIM_QB = NT
NO_POOL = False
ACC_DEPTH = 2
FLUSH_BG_BEFORE_C = False


class Buf:
    __slots__ = ("name", "last_w", "readers", "dsem", "bg")

    def __init__(self, name, bg=False):
        self.name = name
        self.last_w = None
        self.readers = []
        self.dsem = {}
        self.bg = bg


class Tracker:
    def __init__(self, nc):
        self.nc = nc
        self.eng = {"pe": nc.tensor, "act": nc.scalar, "dve": nc.vector,
                    "pool": nc.gpsimd, "sp": nc.sync}
        self.cnt = {e: 0 for e in self.eng}
        self.sems = {e: [] for e in self.eng}
        self.seen = {e: {} for e in self.eng}
        self.nsem = 0
        self.dma_bufs = []
        self.free_dsems = {"hw": [], "sw": []}
        self.nwaits = 0
        self.ninstr = 0

    def _newsem(self, name):
        self.nsem += 1
        return self.nc.alloc_semaphore(name=name)

    def _wait(self, e, tok):
        sem, val, src = tok
        key = id(sem)
        if self.seen[e].get(key, 0) >= val:
            return
        self.seen[e][key] = val
        self.eng[e].wait_ge(sem, val)
        self.nwaits += 1

    def _deps(self, e, reads, writes):
        for b in reads:
            if b.last_w is not None and not (b.last_w[2] == e and e == "pe"):
                self._wait(e, b.last_w)
        for b in writes:
            if b.last_w is not None and not (b.last_w[2] == e and e == "pe"):
                self._wait(e, b.last_w)
            for t in b.readers:
                if t[2] == e:
                    continue
                self._wait(e, t)

    def _commit(self, tok, reads, writes):
        for b in reads:
            b.readers.append(tok)
            if len(b.readers) > 12:
                d = {}
                for t in b.readers:
                    k = id(t[0])
                    if k not in d or d[k][1] < t[1]:
                        d[k] = t
                b.readers = list(d.values())
        for b in writes:
            b.last_w = tok
            b.readers = []

    def op(self, e, fn, reads=(), writes=()):
        self._deps(e, reads, writes)
        n = self.cnt[e]
        ep, v = divmod(n, EPOCH)
        while len(self.sems[e]) <= ep:
            self.sems[e].append(self._newsem(f"c_{e}_{len(self.sems[e])}"))
        sem = self.sems[e][ep]
        ins = fn(self.eng[e])
        ins.then_inc(sem, 1)
        self.cnt[e] = n + 1
        self.ninstr += 1
        tok = (sem, v + 1, e)
        self._commit(tok, reads, writes)
        return tok

    def dma(self, q, out, in_, sb, reads=(), writes=(), **kw):
        self._deps(q, reads, writes)
        kind = "sw" if q == "pool" else "hw"
        if kind not in sb.dsem:
            if self.free_dsems[kind]:
                sb.dsem[kind] = list(self.free_dsems[kind].pop())
            else:
                sb.dsem[kind] = [self._newsem(f"d{kind}_{sb.name}_{self.nsem}"), 0]
            self.dma_bufs.append((sb, kind))
        ent = sb.dsem[kind]
        ent[1] += 16
        ins = self.eng[q].dma_start(out=out, in_=in_, **kw)
        ins.then_inc(ent[0], 16)
        self.ninstr += 1
        tok = (ent[0], ent[1], "dma")
        self._commit(tok, reads, writes)
        return tok

    def barrier(self):
        toks = []
        for f in self.eng:
            n = self.cnt[f]
            if n == 0:
                continue
            ep, v = divmod(n - 1, EPOCH)
            toks.append((self.sems[f][ep], v + 1, f))
        for b, kind in self.dma_bufs:
            if not b.bg:
                toks.append((b.dsem[kind][0], b.dsem[kind][1], "dma"))
        for e in self.eng:
            for t in toks:
                if t[2] == e:
                    continue
                self._wait(e, t)
        keep = []
        for b, kind in self.dma_bufs:
            if b.bg:
                keep.append((b, kind))
            else:
                self.free_dsems[kind].append(tuple(b.dsem.pop(kind)))
        self.dma_bufs = keep


def build_program(nlayers=2, debug=False, phases="ABCDE"):
    nc = bass.Bass("TRN2", target_bir_lowering=False)
    T = Tracker(nc)
    uid = [0]

    def din(name, shape, dt=F32):
        return nc.dram_tensor(name, list(shape), dt, kind="ExternalInput").ap()

    def dscr(name, shape, dt):
        kind = "ExternalOutput" if debug else "Internal"
        return nc.dram_tensor(name, list(shape), dt, kind=kind).ap()

    x_in = din("x", [NSEQ, S, D])
    cT = din("cT", [128, 8, NSEQ])
    w_mod = din("w_mod", [2, D, 6 * D])
    b_modT = din("b_modT", [128, 2, 48, NSEQ])
    g_attnT = din("g_attnT", [128, 2, 8, NSEQ])
    g_mlpT = din("g_mlpT", [128, 2, 8, NSEQ])
    w_in = din("w_in", [2, D, DIN])
    kvg_bc = din("kvg_bc", [128, 2, 128])
    w_uv = din("w_uv", [2, 8, 128, 64])
    goa_bc = din("goa_bc", [128, 2, 512])
    gob_bc = din("gob_bc", [128, 2, 512])
    w_out = din("w_out", [2, D, D])
    w_up = din("w_up", [2, D, DFF])
    w_down = din("w_down", [2, DFF, D])
    biasn = din("biasn", [128, 2, 8, 128])
    bfar_bc = din("bfar_bc", [128, 8, 128])
    gfin_bc = din("gfin_bc", [128, D])
    c_ident = din("c_ident", [128, 128])
    c_tri8 = din("c_tri8", [128, 128])
    c_neg8 = din("c_neg8", [128, 128])
    c_cmask = din("c_cmask", [128, 8, 128])
    c_adm = din("c_adm", [128, 8, 128])
    c_admneg = din("c_admneg", [128, 8, 128])
    c_pow2 = din("c_pow2", [128, KBIS])
    c_ones = din("c_ones", [128, 128])
    out_d = nc.dram_tensor("out", [NSEQ, S, D], F32, kind="ExternalOutput").ap()

    xres = dscr("xres", [NSEQ, S, D], F32)
    featT = dscr("featT", [NSEQ, 21, 128, S], BF16)
    v_scr = dscr("v_scr", [NSEQ, S, 512], BF16)
    kv_tok = dscr("kv_tok", [NSEQ, S, 128], BF16)
    kvT_scr = dscr("kvT_scr", [NSEQ, 128, S], BF16)
    widx = dscr("widx", [NSEQ, S, 8], F32)
    ocat = dscr("ocat", [NSEQ, S, D], BF16)

    wb_in = [nc.dram_tensor(f"wb_in{l}", [D, DIN], BF16, kind="Internal").ap() for l in range(2)]
    wb_out = [nc.dram_tensor(f"wb_out{l}", [D, D], BF16, kind="Internal").ap() for l in range(2)]
    wb_up = [nc.dram_tensor(f"wb_up{l}", [D, DFF], BF16, kind="Internal").ap() for l in range(2)]
    wb_dn = [nc.dram_tensor(f"wb_dn{l}", [DFF, D], BF16, kind="Internal").ap() for l in range(2)]
    B_wb_in = [Buf(f"wb_in{l}", bg=True) for l in range(2)]
    B_wb_out = [Buf(f"wb_out{l}", bg=True) for l in range(2)]
    B_wb_up = [Buf(f"wb_up{l}", bg=True) for l in range(2)]
    B_wb_dn = [Buf(f"wb_dn{l}", bg=True) for l in range(2)]

    bgq = []

    def convert_weights():
        for l in range(nlayers):
            for (dst, src, bb, rows, step) in ((wb_in[l], w_in[l], B_wb_in[l], D, 256), (wb_out[l], w_out[l], B_wb_out[l], D, 512),
                                               (wb_up[l], w_up[l], B_wb_up[l], D, 256), (wb_dn[l], w_down[l], B_wb_dn[l], DFF, 1024)):
                for r0 in range(0, rows, step):
                    f = (lambda dst=dst, src=src, bb=bb, r0=r0, step=step:
                         T.dma("pool", dst[r0:r0 + step, :], src[r0:r0 + step, :], bb, writes=[bb]))
                    if l == 0 and dst is wb_in[0]:
                        f()
                    else:
                        bgq.append((l, f))

    def bg_step(n=1):
        for _ in range(n):
            if bgq:
                bgq.pop(0)[1]()

    def bg_flush(layer):
        while bgq and bgq[0][0] <= layer:
            bgq.pop(0)[1]()

    def salloc(es, name, shape, dt):
        uid[0] += 1
        return es.enter_context(nc.sbuf_tensor(f"{name}_{uid[0]}", list(shape), dt))

    ges = ExitStack()
    pb2 = [ges.enter_context(nc.psum_tensor(f"pbp{i}", [128, 1024], F32)) for i in range(4)]
    pb = [pb2[i // 2][:, (i % 2) * 512:(i % 2 + 1) * 512] for i in range(8)]
    PB = [Buf(f"pb{i}") for i in range(8)]

    def pbf(i):
        return pb[i].bitcast(BF16)

    ident_f = salloc(ges, "identf", [128, 128], F32); b_identf = Buf("identf")
    ident_b = salloc(ges, "identb", [128, 128], BF16); b_identb = Buf("identb")
    ones_f = salloc(ges, "onesf", [128, 128], F32); b_onesf = Buf("onesf")
    ones_b = salloc(ges, "onesb", [128, 128], BF16); b_onesb = Buf("onesb")
    modT = salloc(ges, "modT", [128, 2, 48, NSEQ], F32); b_modT_ = Buf("modT")
    gm1T = salloc(ges, "gm1T", [128, 2, 8, NSEQ], F32); b_gm1 = Buf("gm1T")
    gm2T = salloc(ges, "gm2T", [128, 2, 8, NSEQ], F32); b_gm2 = Buf("gm2T")
    stat = salloc(ges, "stat", [128, 8, 4], F32)
    STB = [Buf(f"stat{i}") for i in range(8)]
    junk = salloc(ges, "junk", [128, 2048], BF16); b_junk = Buf("junk")
    stat_i = [0]

    T.dma("sp", ident_f[:], c_ident, b_identf, writes=[b_identf])
    T.dma("pool", ident_b[:], c_ident, b_identb, writes=[b_identb])
    T.dma("sp", ones_f[:], c_ones, b_onesf, writes=[b_onesf])
    T.dma("pool", ones_b[:], c_ones, b_onesb, writes=[b_onesb])

    def prologue(preA=None):
        with ExitStack() as es:
            sT = salloc(es, "sT", [128, 8, NSEQ], F32); b_sT = Buf("sT")
            cTs = salloc(es, "cTs", [128, 8, NSEQ], F32); b_cTs = Buf("cTs")
            bm = salloc(es, "bm", [128, 2, 48, NSEQ], F32); b_bm = Buf("bm")
            ga = salloc(es, "ga", [128, 2, 8, NSEQ], F32); b_ga = Buf("ga")
            gmm = salloc(es, "gmm", [128, 2, 8, NSEQ], F32); b_gmm = Buf("gmm")
            NW = 4
            wms = [salloc(es, f"wm{i}", [128, 8, 512], F32) for i in range(NW)]
            b_wms = [Buf(f"wm{i}") for i in range(NW)]
            modrow = salloc(es, "modrow", [NSEQ, 6 * D], F32); b_mrow = Buf("modrow")
            T.dma("sp", cTs[:], cT, b_cTs, writes=[b_cTs])
            T.dma("sp", bm[:], b_modT, b_bm, writes=[b_bm])
            T.dma("sp", ga[:], g_attnT, b_ga, writes=[b_ga])
            T.dma("sp", gmm[:], g_mlpT, b_gmm, writes=[b_gmm])
            T.op("act", lambda e: e.activation(out=sT[:], in_=cTs[:], func=AF.Silu),
                 reads=[b_cTs], writes=[b_sT])
            it = 0
            for l in range(nlayers):
                wl = w_mod[l].rearrange("(c p) n -> p c n", p=128)
                for ns in range(12):
                    sl = it % NW
                    bank = it % 2
                    it += 1
                    T.dma("sp", wms[sl][:], wl[:, :, ns * 512:(ns + 1) * 512], b_wms[sl],
                          writes=[b_wms[sl]])
                    for c in range(8):
                        T.op("pe", lambda e: e.matmul(
                            pb[bank][0:NSEQ, 0:512], lhsT=sT[:, c, :], rhs=wms[sl][:, c, :],
                            start=(c == 0), stop=(c == 7)),
                            reads=[b_wms[sl], b_sT], writes=[PB[bank]])
                    T.op("act", lambda e: e.activation(out=modrow[:, ns * 512:(ns + 1) * 512],
                                                       in_=pb[bank][0:NSEQ, 0:512], func=AF.Copy),
                         reads=[PB[bank]], writes=[b_mrow])
                for j in range(48):
                    T.op("pe", lambda e: e.transpose(pb[2][:, j * NSEQ:(j + 1) * NSEQ],
                                                     modrow[0:NSEQ, j * 128:(j + 1) * 128],
                                                     ident_f[0:NSEQ, 0:NSEQ]),
                         reads=[b_mrow, b_identf], writes=[PB[2]])
                T.op("dve", lambda e: e.tensor_tensor(
                    out=modT[:, l, :, :],
                    in0=pb[2][:, 0:48 * NSEQ].rearrange("p (j b) -> p j b", b=NSEQ),
                    in1=bm[:, l, :, :], op=ALU.add),
                    reads=[PB[2], b_bm], writes=[b_modT_])
                T.op("dve", lambda e: e.scalar_tensor_tensor(
                    out=gm1T[:, l], in0=modT[:, l, 8:16, :], scalar=1.0, in1=ga[:, l],
                    op0=ALU.add, op1=ALU.mult), reads=[b_modT_, b_ga], writes=[b_gm1])
                T.op("dve", lambda e: e.scalar_tensor_tensor(
                    out=gm2T[:, l], in0=modT[:, l, 32:40, :], scalar=1.0, in1=gmm[:, l],
                    op0=ALU.add, op1=ALU.mult), reads=[b_modT_, b_gmm], writes=[b_gm2])
            if preA is not None:
                load_A_weights(0, *preA)
            T.barrier()

    def next_stat():
        i = stat_i[0] % 8
        stat_i[0] += 1
        return stat[:, i, :], STB[i]

    def rstd_from(src_ap, src_bufs, n, from_psum=False):
        st, sbuf_ = next_stat()
        T.op("act", lambda e: e.activation(out=junk[:, 0:n], in_=src_ap, func=AF.Square,
                                           accum_out=st[:, 0:1]),
             reads=src_bufs, writes=[b_junk, sbuf_])
        T.op("dve", lambda e: e.tensor_scalar(out=st[:, 1:2], in0=st[:, 0:1], scalar1=1.0 / n,
                                              scalar2=EPS, op0=ALU.mult, op1=ALU.add),
             reads=[sbuf_], writes=[sbuf_])
        T.op("act", lambda e: e.activation(out=st[:, 2:3], in_=st[:, 1:2], func=AF.Sqrt),
             reads=[sbuf_], writes=[sbuf_])
        T.op("dve", lambda e: e.reciprocal(out=st[:, 3:4], in_=st[:, 2:3]),
             reads=[sbuf_], writes=[sbuf_])
        return st[:, 3:4], sbuf_

    def norm_to_T(xt_ap, bx, gmT_ap, shT_ap, bmods, xn, bxn, tbank, hT_dst, bhT):
        rstd, brs = rstd_from(xt_ap, [bx], D)
        T.op("dve", lambda e: e.tensor_scalar(out=xn[:], in0=xt_ap, scalar1=rstd, scalar2=None,
                                              op0=ALU.mult), reads=[bx, brs], writes=[bxn])
        pv = pbf(tbank).rearrange("p (c t) -> p c t", c=8)
        for c in range(8):
            T.op("pe", lambda e: e.transpose(pv[:, c, :], xn[:, c * 128:(c + 1) * 128], ident_b[:]),
                 reads=[bxn, b_identb], writes=[PB[tbank]])
        for c in range(8):
            if c % 2 == 0:
                T.op("act", lambda e: e.activation(out=hT_dst(c), in_=pv[:, c, :], func=AF.Identity,
                                                   scale=gmT_ap(c), bias=shT_ap(c)),
                     reads=[PB[tbank]] + bmods, writes=[bhT])
            else:
                T.op("dve", lambda e: e.tensor_scalar(out=hT_dst(c), in0=pv[:, c, :],
                                                      scalar1=gmT_ap(c), scalar2=shT_ap(c),
                                                      op0=ALU.mult, op1=ALU.add),
                     reads=[PB[tbank]] + bmods, writes=[bhT])

    def ga_bcast(es, l, chunk0, name):
        gb = salloc(es, name, [128, NSEQ, D], F32); bgb = Buf(name)
        dg = salloc(es, name + "dg", [128, 2, 128], F32); bdg = [Buf(name + "dg0"), Buf(name + "dg1")]
        k = 0
        for b in range(NSEQ):
            for half in range(2):
                bank = 6 + half
                for cc in range(4):
                    c = half * 4 + cc
                    sl = k % 2
                    k += 1
                    T.op("dve", lambda e: e.tensor_scalar(
                        out=dg[:, sl, :], in0=ident_f[:], scalar1=modT[:, l, chunk0 + c, b:b + 1],
                        scalar2=None, op0=ALU.mult), reads=[b_identf, b_modT_], writes=[bdg[sl]])
                    T.op("pe", lambda e: e.matmul(pb[bank][:, cc * 128:(cc + 1) * 128], lhsT=ones_f[:],
                                                  rhs=dg[:, sl, :], start=(cc == 0), stop=True,
                                                  skip_group_check=True),
                         reads=[b_onesf, bdg[sl]], writes=[PB[bank]])
                T.op("act", lambda e: e.activation(out=gb[:, b, half * 512:(half + 1) * 512],
                                                   in_=pb[bank][:], func=AF.Copy),
                     reads=[PB[bank]], writes=[bgb])
        return gb, bgb

    def load_A_weights(l, Wf, bWf, Wt, bWt):
        wl = wb_in[l].rearrange("(c p) n -> p c n", p=128)
        for (a, b, o) in [(1024, 1536, 0), (2560, 2688, 512), (3264, 3272, 640)]:
            T.dma("sp", Wt[:, :, o:o + (b - a)], wl[:, :, a:b], bWt, reads=[B_wb_in[l]], writes=[bWt])
        for (a, b, o, sg) in [(0, 1024, 0, 0), (1536, 2560, 1024, 1), (2688, 3200, 2048, 2),
                              (3200, 3264, 2560, 3), (3200, 3264, 2624, 3)]:
            for c in range(8):
                T.dma("sp", Wf[:, c, o:o + (b - a)], wl[:, c, a:b], bWf[sg], reads=[B_wb_in[l]], writes=[bWf[sg]])

    def phase_A(l, pre=None):
        with ExitStack() as es:
            if pre is None:
                bg_flush(l)
                Wf = salloc(es, "Wf", [128, 8, 2688], BF16); bWf = [Buf(f"Wf{i}") for i in range(4)]
                Wt = salloc(es, "Wt", [128, 8, 648], BF16); bWt = Buf("Wt")
                load_A_weights(l, Wf, bWf, Wt, bWt)
            else:
                Wf, bWf, Wt, bWt = pre
            kvg = salloc(es, "kvg", [128, 128], F32); bkvg = Buf("kvg")
            T.dma("sp", kvg[:], kvg_bc[:, l, :], bkvg, writes=[bkvg])
            xts = [salloc(es, f"xt{i}", [128, D], F32) for i in range(3)]
            bxts = [Buf(f"xt{i}") for i in range(3)]
            xns = [salloc(es, f"xn{i}", [128, D], BF16) for i in range(2)]
            bxns = [Buf(f"xn{i}") for i in range(2)]
            hTs = [salloc(es, f"hT{i}", [128, 8, 512], BF16) for i in range(2)]
            bhTs = [Buf(f"hT{i}") for i in range(2)]
            vts = [salloc(es, f"vt{i}", [128, 512], BF16) for i in range(2)]
            bvts = [Buf(f"vt{i}") for i in range(2)]
            kvn = [salloc(es, f"kvn{i}", [128, 128], BF16) for i in range(2)]
            bkvn = [Buf(f"kvn{i}") for i in range(2)]
            kvTt = [salloc(es, f"kvTt{i}", [128, 128], BF16) for i in range(2)]
            bkvTt = [Buf(f"kvTt{i}") for i in range(2)]
            wis = [salloc(es, f"wis{i}", [128, 8], F32) for i in range(2)]
            bwis = [Buf(f"wis{i}") for i in range(2)]
            fos = [salloc(es, f"fo{i}", [128, 512], BF16) for i in range(3)]
            bfos = [Buf(f"fo{i}") for i in range(3)]
            src = x_in if l == 0 else xres
            cnt = {"ti": 0, "fi": 0}
            groups = [(s, g) for s in range(NSEQ) for g in range(4)]

            def prep_a(gi, j):
                s, g = groups[gi]
                hT = hTs[gi % 2]; bhT = bhTs[gi % 2]
                tt = g * 4 + j
                t0 = tt * 128
                ti = cnt["ti"]
                cnt["ti"] += 1
                xt = xts[ti % 3]; bxt = bxts[ti % 3]
                xn = xns[ti % 2]; bxn = bxns[ti % 2]
                T.dma("sp", xt[:], src[s, t0:t0 + 128, :], bxt, writes=[bxt])
                norm_to_T(xt[:], bxt,
                          lambda c: gm1T[:, l, c, s:s + 1],
                          lambda c: modT[:, l, 0 + c, s:s + 1],
                          [b_gm1, b_modT_], xn, bxn, ti % 2,
                          lambda c: hT[:, c, j * 128:(j + 1) * 128], bhT)
                return ti

            def prep_b(gi, j, ti):
                s, g = groups[gi]
                hT = hTs[gi % 2]; bhT = bhTs[gi % 2]
                t0 = (g * 4 + j) * 128
                k2 = ti % 2
                for c in range(8):
                    T.op("pe", lambda e: e.matmul(pb[2][:, 0:512], lhsT=hT[:, c, j * 128:(j + 1) * 128],
                                                  rhs=Wt[:, c, 0:512], start=(c == 0), stop=(c == 7)),
                         reads=[bhT, bWt], writes=[PB[2]])
                for c in range(8):
                    T.op("pe", lambda e: e.matmul(pb[3][:, 0:136], lhsT=hT[:, c, j * 128:(j + 1) * 128],
                                                  rhs=Wt[:, c, 512:648], start=(c == 0), stop=(c == 7)),
                         reads=[bhT, bWt], writes=[PB[3]])
                T.op("act", lambda e: e.activation(out=vts[k2][:], in_=pb[2][:, 0:512], func=AF.Copy),
                     reads=[PB[2]], writes=[bvts[k2]])
                T.dma("pool", v_scr[s, t0:t0 + 128, :], vts[k2][:], bvts[k2], reads=[bvts[k2]])
                rs2, brs2 = rstd_from(pb[3][:, 0:128], [PB[3]], 128)
                T.op("dve", lambda e: e.scalar_tensor_tensor(
                    out=kvn[k2][:], in0=pb[3][:, 0:128], scalar=rs2, in1=kvg[:],
                    op0=ALU.mult, op1=ALU.mult), reads=[PB[3], brs2, bkvg], writes=[bkvn[k2]])
                T.op("dve", lambda e: e.tensor_copy(out=wis[k2][:], in_=pb[3][:, 128:136]),
                     reads=[PB[3]], writes=[bwis[k2]])
                T.dma("pool", kv_tok[s, t0:t0 + 128, :], kvn[k2][:], bkvn[k2], reads=[bkvn[k2]])
                T.dma("pool", widx[s, t0:t0 + 128, :], wis[k2][:], bwis[k2], reads=[bwis[k2]])
                T.op("pe", lambda e: e.transpose(pbf(4)[:, 0:128], kvn[k2][:], ident_b[:]),
                     reads=[bkvn[k2], b_identb], writes=[PB[4]])
                T.op("act", lambda e: e.activation(out=kvTt[k2][:], in_=pbf(4)[:, 0:128], func=AF.Copy),
                     reads=[PB[4]], writes=[bkvTt[k2]])
                T.dma("pool", kvT_scr[s, :, t0:t0 + 128], kvTt[k2][:], bkvTt[k2], reads=[bkvTt[k2]])

            def fm_chunk(gi, ch):
                s, g = groups[gi]
                hT = hTs[gi % 2]; bhT = bhTs[gi % 2]
                fi = cnt["fi"]
                cnt["fi"] += 1
                bank = 5 + (fi % 3)
                fo = fos[fi % 3]; bfo = bfos[fi % 3]
                for c in range(8):
                    T.op("pe", lambda e: e.matmul(pb[bank][:, 0:512], lhsT=Wf[:, c, ch * 128:(ch + 1) * 128],
                                                  rhs=hT[:, c, :], start=(c == 0), stop=(c == 7)),
                         reads=[bhT, bWf[0 if ch < 8 else (1 if ch < 16 else (2 if ch < 20 else 3))]], writes=[PB[bank]])
                if fi % 2 == 0:
                    T.op("act", lambda e: e.activation(out=fo[:], in_=pb[bank][:, 0:512], func=AF.Copy),
                         reads=[PB[bank]], writes=[bfo])
                else:
                    T.op("dve", lambda e: e.tensor_copy(out=fo[:], in_=pb[bank][:, 0:512]),
                         reads=[PB[bank]], writes=[bfo])
                T.dma("pool", featT[s, ch, :, g * 512:(g + 1) * 512], fo[:], bfo, reads=[bfo])

            for j in range(4):
                ti0 = prep_a(0, j)
                prep_b(0, j, ti0)
            for gi in range(len(groups)):
                pend_b = None
                for ch in range(21):
                    fm_chunk(gi, ch)
                    if gi + 1 < len(groups):
                        if ch in (1, 6, 11, 16):
                            j = (1, 6, 11, 16).index(ch)
                            pend_b = (j, prep_a(gi + 1, j))
                        if ch in (4, 9, 14, 19) and pend_b is not None:
                            prep_b(gi + 1, pend_b[0], pend_b[1])
                            pend_b = None
            T.barrier()

    def phase_B(l, cin0=None):
        with ExitStack() as es:
            qT = salloc(es, "qT", [128, 4, S], BF16); bqT = Buf("qT")
            kT = salloc(es, "kT", [128, 4, S], BF16); bkT = Buf("kT")
            vv = salloc(es, "vv", [128, NT, 512], BF16); bvv = Buf("vv")
            tri8 = salloc(es, "tri8", [128, 128], BF16); btri = Buf("tri8")
            neg8 = salloc(es, "neg8", [128, 128], BF16); bneg = Buf("neg8")
            cm = salloc(es, "cm", [128, 512], BF16); bcm = Buf("cm")
            goa = salloc(es, "goa", [128, 512], F32); bgoa = Buf("goa")
            T.dma("pool", tri8[:], c_tri8, btri, writes=[btri])
            T.dma("pool", neg8[:], c_neg8, bneg, writes=[bneg])
            T.dma("pool", cm[:], c_cmask[:, 0:4, :].rearrange("p h t -> p (h t)"), bcm, writes=[bcm])
            T.dma("sp", goa[:], goa_bc[:, l, :], bgoa, writes=[bgoa])
            e32a = [salloc(es, f"e32a{i}", [128, 1024], F32) for i in range(2)]
            spba = [salloc(es, f"spba{i}", [128, 1024], BF16) for i in range(2)]
            wba = [salloc(es, f"wba{i}", [128, 1024], BF16) for i in range(2)]
            e32 = [[e32a[i][:, g * 512:(g + 1) * 512] for i in range(2)] for g in range(2)]
            spb = [[spba[i][:, g * 512:(g + 1) * 512] for i in range(2)] for g in range(2)]
            wb = [[wba[i][:, g * 512:(g + 1) * 512] for i in range(2)] for g in range(2)]
            be32 = [[Buf(f"e32{g}{i}") for i in range(2)] for g in range(2)]
            bspb = [[Buf(f"spb{g}{i}") for i in range(2)] for g in range(2)]
            bwb = [[Buf(f"wb{g}{i}") for i in range(2)] for g in range(2)]
            sps = [salloc(es, f"sps{g}", [128, 512], F32) for g in range(2)]
            bsps = [Buf(f"sps{g}") for g in range(2)]
            spsb = [[salloc(es, f"spsb{g}{i}", [128, 512], BF16) for i in range(2)] for g in range(2)]
            bspsb = [[Buf(f"spsb{g}{i}") for i in range(2)] for g in range(2)]
            oan = [salloc(es, f"oan{i}", [128, 512], BF16) for i in range(2)]
            boan = [Buf(f"oan{i}") for i in range(2)]
            ZB = [[0, 2], [1, 3]]
            AB = [4, 5]
            OB = [6, 7]
            carry_slot = [0, 0]

            def zmm(bank, hg, qb, kb, start_first):
                for i in range(4):
                    h = 2 * i + hg
                    ch = h // 2
                    r0 = (h % 2) * 64
                    T.op("pe", lambda e: e.matmul(
                        pb[bank][:, i * 128:(i + 1) * 128],
                        lhsT=kT[r0:r0 + 64, ch, kb * 128:(kb + 1) * 128],
                        rhs=qT[r0:r0 + 64, ch, qb * 128:(qb + 1) * 128],
                        start=(start_first and i == 0), stop=(i == 3),
                        skip_group_check=True),
                        reads=[bkT, bqT], writes=[PB[bank]])

            def stage_Z(n, qb, kb):
                sl = n % 2
                for hg in range(2):
                    zmm(ZB[hg][sl], hg, qb, kb, True)
                T.op("act", lambda e: e.activation(out=e32a[sl][:], in_=pb2[sl][:], func=AF.Exp, scale=0.125),
                     reads=[PB[ZB[0][sl]], PB[ZB[1][sl]]], writes=[be32[0][sl], be32[1][sl]])
                T.op("act", lambda e: e.activation(out=spba[sl][:], in_=e32a[sl][:], func=AF.Ln, bias=1.0),
                     reads=[be32[0][sl], be32[1][sl]], writes=[bspb[0][sl], bspb[1][sl]])
                for hg in range(2):
                    if kb == qb:
                        T.op("dve", lambda e: e.tensor_tensor(out=spb[hg][sl][:], in0=spb[hg][sl][:], in1=cm[:], op=ALU.mult),
                             reads=[bspb[hg][sl], bcm], writes=[bspb[hg][sl]])

            def stage_A(n, qb, kb):
                sl = n % 2
                for hg in range(2):
                    ab = AB[hg]
                    T.op("pe", lambda e: e.matmul(pb[ab][:], lhsT=tri8[:], rhs=spb[hg][sl][:], start=True, stop=False,
                                                  skip_group_check=True),
                         reads=[btri, bspb[hg][sl]], writes=[PB[ab]])
                    if kb < qb:
                        cs = carry_slot[hg]
                        T.op("pe", lambda e: e.matmul(pb[ab][:], lhsT=neg8[:], rhs=spsb[hg][cs][:], start=False, stop=False,
                                                      skip_group_check=True),
                             reads=[bneg, bspsb[hg][cs]], writes=[PB[ab]])
                for hg in range(2):
                    zmm(AB[hg], hg, qb, kb, False)
                T.op("act", lambda e: e.activation(out=wba[sl][:], in_=pb2[2][:], func=AF.Exp, scale=0.125),
                     reads=[PB[AB[0]], PB[AB[1]]], writes=[bwb[0][sl], bwb[1][sl]])
                for hg in range(2):
                    ab = AB[hg]
                    if kb == qb:
                        T.op("dve", lambda e: e.tensor_tensor(out=wb[hg][sl][:], in0=wb[hg][sl][:], in1=cm[:], op=ALU.mult),
                             reads=[bwb[hg][sl], bcm], writes=[bwb[hg][sl]])
                    if kb > 0:
                        if kb == qb:
                            T.op("dve", lambda e: e.tensor_copy(out=sps[hg][:], in_=spb[hg][sl][:]),
                                 reads=[bspb[hg][sl]], writes=[bsps[hg]])
                        else:
                            T.op("dve", lambda e: e.tensor_tensor(out=sps[hg][:], in0=sps[hg][:], in1=spb[hg][sl][:], op=ALU.add),
                                 reads=[bsps[hg], bspb[hg][sl]], writes=[bsps[hg]])
                        carry_slot[hg] ^= 1
                        cs = carry_slot[hg]
                        T.op("dve", lambda e: e.tensor_copy(out=spsb[hg][cs][:], in_=sps[hg][:]),
                             reads=[bsps[hg]], writes=[bspsb[hg][cs]])

            def stage_PV(n, s, qb, kb):
                sl = n % 2
                ob = OB[qb % 2]
                for hg in range(2):
                    for i in range(4):
                        h = 2 * i + hg
                        T.op("pe", lambda e: e.matmul(
                            pb[ob][:, h * 64:(h + 1) * 64], lhsT=wb[hg][sl][:, i * 128:(i + 1) * 128],
                            rhs=vv[:, kb, h * 64:(h + 1) * 64], start=(kb == qb and hg == 0 and i == 0), stop=False,
                            skip_group_check=True),
                            reads=[bwb[hg][sl], bvv], writes=[PB[ob]])
                if kb == 0:
                    osl = qb % 2
                    rs, brs = rstd_from(pb[ob][:], [PB[ob]], 512)
                    T.op("dve", lambda e: e.scalar_tensor_tensor(out=oan[osl][:], in0=pb[ob][:], scalar=rs, in1=goa[:],
                                                                 op0=ALU.mult, op1=ALU.mult),
                         reads=[PB[ob], brs, bgoa], writes=[boan[osl]])
                    T.dma("pool", ocat[s, qb * 128:(qb + 1) * 128, 0:512], oan[osl][:], boan[osl], reads=[boan[osl]])
                    bg_step(1)
                    if cin0 is not None and s == LIM_SEQ - 1 and qb == min(9, LIM_QB - 1):
                        load_cin(cin0, 0, gate=[boan[osl]])

            for s in range(LIM_SEQ):
                T.dma("sp", qT[:], featT[s, 0:4].rearrange("c p t -> p c t"), bqT, writes=[bqT])
                T.dma("sp", kT[:], featT[s, 4:8].rearrange("c p t -> p c t"), bkT, writes=[bkT])
                T.dma("sp", vv[:], v_scr[s].rearrange("(n p) f -> p n f", p=128), bvv, writes=[bvv])
                its = [(qb, kb) for qb in range(LIM_QB) for kb in range(qb, -1, -1)]
                N = len(its)
                for t in range(N + 2):
                    if t < N:
                        stage_Z(t, *its[t])
                    if 0 <= t - 1 < N:
                        stage_A(t - 1, *its[t - 1])
                    if 0 <= t - 2 < N:
                        stage_PV(t - 2, s, *its[t - 2])
            T.barrier()

    def alloc_cin(es, s_):
        return dict(
            dqT=salloc(es, f"dqT{s_}", [128, 8, S], BF16), bdq=Buf(f"dqT{s_}"),
            iqT=salloc(es, f"iqT{s_}", [128, 4, S], BF16), biq=Buf(f"iqT{s_}"),
            ikT=salloc(es, f"ikT{s_}", [128, S], BF16), bik=Buf(f"ikT{s_}"),
            kvT=salloc(es, f"kvT{s_}", [128, S], BF16), bkvT=Buf(f"kvT{s_}"),
            kvt=salloc(es, f"kvt{s_}", [128, NT, 128], BF16), bkvt=Buf(f"kvt{s_}"),
            wi=salloc(es, f"wi{s_}", [128, NT, 8], F32), bwi=Buf(f"wi{s_}"))

    def load_cin(d_, s_, gate=()):
        g = list(gate)
        T.dma("sp", d_["iqT"][:], featT[s_, 16:20].rearrange("c p t -> p c t"), d_["biq"], reads=g, writes=[d_["biq"]])
        T.dma("sp", d_["ikT"][:], featT[s_, 20], d_["bik"], reads=g, writes=[d_["bik"]])
        T.dma("sp", d_["wi"][:], widx[s_].rearrange("(n p) j -> p n j", p=128), d_["bwi"], reads=g, writes=[d_["bwi"]])
        T.dma("sp", d_["kvT"][:], kvT_scr[s_], d_["bkvT"], reads=g, writes=[d_["bkvT"]])
        T.dma("sp", d_["kvt"][:], kv_tok[s_].rearrange("(n p) r -> p n r", p=128), d_["bkvt"], reads=g, writes=[d_["bkvt"]])
        T.dma("sp", d_["dqT"][:], featT[s_, 8:16].rearrange("c p t -> p c t"), d_["bdq"], reads=g, writes=[d_["bdq"]])

    def phase_C(l, cin0=None):
        if FLUSH_BG_BEFORE_C:
            bg_flush(99)
        with ExitStack() as es:
            cin = [cin0 if cin0 is not None else alloc_cin(es, 0)]
            for s_ in range(1, NSEQ):
                cin.append(alloc_cin(es, s_))
            cur = {}

            def load_seq_C(s_, gate=()):
                d_ = cin[s_]
                g = list(gate)
                T.dma("sp", d_["iqT"][:], featT[s_, 16:20].rearrange("c p t -> p c t"), d_["biq"], reads=g, writes=[d_["biq"]])
                T.dma("sp", d_["ikT"][:], featT[s_, 20], d_["bik"], reads=g, writes=[d_["bik"]])
                T.dma("sp", d_["wi"][:], widx[s_].rearrange("(n p) j -> p n j", p=128), d_["bwi"], reads=g, writes=[d_["bwi"]])
                T.dma("sp", d_["kvT"][:], kvT_scr[s_], d_["bkvT"], reads=g, writes=[d_["bkvT"]])
                T.dma("sp", d_["kvt"][:], kv_tok[s_].rearrange("(n p) r -> p n r", p=128), d_["bkvt"], reads=g, writes=[d_["bkvt"]])
                T.dma("sp", d_["dqT"][:], featT[s_, 8:16].rearrange("c p t -> p c t"), d_["bdq"], reads=g, writes=[d_["bdq"]])
            wuv = salloc(es, "wuv", [128, 8, 64], BF16); bwuv = Buf("wuv")
            gob = salloc(es, "gob", [128, 512], F32); bgob = Buf("gob")
            BN = salloc(es, "BN", [128, 2, 1024], BF16); bBN = Buf("BN")
            id4 = salloc(es, "id4", [128, 512], BF16); bid4 = Buf("id4")
            pw2 = salloc(es, "pw2", [128, KBIS], F32); bpw2 = Buf("pw2")
            T.dma("pool", wuv[:], w_uv[l].rearrange("h r d -> r h d"), bwuv, writes=[bwuv])
            T.dma("sp", gob[:], gob_bc[:, l, :], bgob, writes=[bgob])
            T.dma("sp", pw2[:], c_pow2, bpw2, writes=[bpw2])
            for i in range(4):
                T.dma("pool", id4[:, i * 128:(i + 1) * 128], c_ident, bid4, writes=[bid4])
            score = [salloc(es, f"score{i}", [128, S], F32) for i in range(2)]
            bsc = [Buf(f"score{i}") for i in range(2)]
            nmask = [salloc(es, f"nmask{i}", [128, S], BF16) for i in range(2)]
            bnm = [Buf(f"nmask{i}") for i in range(2)]
            PP = [salloc(es, f"PP{i}", [128, 1024], BF16) for i in range(2)]
            bPP = [Buf(f"PP{i}") for i in range(2)]
            oTs = salloc(es, "oTs", [128, 1024], BF16); boTs = Buf("oTs")
            bis = salloc(es, "bis", [128, 8 + 2 * KBIS], F32); bbis = Buf("bis")
            rden = salloc(es, "rden", [128, 8], F32); brden = Buf("rden")
            obf = salloc(es, "obf", [128, 512], F32); bobf = Buf("obf")
            obn = [salloc(es, f"obn{i}", [128, 512], BF16) for i in range(2)]
            bobn = [Buf(f"obn{i}") for i in range(2)]
            lsc = 128 ** -0.5
            isc = (64 ** -0.5) * (8 ** -0.5)
            with ExitStack() as es2:
                bn = salloc(es2, "bn", [128, 2, 1024], F32); bbn = Buf("bn")
                bf = salloc(es2, "bf", [128, 1024], F32); bbf = Buf("bf")
                adn = salloc(es2, "adn", [128, 1024], F32); badn = Buf("adn")
                T.dma("sp", bn[:], biasn.rearrange("p o h t -> p o (h t)"), bbn, writes=[bbn])
                T.dma("sp", bf[:], bfar_bc.rearrange("p h t -> p (h t)"), bbf, writes=[bbf])
                T.dma("sp", adn[:], c_admneg.rearrange("p h t -> p (h t)"), badn, writes=[badn])
                for o in range(2):
                    T.op("dve", lambda e: e.tensor_tensor(out=bn[:, o, :], in0=bn[:, o, :], in1=bf[:], op=ALU.subtract),
                         reads=[bbn, bbf], writes=[bbn])
                    if o == 0:
                        T.op("dve", lambda e: e.scalar_tensor_tensor(out=BN[:, o, :], in0=bn[:, o, :], scalar=1.0 / lsc, in1=adn[:],
                                                                     op0=ALU.mult, op1=ALU.add),
                             reads=[bbn, badn], writes=[bBN])
                    else:
                        T.op("dve", lambda e: e.tensor_scalar(out=BN[:, o, :], in0=bn[:, o, :], scalar1=1.0 / lsc, scalar2=None,
                                                              op0=ALU.mult), reads=[bbn], writes=[bBN])
                T.barrier()
            cnt_i = {"ii": 0, "pi": 0, "oi": 0, "lp": 0, "ib": 0}
            IBS = (2, 3)
            SB = 4
            NRB = 2 * ACC_DEPTH + 2
            Rb = [salloc(es, f"Rb{i}", [128, 512], BF16) for i in range(NRB)]
            bRb = [Buf(f"Rb{i}") for i in range(NRB)]
            dg = [salloc(es, f"dg{i}", [128, 8, 128], BF16) for i in range(2)]
            bdg = [Buf(f"dg{i}") for i in range(2)]
            absw = salloc(es, "absw", [128, NT, 8], F32); babsw = Buf("absw")
            sgn = salloc(es, "sgn", [128, NT, 8], F32); bsgn = Buf("sgn")
            OTB = (5, 6)
            DB = 7

            pend_acc = []

            def flush_acc(keep=0):
                while len(pend_acc) > keep:
                    (qb, c0, w, j, rsl, dsl) = pend_acc.pop(0)
                    sc_ = score[qb % 2]; bsc_ = bsc[qb % 2]
                    T.op("pe", lambda e: e.matmul(pb[SB][:, 0:w], lhsT=dg[dsl][:, j, :], rhs=Rb[rsl][:, 0:w],
                                                  start=(j == 0), stop=(j == 7), skip_group_check=True),
                         reads=[bdg[dsl], bRb[rsl]], writes=[PB[SB]])
                    if j == 7:
                        T.op("act", lambda e: e.activation(out=sc_[:, c0:c0 + w], in_=pb[SB][:, 0:w], func=AF.Copy),
                             reads=[PB[SB]], writes=[bsc_])

            def make_diag(qb):
                dsl = qb % 2
                for j in range(8):
                    T.op("act", lambda e: e.activation(out=dg[dsl][:, j, :], in_=ident_b[:], func=AF.Identity,
                                                       scale=sgn[:, qb, j:j + 1]),
                         reads=[b_identb, bsgn], writes=[bdg[dsl]])

            def idx_unit(s, qb, c, j):
                n = (qb + 1) * 128
                q0 = qb * 128
                c0 = c * 512
                w = min(512, n - c0)
                rsl = cnt_i["ii"] % NRB
                cnt_i["ii"] += 1
                ch = j // 2
                r0 = (j % 2) * 64
                IB = IBS[cnt_i["ib"] % 2]
                cnt_i["ib"] += 1
                T.op("pe", lambda e: e.matmul(pb[IB][:, 0:w], lhsT=cur['iqT'][r0:r0 + 64, ch, q0:q0 + 128],
                                              rhs=cur['ikT'][r0:r0 + 64, c0:c0 + w], start=True, stop=True),
                     reads=[cur['biq'], cur['bik']], writes=[PB[IB]])
                T.op("act", lambda e: e.activation(out=Rb[rsl][:, 0:w], in_=pb[IB][:, 0:w], func=AF.Relu,
                                                   scale=absw[:, qb, j:j + 1]),
                     reads=[PB[IB], babsw], writes=[bRb[rsl]])
                flush_acc(keep=ACC_DEPTH - 1)
                pend_acc.append((qb, c0, w, j, rsl, qb % 2))

            def select(qb):
                n = (qb + 1) * 128
                nm = nmask[qb % 2]; bn_ = bnm[qb % 2]
                score_ = score[qb % 2]; bsc_ = bsc[qb % 2]
                T.op("dve", lambda e: e.memset(score_[0:64, n - 64:n], -1.0e30), reads=[bsc_], writes=[bsc_])
                if qb >= 2:
                    hi = bis[:, 0:1]; lo = bis[:, 1:2]; w0 = bis[:, 2:3]; mid = bis[:, 3:4]
                    cnt = bis[:, 4:5]; sv = bis[:, 5:6]; thr = bis[:, 6:7]
                    H = bis[:, 8:8 + KBIS]; H2 = bis[:, 8 + KBIS:8 + 2 * KBIS]
                    T.op("dve", lambda e: e.tensor_reduce(out=hi, in_=score_[:, 0:n], axis=AX.X, op=ALU.max),
                         reads=[bsc_], writes=[bbis])
                    T.op("dve", lambda e: e.tensor_reduce(out=lo, in_=score_[:, 0:n - 64], axis=AX.X, op=ALU.min),
                         reads=[bsc_], writes=[bbis])
                    T.op("dve", lambda e: e.tensor_tensor(out=w0, in0=hi, in1=lo, op=ALU.subtract),
                         reads=[bbis], writes=[bbis])
                    T.op("dve", lambda e: e.tensor_scalar(out=H, in0=pw2[:], scalar1=w0, scalar2=None, op0=ALU.mult),
                         reads=[bbis, bpw2], writes=[bbis])
                    T.op("dve", lambda e: e.tensor_scalar(out=H2, in0=H, scalar1=2.0, scalar2=None, op0=ALU.mult),
                         reads=[bbis], writes=[bbis])
                    T.op("dve", lambda e: e.tensor_tensor(out=mid, in0=lo, in1=bis[:, 8:9], op=ALU.add),
                         reads=[bbis], writes=[bbis])
                    for k in range(KBIS):
                        T.op("dve", lambda e: e.tensor_scalar(out=junk[:, 0:n], in0=score_[:, 0:n], scalar1=mid, scalar2=None,
                                                              op0=ALU.is_ge, op1=ALU.add, accum_out=cnt),
                             reads=[bsc_, bbis], writes=[b_junk, bbis])
                        if k < KBIS - 1:
                            T.op("dve", lambda e: e.tensor_scalar(out=sv, in0=cnt, scalar1=float(TOPK),
                                                                  scalar2=bis[:, 8 + KBIS + k + 1:8 + KBIS + k + 2],
                                                                  op0=ALU.is_ge, op1=ALU.mult),
                                 reads=[bbis], writes=[bbis])
                            T.op("dve", lambda e: e.scalar_tensor_tensor(out=mid, in0=sv, scalar=bis[:, 8 + k + 1:8 + k + 2],
                                                                         in1=mid, op0=ALU.subtract, op1=ALU.add),
                                 reads=[bbis], writes=[bbis])
                        else:
                            T.op("dve", lambda e: e.tensor_scalar(out=sv, in0=cnt, scalar1=float(TOPK),
                                                                  scalar2=bis[:, 8 + k:8 + k + 1],
                                                                  op0=ALU.is_ge, op1=ALU.mult),
                                 reads=[bbis], writes=[bbis])
                            T.op("dve", lambda e: e.scalar_tensor_tensor(out=thr, in0=sv, scalar=bis[:, 8 + k:8 + k + 1],
                                                                         in1=mid, op0=ALU.subtract, op1=ALU.add),
                                 reads=[bbis], writes=[bbis])
                    T.op("dve", lambda e: e.tensor_scalar(out=nm[:, 0:n], in0=score_[:, 0:n], scalar1=thr, scalar2=-1.0e5,
                                                          op0=ALU.is_lt, op1=ALU.mult), reads=[bsc_, bbis], writes=[bn_])
                else:
                    T.op("dve", lambda e: e.tensor_scalar(out=nm[:, 0:n], in0=score_[:, 0:n], scalar1=-1.0e29, scalar2=-1.0e5,
                                                          op0=ALU.is_lt, op1=ALU.mult), reads=[bsc_], writes=[bn_])

            def att_logits(s, qb, kb):
                q0 = qb * 128
                nm = nmask[qb % 2]; bn_ = bnm[qb % 2]
                lp = cnt_i["lp"] % 2
                cnt_i["lp"] += 1
                P = PP[lp]; bP = bPP[lp]
                off = qb - kb
                for (bank, h0) in ((0, 0), (1, 4)):
                    T.op("pe", lambda e: e.matmul(
                        pb[bank][:].rearrange("p (h t) -> p h t", h=4),
                        lhsT=cur['kvT'][:, kb * 128:(kb + 1) * 128], rhs=cur['dqT'][:, h0:h0 + 4, q0:q0 + 128],
                        start=True, stop=False, skip_group_check=True), reads=[cur['bkvT'], cur['bdq']], writes=[PB[bank]])
                    T.op("pe", lambda e: e.matmul(pb[bank][:], lhsT=nm[:, kb * 128:(kb + 1) * 128], rhs=id4[:],
                                                  start=False, stop=(off >= 2), skip_group_check=True),
                         reads=[bn_, bid4], writes=[PB[bank]])
                    if off < 2:
                        T.op("pe", lambda e: e.matmul(pb[bank][:], lhsT=ident_b[:], rhs=BN[:, off, h0 * 128:(h0 + 4) * 128],
                                                      start=False, stop=True, skip_group_check=True),
                             reads=[b_identb, bBN], writes=[PB[bank]])
                T.op("act", lambda e: e.activation(out=P[:], in_=pb2[0][:], func=AF.Exp, scale=lsc),
                     reads=[PB[0], PB[1]], writes=[bP])
                return lp

            def att_pv(s, qb, kb, lp):
                P = PP[lp]; bP = bPP[lp]
                for (bank, h0) in ((OTB[0], 0), (OTB[1], 4)):
                    T.op("pe", lambda e: e.matmul(pb[bank][:], lhsT=cur['kvt'][:, kb, :], rhs=P[:, h0 * 128:(h0 + 4) * 128],
                                                  start=(kb == 0), stop=(kb == qb)),
                         reads=[cur['bkvt'], bP], writes=[PB[bank]])
                for h in range(8):
                    T.op("pe", lambda e: e.matmul(pb[DB][:, h:h + 1], lhsT=P[:, h * 128:(h + 1) * 128], rhs=ones_b[:, 0:1],
                                                  start=(kb == 0 and h == 0), stop=(kb == qb), skip_group_check=True),
                         reads=[bP, b_onesb], writes=[PB[DB]])

            def epilogue(s, qb):
                q0 = qb * 128
                T.op("act", lambda e: e.activation(out=oTs[:, 0:512], in_=pb[OTB[0]][:], func=AF.Copy), reads=[PB[OTB[0]]], writes=[boTs])
                T.op("act", lambda e: e.activation(out=oTs[:, 512:1024], in_=pb[OTB[1]][:], func=AF.Copy), reads=[PB[OTB[1]]], writes=[boTs])
                T.op("dve", lambda e: e.reciprocal(out=rden[:], in_=pb[DB][:, 0:8]), reads=[PB[DB]], writes=[brden])
                eb = IBS[cnt_i["ib"] % 2]
                cnt_i["ib"] += 1
                for h in range(8):
                    T.op("pe", lambda e: e.matmul(pb[eb][:, h * 64:(h + 1) * 64], lhsT=oTs[:, h * 128:(h + 1) * 128],
                                                  rhs=wuv[:, h, :], start=(h == 0), stop=True, skip_group_check=True),
                         reads=[boTs, bwuv], writes=[PB[eb]])
                for h in range(8):
                    T.op("dve", lambda e: e.tensor_scalar(out=obf[:, h * 64:(h + 1) * 64], in0=pb[eb][:, h * 64:(h + 1) * 64],
                                                          scalar1=rden[:, h:h + 1], scalar2=None, op0=ALU.mult),
                         reads=[PB[eb], brden], writes=[bobf])
                rs, brs = rstd_from(obf[:], [bobf], 512)
                osl = cnt_i["oi"] % 2
                cnt_i["oi"] += 1
                T.op("dve", lambda e: e.scalar_tensor_tensor(out=obn[osl][:], in0=obf[:], scalar=rs, in1=gob[:],
                                                             op0=ALU.mult, op1=ALU.mult),
                     reads=[bobf, brs, bgob], writes=[bobn[osl]])
                T.dma("pool", ocat[s, q0:q0 + 128, 512:1024], obn[osl][:], bobn[osl], reads=[bobn[osl]])
                if s == 0 and LIM_SEQ > 1 and qb == min(8, LIM_QB - 1):
                    load_seq_C(1, gate=[bobn[osl]])

            for s in range(LIM_SEQ):
                if s == 0 and cin0 is None:
                    load_seq_C(0)
                cur.clear(); cur.update(cin[s])
                wi = cur["wi"]; bwi = cur["bwi"]
                T.op("act", lambda e: e.activation(out=absw[:], in_=wi[:], func=AF.Abs, scale=isc),
                     reads=[bwi], writes=[babsw])
                T.op("dve", lambda e: e.tensor_scalar(out=sgn[:], in0=wi[:], scalar1=0.0, scalar2=2.0,
                                                      op0=ALU.is_ge, op1=ALU.mult), reads=[bwi], writes=[bsgn])
                T.op("dve", lambda e: e.tensor_scalar(out=sgn[:], in0=sgn[:], scalar1=-1.0, scalar2=None,
                                                      op0=ALU.add), reads=[bsgn], writes=[bsgn])
                for step in range(LIM_QB + 2):
                    qi = step
                    qs = step - 1
                    qa = step - 2
                    iu = []
                    if qi < LIM_QB:
                        n = (qi + 1) * 128
                        iu = [(c, j) for c in range((n + 511) // 512) for j in range(8)]
                        make_diag(qi)
                    if 0 <= qs < LIM_QB:
                        select(qs)
                    au = list(range(qa + 1)) if qa >= 0 else []
                    na, ni = len(au), len(iu)
                    ai = 0
                    ii_ = 0
                    pend = None
                    total = max(na, 1)
                    while ai < na or ii_ < ni:
                        tgt = ni if ai >= na else (ni * (ai + 1)) // total
                        while ii_ < tgt:
                            idx_unit(s, qi, *iu[ii_])
                            ii_ += 1
                        if ai < na:
                            lp = att_logits(s, qa, au[ai])
                            if pend is not None:
                                att_pv(s, qa, *pend)
                            pend = (au[ai], lp)
                            ai += 1
                    if pend is not None:
                        att_pv(s, qa, *pend)
                    flush_acc()
                    if qa >= 0:
                        epilogue(s, qa)
                        bg_step(1)
            T.barrier()

    def phase_D(l, prefetch=()):
        prefetch = list(prefetch)
        bg_flush(l)
        with ExitStack() as es:
            Wo = salloc(es, "Wo", [128, 8, D], BF16); bWo = Buf("Wo")
            wl = wb_out[l].rearrange("(c p) n -> p c n", p=128)
            for c in range(8):
                T.dma("sp", Wo[:, c, :], wl[:, c, :], bWo, reads=[B_wb_out[l]], writes=[bWo])
            gb, bgb = ga_bcast(es, l, 16, "ga1")
            oc = [salloc(es, f"oc{i}", [128, D], BF16) for i in range(2)]
            boc = [Buf(f"oc{i}") for i in range(2)]
            oT = [salloc(es, f"oT{i}", [128, 8, 128], BF16) for i in range(2)]
            boT = [Buf(f"oT{i}") for i in range(2)]
            xts = [salloc(es, f"xd{i}", [128, D], F32) for i in range(2)]
            bxts = [Buf(f"xd{i}") for i in range(2)]
            tmp = [salloc(es, f"tm{i}", [128, D], F32) for i in range(2)]
            btmp = [Buf(f"tm{i}") for i in range(2)]
            src = x_in if l == 0 else xres
            ti = 0
            for s in range(NSEQ):
                for tt in range(NT):
                    t0 = tt * 128
                    k2 = ti % 2
                    ti += 1
                    T.dma("sp", oc[k2][:], ocat[s, t0:t0 + 128, :], boc[k2], writes=[boc[k2]])
                    T.dma("sp", xts[k2][:], src[s, t0:t0 + 128, :], bxts[k2], writes=[bxts[k2]])
                    if prefetch:
                        prefetch.pop(0)()
                    pv = pbf(k2).rearrange("p (c t) -> p c t", c=8)
                    for c in range(8):
                        T.op("pe", lambda e: e.transpose(pv[:, c, :], oc[k2][:, c * 128:(c + 1) * 128], ident_b[:]),
                             reads=[boc[k2], b_identb], writes=[PB[k2]])
                    T.op("act", lambda e: e.activation(out=oT[k2][:].rearrange("p c t -> p (c t)"), in_=pbf(k2)[:, 0:1024], func=AF.Copy),
                         reads=[PB[k2]], writes=[boT[k2]])
                    for half in range(2):
                        bank = 2 + k2 * 2 + half
                        for c in range(8):
                            T.op("pe", lambda e: e.matmul(pb[bank][:], lhsT=oT[k2][:, c, :], rhs=Wo[:, c, half * 512:(half + 1) * 512],
                                                          start=(c == 0), stop=(c == 7)),
                                 reads=[boT[k2], bWo], writes=[PB[bank]])
                        T.op("dve", lambda e: e.tensor_tensor(out=tmp[k2][:, half * 512:(half + 1) * 512], in0=pb[bank][:],
                                                              in1=gb[:, s, half * 512:(half + 1) * 512], op=ALU.mult),
                             reads=[PB[bank], bgb], writes=[btmp[k2]])
                    T.op("dve", lambda e: e.tensor_tensor(out=tmp[k2][:], in0=tmp[k2][:], in1=xts[k2][:], op=ALU.add),
                         reads=[btmp[k2], bxts[k2]], writes=[btmp[k2]])
                    T.dma("pool", xres[s, t0:t0 + 128, :], tmp[k2][:], btmp[k2], reads=[btmp[k2]])
            while prefetch:
                prefetch.pop(0)()
            T.barrier()

    def phase_DE(l, last):
        with ExitStack() as esw:
            Wu = salloc(esw, "Wu", [128, 8, DFF], BF16); bWu = Buf("Wu")
            Wd = salloc(esw, "Wd", [128, 32, D], BF16); bWd = Buf("Wd")
            wul = wb_up[l].rearrange("(c p) n -> p c n", p=128)
            wdl = wb_dn[l].rearrange("(c p) n -> p c n", p=128)
            pf = []
            for c in range(8):
                for hh in range(2):
                    pf.append(lambda c=c, hh=hh: T.dma(
                        "sp", Wu[:, c, hh * 2048:(hh + 1) * 2048], wul[:, c, hh * 2048:(hh + 1) * 2048], bWu,
                        reads=[B_wb_up[l]], writes=[bWu]))
            for c4 in range(8):
                pf.append(lambda c4=c4: T.dma(
                    "sp", Wd[:, c4 * 4:(c4 + 1) * 4, :], wdl[:, c4 * 4:(c4 + 1) * 4, :], bWd,
                    reads=[B_wb_dn[l]], writes=[bWd]))
            phase_D(l, prefetch=pf)
            phase_E(l, last, Wu, bWu, Wd, bWd)

    def phase_E(l, last, Wu, bWu, Wd, bWd):
        with ExitStack() as es:
            gb, bgb = ga_bcast(es, l, 40, "ga2")
            if last:
                gf = salloc(es, "gf", [128, D], F32); bgf = Buf("gf")
                T.dma("sp", gf[:], gfin_bc, bgf, writes=[bgf])
            TG = 256
            xts = [salloc(es, f"xe{i}", [128, D], F32) for i in range(4)]
            bxts = [Buf(f"xe{i}") for i in range(4)]
            xns = [salloc(es, f"xne{i}", [128, D], BF16) for i in range(2)]
            bxns = [Buf(f"xne{i}") for i in range(2)]
            hTs = [salloc(es, f"hTe{i}", [128, 8, TG], BF16) for i in range(2)]
            bhTs = [Buf(f"hTe{i}") for i in range(2)]
            aT = salloc(es, "aT", [128, 32, TG], BF16); baT = [Buf(f"aT{i}") for i in range(32)]
            rl = [salloc(es, f"rl{i}", [128, TG], BF16) for i in range(2)]
            brl = [Buf(f"rl{i}") for i in range(2)]
            ti = 0
            gi = 0
            ui = 0
            for s in range(NSEQ):
                for g in range(S // TG):
                    hT = hTs[gi % 2]; bhT = bhTs[gi % 2]
                    gi += 1
                    tiles = []
                    for j in range(TG // 128):
                        tt = g * (TG // 128) + j
                        t0 = tt * 128
                        xt = xts[ti % 4]; bxt = bxts[ti % 4]
                        xn = xns[ti % 2]; bxn = bxns[ti % 2]
                        tb = ti % 2
                        ti += 1
                        tiles.append((t0, xt, bxt))
                        T.dma("sp", xt[:], xres[s, t0:t0 + 128, :], bxt, writes=[bxt])
                        norm_to_T(xt[:], bxt,
                                  lambda c: gm2T[:, l, c, s:s + 1],
                                  lambda c: modT[:, l, 24 + c, s:s + 1],
                                  [b_gm2, b_modT_], xn, bxn, tb,
                                  lambda c: hT[:, c, j * 128:(j + 1) * 128], bhT)
                    for f in range(32):
                        bank = 2 + (ui % 2)
                        sl = ui % 2
                        ui += 1
                        for c in range(8):
                            T.op("pe", lambda e: e.matmul(pb[bank][:, 0:TG], lhsT=Wu[:, c, f * 128:(f + 1) * 128], rhs=hT[:, c, :],
                                                          start=(c == 0), stop=(c == 7)),
                                 reads=[bWu, bhT], writes=[PB[bank]])
                        T.op("act", lambda e: e.activation(out=rl[sl][:], in_=pb[bank][:, 0:TG], func=AF.Relu),
                             reads=[PB[bank]], writes=[brl[sl]])
                        T.op("dve", lambda e: e.tensor_tensor(out=aT[:, f, :], in0=rl[sl][:], in1=rl[sl][:], op=ALU.mult),
                             reads=[brl[sl]], writes=[baT[f]])
                    for j, (t0, xt, bxt) in enumerate(tiles):
                        for half in range(2):
                            bank = 4 + (j % 2) * 2 + half
                            for f in range(32):
                                T.op("pe", lambda e: e.matmul(pb[bank][:], lhsT=aT[:, f, j * 128:(j + 1) * 128],
                                                              rhs=Wd[:, f, half * 512:(half + 1) * 512], start=(f == 0), stop=(f == 31)),
                                     reads=[baT[f], bWd], writes=[PB[bank]])
                            T.op("dve", lambda e: e.tensor_tensor(out=tmpE[j % 2][:, half * 512:(half + 1) * 512], in0=pb[bank][:],
                                                                  in1=gb[:, s, half * 512:(half + 1) * 512], op=ALU.mult),
                                 reads=[PB[bank], bgb], writes=[btmpE[j % 2]])
                        T.op("dve", lambda e: e.tensor_tensor(out=xt[:], in0=tmpE[j % 2][:], in1=xt[:], op=ALU.add),
                             reads=[btmpE[j % 2], bxt], writes=[bxt])
                        if not last:
                            T.dma("pool", xres[s, t0:t0 + 128, :], xt[:], bxt, reads=[bxt])
                        else:
                            rs, brs = rstd_from(xt[:], [bxt], D)
                            T.op("dve", lambda e: e.scalar_tensor_tensor(out=tmpE[j % 2][:], in0=xt[:], scalar=rs, in1=gf[:],
                                                                         op0=ALU.mult, op1=ALU.mult),
                                 reads=[bxt, brs, bgf], writes=[btmpE[j % 2]])
                            T.dma("pool", out_d[s, t0:t0 + 128, :], tmpE[j % 2][:], btmpE[j % 2], reads=[btmpE[j % 2]])
            T.barrier()

    tmpE = []
    btmpE = [Buf("tmpE0"), Buf("tmpE1")]

    tmpE.append(salloc(ges, "tmpE0", [128, D], F32))
    tmpE.append(salloc(ges, "tmpE1", [128, D], F32))

    convert_weights()
    esA0 = ExitStack()
    Wf0 = salloc(esA0, "Wf0", [128, 8, 2688], BF16); bWf0 = [Buf(f"Wf0{i}") for i in range(4)]
    Wt0 = salloc(esA0, "Wt0", [128, 8, 648], BF16); bWt0 = Buf("Wt0")
    preA = (Wf0, bWf0, Wt0, bWt0)
    prologue(preA)
    for l in range(nlayers):
        if "A" in phases:
            phase_A(l, pre=preA if l == 0 else None)
        if l == 0:
            esA0.close()
        if "B" in phases and "C" in phases:
            with ExitStack() as esC0:
                cin0 = alloc_cin(esC0, 0)
                phase_B(l, cin0)
                phase_C(l, cin0)
        else:
            if "B" in phases:
                phase_B(l)
            if "C" in phases:
                phase_C(l)
        if "D" in phases and "E" in phases:
            phase_DE(l, last=(l == nlayers - 1))
        elif "D" in phases:
            phase_D(l)
    T.barrier()
    ges.close()
    return nc, T


def _t5_bucket(rel):
    nb = 16
    max_exact = 8
    base = np.where(rel > 0, nb, 0)
    n = np.abs(rel)
    nf = np.maximum(n, max_exact).astype(np.float32)
    large = max_exact + (np.log(nf / np.float32(max_exact)) / np.float32(math.log(128 / max_exact))
                         * np.float32(nb - max_exact)).astype(np.int32)
    large = np.minimum(large, nb - 1)
    return base + np.where(n < max_exact, n, large)


def _consts():
    p = np.arange(128)
    c = {}
    c["c_ident"] = np.eye(128, dtype=np.float32)
    c["c_tri8"] = np.where(p[:, None] >= p[None, :], -8.0, 0.0).astype(np.float32)
    c["c_neg8"] = np.full((128, 128), -8.0, np.float32)
    cm = (p[:, None] < p[None, :]).astype(np.float32)
    c["c_cmask"] = np.ascontiguousarray(np.broadcast_to(cm[:, None, :], (128, 8, 128)))
    adm = ((p[:, None] // 64) <= (p[None, :] // 64)).astype(np.float32)
    c["c_adm"] = np.ascontiguousarray(np.broadcast_to(adm[:, None, :], (128, 8, 128)))
    c["c_admneg"] = np.ascontiguousarray(np.broadcast_to(np.where(adm > 0, 0.0, -1.0e5).astype(np.float32)[:, None, :], (128, 8, 128)))
    c["c_pow2"] = np.ascontiguousarray(np.broadcast_to(
        (0.5 ** np.arange(1, KBIS + 1)).astype(np.float32)[None, :], (128, KBIS)))
    c["c_ones"] = np.ones((128, 128), np.float32)
    return c


def _prep_inputs(inp, core):
    f = np.float32
    b0 = core * NSEQ
    bs = slice(b0, b0 + NSEQ)
    m = {}
    m["x"] = np.ascontiguousarray(inp["x"][bs], dtype=f)
    c = np.asarray(inp["c"], dtype=f)[bs]
    m["cT"] = np.ascontiguousarray(c.reshape(NSEQ, 8, 128).transpose(2, 1, 0))
    m["w_mod"] = np.ascontiguousarray(inp["w_mod"], dtype=f)
    bm = np.asarray(inp["b_mod"], dtype=f).reshape(2, 48, 128).transpose(2, 0, 1)
    m["b_modT"] = np.ascontiguousarray(np.broadcast_to(bm[..., None], (128, 2, 48, NSEQ)))
    ga = np.asarray(inp["g_attn"], dtype=f).reshape(2, 8, 128).transpose(2, 0, 1)
    m["g_attnT"] = np.ascontiguousarray(np.broadcast_to(ga[..., None], (128, 2, 8, NSEQ)))
    gm = np.asarray(inp["g_mlp"], dtype=f).reshape(2, 8, 128).transpose(2, 0, 1)
    m["g_mlpT"] = np.ascontiguousarray(np.broadcast_to(gm[..., None], (128, 2, 8, NSEQ)))
    m["w_in"] = np.ascontiguousarray(inp["w_in"], dtype=f)
    m["kvg_bc"] = np.ascontiguousarray(np.broadcast_to(np.asarray(inp["kv_norm_g"], dtype=f)[None], (128, 2, 128)))
    m["w_uv"] = np.ascontiguousarray(inp["w_uv"], dtype=f)
    m["goa_bc"] = np.ascontiguousarray(np.broadcast_to(np.asarray(inp["g_out_a"], dtype=f)[None], (128, 2, 512)))
    m["gob_bc"] = np.ascontiguousarray(np.broadcast_to(np.asarray(inp["g_out_b"], dtype=f)[None], (128, 2, 512)))
    m["w_out"] = np.ascontiguousarray(inp["w_out"], dtype=f)
    m["w_up"] = np.ascontiguousarray(inp["w_up"], dtype=f)
    m["w_down"] = np.ascontiguousarray(inp["w_down"], dtype=f)
    rb = np.asarray(inp["rel_bias"], dtype=f)
    p = np.arange(128)
    bn = np.empty((128, 2, 8, 128), f)
    for off in range(2):
        rel = (p[:, None] - off * 128) - p[None, :]
        bk = _t5_bucket(rel.astype(np.int32))
        bn[:, off] = rb[bk].transpose(0, 2, 1)
    m["biasn"] = bn
    far = rb[_t5_bucket(np.array([-1000], np.int32))[0]]
    m["bfar_bc"] = np.ascontiguousarray(np.broadcast_to(far[None, :, None], (128, 8, 128)))
    m["gfin_bc"] = np.ascontiguousarray(np.broadcast_to(np.asarray(inp["g_final"], dtype=f)[None], (128, D)))
    m.update(_consts())
    return m


_CACHE = {}


def kernel(**inputs):
    if "nc" not in _CACHE:
        _CACHE["nc"] = build_program()[0]
    nc = _CACHE["nc"]
    in_maps = [_prep_inputs(inputs, core) for core in range(8)]
    res = run_bass_kernel_spmd(nc, in_maps, core_ids=list(range(8)))
    out = np.concatenate([np.asarray(r["out"]) for r in res.results], axis=0)
    return out.astype(np.float32, copy=False)
```
